# Optimizing a Trainium2 kernel written in Bass

```python
import math
import jax, jax.numpy as jnp
from jax import lax
import numpy as np

D_MODEL = 1024
BATCH = 8
SEQ = 4096
DEPTH = 2

HEAD_DIM = 64
ROPE_THETA = 10000.0
SWA_HEADS = 16
SWA_KV_HEADS = 4
SWA_WINDOW = 128
SWA_BLOCK = 128
SWA_Q = SWA_HEADS * HEAD_DIM
SWA_KV = SWA_KV_HEADS * HEAD_DIM
SWA_IN = SWA_Q + 2 * SWA_KV
NSA_HEADS = 16
NSA_KV_HEADS = 4
CMP_LEN = 32
CMP_STRIDE = 16
CMP_HIDDEN = 256
SEL_BLOCK = 64
SEL_TOPN = 16
NSA_WINDOW = 512
NSA_QCHUNK = 32
SEL_FORCE = 1e4
NSA_Q = NSA_HEADS * HEAD_DIM
NSA_KV = NSA_KV_HEADS * HEAD_DIM
NSA_IN = NSA_Q + 6 * NSA_KV + 3 * NSA_HEADS
D_FF = 3584
N_EXPERTS = 8
TOP_K = 2
DN_ALPHA = (2 * DEPTH) ** 0.25
DN_BETA = (8 * DEPTH) ** -0.25
LN_EPS = 1e-5
NEG_INF = -1e30
N_EVEN = (DEPTH + 1) // 2
N_ODD = DEPTH // 2

kernel_name = 'hybrid_swa_sink_nsa_moe_deepnorm'


def _layer_norm(x, g, b):
    xf = x.astype(jnp.float32)
    mu = jnp.mean(xf, axis=-1, keepdims=True)
    var = jnp.mean(jnp.square(xf - mu), axis=-1, keepdims=True)
    return ((xf - mu) * lax.rsqrt(var + LN_EPS)).astype(x.dtype) * g + b


def _rope_tables(seq):
    inv = 1.0 / (ROPE_THETA ** (jnp.arange(0, HEAD_DIM, 2, dtype=jnp.float32) / HEAD_DIM))
    ang = jnp.arange(seq, dtype=jnp.float32)[:, None] * inv[None, :]
    return jnp.cos(ang), jnp.sin(ang)


def _rope(x, cos, sin):
    c = cos[None, :, None, :].astype(x.dtype)
    s = sin[None, :, None, :].astype(x.dtype)
    x1, x2 = jnp.split(x, 2, axis=-1)
    return jnp.concatenate([x1 * c - x2 * s, x2 * c + x1 * s], axis=-1)


def _masked_softmax(s, mask):
    s = jnp.where(mask, s, NEG_INF)
    m = jnp.max(s, axis=-1, keepdims=True)
    p = jnp.where(mask, jnp.exp(s - m), 0.0)
    return p / jnp.maximum(jnp.sum(p, axis=-1, keepdims=True), 1e-30)


def _swa_sink_attention(x, w_in, w_out, sinks, cos, sin):
    B, S, _ = x.shape
    G = SWA_HEADS // SWA_KV_HEADS
    nb = S // SWA_BLOCK
    proj = x @ w_in
    q, k, v = jnp.split(proj, [SWA_Q, SWA_Q + SWA_KV], axis=-1)
    q = _rope(q.reshape(B, S, SWA_HEADS, HEAD_DIM), cos, sin)
    k = _rope(k.reshape(B, S, SWA_KV_HEADS, HEAD_DIM), cos, sin)
    v = v.reshape(B, S, SWA_KV_HEADS, HEAD_DIM)
    qb = q.reshape(B, nb, SWA_BLOCK, SWA_KV_HEADS, G, HEAD_DIM)
    kb = k.reshape(B, nb, SWA_BLOCK, SWA_KV_HEADS, HEAD_DIM)
    vb = v.reshape(B, nb, SWA_BLOCK, SWA_KV_HEADS, HEAD_DIM)
    kk = jnp.concatenate([jnp.concatenate([jnp.zeros_like(kb[:, :1]), kb[:, :-1]], axis=1), kb], axis=2)
    vv = jnp.concatenate([jnp.concatenate([jnp.zeros_like(vb[:, :1]), vb[:, :-1]], axis=1), vb], axis=2)
    scores = jnp.einsum('bnqkgd,bnskd->bnkgqs', qb, kk).astype(jnp.float32) * (HEAD_DIM ** -0.5)
    n_i = jnp.arange(nb)[:, None, None]
    q_i = jnp.arange(SWA_BLOCK)[None, :, None] + SWA_BLOCK
    s_i = jnp.arange(2 * SWA_BLOCK)[None, None, :]
    diff = q_i - s_i
    abs_s = n_i * SWA_BLOCK - SWA_BLOCK + s_i
    mask = ((diff >= 0) & (diff < SWA_WINDOW) & (abs_s >= 0))[None, :, None, None]
    sink = sinks.astype(jnp.float32).reshape(1, 1, SWA_KV_HEADS, G, 1, 1)
    s = jnp.where(mask, scores, NEG_INF)
    m = jnp.maximum(jnp.max(s, axis=-1, keepdims=True), sink)
    p = jnp.where(mask, jnp.exp(s - m), 0.0)
    probs = p / (jnp.sum(p, axis=-1, keepdims=True) + jnp.exp(sink - m))
    o = jnp.einsum('bnkgqs,bnskd->bnqkgd', probs.astype(vv.dtype), vv)
    return o.reshape(B, S, SWA_Q) @ w_out


def _overlap_matrix(nc, nsel):
    cs = CMP_STRIDE * np.arange(nc)
    ce = cs + CMP_LEN
    ss = SEL_BLOCK * np.arange(nsel)
    se = ss + SEL_BLOCK
    ov = np.clip(np.minimum(ce[:, None], se[None, :]) - np.maximum(cs[:, None], ss[None, :]), 0, None)
    return jnp.asarray(ov / CMP_STRIDE, dtype=jnp.float32)


def _nsa_attention(x, w_in, w_out, pos_k, pos_v, ck_w1, ck_w2, cv_w1, cv_w2, cos, sin):
    B, S, _ = x.shape
    Hkv = NSA_KV_HEADS
    G = NSA_HEADS // NSA_KV_HEADS
    scale = HEAD_DIM ** -0.5
    proj = x @ w_in
    splits = [NSA_Q + i * NSA_KV for i in range(7)]
    q, kc, vc, ks, vs, kw, vw, gl = jnp.split(proj, splits, axis=-1)
    q = _rope(q.reshape(B, S, NSA_HEADS, HEAD_DIM), cos, sin)
    kc = _rope(kc.reshape(B, S, Hkv, HEAD_DIM), cos, sin)
    ks = _rope(ks.reshape(B, S, Hkv, HEAD_DIM), cos, sin)
    kw = _rope(kw.reshape(B, S, Hkv, HEAD_DIM), cos, sin)
    vc = vc.reshape(B, S, Hkv, HEAD_DIM)
    vs = vs.reshape(B, S, Hkv, HEAD_DIM)
    vw = vw.reshape(B, S, Hkv, HEAD_DIM)
    gates = jax.nn.sigmoid(gl).reshape(B, S, Hkv, G, 3)

    nc = (S - CMP_LEN) // CMP_STRIDE + 1
    idx = CMP_STRIDE * np.arange(nc)[:, None] + np.arange(CMP_LEN)[None, :]

    def compress(t, pos, w1, w2):
        blk = t[:, idx] + pos[None, None, :, None, :]
        flat = blk.transpose(0, 1, 3, 2, 4).reshape(B, nc, Hkv, CMP_LEN * HEAD_DIM)
        return jax.nn.gelu(flat @ w1) @ w2

    kcc = compress(kc, pos_k, ck_w1, ck_w2)
    vcc = compress(vc, pos_v, cv_w1, cv_w2)
    cmp_end = jnp.asarray(idx[:, -1])

    nsel = S // SEL_BLOCK
    topn = min(SEL_TOPN, nsel)
    ov = _overlap_matrix(nc, nsel)
    kblk = ks.reshape(B, nsel, SEL_BLOCK, Hkv, HEAD_DIM).transpose(0, 3, 1, 2, 4)
    vblk = vs.reshape(B, nsel, SEL_BLOCK, Hkv, HEAD_DIM).transpose(0, 3, 1, 2, 4)
    b_i = jnp.arange(B)[:, None, None, None]
    h_i = jnp.arange(Hkv)[None, :, None, None]
    blk_ids = jnp.arange(nsel)

    kw_pad = jnp.pad(kw, ((0, 0), (NSA_WINDOW, 0), (0, 0), (0, 0)))
    vw_pad = jnp.pad(vw, ((0, 0), (NSA_WINDOW, 0), (0, 0), (0, 0)))

    def chunk(ci):
        c0 = ci * NSA_QCHUNK
        tpos = c0 + jnp.arange(NSA_QCHUNK)
        qc = lax.dynamic_slice_in_dim(q, c0, NSA_QCHUNK, axis=1).reshape(B, NSA_QCHUNK, Hkv, G, HEAD_DIM)
        s_c = jnp.einsum('bqkgd,bckd->bkgqc', qc, kcc).astype(jnp.float32) * scale
        p_c = _masked_softmax(s_c, cmp_end[None, :] <= tpos[:, None])
        o_c = jnp.einsum('bkgqc,bckd->bqkgd', p_c.astype(vcc.dtype), vcc)
        imp = jnp.sum(p_c, axis=2) @ ov
        cur = (tpos // SEL_BLOCK)[:, None]
        forced = (blk_ids[None, :] == 0) | (blk_ids[None, :] == cur) | (blk_ids[None, :] == cur - 1)
        imp = jnp.where(forced, SEL_FORCE, imp)
        imp = jnp.where(blk_ids[None, :] <= cur, imp, -1.0)
        _, sel = lax.top_k(imp, topn)
        ksel = kblk[b_i, h_i, sel].reshape(B, Hkv, NSA_QCHUNK, topn * SEL_BLOCK, HEAD_DIM)
        vsel = vblk[b_i, h_i, sel].reshape(B, Hkv, NSA_QCHUNK, topn * SEL_BLOCK, HEAD_DIM)
        spos = (sel[..., None] * SEL_BLOCK + jnp.arange(SEL_BLOCK)).reshape(B, Hkv, NSA_QCHUNK, topn * SEL_BLOCK)
        mask_s = (spos <= tpos[None, None, :, None])[:, :, None]
        s_s = jnp.einsum('bqkgd,bkqsd->bkgqs', qc, ksel).astype(jnp.float32) * scale
        p_s = _masked_softmax(s_s, mask_s)
        o_s = jnp.einsum('bkgqs,bkqsd->bqkgd', p_s.astype(vsel.dtype), vsel)
        kwc = lax.dynamic_slice_in_dim(kw_pad, c0, NSA_WINDOW + NSA_QCHUNK, axis=1)
        vwc = lax.dynamic_slice_in_dim(vw_pad, c0, NSA_WINDOW + NSA_QCHUNK, axis=1)
        wpos = c0 - NSA_WINDOW + jnp.arange(NSA_WINDOW + NSA_QCHUNK)
        diff = tpos[:, None] - wpos[None, :]
        mask_w = (diff >= 0) & (diff < NSA_WINDOW) & (wpos[None, :] >= 0)
        s_w = jnp.einsum('bqkgd,bskd->bkgqs', qc, kwc).astype(jnp.float32) * scale
        p_w = _masked_softmax(s_w, mask_w)
        o_w = jnp.einsum('bkgqs,bskd->bqkgd', p_w.astype(vwc.dtype), vwc)
        g = lax.dynamic_slice_in_dim(gates, c0, NSA_QCHUNK, axis=1)
        o = g[..., 0:1] * o_c + g[..., 1:2] * o_s + g[..., 2:3] * o_w
        return o.reshape(B, NSA_QCHUNK, NSA_Q)

    outs = lax.map(chunk, jnp.arange(S // NSA_QCHUNK))
    o = outs.transpose(1, 0, 2, 3).reshape(B, S, NSA_Q)
    return o @ w_out


def _swiglu(x, w_gate, w_up, w_down):
    return (jax.nn.silu(x @ w_gate) * (x @ w_up)) @ w_down


def _moe(x, router, w_gate, w_up, w_down):
    B, S, D = x.shape
    xt = x.reshape(B * S, D)
    logits = (xt @ router).astype(jnp.float32)
    vals, ids = lax.top_k(logits, TOP_K)
    w = jax.nn.softmax(vals, axis=-1)
    gate = jnp.sum(jax.nn.one_hot(ids, N_EXPERTS, dtype=jnp.float32) * w[..., None], axis=1)
    out = jnp.zeros_like(xt)
    for e in range(N_EXPERTS):
        out = out + gate[:, e:e + 1].astype(xt.dtype) * _swiglu(xt, w_gate[e], w_up[e], w_down[e])
    return out.reshape(B, S, D)


def _normal(k, shape, scale):
    return jax.random.normal(k, shape, jnp.float32) * scale


def setup_inputs(seed: int = 0) -> dict:
    key = jax.random.key(seed)
    ks = jax.random.split(key, 24)
    D = D_MODEL
    a_col = jnp.concatenate([jnp.ones((SWA_Q + SWA_KV,), jnp.float32), jnp.full((SWA_KV,), DN_BETA, jnp.float32)])
    b_col = jnp.concatenate([
        jnp.ones((NSA_Q + NSA_KV,), jnp.float32),
        jnp.full((NSA_KV,), DN_BETA, jnp.float32),
        jnp.ones((NSA_KV,), jnp.float32),
        jnp.full((NSA_KV,), DN_BETA, jnp.float32),
        jnp.ones((NSA_KV,), jnp.float32),
        jnp.full((NSA_KV,), DN_BETA, jnp.float32),
        jnp.ones((3 * NSA_HEADS,), jnp.float32)])
    return {
        'x': _normal(ks[0], (BATCH, SEQ, D), 1.0),
        'a_w_in': _normal(ks[1], (N_EVEN, D, SWA_IN), D ** -0.5) * a_col,
        'a_w_out': _normal(ks[2], (N_EVEN, SWA_Q, D), DN_BETA * SWA_Q ** -0.5),
        'a_sinks': _normal(ks[3], (N_EVEN, SWA_HEADS), 1.0),
        'b_w_in': _normal(ks[4], (N_ODD, D, NSA_IN), D ** -0.5) * b_col,
        'b_w_out': _normal(ks[5], (N_ODD, NSA_Q, D), DN_BETA * NSA_Q ** -0.5),
        'b_cmp_pos_k': _normal(ks[6], (N_ODD, CMP_LEN, HEAD_DIM), 0.1),
        'b_cmp_pos_v': _normal(ks[7], (N_ODD, CMP_LEN, HEAD_DIM), 0.1),
        'b_cmp_k_w1': _normal(ks[8], (N_ODD, CMP_LEN * HEAD_DIM, CMP_HIDDEN), (CMP_LEN * HEAD_DIM) ** -0.5),
        'b_cmp_k_w2': _normal(ks[9], (N_ODD, CMP_HIDDEN, HEAD_DIM), CMP_HIDDEN ** -0.5),
        'b_cmp_v_w1': _normal(ks[10], (N_ODD, CMP_LEN * HEAD_DIM, CMP_HIDDEN), (CMP_LEN * HEAD_DIM) ** -0.5),
        'b_cmp_v_w2': _normal(ks[11], (N_ODD, CMP_HIDDEN, HEAD_DIM), CMP_HIDDEN ** -0.5),
        'ffn_w_gate': _normal(ks[12], (N_EVEN, D, D_FF), D ** -0.5),
        'ffn_w_up': _normal(ks[13], (N_EVEN, D, D_FF), D ** -0.5),
        'ffn_w_down': _normal(ks[14], (N_EVEN, D_FF, D), DN_BETA * D_FF ** -0.5),
        'moe_router': _normal(ks[15], (N_ODD, D, N_EXPERTS), D ** -0.5),
        'moe_w_gate': _normal(ks[16], (N_ODD, N_EXPERTS, D, D_FF), D ** -0.5),
        'moe_w_up': _normal(ks[17], (N_ODD, N_EXPERTS, D, D_FF), D ** -0.5),
        'moe_w_down': _normal(ks[18], (N_ODD, N_EXPERTS, D_FF, D), DN_BETA * D_FF ** -0.5),
        'ln_gain': 1.0 + _normal(ks[19], (DEPTH, 2, D), 0.02),
        'ln_bias': _normal(ks[20], (DEPTH, 2, D), 0.02),
    }


def reference(x, a_w_in, a_w_out, a_sinks, b_w_in, b_w_out, b_cmp_pos_k, b_cmp_pos_v,
              b_cmp_k_w1, b_cmp_k_w2, b_cmp_v_w1, b_cmp_v_w2, ffn_w_gate, ffn_w_up, ffn_w_down,
              moe_router, moe_w_gate, moe_w_up, moe_w_down, ln_gain, ln_bias):
    cos, sin = _rope_tables(x.shape[1])
    for i in range(DEPTH):
        j = i // 2
        if i % 2 == 0:
            h = _swa_sink_attention(x, a_w_in[j], a_w_out[j], a_sinks[j], cos, sin)
        else:
            h = _nsa_attention(x, b_w_in[j], b_w_out[j], b_cmp_pos_k[j], b_cmp_pos_v[j],
                               b_cmp_k_w1[j], b_cmp_k_w2[j], b_cmp_v_w1[j], b_cmp_v_w2[j], cos, sin)
        x = _layer_norm(DN_ALPHA * x + h, ln_gain[i, 0], ln_bias[i, 0])
        if i % 2 == 0:
            f = _swiglu(x, ffn_w_gate[j], ffn_w_up[j], ffn_w_down[j])
        else:
            f = _moe(x, moe_router[j], moe_w_gate[j], moe_w_up[j], moe_w_down[j])
        x = _layer_norm(DN_ALPHA * x + f, ln_gain[i, 1], ln_bias[i, 1])
    return x
```

```python
import numpy as np
import ml_dtypes
from contextlib import ExitStack
import concourse.bass as bass
import concourse.mybir as mybir
from concourse.bass_utils import run_bass_kernel_spmd

F32 = mybir.dt.float32
BF16 = mybir.dt.bfloat16
AF = mybir.ActivationFunctionType
ALU = mybir.AluOpType

S = 4096
D = 1024
NT = 32
FF = 3584
NE = 8
ALPHA = float(4.0 ** 0.25)
EPS = 1e-5
NCORES = 8


class SemObj:
    def __init__(self, h):
        self.h = h
        self.n = 0


class Buf:
    def __init__(self, ap):
        self.ap = ap
        self.w = {}
        self.r = {}

    @staticmethod
    def _add(d, tok):
        if tok is None:
            return
        s, v = tok
        if d.get(s, 0) < v:
            d[s] = v

    def deps_r(self):
        return list(self.w.items())

    def deps_w(self):
        return list(self.w.items()) + list(self.r.items())

    def wrote(self, tok, fresh=True):
        if fresh:
            self.w = {}
            self.r = {}
        self._add(self.w, tok)

    def read(self, tok):
        self._add(self.r, tok)


class Eng:
    def __init__(self, fl, name, eng):
        self.fl = fl
        self.name = name
        self.e = eng
        self.sem = SemObj(fl.gstack.enter_context(fl.nc.semaphore("s_" + name)))
        self.seen = {}

    def wait(self, *toks):
        for tok in toks:
            if tok is None:
                continue
            if isinstance(tok, list) or (isinstance(tok, tuple) and (len(tok) != 2 or not isinstance(tok[0], SemObj))):
                self.wait(*tok)
                continue
            sem, v = tok
            if sem is None or v <= 0:
                continue
            if self.seen.get(sem, 0) >= v:
                continue
            if sem is self.sem and self.name == "pe":
                continue
            self.e.wait_ge(sem.h, v)
            self.seen[sem] = v

    def done(self, ins):
        self.sem.n += 1
        ins.then_inc(self.sem.h, 1)
        return (self.sem, self.sem.n)

    def dma(self, out, in_, sem, **kw):
        ins = self.e.dma_start(out=out, in_=in_, **kw)
        sem.n += 16
        ins.then_inc(sem.h, 16)
        return (sem, sem.n)


class Flow:
    def __init__(self, nc):
        self.nc = nc
        self.gstack = ExitStack()
        self.pe = Eng(self, "pe", nc.tensor)
        self.act = Eng(self, "act", nc.scalar)
        self.dve = Eng(self, "dve", nc.vector)
        self.pool = Eng(self, "pool", nc.gpsimd)
        self.sp = Eng(self, "sp", nc.sync)
        self.engines = [self.pe, self.act, self.dve, self.pool, self.sp]
        self.dsems = {}
        self.pstack = None
        self.uid = 0

    def dsem(self, name):
        if name not in self.dsems:
            self.dsems[name] = SemObj(self.gstack.enter_context(self.nc.semaphore("d_" + name)))
        return self.dsems[name]

    def begin(self):
        self.pstack = ExitStack()

    def end(self):
        self.barrier()
        self.pstack.close()
        self.pstack = None

    def barrier(self):
        toks = [(e.sem, e.sem.n) for e in self.engines if e.sem.n > 0]
        toks += [(s, s.n) for s in self.dsems.values() if s.n > 0]
        for e in self.engines:
            e.wait([t for t in toks if t[0] is not e.sem])

    def sb(self, shape, dt, name=None):
        self.uid += 1
        return self.pstack.enter_context(self.nc.sbuf_tensor(f"{name or 'sb'}_{self.uid}", list(shape), dt))

    def ps(self, shape, dt=F32, name=None):
        self.uid += 1
        return self.pstack.enter_context(self.nc.psum_tensor(f"{name or 'ps'}_{self.uid}", list(shape), dt))


def load_xT(fl, XT, xb_d, deps, semname, ncols=D, t0=0, nt=S):
    sp = fl.sp
    sem = fl.dsem(semname)
    sp.wait(deps, XT.deps_w())
    tok = None
    step = 1024 if nt >= 1024 else nt
    for kc in range(ncols // 128):
        for q in range(nt // step):
            tok = sp.dma(XT.ap[:, kc, q * step:(q + 1) * step],
                         xb_d[t0 + q * step:t0 + (q + 1) * step, kc * 128:(kc + 1) * 128], sem, transpose=True)
    XT.wrote(tok)
    return tok


def layernorm_tile(fl, r, stats, mv, rs, xn, out_t, G, Bt):
    dve, pool = fl.dve, fl.pool
    dve.wait(r.deps_r())
    t1 = dve.done(dve.e.bn_stats(out=stats[:, 0, :], in_=r.ap[:, 0:512]))
    t2 = dve.done(dve.e.bn_stats(out=stats[:, 1, :], in_=r.ap[:, 512:1024]))
    dve.wait(t1, t2)
    t3 = dve.done(dve.e.bn_aggr(out=mv[:, :], in_=stats[:, :, :].rearrange("p a b -> p (a b)")))
    dve.wait(t3)
    t4a = dve.done(dve.e.tensor_scalar(out=rs[:, :], in0=mv[:, 1:2], scalar1=EPS, scalar2=None, op0=ALU.add))
    fl.act.wait(t4a)
    t4b = fl.act.done(fl.act.e.activation(out=rs[:, :], in_=rs[:, :], func=AF.Sqrt))
    dve.wait(t4b)
    t4 = dve.done(dve.e.reciprocal(out=rs[:, :], in_=rs[:, :]))
    dve.wait(t4, xn.deps_w())
    t5 = dve.done(dve.e.tensor_scalar(out=xn.ap[:, :], in0=r.ap[:, :], scalar1=mv[:, 0:1], scalar2=rs[:, 0:1],
                                      op0=ALU.subtract, op1=ALU.mult))
    r.read(t5)
    xn.wrote(t5)
    pool.wait(t5, out_t.deps_w())
    t6 = pool.done(pool.e.tensor_tensor(out=xn.ap[:, :], in0=xn.ap[:, :], in1=G[:, :], op=ALU.mult))
    pool.wait(t6)
    t7 = pool.done(pool.e.tensor_tensor(out=out_t.ap[:, :], in0=xn.ap[:, :], in1=Bt[:, :], op=ALU.add))
    xn.read(t7)
    xn.wrote(t6, fresh=False)
    out_t.wrote(t7)
    return t7


def load_ln_params(fl, ln_gain_d, ln_bias_d, i, j, ready):
    G = fl.sb([128, D], F32, "lnG")
    Bt = fl.sb([128, D], F32, "lnB")
    sem = fl.dsem("lnp")
    fl.sp.wait(ready)
    fl.sp.dma(G[:, :], ln_gain_d[i, j, :].partition_broadcast(128), sem)
    tok = fl.sp.dma(Bt[:, :], ln_bias_d[i, j, :].partition_broadcast(128), sem)
    return G, Bt, tok


def phase_cast_x(fl, x_d, xb_d):
    sem = fl.dsem("castx")
    tok = None
    for q in range(4):
        tok = fl.pool.dma(xb_d[q * 1024:(q + 1) * 1024, :], x_d[q * 1024:(q + 1) * 1024, :], sem)
    return tok


def rope_proj_chunk(fl, XT, W, Wsw, M, tt, PJ, pj_i, CT, ST, tmps, tmp_i, dests, x_ready):
    pe, dve, pool = fl.pe, fl.dve, fl.pool
    P1 = PJ[(2 * pj_i) % len(PJ)]
    P2 = PJ[(2 * pj_i + 1) % len(PJ)]
    for (P, Wt) in ((P1, W), (P2, Wsw)):
        pe.wait(P.deps_w(), Wt.deps_r(), x_ready)
        tok = None
        for kc in range(8):
            tok = pe.done(pe.e.matmul(P.ap[0:M, :], Wt.ap[:, kc, 0:M], XT.ap[:, kc, tt * 512:(tt + 1) * 512],
                                      start=(kc == 0), stop=(kc == 7)))
        Wt.read(tok)
        XT.read(tok)
        P.wrote(tok)
    T1 = tmps[(2 * tmp_i) % len(tmps)]
    T2 = tmps[(2 * tmp_i + 1) % len(tmps)]
    dve.wait(P1.deps_r(), T1.deps_w())
    t1 = dve.done(dve.e.tensor_tensor(out=T1.ap[0:M, :], in0=P1.ap[0:M, :], in1=CT[0:M, tt * 512:(tt + 1) * 512], op=ALU.mult))
    P1.read(t1)
    T1.wrote(t1)
    dve.wait(P2.deps_r(), T2.deps_w())
    t2 = dve.done(dve.e.tensor_tensor(out=T2.ap[0:M, :], in0=P2.ap[0:M, :], in1=ST[0:M, tt * 512:(tt + 1) * 512], op=ALU.mult))
    P2.read(t2)
    T2.wrote(t2)
    for (row0, dbuf, out_ap) in dests:
        pool.wait(t1, t2, dbuf.deps_w())
        t3 = pool.done(pool.e.tensor_tensor(out=out_ap, in0=T1.ap[row0:row0 + 64, :], in1=T2.ap[row0:row0 + 64, :], op=ALU.add))
        T1.read(t3)
        T2.read(t3)
        dbuf.wrote(t3, fresh=False)


def load_w_cols(fl, Wb, w_d, c0, M, semname, swap=False):
    pool = fl.pool
    sem = fl.dsem(semname)
    pool.wait(Wb.deps_w())
    if not swap:
        src = w_d.rearrange("(kc p) n -> p kc n", p=128)[:, :, c0:c0 + M]
        tok = pool.dma(Wb.ap[:, :, 0:M], src, sem)
    else:
        nh = M // 64
        ncol = w_d.shape[1]
        src5 = w_d[:, 0:(ncol // 64) * 64].rearrange("(kc p) (h two i) -> p kc h two i", p=128, two=2, i=32)
        dst5 = Wb.ap[:, :, 0:M].rearrange("p kc (h two i) -> p kc h two i", two=2, i=32)
        h0 = c0 // 64
        tok = None
        for two in range(2):
            for hh in range(nh):
                tok = pool.dma(dst5[:, :, hh, two, :], src5[:, :, h0 + hh, 1 - two, :], sem)
    Wb.wrote(tok)
    return tok


def phase_l0_attn(fl, d):
    nc = fl.nc
    pe, act, dve, pool, sp = fl.pe, fl.act, fl.dve, fl.pool, fl.sp
    fl.begin()
    XT = Buf(fl.sb([128, 8, S], BF16, "XT"))
    CT = fl.sb([128, S], F32, "CT")
    ST = fl.sb([128, S], F32, "ST")
    MK = fl.sb([128, 256], BF16, "MK")
    es = fl.sb([128, 16], F32, "es")
    KT = Buf(fl.sb([64, S], BF16, "KT"))
    VA = Buf(fl.sb([128, NT, 65], BF16, "VA"))
    QT = Buf(fl.sb([64, 4, S], BF16, "QT"))
    OG = Buf(fl.sb([128, NT, 256], BF16, "OG"))
    Wn = [Buf(fl.sb([128, 8, 128], BF16, "Wn")) for _ in range(2)]
    Ws = [Buf(fl.sb([128, 8, 128], BF16, "Ws")) for _ in range(2)]
    tmps = [Buf(fl.sb([128, 512], F32, "tmp")) for _ in range(4)]
    PTs = [Buf(fl.sb([128, 256], BF16, "PT")) for _ in range(4)]
    lts = [fl.sb([128, 4], F32, "lt") for _ in range(4)]
    banks = [Buf(fl.ps([128, 512], F32, "bank")) for _ in range(8)]
    PJ = banks[0:4]
    STs = []
    for b in banks[4:6]:
        STs.append((Buf(b.ap[:, 0:256]), 0))
        STs.append((Buf(b.ap[:, 256:512]), 0))
    OPs = [banks[6], banks[7], banks[3]]

    csem = fl.dsem("const")
    sp.dma(CT[:, :], d["cosT"][:, :], csem)
    sp.dma(ST[:, :], d["sinT"][:, :], csem)
    sp.dma(es[:, :], d["a_sinks"][0, :].partition_broadcast(128), csem)
    tc1 = (csem, csem.n)
    msem = fl.dsem("constp")
    tmk = pool.dma(MK[:, :], d["swa_mask"][:, :], msem)
    act.wait(tc1)
    tes = act.done(act.e.activation(out=es[:, :], in_=es[:, :], func=AF.Exp))
    tva = dve.done(dve.e.memset(VA.ap[:, :, 64:65], 1.0))
    VA.wrote(tva)
    tx = load_xT(fl, XT, d["xb0"], d["t_castx"], "xt")
    x_ready = [tx, tc1]

    w_in = d["a_w_in"][0]
    pj_i = 0
    tmp_i = 0
    wi = 0
    it = 0
    for j in range(4):
        for c in range(2):
            Wb, Wsb = Wn[wi % 2], Ws[wi % 2]
            wi += 1
            c0 = (4 * j + 2 * c) * 64
            load_w_cols(fl, Wb, w_in, c0, 128, "wn")
            load_w_cols(fl, Wsb, w_in, c0, 128, "ws", swap=True)
            for tt in range(8):
                dests = [(0, QT, QT.ap[0:64, 2 * c, tt * 512:(tt + 1) * 512]),
                         (64, QT, QT.ap[0:64, 2 * c + 1, tt * 512:(tt + 1) * 512])]
                rope_proj_chunk(fl, XT, Wb, Wsb, 128, tt, PJ, pj_i, CT, ST, tmps, tmp_i, dests, x_ready)
                pj_i += 1
                tmp_i += 1
        Wb, Wsb = Wn[wi % 2], Ws[wi % 2]
        wi += 1
        c0 = 1024 + j * 64
        load_w_cols(fl, Wb, w_in, c0, 64, "wn")
        load_w_cols(fl, Wsb, w_in, c0, 64, "ws", swap=True)
        for tt in range(8):
            dests = [(0, KT, KT.ap[0:64, tt * 512:(tt + 1) * 512])]
            rope_proj_chunk(fl, XT, Wb, Wsb, 64, tt, PJ, pj_i, CT, ST, tmps, tmp_i, dests, x_ready)
            pj_i += 1
            tmp_i += 1
        Wb = Wn[wi % 2]
        wi += 1
        c0 = 1280 + j * 64
        load_w_cols(fl, Wb, w_in, c0, 64, "wn")
        for g8 in range(4):
            P = PJ[pj_i % 4]
            pj_i += 1
            pe.wait(P.deps_w(), Wb.deps_r(), x_ready)
            tok = None
            for t8 in range(8):
                tb = g8 * 8 + t8
                for kc in range(8):
                    tok = pe.done(pe.e.matmul(P.ap[:, t8 * 64:(t8 + 1) * 64], XT.ap[:, kc, tb * 128:(tb + 1) * 128],
                                              Wb.ap[:, kc, 0:64], start=(kc == 0), stop=(kc == 7)))
            Wb.read(tok)
            XT.read(tok)
            P.wrote(tok)
            act.wait(tok, VA.deps_w())
            tv = act.done(act.e.activation(out=VA.ap[:, g8 * 8:(g8 + 1) * 8, 0:64],
                                           in_=P.ap[:, :].rearrange("p (a b) -> p a b", a=8), func=AF.Copy))
            P.read(tv)
            VA.wrote(tv, fresh=False)

        for kt in range(NT):
            nq = 256 if kt < NT - 1 else 128
            for hl in range(4):
                sb_, so = STs[it % 4]
                p = PTs[it % 4]
                it += 1
                pe.wait(sb_.deps_w(), KT.deps_r(), QT.deps_r())
                tok = pe.done(pe.e.matmul(sb_.ap[:, so:so + nq], KT.ap[0:64, kt * 128:(kt + 1) * 128],
                                          QT.ap[0:64, hl, kt * 128:kt * 128 + nq], start=True, stop=True))
                sb_.wrote(tok)
                KT.read(tok)
                QT.read(tok)
                act.wait(tok, p.deps_w())
                ta = act.done(act.e.activation(out=p.ap[:, 0:nq], in_=sb_.ap[:, so:so + nq], func=AF.Exp, scale=0.125))
                sb_.read(ta)
                p.wrote(ta)
                dve.wait(ta, tmk)
                td = dve.done(dve.e.tensor_tensor(out=p.ap[:, 0:nq], in0=p.ap[:, 0:nq], in1=MK[:, 0:nq], op=ALU.mult))
                p.wrote(td)
                for qb in ([kt, kt + 1] if kt < NT - 1 else [kt]):
                    o = OPs[qb % 3]
                    first = (qb == kt + 1) or (kt == 0)
                    last = (qb == kt)
                    if first and hl == 0:
                        pe.wait(o.deps_w())
                    pe.wait(td, VA.deps_r())
                    tok = pe.done(pe.e.matmul(o.ap[:, hl * 65:hl * 65 + 65], p.ap[:, (qb - kt) * 128:(qb - kt + 1) * 128],
                                              VA.ap[:, kt, :], start=(first and hl == 0), stop=last))
                    p.read(tok)
                    VA.read(tok)
                    o.wrote(tok, fresh=(first and hl == 0))
            o = OPs[kt % 3]
            lt = lts[kt % 4]
            o4 = o.ap[:, 0:260].rearrange("p (h c) -> p h c", h=4)
            dve.wait(o.deps_r(), tes, OG.deps_w())
            f1 = dve.done(dve.e.tensor_tensor(out=lt[:, :], in0=o4[:, :, 64], in1=es[:, 4 * j:4 * j + 4], op=ALU.add))
            dve.wait(f1)
            f2 = dve.done(dve.e.reciprocal(out=lt[:, :], in_=lt[:, :]))
            dve.wait(f2)
            f3 = dve.done(dve.e.tensor_tensor(out=OG.ap[:, kt, :].rearrange("p (h c) -> p h c", h=4), in0=o4[:, :, 0:64],
                                              in1=lt[:, :].unsqueeze(2).to_broadcast([128, 4, 64]), op=ALU.mult))
            o.read(f1)
            o.read(f3)
            OG.wrote(f3, fresh=False)
        osem = fl.dsem("ostore")
        sp.wait(OG.deps_r())
        tso = sp.dma(d["attnO"].rearrange("(t p) c -> p t c", p=128)[:, :, j * 256:(j + 1) * 256], OG.ap[:, :, :], osem)
        OG.read(tso)
        OG.w = {}
    fl.end()


def phase_outproj_ln(fl, d, attn_d, w_out_d, xres_d, ln_i, ln_j, out_d, outb_d, router_d=None, gate_d=None):
    pe, act, dve, pool, sp = fl.pe, fl.act, fl.dve, fl.pool, fl.sp
    fl.begin()
    OT = Buf(fl.sb([128, 8, S], BF16, "OT"))
    Wo = Buf(fl.sb([128, 8, D], BF16, "Wo"))
    G, Bt, tln = load_ln_params(fl, d["ln_gain"], d["ln_bias"], ln_i, ln_j, None)
    xts = [Buf(fl.sb([128, D], F32, "xt")) for _ in range(2)]
    rts = [Buf(fl.sb([128, D], F32, "rt")) for _ in range(2)]
    xns = [Buf(fl.sb([128, D], F32, "xn")) for _ in range(2)]
    ots = [Buf(fl.sb([128, D], F32, "ot")) for _ in range(2)]
    obs = [Buf(fl.sb([128, D], BF16, "ob")) for _ in range(2)]
    stats = [fl.sb([128, 2, 6], F32, "st") for _ in range(2)]
    mvs = [fl.sb([128, 2], F32, "mv") for _ in range(2)]
    rss = [fl.sb([128, 1], F32, "rs") for _ in range(2)]
    Ys = [Buf(fl.ps([128, 1024], F32, "Y")) for _ in range(2)]
    if router_d is not None:
        RB = fl.sb([128, NE, D], F32, "RB")
        rsem = fl.dsem("rb")
        trb = None
        for e in range(NE):
            trb = sp.dma(RB[:, e, :], d["routerT"][e, :].partition_broadcast(128), rsem)
        junk = fl.sb([128, D], F32, "junk")
        lgs = [fl.sb([128, 8], F32, "lg") for _ in range(2)]
        mx8 = [fl.sb([128, 8], F32, "mx8") for _ in range(2)]
        gts = [Buf(fl.sb([128, 8], F32, "gt")) for _ in range(2)]
        g2 = [fl.sb([128, 8], F32, "g2") for _ in range(2)]
        w12 = [fl.sb([128, 2], F32, "w12") for _ in range(2)]
    wsem = fl.dsem("wo")
    pool.wait(Wo.deps_w())
    tw = pool.dma(Wo.ap[:, :, :], w_out_d.rearrange("(kc p) n -> p kc n", p=128), wsem)
    Wo.wrote(tw)
    tot = load_xT(fl, OT, attn_d, None, "xt")
    xsem = [fl.dsem("xl0"), fl.dsem("xl1")]
    ssem = [fl.dsem("st0"), fl.dsem("st1")]
    for tb in range(NT):
        k = tb % 2
        xt, rt, xn, ot, ob, Y = xts[k], rts[k], xns[k], ots[k], obs[k], Ys[k]
        sp.wait(xt.deps_w())
        tx = sp.dma(xt.ap[:, :], xres_d[tb * 128:(tb + 1) * 128, :], xsem[k])
        xt.wrote(tx)
        pe.wait(Y.deps_w(), tw, tot)
        tok = None
        for half in range(2):
            for kc in range(8):
                tok = pe.done(pe.e.matmul(Y.ap[:, half * 512:(half + 1) * 512], OT.ap[:, kc, tb * 128:(tb + 1) * 128],
                                          Wo.ap[:, kc, half * 512:(half + 1) * 512], start=(kc == 0), stop=(kc == 7)))
        Y.wrote(tok)
        dve.wait(tok, tx, rt.deps_w())
        tr = None
        for half in range(2):
            sl = slice(half * 512, (half + 1) * 512)
            tr = dve.done(dve.e.scalar_tensor_tensor(out=rt.ap[:, sl], in0=xt.ap[:, sl], scalar=ALPHA, in1=Y.ap[:, sl],
                                                     op0=ALU.mult, op1=ALU.add))
        Y.read(tr)
        xt.read(tr)
        rt.wrote(tr)
        pool.wait(tln)
        t7 = layernorm_tile(fl, rt, stats[k], mvs[k], rss[k], xn, ot, G, Bt)
        sp.wait(t7)
        ts1 = sp.dma(out_d[tb * 128:(tb + 1) * 128, :], ot.ap[:, :], ssem[k])
        ot.read(ts1)
        act.wait(t7, ob.deps_w())
        tc = act.done(act.e.activation(out=ob.ap[:, :], in_=ot.ap[:, :], func=AF.Copy))
        ot.read(tc)
        ob.wrote(tc)
        sp.wait(tc)
        ts2 = sp.dma(outb_d[tb * 128:(tb + 1) * 128, :], ob.ap[:, :], ssem[k])
        ob.read(ts2)
        if router_d is not None:
            lg, m8, gt, gg, ww = lgs[k], mx8[k], gts[k], g2[k], w12[k]
            dve.wait(t7, trb)
            tl = None
            for e in range(NE):
                tl = dve.done(dve.e.scalar_tensor_tensor(out=junk[:, :], in0=ot.ap[:, :], scalar=1.0, in1=RB[:, e, :],
                                                         op0=ALU.mult, op1=ALU.mult, accum_out=lg[:, e:e + 1]))
            ot.read(tl)
            dve.wait(tl)
            tm = dve.done(dve.e.max(out=m8[:, :], in_=lg[:, :]))
            dve.wait(tm)
            t_a = dve.done(dve.e.tensor_tensor(out=ww[:, 0:1], in0=m8[:, 0:1], in1=m8[:, 1:2], op=ALU.subtract))
            t_b = dve.done(dve.e.tensor_tensor(out=ww[:, 1:2], in0=m8[:, 1:2], in1=m8[:, 0:1], op=ALU.subtract))
            act.wait(t_a, t_b)
            t_s = act.done(act.e.activation(out=ww[:, :], in_=ww[:, :], func=AF.Sigmoid))
            dve.wait(t_s, gt.deps_w())
            t_g1 = dve.done(dve.e.tensor_scalar(out=gt.ap[:, :], in0=lg[:, :], scalar1=m8[:, 0:1], scalar2=ww[:, 0:1],
                                                op0=ALU.is_equal, op1=ALU.mult))
            t_g2 = dve.done(dve.e.tensor_scalar(out=gg[:, :], in0=lg[:, :], scalar1=m8[:, 1:2], scalar2=ww[:, 1:2],
                                                op0=ALU.is_equal, op1=ALU.mult))
            dve.wait(t_g1, t_g2)
            t_g3 = dve.done(dve.e.tensor_tensor(out=gt.ap[:, :], in0=gt.ap[:, :], in1=gg[:, :], op=ALU.add))
            gt.wrote(t_g3)
            sp.wait(t_g3)
            ts3 = sp.dma(gate_d[tb * 128:(tb + 1) * 128, :], gt.ap[:, :], ssem[k])
            gt.read(ts3)
    fl.end()


def phase_ffn(fl, d, xb_d, xres_d, wg_list, wu_list, wd_list, gate_d, ln_i, ln_j, out_d, outb_d, TG=1024):
    pe, act, dve, pool, sp = fl.pe, fl.act, fl.dve, fl.pool, fl.sp
    fl.begin()
    ne = len(wg_list)
    ntile = TG // 128
    XTg = Buf(fl.sb([128, 8, TG], BF16, "XTg"))
    acc = [Buf(fl.sb([128, D], F32, "acc")) for _ in range(ntile)]
    Wg = [Buf(fl.sb([128, 8, 512], BF16, "Wg")) for _ in range(2)]
    Wu = [Buf(fl.sb([128, 8, 512], BF16, "Wu")) for _ in range(2)]
    Wd = [Buf(fl.sb([128, 4, D], BF16, "Wd")) for _ in range(2)]
    Hs = [Buf(fl.sb([128, 4, 512], BF16, "H")) for _ in range(2)]
    Ss = [Buf(fl.sb([128, 512], F32, "Ssil")) for _ in range(2)]
    G, Bt, tln = load_ln_params(fl, d["ln_gain"], d["ln_bias"], ln_i, ln_j, None)
    xts = [Buf(fl.sb([128, D], F32, "xt")) for _ in range(2)]
    xns = [Buf(fl.sb([128, D], F32, "xn")) for _ in range(2)]
    ots = [Buf(fl.sb([128, D], F32, "ot")) for _ in range(2)]
    obs = [Buf(fl.sb([128, D], BF16, "ob")) for _ in range(2)]
    stats = [fl.sb([128, 2, 6], F32, "st") for _ in range(2)]
    mvs = [fl.sb([128, 2], F32, "mv") for _ in range(2)]
    rss = [fl.sb([128, 1], F32, "rs") for _ in range(2)]
    GPs = [Buf(fl.ps([128, 512], F32, "GP")) for _ in range(2)]
    UPs = [Buf(fl.ps([128, 512], F32, "UP")) for _ in range(2)]
    Ys = [Buf(fl.ps([128, 1024], F32, "Y")) for _ in range(2)]
    if gate_d is not None:
        GT = Buf(fl.sb([128, ntile, NE], F32, "GT"))
    wsems = [fl.dsem("ffw0"), fl.dsem("ffw1")]
    xsem = [fl.dsem("xl0"), fl.dsem("xl1")]
    ssem = [fl.dsem("st0"), fl.dsem("st1")]
    gsem = fl.dsem("gl")
    wi = 0
    gi = 0
    yi = 0
    hi = 0
    ei = 0
    for tg in range(S // TG):
        t0 = tg * TG
        tx = load_xT(fl, XTg, xb_d, None, "xt", t0=t0, nt=TG)
        if gate_d is not None:
            sp.wait(GT.deps_w())
            tgl = sp.dma(GT.ap[:, :, :], gate_d[t0:t0 + TG, :].rearrange("(t p) e -> p t e", p=128), gsem)
            GT.wrote(tgl)
        for e in range(ne):
            for fg in range(FF // 512):
                k = wi % 2
                wi += 1
                wg, wu, wd = Wg[k], Wu[k], Wd[k]
                pool.wait(wg.deps_w(), wu.deps_w(), wd.deps_w())
                f0 = fg * 512
                pool.dma(wg.ap[:, :, :], wg_list[e].rearrange("(kc p) n -> p kc n", p=128)[:, :, f0:f0 + 512], wsems[k])
                pool.dma(wu.ap[:, :, :], wu_list[e].rearrange("(kc p) n -> p kc n", p=128)[:, :, f0:f0 + 512], wsems[k])
                tw = pool.dma(wd.ap[:, :, :], wd_list[e][f0:f0 + 512, :].rearrange("(fc p) n -> p fc n", p=128), wsems[k])
                wg.wrote(tw)
                wu.wrote(tw)
                wd.wrote(tw)
                first_acc = (e == 0 and fg == 0)
                for tt in range(TG // 512):
                    H = Hs[hi % 2]
                    hi += 1
                    for fc in range(4):
                        GP, UP, Sb = GPs[gi % 2], UPs[gi % 2], Ss[gi % 2]
                        gi += 1
                        for (P, Wt) in ((GP, wg), (UP, wu)):
                            pe.wait(P.deps_w(), tw, tx)
                            tok = None
                            for kc in range(8):
                                tok = pe.done(pe.e.matmul(P.ap[:, :], Wt.ap[:, kc, fc * 128:(fc + 1) * 128],
                                                          XTg.ap[:, kc, tt * 512:(tt + 1) * 512], start=(kc == 0), stop=(kc == 7)))
                            P.wrote(tok)
                            Wt.read(tok)
                            XTg.read(tok)
                        act.wait(GP.deps_r(), Sb.deps_w())
                        ta = act.done(act.e.activation(out=Sb.ap[:, :], in_=GP.ap[:, :], func=AF.Silu))
                        GP.read(ta)
                        Sb.wrote(ta)
                        dve.wait(ta, UP.deps_r(), H.deps_w())
                        th = dve.done(dve.e.tensor_tensor(out=H.ap[:, fc, :], in0=UP.ap[:, :], in1=Sb.ap[:, :], op=ALU.mult))
                        UP.read(th)
                        Sb.read(th)
                        H.wrote(th, fresh=(fc == 0))
                    for tb in range(4):
                        Y = Ys[yi % 2]
                        yi += 1
                        tile_i = tt * 4 + tb
                        pe.wait(Y.deps_w(), H.deps_r(), tw)
                        tok = None
                        for half in range(2):
                            for fc in range(4):
                                tok = pe.done(pe.e.matmul(Y.ap[:, half * 512:(half + 1) * 512], H.ap[:, fc, tb * 128:(tb + 1) * 128],
                                                          wd.ap[:, fc, half * 512:(half + 1) * 512], start=(fc == 0), stop=(fc == 3)))
                        Y.wrote(tok)
                        H.read(tok)
                        wd.read(tok)
                        a = acc[tile_i]
                        dve.wait(tok, a.deps_w())
                        ty = None
                        for half in range(2):
                            sl = slice(half * 512, (half + 1) * 512)
                            if gate_d is None:
                                if first_acc:
                                    ty = dve.done(dve.e.tensor_copy(out=a.ap[:, sl], in_=Y.ap[:, sl]))
                                else:
                                    ty = dve.done(dve.e.tensor_tensor(out=a.ap[:, sl], in0=Y.ap[:, sl], in1=a.ap[:, sl], op=ALU.add))
                            else:
                                dve.wait(GT.deps_r())
                                gsc = GT.ap[:, tile_i, e:e + 1]
                                if first_acc:
                                    ty = dve.done(dve.e.tensor_scalar(out=a.ap[:, sl], in0=Y.ap[:, sl], scalar1=gsc, scalar2=None,
                                                                      op0=ALU.mult))
                                else:
                                    ty = dve.done(dve.e.scalar_tensor_tensor(out=a.ap[:, sl], in0=Y.ap[:, sl], scalar=gsc,
                                                                             in1=a.ap[:, sl], op0=ALU.mult, op1=ALU.add))
                        Y.read(ty)
                        a.wrote(ty)
                        if gate_d is not None:
                            GT.read(ty)
        for ti in range(ntile):
            k = ei % 2
            ei += 1
            tbg = t0 // 128 + ti
            xt, xn, ot, ob = xts[k], xns[k], ots[k], obs[k]
            a = acc[ti]
            sp.wait(xt.deps_w())
            txl = sp.dma(xt.ap[:, :], xres_d[tbg * 128:(tbg + 1) * 128, :], xsem[k])
            xt.wrote(txl)
            dve.wait(txl, a.deps_r())
            tr = dve.done(dve.e.scalar_tensor_tensor(out=a.ap[:, :], in0=xt.ap[:, :], scalar=ALPHA, in1=a.ap[:, :],
                                                     op0=ALU.mult, op1=ALU.add))
            xt.read(tr)
            a.wrote(tr)
            pool.wait(tln)
            t7 = layernorm_tile(fl, a, stats[k], mvs[k], rss[k], xn, ot, G, Bt)
            sp.wait(t7)
            ts1 = sp.dma(out_d[tbg * 128:(tbg + 1) * 128, :], ot.ap[:, :], ssem[k])
            ot.read(ts1)
            if outb_d is not None:
                act.wait(t7, ob.deps_w())
                tc = act.done(act.e.activation(out=ob.ap[:, :], in_=ot.ap[:, :], func=AF.Copy))
                ot.read(tc)
                ob.wrote(tc)
                sp.wait(tc)
                ts2 = sp.dma(outb_d[tbg * 128:(tbg + 1) * 128, :], ob.ap[:, :], ssem[k])
                ob.read(ts2)
    fl.end()


NSA_FM = [(0, True), (128, True), (256, True), (384, True), (512, True), (640, True), (768, True), (896, True),
          (1024, True), (1152, True),
          (1280, False), (1408, False),
          (1536, True), (1664, True),
          (2048, True), (2176, True)]


def nsa_projT_row(c0):
    return c0 if c0 < 1792 else c0 - 256


def phase_nsa_proj(fl, d):
    pe, act, dve, pool, sp = fl.pe, fl.act, fl.dve, fl.pool, fl.sp
    fl.begin()
    XT = Buf(fl.sb([128, 8, S], BF16, "XT"))
    CT = fl.sb([128, S], F32, "CT")
    ST = fl.sb([128, S], F32, "ST")
    Wn = [Buf(fl.sb([128, 8, 128], BF16, "Wn")) for _ in range(2)]
    Ws = [Buf(fl.sb([128, 8, 128], BF16, "Ws")) for _ in range(2)]
    Wt = Buf(fl.sb([128, 8, 560], BF16, "Wt"))
    tmps = [Buf(fl.sb([128, 512], F32, "tmp")) for _ in range(4)]
    stg = [Buf(fl.sb([128, S], BF16, "stg")) for _ in range(2)]
    vst = [Buf(fl.sb([128, 512], BF16, "vst")) for _ in range(2)]
    gst = [Buf(fl.sb([128, 48], F32, "gst")) for _ in range(2)]
    PJ = [Buf(fl.ps([128, 512], F32, "bank")) for _ in range(6)]
    csem = fl.dsem("const")
    sp.dma(CT[:, :], d["cosT"][:, :], csem)
    sp.dma(ST[:, :], d["sinT"][:, :], csem)
    tc1 = (csem, csem.n)
    tx = load_xT(fl, XT, d["x2b"], None, "xt")
    x_ready = [tx, tc1]
    w_in = d["b_w_in"][0]
    projT = d["projT"]
    ssem = [fl.dsem("st0"), fl.dsem("st1")]
    pj_i = 0
    tmp_i = 0
    for ci, (c0, roped) in enumerate(NSA_FM):
        Wb, Wsb = Wn[ci % 2], Ws[ci % 2]
        sg = stg[ci % 2]
        load_w_cols(fl, Wb, w_in, c0, 128, "wn")
        if roped:
            load_w_cols(fl, Wsb, w_in, c0, 128, "ws", swap=True)
        for tt in range(8):
            if roped:
                dests = [(0, sg, sg.ap[0:64, tt * 512:(tt + 1) * 512]), (64, sg, sg.ap[64:128, tt * 512:(tt + 1) * 512])]
                rope_proj_chunk(fl, XT, Wb, Wsb, 128, tt, PJ[0:4], pj_i, CT, ST, tmps, tmp_i, dests, x_ready)
                pj_i += 1
                tmp_i += 1
            else:
                P = PJ[4 + (tt % 2)]
                pe.wait(P.deps_w(), Wb.deps_r(), x_ready)
                tok = None
                for kc in range(8):
                    tok = pe.done(pe.e.matmul(P.ap[:, :], Wb.ap[:, kc, :], XT.ap[:, kc, tt * 512:(tt + 1) * 512],
                                              start=(kc == 0), stop=(kc == 7)))
                Wb.read(tok)
                XT.read(tok)
                P.wrote(tok)
                act.wait(tok, sg.deps_w())
                tv = act.done(act.e.activation(out=sg.ap[:, tt * 512:(tt + 1) * 512], in_=P.ap[:, :], func=AF.Copy))
                P.read(tv)
                sg.wrote(tv, fresh=False)
        r0 = nsa_projT_row(c0)
        sp.wait(sg.deps_r())
        ts = sp.dma(projT[r0:r0 + 128, :], sg.ap[:, :], ssem[ci % 2])
        sg.read(ts)
        sg.w = {}
    wsem = fl.dsem("wn")
    pool.wait(Wt.deps_w())
    wsrc = w_in.rearrange("(kc p) n -> p kc n", p=128)
    pool.dma(Wt.ap[:, :, 0:256], wsrc[:, :, 1792:2048], wsem)
    pool.dma(Wt.ap[:, :, 256:512], wsrc[:, :, 2304:2560], wsem)
    tw = pool.dma(Wt.ap[:, :, 512:560], wsrc[:, :, 2560:2608], wsem)
    Wt.wrote(tw)
    for tb in range(NT):
        P = PJ[(2 * tb) % 6]
        Pg = PJ[(2 * tb + 1) % 6]
        pe.wait(P.deps_w(), Pg.deps_w(), tw, x_ready)
        tok = None
        for kc in range(8):
            tok = pe.done(pe.e.matmul(P.ap[:, :], XT.ap[:, kc, tb * 128:(tb + 1) * 128], Wt.ap[:, kc, 0:512],
                                      start=(kc == 0), stop=(kc == 7)))
        P.wrote(tok)
        tokg = None
        for kc in range(8):
            tokg = pe.done(pe.e.matmul(Pg.ap[:, 0:48], XT.ap[:, kc, tb * 128:(tb + 1) * 128], Wt.ap[:, kc, 512:560],
                                       start=(kc == 0), stop=(kc == 7)))
        Pg.wrote(tokg)
        vs_, gs_ = vst[tb % 2], gst[tb % 2]
        act.wait(tok, vs_.deps_w())
        tv = act.done(act.e.activation(out=vs_.ap[:, :], in_=P.ap[:, :], func=AF.Copy))
        P.read(tv)
        vs_.wrote(tv)
        act.wait(tokg, gs_.deps_w())
        tg_ = act.done(act.e.activation(out=gs_.ap[:, :], in_=Pg.ap[:, 0:48], func=AF.Sigmoid))
        Pg.read(tg_)
        gs_.wrote(tg_)
        sp.wait(tv, tg_)
        t1 = sp.dma(d["vtok"][tb * 128:(tb + 1) * 128, :], vs_.ap[:, :], ssem[tb % 2])
        t2 = sp.dma(d["gl"][tb * 128:(tb + 1) * 128, :], gs_.ap[:, :], ssem[tb % 2])
        vs_.read(t1)
        gs_.read(t2)
    fl.end()


def attn_qtile(fl, S_ring, s_i, PT_ring, O, Kbuf, krows, Qbuf, qsel, q0, nqb, Vbuf, W, ktiles, pe_extra_deps=()):
    pe, act, dve = fl.pe, fl.act, fl.dve
    first_mm = True
    nk = len(ktiles)
    last_for = {}
    for i, (_, _, qlo, qhi, _) in enumerate(ktiles):
        for qb in range(qlo, qhi):
            last_for[qb] = i
    for i, (kc0, vidx, qlo, qhi, masks) in enumerate(ktiles):
        Sb = S_ring[s_i % len(S_ring)]
        p = PT_ring[s_i % len(PT_ring)]
        s_i += 1
        n0, n1 = qlo * 128, qhi * 128
        pe.wait(Sb.deps_w(), Kbuf.deps_r(), Qbuf.deps_r(), pe_extra_deps)
        tok = pe.done(pe.e.matmul(Sb.ap[:, n0:n1], Kbuf.ap[0:krows, kc0:kc0 + 128],
                                  Qbuf.ap[0:krows, qsel, q0 + n0:q0 + n1], start=True, stop=True))
        Sb.wrote(tok)
        Kbuf.read(tok)
        Qbuf.read(tok)
        act.wait(tok, p.deps_w())
        ta = act.done(act.e.activation(out=p.ap[:, n0:n1], in_=Sb.ap[:, n0:n1], func=AF.Exp, scale=0.125))
        Sb.read(ta)
        p.wrote(ta)
        tlast = ta
        for (qb, mask_ap, mdeps) in masks:
            dve.wait(ta, mdeps)
            tlast = dve.done(dve.e.tensor_tensor(out=p.ap[:, qb * 128:(qb + 1) * 128], in0=p.ap[:, qb * 128:(qb + 1) * 128],
                                                 in1=mask_ap, op=ALU.mult))
            p.wrote(tlast, fresh=False)
        for qb in range(qlo, qhi):
            if first_mm:
                pe.wait(O.deps_w())
            pe.wait(p.deps_r(), Vbuf.deps_r())
            tok = pe.done(pe.e.matmul(O.ap[:, qb * W:(qb + 1) * W], p.ap[:, qb * 128:(qb + 1) * 128], Vbuf.ap[:, vidx, 0:W],
                                      start=first_mm, stop=(last_for[qb] == i)))
            O.wrote(tok, fresh=first_mm)
            first_mm = False
            p.read(tok)
            Vbuf.read(tok)
    return s_i


def phase_nsa_attn(fl, d):
    pe, act, dve, pool, sp = fl.pe, fl.act, fl.dve, fl.pool, fl.sp
    fl.begin()
    projT, vtok, gl = d["projT"], d["vtok"], d["gl"]
    QN = Buf(fl.sb([128, 4, S], BF16, "QN"))
    KE = Buf(fl.sb([128, S], BF16, "KE"))
    KW = Buf(fl.sb([64, S], BF16, "KW"))
    KC = Buf(fl.sb([64, S], BF16, "KC"))
    VC = Buf(fl.sb([64, S], BF16, "VC"))
    VS = Buf(fl.sb([128, NT, 65], BF16, "VS"))
    VW = Buf(fl.sb([128, NT, 65], BF16, "VW"))
    KCC = Buf(fl.sb([64, 256], BF16, "KCC"))
    RC = Buf(fl.sb([128, 2, 129], BF16, "RC"))
    HK = Buf(fl.sb([128, 2, 256], BF16, "HK"))
    HV = Buf(fl.sb([128, 2, 256], BF16, "HV"))
    W1K = fl.sb([64, 32, 256], BF16, "W1K")
    W1V = fl.sb([64, 32, 256], BF16, "W1V")
    W2K = fl.sb([128, 2, 64], BF16, "W2K")
    W2V = fl.sb([128, 2, 64], BF16, "W2V")
    POSK = fl.sb([64, 32], BF16, "POSK")
    POSV = fl.sb([64, 32], BF16, "POSV")
    CB = fl.sb([128, 4], F32, "CB")
    MK = fl.sb([128, 256], BF16, "MK")
    MCM = fl.sb([128, 9, 512], BF16, "MCM")
    FA = fl.sb([128, NT, 64], BF16, "FA")
    FB = fl.sb([128, NT, 64], BF16, "FB")
    IDN = fl.sb([128, 128], BF16, "IDN")
    GS = fl.sb([128, NT, 48], F32, "GS")
    IMP = Buf(fl.sb([128, NT, 64], F32, "IMP"))
    OG = Buf(fl.sb([128, NT, 256], F32, "OG"))
    NBT = [Buf(fl.sb([128, 128], BF16, "NBT")) for _ in range(2)]
    PTs = [Buf(fl.sb([128, 512], BF16, "PT")) for _ in range(3)]
    smalls = [fl.sb([128, 16], F32, "sm") for _ in range(4)]
    otmp = [Buf(fl.sb([128, 4, 64], F32, "otmp")) for _ in range(2)]
    im2 = [fl.sb([128, 64], F32, "im2") for _ in range(2)]
    imm = [fl.sb([128, 64], F32, "imm") for _ in range(2)]
    m8 = [fl.sb([128, 16], F32, "m8") for _ in range(2)]
    S_ring = [Buf(fl.ps([128, 512], F32, "Sb")) for _ in range(3)]
    O_ring = [Buf(fl.ps([128, 512], F32, "Ob")) for _ in range(2)]
    TP = Buf(fl.ps([128, 512], BF16, "TP"))
    CPS = [Buf(fl.ps([128, 512], F32, "CPS")) for _ in range(2)]

    csem = fl.dsem("const")
    cpsem = fl.dsem("constp")
    pool.dma(MK[:, :], d["swa_mask"][:, :], cpsem)
    pool.dma(MCM[:, :, :], d["cmp_mask"].rearrange("n p c -> p n c"), cpsem)
    pool.dma(FA[:, :, :], d["sel_fa"].rearrange("(t p) c -> p t c", p=128), cpsem)
    pool.dma(FB[:, :, :], d["sel_fb"].rearrange("(t p) c -> p t c", p=128), cpsem)
    pool.dma(IDN[:, :], d["ident"][:, :], cpsem)
    pool.dma(KE.ap[64:128, :], d["emat"][:, :], cpsem)
    pool.dma(RC.ap[:, :, 65:129], d["ov_pad"].rearrange("(c p) j -> p c j", p=128), cpsem)
    pool.dma(W1K[:, :, :], d["b_cmp_k_w1"][0].rearrange("(l dd) m -> dd l m", dd=64), cpsem)
    pool.dma(W1V[:, :, :], d["b_cmp_v_w1"][0].rearrange("(l dd) m -> dd l m", dd=64), cpsem)
    pool.dma(W2K[:, :, :], d["b_cmp_k_w2"][0].rearrange("(mc p) dd -> p mc dd", p=128), cpsem)
    pool.dma(W2V[:, :, :], d["b_cmp_v_w2"][0].rearrange("(mc p) dd -> p mc dd", p=128), cpsem)
    pool.dma(POSK[:, :], d["posT_k"][:, :], cpsem)
    pool.dma(POSV[:, :], d["posT_v"][:, :], cpsem)
    tcp = (cpsem, cpsem.n + 0)
    tgs = sp.dma(GS[:, :, :], gl.rearrange("(t p) c -> p t c", p=128), csem)
    dve.wait(tcp)
    t_m = dve.done(dve.e.memset(VS.ap[:, :, 64:65], 1.0))
    t_m = dve.done(dve.e.memset(VW.ap[:, :, 64:65], 1.0))
    t_m = dve.done(dve.e.memset(RC.ap[:, :, 64:65], 1.0))
    t_m = dve.done(dve.e.memset(HK.ap[:, :, :], 0.0))
    t_m = dve.done(dve.e.memset(HV.ap[:, :, :], 0.0))
    t_m = dve.done(dve.e.memset(KCC.ap[:, :], 0.0))
    for nb in NBT:
        t_m = dve.done(dve.e.memset(nb.ap[:, :], 0.0))
    t_init = t_m
    pe.wait(tcp)
    P = CPS[0]
    tok = None
    for which, (W1, POS) in enumerate(((W1K, POSK), (W1V, POSV))):
        for mc in range(2):
            col = which * 2 + mc
            for l in range(32):
                tok = pe.done(pe.e.matmul(P.ap[:, col:col + 1], W1[0:64, l, mc * 128:(mc + 1) * 128], POS[0:64, l:l + 1],
                                          start=(l == 0 and col == 0), stop=(l == 31)))
    P.wrote(tok)
    dve.wait(tok)
    t_cb = dve.done(dve.e.tensor_copy(out=CB[:, :], in_=P.ap[:, 0:4]))
    P.read(t_cb)

    lsem = [fl.dsem("xl0"), fl.dsem("xl1")]
    osem = fl.dsem("ostore")
    s_i = 0
    o_i = 0
    sm_i = 0
    for j in range(4):
        sp.wait(QN.deps_w(), KE.deps_w(), KW.deps_w(), KC.deps_w(), VC.deps_w(), VS.deps_w(), VW.deps_w(), t_init)
        ls = lsem[j % 2]
        for hl in range(4):
            sp.dma(QN.ap[0:64, hl, :], projT[(4 * j + hl) * 64:(4 * j + hl + 1) * 64, :], ls)
        sp.dma(KC.ap[0:64, :], projT[1024 + j * 64:1024 + (j + 1) * 64, :], ls)
        sp.dma(VC.ap[0:64, :], projT[1280 + j * 64:1280 + (j + 1) * 64, :], ls)
        sp.dma(KE.ap[0:64, :], projT[1536 + j * 64:1536 + (j + 1) * 64, :], ls)
        sp.dma(KW.ap[0:64, :], projT[1792 + j * 64:1792 + (j + 1) * 64, :], ls)
        vt3 = vtok.rearrange("(t p) c -> p t c", p=128)
        sp.dma(VS.ap[:, :, 0:64], vt3[:, :, j * 64:(j + 1) * 64], ls)
        tl = sp.dma(VW.ap[:, :, 0:64], vt3[:, :, 256 + j * 64:256 + (j + 1) * 64], ls)
        for b in (QN, KE, KW, KC, VC, VS, VW):
            b.wrote(tl, fresh=False)
            b.r = {}
        ld = [tl, tcp, t_init]

        for which, (src, W1, W2, Hb) in enumerate(((KC, W1K, W2K, HK), (VC, W1V, W2V, HV))):
            for mc in range(2):
                P = CPS[(which * 2 + mc) % 2]
                pe.wait(P.deps_w(), ld)
                tok = None
                for l in range(32):
                    tok = pe.done(pe.e.matmul(P.ap[:, 0:255], W1[0:64, l, mc * 128:(mc + 1) * 128], src.ap[0:64, l:l + 4065:16],
                                              start=(l == 0), stop=(l == 31)))
                P.wrote(tok)
                src.read(tok)
                act.wait(tok, t_cb, Hb.deps_w(), t_init)
                th = act.done(act.e.activation(out=Hb.ap[:, mc, 0:255], in_=P.ap[:, 0:255], func=AF.Gelu_apprx_tanh,
                                               bias=CB[:, which * 2 + mc:which * 2 + mc + 1]))
                P.read(th)
                Hb.wrote(th, fresh=False)
        P = CPS[0]
        pe.wait(P.deps_w(), HK.deps_r())
        tok = None
        for mc in range(2):
            tok = pe.done(pe.e.matmul(P.ap[0:64, 0:255], W2K[:, mc, :], HK.ap[:, mc, 0:255], start=(mc == 0), stop=(mc == 1)))
        P.wrote(tok)
        HK.read(tok)
        act.wait(tok, KCC.deps_w())
        tk = act.done(act.e.activation(out=KCC.ap[0:64, 0:255], in_=P.ap[0:64, 0:255], func=AF.Copy))
        P.read(tk)
        KCC.wrote(tk)
        P = CPS[1]
        pe.wait(P.deps_w(), HV.deps_r())
        tok = None
        for ct in range(2):
            for mc in range(2):
                tok = pe.done(pe.e.matmul(P.ap[:, ct * 64:(ct + 1) * 64], HV.ap[:, mc, ct * 128:(ct + 1) * 128], W2V[:, mc, :],
                                          start=(mc == 0 and ct == 0), stop=(mc == 1)))
        P.wrote(tok)
        HV.read(tok)
        act.wait(tok, RC.deps_w())
        tr_ = act.done(act.e.activation(out=RC.ap[:, :, 0:64], in_=P.ap[:, 0:128].rearrange("p (c x) -> p c x", c=2), func=AF.Copy))
        P.read(tr_)
        RC.wrote(tr_)
        HK.w = {}
        HV.w = {}

        mcm_idx = {(0, 0): 0, (0, 1): 1, (0, 2): 2, (0, 3): 3, (0, 4): 4, (1, 4): 5, (1, 5): 6, (1, 6): 7, (1, 7): 8}
        for hl in range(4):
            h = 4 * j + hl
            for qh in range(16):
                tt = qh // 2
                q0 = qh * 256
                ktl = []
                for ct in range(2):
                    if ct == 1 and tt <= 3:
                        continue
                    masks = []
                    if (ct, tt) in mcm_idx:
                        mi = mcm_idx[(ct, tt)]
                        off = (qh % 2) * 256
                        masks = [(0, MCM[:, mi, off:off + 128], tcp), (1, MCM[:, mi, off + 128:off + 256], tcp)]
                    ktl.append((ct * 128, ct, 0, 2, masks))
                O = O_ring[o_i % 2]
                o_i += 1
                s_i = attn_qtile(fl, S_ring, s_i, PTs, O, KCC, 64, QN, hl, q0, 2, RC, 129, ktl)
                sm = smalls[sm_i % 4]
                sm_i += 1
                O3 = O.ap[:, 0:258].rearrange("p (b w) -> p b w", b=2)
                tile0 = qh * 2
                dve.wait(O.deps_r(), tgs, OG.deps_w(), IMP.deps_w())
                f1 = dve.done(dve.e.tensor_scalar(out=sm[:, 0:2], in0=O3[:, :, 64], scalar1=1e-30, scalar2=None, op0=ALU.max))
                dve.wait(f1)
                f2 = dve.done(dve.e.reciprocal(out=sm[:, 0:2], in_=sm[:, 0:2]))
                dve.wait(f2)
                f3 = dve.done(dve.e.tensor_tensor(out=sm[:, 2:4], in0=sm[:, 0:2], in1=GS[:, tile0:tile0 + 2, h * 3 + 0], op=ALU.mult))
                dve.wait(f3)
                f4 = dve.done(dve.e.tensor_tensor(out=OG.ap[:, tile0:tile0 + 2, hl * 64:(hl + 1) * 64], in0=O3[:, :, 0:64],
                                                  in1=sm[:, 2:4].unsqueeze(2).to_broadcast([128, 2, 64]), op=ALU.mult))
                OG.wrote(f4, fresh=False)
                f5 = None
                for b2 in range(2):
                    if hl == 0:
                        f5 = dve.done(dve.e.tensor_scalar(out=IMP.ap[:, tile0 + b2, :], in0=O3[:, b2, 65:129], scalar1=sm[:, b2:b2 + 1],
                                                          scalar2=None, op0=ALU.mult))
                    else:
                        f5 = dve.done(dve.e.scalar_tensor_tensor(out=IMP.ap[:, tile0 + b2, :], in0=O3[:, b2, 65:129],
                                                                 scalar=sm[:, b2:b2 + 1], in1=IMP.ap[:, tile0 + b2, :],
                                                                 op0=ALU.mult, op1=ALU.add))
                IMP.wrote(f5, fresh=False)
                O.read(f1)
                O.read(f5)

        for tile in range(NT):
            k2 = tile % 2
            i2, im_, mm = im2[k2], imm[k2], m8[k2]
            nb = NBT[k2]
            dve.wait(IMP.deps_r(), tcp)
            g1 = dve.done(dve.e.tensor_tensor(out=im_[:, :], in0=IMP.ap[:, tile, :], in1=FA[:, tile, :], op=ALU.mult))
            dve.wait(g1)
            g2 = dve.done(dve.e.tensor_tensor(out=im_[:, :], in0=im_[:, :], in1=FB[:, tile, :], op=ALU.add))
            dve.wait(g2)
            g3 = dve.done(dve.e.max(out=mm[:, 0:8], in_=im_[:, :]))
            dve.wait(g3)
            g4 = dve.done(dve.e.match_replace(out=i2[:, :], in_to_replace=mm[:, 0:8], in_values=im_[:, :], imm_value=-2.0))
            dve.wait(g4)
            g5 = dve.done(dve.e.max(out=mm[:, 8:16], in_=i2[:, :]))
            dve.wait(g5)
            g6 = dve.done(dve.e.tensor_scalar(out=mm[:, 0:1], in0=mm[:, 15:16], scalar1=0.0, scalar2=None, op0=ALU.max))
            dve.wait(g6, nb.deps_w())
            g7 = dve.done(dve.e.tensor_scalar(out=nb.ap[:, 64:128], in0=im_[:, :], scalar1=mm[:, 0:1], scalar2=1.0,
                                              op0=ALU.is_ge, op1=ALU.subtract))
            nb.wrote(g7)
            IMP.read(g7)
            pe.wait(g7, TP.deps_w(), tcp)
            tt_ = pe.done(pe.e.transpose(TP.ap[:, 0:128], nb.ap[:, :], IDN[:, :]))
            nb.read(tt_)
            TP.wrote(tt_)
            act.wait(tt_, QN.deps_w())
            tq = act.done(act.e.activation(out=QN.ap[64:128, :, tile * 128:(tile + 1) * 128],
                                           in_=TP.ap[64:128, 0:128].unsqueeze(1).to_broadcast([64, 4, 128]), func=AF.Copy))
            TP.read(tq)
            QN.wrote(tq, fresh=False)

        for br in (2, 1):
            for hl in range(4):
                h = 4 * j + hl
                for tt in range(8):
                    ktl = []
                    if br == 2:
                        for kt in range(max(0, 4 * tt - 4), 4 * tt + 4):
                            qlo = max(4 * tt, kt) - 4 * tt
                            qhi = min(4 * tt + 3, kt + 4) - 4 * tt + 1
                            masks = []
                            if 4 * tt <= kt:
                                masks.append((kt - 4 * tt, MK[:, 0:128], tcp))
                            if kt + 4 <= 4 * tt + 3 and kt + 4 >= 4 * tt:
                                masks.append((kt + 4 - 4 * tt, MK[:, 128:256], tcp))
                            ktl.append((kt * 128, kt, qlo, qhi, masks))
                        Kb, krows, Vb = KW, 64, VW
                    else:
                        for kt in range(0, 4 * tt + 4):
                            qlo = max(4 * tt, kt) - 4 * tt
                            masks = []
                            if kt >= 4 * tt:
                                masks.append((kt - 4 * tt, MK[:, 0:128], tcp))
                            ktl.append((kt * 128, kt, qlo, 4, masks))
                        Kb, krows, Vb = KE, 128, VS
                    O = O_ring[o_i % 2]
                    o_i += 1
                    s_i = attn_qtile(fl, S_ring, s_i, PTs, O, Kb, krows, QN, hl, tt * 512, 4, Vb, 65, ktl)
                    sm = smalls[sm_i % 4]
                    sm_i += 1
                    ot_ = otmp[sm_i % 2]
                    O3 = O.ap[:, 0:260].rearrange("p (b w) -> p b w", b=4)
                    dve.wait(O.deps_r(), tgs, ot_.deps_w())
                    f1 = dve.done(dve.e.tensor_scalar(out=sm[:, 0:4], in0=O3[:, :, 64], scalar1=1e-30, scalar2=None, op0=ALU.max))
                    dve.wait(f1)
                    f2 = dve.done(dve.e.reciprocal(out=sm[:, 0:4], in_=sm[:, 0:4]))
                    dve.wait(f2)
                    f3 = dve.done(dve.e.tensor_tensor(out=sm[:, 4:8], in0=sm[:, 0:4], in1=GS[:, 4 * tt:4 * tt + 4, h * 3 + br], op=ALU.mult))
                    dve.wait(f3)
                    f4 = dve.done(dve.e.tensor_tensor(out=ot_.ap[:, :, :], in0=O3[:, :, 0:64],
                                                      in1=sm[:, 4:8].unsqueeze(2).to_broadcast([128, 4, 64]), op=ALU.mult))
                    O.read(f1)
                    O.read(f4)
                    ot_.wrote(f4)
                    pool.wait(f4, OG.deps_r())
                    f5 = pool.done(pool.e.tensor_tensor(out=OG.ap[:, 4 * tt:4 * tt + 4, hl * 64:(hl + 1) * 64],
                                                        in0=OG.ap[:, 4 * tt:4 * tt + 4, hl * 64:(hl + 1) * 64], in1=ot_.ap[:, :, :], op=ALU.add))
                    ot_.read(f5)
                    OG.wrote(f5, fresh=False)
        pool.wait(OG.deps_r())
        tso = pool.dma(d["attnO"].rearrange("(t p) c -> p t c", p=128)[:, :, j * 256:(j + 1) * 256], OG.ap[:, :, :], osem)
        OG.w = {}
        OG.r = {}
        OG.read(tso)
        IMP.r = {}
    fl.end()


def host_constants():
    inv = (1.0 / (np.float32(10000.0) ** (np.arange(0, 64, 2, dtype=np.float32) / np.float32(64)))).astype(np.float32)
    ang = np.arange(S, dtype=np.float32)[:, None] * inv[None, :]
    cos = np.cos(ang).astype(np.float32).T
    sin = np.sin(ang).astype(np.float32).T
    cosT = np.concatenate([cos, cos, cos, cos], axis=0)
    sinT = np.concatenate([-sin, sin, -sin, sin], axis=0)
    k = np.arange(128)[:, None]
    c = np.arange(256)[None, :]
    swa_mask = np.where(c < 128, c >= k, (c - 128) < k).astype(np.float32)
    out = {"cosT": np.ascontiguousarray(cosT), "sinT": np.ascontiguousarray(sinT), "swa_mask": swa_mask}
    cidx = np.arange(256)
    cs = 16 * cidx
    ce = cs + 32
    ss = 64 * np.arange(64)
    se = ss + 64
    ov = np.clip(np.minimum(ce[:, None], se[None, :]) - np.maximum(cs[:, None], ss[None, :]), 0, None) / 16.0
    ov[255, :] = 0.0
    out["ov_pad"] = ov.astype(np.float32)
    tiles = [(0, 0), (0, 1), (0, 2), (0, 3), (0, 4), (1, 4), (1, 5), (1, 6), (1, 7)]
    cm = np.zeros((9, 128, 512), np.float32)
    for i, (ct, tt) in enumerate(tiles):
        cc = ct * 128 + np.arange(128)[:, None]
        t = tt * 512 + np.arange(512)[None, :]
        cm[i] = ((16 * cc + 31 <= t) & (cc < 255)).astype(np.float32)
    out["cmp_mask"] = cm
    t = np.arange(S)[:, None]
    b = np.arange(64)[None, :]
    cur = t // 64
    forced = (b == 0) | (b == cur) | (b == cur - 1)
    fa = ((b <= cur) & (~forced)).astype(np.float32)
    fb = np.where(b > cur, -1.0, 0.0).astype(np.float32)
    fb = np.where(b == cur - 1, 1.0e4, fb)
    fb = np.where(b == cur, 2.0e4, fb)
    fb = np.where(b == 0, 3.0e4, fb).astype(np.float32)
    out["sel_fa"] = fa
    out["sel_fb"] = fb
    out["ident"] = np.eye(128, dtype=np.float32)
    em = np.zeros((64, S), np.float32)
    em[np.arange(S) // 64, np.arange(S)] = 30000.0
    out["emat"] = em
    return out


INPUT_SHAPES = {
    "a_w_in": [1, 1024, 1536], "a_w_out": [1, 1024, 1024], "a_sinks": [1, 16],
    "b_w_in": [1, 1024, 2608], "b_w_out": [1, 1024, 1024], "b_cmp_pos_k": [1, 32, 64], "b_cmp_pos_v": [1, 32, 64],
    "b_cmp_k_w1": [1, 2048, 256], "b_cmp_k_w2": [1, 256, 64], "b_cmp_v_w1": [1, 2048, 256], "b_cmp_v_w2": [1, 256, 64],
    "ffn_w_gate": [1, 1024, 3584], "ffn_w_up": [1, 1024, 3584], "ffn_w_down": [1, 3584, 1024],
    "moe_router": [1, 1024, 8], "moe_w_gate": [1, 8, 1024, 3584], "moe_w_up": [1, 8, 1024, 3584],
    "moe_w_down": [1, 8, 3584, 1024], "ln_gain": [2, 2, 1024], "ln_bias": [2, 2, 1024],
}


def _needed(k, stop_after):
    if stop_after >= 4:
        return True
    if k.startswith("moe_w"):
        return False
    if stop_after <= 2 and (k.startswith("b_") or k.startswith("moe")):
        return False
    if stop_after <= 1 and k.startswith("ffn"):
        return False
    return True


def build(stop_after=99):
    nc = bass.Bass("TRN2", target_bir_lowering=False)
    d = {}
    d["x"] = nc.dram_tensor("x", [S, D], F32, kind="ExternalInput").ap()
    for k, shp in INPUT_SHAPES.items():
        if not _needed(k, stop_after):
            continue
        d[k] = nc.dram_tensor(k, shp, F32, kind="ExternalInput").ap()
    hc = host_constants()
    for k, v in hc.items():
        d[k] = nc.dram_tensor(k, list(v.shape), F32, kind="ExternalInput").ap()
    d["routerT"] = nc.dram_tensor("routerT", [NE, D], F32, kind="ExternalInput").ap()
    d["posT_k"] = nc.dram_tensor("posT_k", [64, 32], F32, kind="ExternalInput").ap()
    d["posT_v"] = nc.dram_tensor("posT_v", [64, 32], F32, kind="ExternalInput").ap()
    d["projT"] = nc.dram_tensor("projT", [2048, S], BF16, kind="Internal").ap()
    d["vtok"] = nc.dram_tensor("vtok", [S, 512], BF16, kind="Internal").ap()
    d["gl"] = nc.dram_tensor("gl", [S, 48], F32, kind="Internal").ap()
    y = nc.dram_tensor("y", [S, D], F32, kind="ExternalOutput").ap()
    for nm in ("xb0", "attnO", "x1b", "x2b", "x3b"):
        d[nm] = nc.dram_tensor(nm, [S, D], BF16, kind="Internal").ap()
    for nm in ("x1", "x2", "x3"):
        d[nm] = nc.dram_tensor(nm, [S, D], F32, kind="Internal").ap()
    d["gate"] = nc.dram_tensor("gate", [S, NE], F32, kind="Internal").ap()
    fl = Flow(nc)
    d["t_castx"] = phase_cast_x(fl, d["x"], d["xb0"])
    phase_l0_attn(fl, d)
    phase_outproj_ln(fl, d, d["attnO"], d["a_w_out"][0], d["x"], 0, 0, y if stop_after == 1 else d["x1"], d["x1b"])
    if stop_after >= 2:
        phase_ffn(fl, d, d["x1b"], d["x1"], [d["ffn_w_gate"][0]], [d["ffn_w_up"][0]], [d["ffn_w_down"][0]], None, 0, 1,
                  y if stop_after == 2 else d["x2"], d["x2b"])
    if stop_after >= 3:
        phase_nsa_proj(fl, d)
        phase_nsa_attn(fl, d)
        phase_outproj_ln(fl, d, d["attnO"], d["b_w_out"][0], d["x2"], 1, 0, y if stop_after == 3 else d["x3"], d["x3b"],
                         router_d=d["moe_router"][0], gate_d=d["gate"])
    if stop_after >= 4:
        phase_ffn(fl, d, d["x3b"], d["x3"], [d["moe_w_gate"][0, e] for e in range(NE)], [d["moe_w_up"][0, e] for e in range(NE)],
                  [d["moe_w_down"][0, e] for e in range(NE)], d["gate"], 1, 1, y, None)
    fl.barrier()
    fl.gstack.close()
    return nc


_CACHE = {}


def kernel(**inputs):
    stop_after = int(inputs.pop("_stop_after", 99))
    if stop_after not in _CACHE:
        _CACHE[stop_after] = build(stop_after)
    nc = _CACHE[stop_after]
    hc = host_constants()
    shared = {k: np.ascontiguousarray(np.asarray(inputs[k], dtype=np.float32)) for k in INPUT_SHAPES if _needed(k, stop_after)}
    shared.update(hc)
    shared["routerT"] = np.ascontiguousarray(np.asarray(inputs["moe_router"], dtype=np.float32)[0].T)
    shared["posT_k"] = np.ascontiguousarray(np.asarray(inputs["b_cmp_pos_k"], dtype=np.float32)[0].T)
    shared["posT_v"] = np.ascontiguousarray(np.asarray(inputs["b_cmp_pos_v"], dtype=np.float32)[0].T)
    x = np.asarray(inputs["x"], dtype=np.float32)
    in_maps = []
    for c in range(NCORES):
        m = dict(shared)
        m["x"] = np.ascontiguousarray(x[c])
        in_maps.append(m)
    res = run_bass_kernel_spmd(nc, in_maps, core_ids=list(range(NCORES)))
    return np.stack([np.asarray(r["y"], dtype=np.float32) for r in res.results], axis=0)
```

```python
import numpy as np
import ml_dtypes
from contextlib import ExitStack
import concourse.bass as bass
import concourse.mybir as mybir
from concourse.bass_utils import run_bass_kernel_spmd

F32 = mybir.dt.float32
BF16 = mybir.dt.bfloat16
AF = mybir.ActivationFunctionType
ALU = mybir.AluOpType

S = 4096
D = 1024
NT = 32
FF = 3584
NE = 8
ALPHA = float(4.0 ** 0.25)
EPS = 1e-5
NCORES = 8


class SemObj:
    def __init__(self, h):
        self.h = h
        self.n = 0


class Buf:
    def __init__(self, ap):
        self.ap = ap
        self.w = {}
        self.r = {}

    @staticmethod
    def _add(d, tok):
        if tok is None:
            return
        s, v = tok
        if d.get(s, 0) < v:
            d[s] = v

    def deps_r(self):
        return list(self.w.items())

    def deps_w(self):
        return list(self.w.items()) + list(self.r.items())

    def wrote(self, tok, fresh=True):
        if fresh:
            self.w = {}
            self.r = {}
        self._add(self.w, tok)

    def read(self, tok):
        self._add(self.r, tok)


class Eng:
    def __init__(self, fl, name, eng):
        self.fl = fl
        self.name = name
        self.e = eng
        self.sem = SemObj(fl.gstack.enter_context(fl.nc.semaphore("s_" + name)))
        self.seen = {}

    def wait(self, *toks):
        for tok in toks:
            if tok is None:
                continue
            if isinstance(tok, list) or (isinstance(tok, tuple) and (len(tok) != 2 or not isinstance(tok[0], SemObj))):
                self.wait(*tok)
                continue
            sem, v = tok
            if sem is None or v <= 0:
                continue
            if self.seen.get(sem, 0) >= v:
                continue
            if sem is self.sem and self.name == "pe":
                continue
            self.e.wait_ge(sem.h, v)
            self.seen[sem] = v

    def done(self, ins):
        self.sem.n += 1
        ins.then_inc(self.sem.h, 1)
        return (self.sem, self.sem.n)

    def dma(self, out, in_, sem, **kw):
        ins = self.e.dma_start(out=out, in_=in_, **kw)
        sem.n += 16
        ins.then_inc(sem.h, 16)
        return (sem, sem.n)


class Flow:
    def __init__(self, nc):
        self.nc = nc
        self.gstack = ExitStack()
        self.pe = Eng(self, "pe", nc.tensor)
        self.act = Eng(self, "act", nc.scalar)
        self.dve = Eng(self, "dve", nc.vector)
        self.pool = Eng(self, "pool", nc.gpsimd)
        self.sp = Eng(self, "sp", nc.sync)
        self.engines = [self.pe, self.act, self.dve, self.pool, self.sp]
        self.dsems = {}
        self.pstack = None
        self.uid = 0

    def dsem(self, name):
        if name not in self.dsems:
            self.dsems[name] = SemObj(self.gstack.enter_context(self.nc.semaphore("d_" + name)))
        return self.dsems[name]

    def begin(self):
        self.pstack = ExitStack()

    def end(self):
        self.barrier()
        self.pstack.close()
        self.pstack = None

    def barrier(self):
        toks = [(e.sem, e.sem.n) for e in self.engines if e.sem.n > 0]
        toks += [(s, s.n) for s in self.dsems.values() if s.n > 0]
        for e in self.engines:
            e.wait([t for t in toks if t[0] is not e.sem])

    def sb(self, shape, dt, name=None):
        self.uid += 1
        return self.pstack.enter_context(self.nc.sbuf_tensor(f"{name or 'sb'}_{self.uid}", list(shape), dt))

    def ps(self, shape, dt=F32, name=None):
        self.uid += 1
        return self.pstack.enter_context(self.nc.psum_tensor(f"{name or 'ps'}_{self.uid}", list(shape), dt))


def load_xT(fl, XT, xb_d, deps, semname, ncols=D, t0=0, nt=S):
    sp = fl.sp
    sem = fl.dsem(semname)
    sp.wait(deps, XT.deps_w())
    tok = None
    step = 1024 if nt >= 1024 else nt
    for kc in range(ncols // 128):
        for q in range(nt // step):
            tok = sp.dma(XT.ap[:, kc, q * step:(q + 1) * step],
                         xb_d[t0 + q * step:t0 + (q + 1) * step, kc * 128:(kc + 1) * 128], sem, transpose=True)
    XT.wrote(tok)
    return tok


def layernorm_tile(fl, r, stats, mv, rs, xn, out_t, G, Bt):
    dve, pool = fl.dve, fl.pool
    dve.wait(r.deps_r())
    t1 = dve.done(dve.e.bn_stats(out=stats[:, 0, :], in_=r.ap[:, 0:512]))
    t2 = dve.done(dve.e.bn_stats(out=stats[:, 1, :], in_=r.ap[:, 512:1024]))
    dve.wait(t1, t2)
    t3 = dve.done(dve.e.bn_aggr(out=mv[:, :], in_=stats[:, :, :].rearrange("p a b -> p (a b)")))
    dve.wait(t3)
    t4a = dve.done(dve.e.tensor_scalar(out=rs[:, :], in0=mv[:, 1:2], scalar1=EPS, scalar2=None, op0=ALU.add))
    fl.act.wait(t4a)
    t4b = fl.act.done(fl.act.e.activation(out=rs[:, :], in_=rs[:, :], func=AF.Sqrt))
    dve.wait(t4b)
    t4 = dve.done(dve.e.reciprocal(out=rs[:, :], in_=rs[:, :]))
    dve.wait(t4, xn.deps_w())
    t5 = dve.done(dve.e.tensor_scalar(out=xn.ap[:, :], in0=r.ap[:, :], scalar1=mv[:, 0:1], scalar2=rs[:, 0:1],
                                      op0=ALU.subtract, op1=ALU.mult))
    r.read(t5)
    xn.wrote(t5)
    pool.wait(t5, out_t.deps_w())
    t6 = pool.done(pool.e.tensor_tensor(out=xn.ap[:, :], in0=xn.ap[:, :], in1=G[:, :], op=ALU.mult))
    pool.wait(t6)
    t7 = pool.done(pool.e.tensor_tensor(out=out_t.ap[:, :], in0=xn.ap[:, :], in1=Bt[:, :], op=ALU.add))
    xn.read(t7)
    xn.wrote(t6, fresh=False)
    out_t.wrote(t7)
    return t7


def load_ln_params(fl, ln_gain_d, ln_bias_d, i, j, ready):
    G = fl.sb([128, D], F32, "lnG")
    Bt = fl.sb([128, D], F32, "lnB")
    sem = fl.dsem("lnp")
    fl.sp.wait(ready)
    fl.sp.dma(G[:, :], ln_gain_d[i, j, :].partition_broadcast(128), sem)
    tok = fl.sp.dma(Bt[:, :], ln_bias_d[i, j, :].partition_broadcast(128), sem)
    return G, Bt, tok


def phase_cast_x(fl, x_d, xb_d):
    sem = fl.dsem("castx")
    tok = None
    for q in range(4):
        tok = fl.pool.dma(xb_d[q * 1024:(q + 1) * 1024, :], x_d[q * 1024:(q + 1) * 1024, :], sem)
    return tok


def rope_proj_chunk(fl, XT, W, Wsw, M, tt, PJ, pj_i, CT, ST, tmps, tmp_i, dests, x_ready):
    pe, dve, pool = fl.pe, fl.dve, fl.pool
    P1 = PJ[(2 * pj_i) % len(PJ)]
    P2 = PJ[(2 * pj_i + 1) % len(PJ)]
    for (P, Wt) in ((P1, W), (P2, Wsw)):
        pe.wait(P.deps_w(), Wt.deps_r(), x_ready)
        tok = None
        for kc in range(8):
            tok = pe.done(pe.e.matmul(P.ap[0:M, :], Wt.ap[:, kc, 0:M], XT.ap[:, kc, tt * 512:(tt + 1) * 512],
                                      start=(kc == 0), stop=(kc == 7)))
        Wt.read(tok)
        XT.read(tok)
        P.wrote(tok)
    T1 = tmps[(2 * tmp_i) % len(tmps)]
    T2 = tmps[(2 * tmp_i + 1) % len(tmps)]
    dve.wait(P1.deps_r(), T1.deps_w())
    t1 = dve.done(dve.e.tensor_tensor(out=T1.ap[0:M, :], in0=P1.ap[0:M, :], in1=CT[0:M, tt * 512:(tt + 1) * 512], op=ALU.mult))
    P1.read(t1)
    T1.wrote(t1)
    dve.wait(P2.deps_r(), T2.deps_w())
    t2 = dve.done(dve.e.tensor_tensor(out=T2.ap[0:M, :], in0=P2.ap[0:M, :], in1=ST[0:M, tt * 512:(tt + 1) * 512], op=ALU.mult))
    P2.read(t2)
    T2.wrote(t2)
    for (row0, dbuf, out_ap) in dests:
        pool.wait(t1, t2, dbuf.deps_w())
        t3 = pool.done(pool.e.tensor_tensor(out=out_ap, in0=T1.ap[row0:row0 + 64, :], in1=T2.ap[row0:row0 + 64, :], op=ALU.add))
        T1.read(t3)
        T2.read(t3)
        dbuf.wrote(t3, fresh=False)


def load_w_cols(fl, Wb, w_d, c0, M, semname, swap=False):
    pool = fl.pool
    sem = fl.dsem(semname)
    pool.wait(Wb.deps_w())
    if not swap:
        src = w_d.rearrange("(kc p) n -> p kc n", p=128)[:, :, c0:c0 + M]
        tok = pool.dma(Wb.ap[:, :, 0:M], src, sem)
    else:
        nh = M // 64
        ncol = w_d.shape[1]
        src5 = w_d[:, 0:(ncol // 64) * 64].rearrange("(kc p) (h two i) -> p kc h two i", p=128, two=2, i=32)
        dst5 = Wb.ap[:, :, 0:M].rearrange("p kc (h two i) -> p kc h two i", two=2, i=32)
        h0 = c0 // 64
        tok = None
        for two in range(2):
            for hh in range(nh):
                tok = pool.dma(dst5[:, :, hh, two, :], src5[:, :, h0 + hh, 1 - two, :], sem)
    Wb.wrote(tok)
    return tok


def phase_l0_attn(fl, d):
    nc = fl.nc
    pe, act, dve, pool, sp = fl.pe, fl.act, fl.dve, fl.pool, fl.sp
    fl.begin()
    XT = Buf(fl.sb([128, 8, S], BF16, "XT"))
    CT = fl.sb([128, S], F32, "CT")
    ST = fl.sb([128, S], F32, "ST")
    MK = fl.sb([128, 256], BF16, "MK")
    es = fl.sb([128, 16], F32, "es")
    KT = Buf(fl.sb([64, S], BF16, "KT"))
    VA = Buf(fl.sb([128, NT, 65], BF16, "VA"))
    QT = Buf(fl.sb([64, 4, S], BF16, "QT"))
    OG = Buf(fl.sb([128, NT, 256], BF16, "OG"))
    Wn = [Buf(fl.sb([128, 8, 128], BF16, "Wn")) for _ in range(2)]
    Ws = [Buf(fl.sb([128, 8, 128], BF16, "Ws")) for _ in range(2)]
    tmps = [Buf(fl.sb([128, 512], F32, "tmp")) for _ in range(4)]
    PTs = [Buf(fl.sb([128, 256], BF16, "PT")) for _ in range(4)]
    lts = [fl.sb([128, 4], F32, "lt") for _ in range(4)]
    banks = [Buf(fl.ps([128, 512], F32, "bank")) for _ in range(8)]
    PJ = banks[0:4]
    STs = []
    for b in banks[4:6]:
        STs.append((Buf(b.ap[:, 0:256]), 0))
        STs.append((Buf(b.ap[:, 256:512]), 0))
    OPs = [banks[6], banks[7], banks[3]]

    csem = fl.dsem("const")
    sp.dma(CT[:, :], d["cosT"][:, :], csem)
    sp.dma(ST[:, :], d["sinT"][:, :], csem)
    sp.dma(es[:, :], d["a_sinks"][0, :].partition_broadcast(128), csem)
    tc1 = (csem, csem.n)
    msem = fl.dsem("constp")
    tmk = pool.dma(MK[:, :], d["swa_mask"][:, :], msem)
    act.wait(tc1)
    tes = act.done(act.e.activation(out=es[:, :], in_=es[:, :], func=AF.Exp))
    tva = dve.done(dve.e.memset(VA.ap[:, :, 64:65], 1.0))
    VA.wrote(tva)
    tx = load_xT(fl, XT, d["xb0"], d["t_castx"], "xt")
    x_ready = [tx, tc1]

    w_in = d["a_w_in"][0]
    pj_i = 0
    tmp_i = 0
    wi = 0
    it = 0
    for j in range(4):
        for c in range(2):
            Wb, Wsb = Wn[wi % 2], Ws[wi % 2]
            wi += 1
            c0 = (4 * j + 2 * c) * 64
            load_w_cols(fl, Wb, w_in, c0, 128, "wn%d" % ((wi - 1) % 2))
            load_w_cols(fl, Wsb, w_in, c0, 128, "ws%d" % ((wi - 1) % 2), swap=True)
            for tt in range(8):
                dests = [(0, QT, QT.ap[0:64, 2 * c, tt * 512:(tt + 1) * 512]),
                         (64, QT, QT.ap[0:64, 2 * c + 1, tt * 512:(tt + 1) * 512])]
                rope_proj_chunk(fl, XT, Wb, Wsb, 128, tt, PJ, pj_i, CT, ST, tmps, tmp_i, dests, x_ready)
                pj_i += 1
                tmp_i += 1
        Wb, Wsb = Wn[wi % 2], Ws[wi % 2]
        wi += 1
        c0 = 1024 + j * 64
        load_w_cols(fl, Wb, w_in, c0, 64, "wn%d" % ((wi - 1) % 2))
        load_w_cols(fl, Wsb, w_in, c0, 64, "ws%d" % ((wi - 1) % 2), swap=True)
        for tt in range(8):
            dests = [(0, KT, KT.ap[0:64, tt * 512:(tt + 1) * 512])]
            rope_proj_chunk(fl, XT, Wb, Wsb, 64, tt, PJ, pj_i, CT, ST, tmps, tmp_i, dests, x_ready)
            pj_i += 1
            tmp_i += 1
        Wb = Wn[wi % 2]
        wi += 1
        c0 = 1280 + j * 64
        load_w_cols(fl, Wb, w_in, c0, 64, "wn%d" % ((wi - 1) % 2))
        for g8 in range(4):
            P = PJ[pj_i % 4]
            pj_i += 1
            pe.wait(P.deps_w(), Wb.deps_r(), x_ready)
            tok = None
            for t8 in range(8):
                tb = g8 * 8 + t8
                for kc in range(8):
                    tok = pe.done(pe.e.matmul(P.ap[:, t8 * 64:(t8 + 1) * 64], XT.ap[:, kc, tb * 128:(tb + 1) * 128],
                                              Wb.ap[:, kc, 0:64], start=(kc == 0), stop=(kc == 7)))
            Wb.read(tok)
            XT.read(tok)
            P.wrote(tok)
            act.wait(tok, VA.deps_w())
            tv = act.done(act.e.activation(out=VA.ap[:, g8 * 8:(g8 + 1) * 8, 0:64],
                                           in_=P.ap[:, :].rearrange("p (a b) -> p a b", a=8), func=AF.Copy))
            P.read(tv)
            VA.wrote(tv, fresh=False)

        for kt in range(NT):
            nq = 256 if kt < NT - 1 else 128
            for hl in range(4):
                sb_, so = STs[it % 4]
                p = PTs[it % 4]
                it += 1
                pe.wait(sb_.deps_w(), KT.deps_r(), QT.deps_r())
                tok = pe.done(pe.e.matmul(sb_.ap[:, so:so + nq], KT.ap[0:64, kt * 128:(kt + 1) * 128],
                                          QT.ap[0:64, hl, kt * 128:kt * 128 + nq], start=True, stop=True))
                sb_.wrote(tok)
                KT.read(tok)
                QT.read(tok)
                act.wait(tok, p.deps_w())
                ta = act.done(act.e.activation(out=p.ap[:, 0:nq], in_=sb_.ap[:, so:so + nq], func=AF.Exp, scale=0.125))
                sb_.read(ta)
                p.wrote(ta)
                dve.wait(ta, tmk)
                td = dve.done(dve.e.tensor_tensor(out=p.ap[:, 0:nq], in0=p.ap[:, 0:nq], in1=MK[:, 0:nq], op=ALU.mult))
                p.wrote(td)
                for qb in ([kt, kt + 1] if kt < NT - 1 else [kt]):
                    o = OPs[qb % 3]
                    first = (qb == kt + 1) or (kt == 0)
                    last = (qb == kt)
                    if first and hl == 0:
                        pe.wait(o.deps_w())
                    pe.wait(td, VA.deps_r())
                    tok = pe.done(pe.e.matmul(o.ap[:, hl * 65:hl * 65 + 65], p.ap[:, (qb - kt) * 128:(qb - kt + 1) * 128],
                                              VA.ap[:, kt, :], start=(first and hl == 0), stop=(last and hl == 3)))
                    p.read(tok)
                    VA.read(tok)
                    o.wrote(tok, fresh=(first and hl == 0))
            o = OPs[kt % 3]
            lt = lts[kt % 4]
            o4 = o.ap[:, 0:260].rearrange("p (h c) -> p h c", h=4)
            dve.wait(o.deps_r(), tes, OG.deps_w())
            f1 = dve.done(dve.e.tensor_tensor(out=lt[:, :], in0=o4[:, :, 64], in1=es[:, 4 * j:4 * j + 4], op=ALU.add))
            dve.wait(f1)
            f2 = dve.done(dve.e.reciprocal(out=lt[:, :], in_=lt[:, :]))
            dve.wait(f2)
            f3 = dve.done(dve.e.tensor_tensor(out=OG.ap[:, kt, :].rearrange("p (h c) -> p h c", h=4), in0=o4[:, :, 0:64],
                                              in1=lt[:, :].unsqueeze(2).to_broadcast([128, 4, 64]), op=ALU.mult))
            o.read(f1)
            o.read(f3)
            OG.wrote(f3, fresh=False)
        osem = fl.dsem("ostore")
        sp.wait(OG.deps_r())
        tso = sp.dma(d["attnO"].rearrange("(t p) c -> p t c", p=128)[:, :, j * 256:(j + 1) * 256], OG.ap[:, :, :], osem)
        OG.read(tso)
        OG.w = {}
    fl.end()


def phase_outproj_ln(fl, d, attn_d, w_out_d, xres_d, ln_i, ln_j, out_d, outb_d, router_d=None, gate_d=None):
    pe, act, dve, pool, sp = fl.pe, fl.act, fl.dve, fl.pool, fl.sp
    fl.begin()
    OT = Buf(fl.sb([128, 8, S], BF16, "OT"))
    Wo = Buf(fl.sb([128, 8, D], BF16, "Wo"))
    G, Bt, tln = load_ln_params(fl, d["ln_gain"], d["ln_bias"], ln_i, ln_j, None)
    xts = [Buf(fl.sb([128, D], F32, "xt")) for _ in range(2)]
    rts = [Buf(fl.sb([128, D], F32, "rt")) for _ in range(2)]
    xns = [Buf(fl.sb([128, D], F32, "xn")) for _ in range(2)]
    ots = [Buf(fl.sb([128, D], F32, "ot")) for _ in range(2)]
    obs = [Buf(fl.sb([128, D], BF16, "ob")) for _ in range(2)]
    stats = [fl.sb([128, 2, 6], F32, "st") for _ in range(2)]
    mvs = [fl.sb([128, 2], F32, "mv") for _ in range(2)]
    rss = [fl.sb([128, 1], F32, "rs") for _ in range(2)]
    Ys = [Buf(fl.ps([128, 1024], F32, "Y")) for _ in range(2)]
    if router_d is not None:
        RB = fl.sb([128, NE, D], F32, "RB")
        rsem = fl.dsem("rb")
        trb = None
        for e in range(NE):
            trb = sp.dma(RB[:, e, :], d["routerT"][e, :].partition_broadcast(128), rsem)
        junk = fl.sb([128, D], F32, "junk")
        jtok = [None]
        lgs = [fl.sb([128, 8], F32, "lg") for _ in range(2)]
        mx8 = [fl.sb([128, 8], F32, "mx8") for _ in range(2)]
        gts = [Buf(fl.sb([128, 8], F32, "gt")) for _ in range(2)]
        g2 = [fl.sb([128, 8], F32, "g2") for _ in range(2)]
        w12 = [fl.sb([128, 2], F32, "w12") for _ in range(2)]
    wsem = fl.dsem("wo")
    pool.wait(Wo.deps_w())
    tw = pool.dma(Wo.ap[:, :, :], w_out_d.rearrange("(kc p) n -> p kc n", p=128), wsem)
    Wo.wrote(tw)
    tot = load_xT(fl, OT, attn_d, None, "xt")
    xsem = [fl.dsem("xl0"), fl.dsem("xl1")]
    ssem = [fl.dsem("st0"), fl.dsem("st1")]
    for tb in range(NT):
        k = tb % 2
        xt, rt, xn, ot, ob, Y = xts[k], rts[k], xns[k], ots[k], obs[k], Ys[k]
        sp.wait(xt.deps_w())
        tx = sp.dma(xt.ap[:, :], xres_d[tb * 128:(tb + 1) * 128, :], xsem[k])
        xt.wrote(tx)
        pe.wait(Y.deps_w(), tw, tot)
        tok = None
        for half in range(2):
            for kc in range(8):
                tok = pe.done(pe.e.matmul(Y.ap[:, half * 512:(half + 1) * 512], OT.ap[:, kc, tb * 128:(tb + 1) * 128],
                                          Wo.ap[:, kc, half * 512:(half + 1) * 512], start=(kc == 0), stop=(kc == 7)))
        Y.wrote(tok)
        dve.wait(tok, tx, rt.deps_w())
        tr = None
        for half in range(2):
            sl = slice(half * 512, (half + 1) * 512)
            tr = dve.done(dve.e.scalar_tensor_tensor(out=rt.ap[:, sl], in0=xt.ap[:, sl], scalar=ALPHA, in1=Y.ap[:, sl],
                                                     op0=ALU.mult, op1=ALU.add))
        Y.read(tr)
        xt.read(tr)
        rt.wrote(tr)
        pool.wait(tln)
        t7 = layernorm_tile(fl, rt, stats[k], mvs[k], rss[k], xn, ot, G, Bt)
        sp.wait(t7)
        ts1 = sp.dma(out_d[tb * 128:(tb + 1) * 128, :], ot.ap[:, :], ssem[k])
        ot.read(ts1)
        act.wait(t7, ob.deps_w())
        tc = act.done(act.e.activation(out=ob.ap[:, :], in_=ot.ap[:, :], func=AF.Copy))
        ot.read(tc)
        ob.wrote(tc)
        sp.wait(tc)
        ts2 = sp.dma(outb_d[tb * 128:(tb + 1) * 128, :], ob.ap[:, :], fl.dsem("sb%d" % k))
        ob.read(ts2)
        if router_d is not None:
            lg, m8, gt, gg, ww = lgs[k], mx8[k], gts[k], g2[k], w12[k]
            dve.wait(t7, trb)
            tl = jtok[0]
            for e in range(NE):
                dve.wait(tl)
                tl = dve.done(dve.e.scalar_tensor_tensor(out=junk[:, :], in0=ot.ap[:, :], scalar=1.0, in1=RB[:, e, :],
                                                         op0=ALU.mult, op1=ALU.mult, accum_out=lg[:, e:e + 1]))
            ot.read(tl)
            jtok[0] = tl
            dve.wait(tl)
            tm = dve.done(dve.e.max(out=m8[:, :], in_=lg[:, :]))
            dve.wait(tm)
            t_a = dve.done(dve.e.tensor_tensor(out=ww[:, 0:1], in0=m8[:, 0:1], in1=m8[:, 1:2], op=ALU.subtract))
            t_b = dve.done(dve.e.tensor_tensor(out=ww[:, 1:2], in0=m8[:, 1:2], in1=m8[:, 0:1], op=ALU.subtract))
            act.wait(t_a, t_b)
            t_s = act.done(act.e.activation(out=ww[:, :], in_=ww[:, :], func=AF.Sigmoid))
            dve.wait(t_s, gt.deps_w())
            t_g1 = dve.done(dve.e.tensor_scalar(out=gt.ap[:, :], in0=lg[:, :], scalar1=m8[:, 0:1], scalar2=ww[:, 0:1],
                                                op0=ALU.is_equal, op1=ALU.mult))
            t_g2 = dve.done(dve.e.tensor_scalar(out=gg[:, :], in0=lg[:, :], scalar1=m8[:, 1:2], scalar2=ww[:, 1:2],
                                                op0=ALU.is_equal, op1=ALU.mult))
            dve.wait(t_g1, t_g2)
            t_g3 = dve.done(dve.e.tensor_tensor(out=gt.ap[:, :], in0=gt.ap[:, :], in1=gg[:, :], op=ALU.add))
            gt.wrote(t_g3)
            sp.wait(t_g3)
            ts3 = sp.dma(gate_d[tb * 128:(tb + 1) * 128, :], gt.ap[:, :], fl.dsem("sg%d" % k))
            gt.read(ts3)
    fl.end()


def phase_ffn(fl, d, xb_d, xres_d, wg_list, wu_list, wd_list, gate_d, ln_i, ln_j, out_d, outb_d, TG=1024):
    pe, act, dve, pool, sp = fl.pe, fl.act, fl.dve, fl.pool, fl.sp
    fl.begin()
    ne = len(wg_list)
    ntile = TG // 128
    XTg = Buf(fl.sb([128, 8, TG], BF16, "XTg"))
    acc = [Buf(fl.sb([128, D], F32, "acc")) for _ in range(ntile)]
    Wg = [Buf(fl.sb([128, 8, 512], BF16, "Wg")) for _ in range(2)]
    Wu = [Buf(fl.sb([128, 8, 512], BF16, "Wu")) for _ in range(2)]
    Wd = [Buf(fl.sb([128, 4, D], BF16, "Wd")) for _ in range(2)]
    Hs = [Buf(fl.sb([128, 4, 512], BF16, "H")) for _ in range(2)]
    Ss = [Buf(fl.sb([128, 512], F32, "Ssil")) for _ in range(2)]
    G, Bt, tln = load_ln_params(fl, d["ln_gain"], d["ln_bias"], ln_i, ln_j, None)
    xts = [Buf(fl.sb([128, D], F32, "xt")) for _ in range(2)]
    xns = [Buf(fl.sb([128, D], F32, "xn")) for _ in range(2)]
    ots = [Buf(fl.sb([128, D], F32, "ot")) for _ in range(2)]
    obs = [Buf(fl.sb([128, D], BF16, "ob")) for _ in range(2)]
    stats = [fl.sb([128, 2, 6], F32, "st") for _ in range(2)]
    mvs = [fl.sb([128, 2], F32, "mv") for _ in range(2)]
    rss = [fl.sb([128, 1], F32, "rs") for _ in range(2)]
    GPs = [Buf(fl.ps([128, 512], F32, "GP")) for _ in range(2)]
    UPs = [Buf(fl.ps([128, 512], F32, "UP")) for _ in range(2)]
    Ys = [Buf(fl.ps([128, 1024], F32, "Y")) for _ in range(2)]
    if gate_d is not None:
        GT = Buf(fl.sb([128, ntile, NE], F32, "GT"))
    wsems = [fl.dsem("ffw0"), fl.dsem("ffw1")]
    xsem = [fl.dsem("xl0"), fl.dsem("xl1")]
    ssem = [fl.dsem("st0"), fl.dsem("st1")]
    gsem = fl.dsem("gl")
    wi = 0
    gi = 0
    yi = 0
    hi = 0
    ei = 0
    for tg in range(S // TG):
        t0 = tg * TG
        tx = load_xT(fl, XTg, xb_d, None, "xt", t0=t0, nt=TG)
        if gate_d is not None:
            sp.wait(GT.deps_w())
            tgl = sp.dma(GT.ap[:, :, :], gate_d[t0:t0 + TG, :].rearrange("(t p) e -> p t e", p=128), gsem)
            GT.wrote(tgl)
        for e in range(ne):
            for fg in range(FF // 512):
                k = wi % 2
                wi += 1
                wg, wu, wd = Wg[k], Wu[k], Wd[k]
                pool.wait(wg.deps_w(), wu.deps_w(), wd.deps_w())
                f0 = fg * 512
                pool.dma(wg.ap[:, :, :], wg_list[e].rearrange("(kc p) n -> p kc n", p=128)[:, :, f0:f0 + 512], wsems[k])
                pool.dma(wu.ap[:, :, :], wu_list[e].rearrange("(kc p) n -> p kc n", p=128)[:, :, f0:f0 + 512], wsems[k])
                tw = pool.dma(wd.ap[:, :, :], wd_list[e][f0:f0 + 512, :].rearrange("(fc p) n -> p fc n", p=128), wsems[k])
                wg.wrote(tw)
                wu.wrote(tw)
                wd.wrote(tw)
                first_acc = (e == 0 and fg == 0)
                for tt in range(TG // 512):
                    H = Hs[hi % 2]
                    hi += 1
                    for fc in range(4):
                        GP, UP, Sb = GPs[gi % 2], UPs[gi % 2], Ss[gi % 2]
                        gi += 1
                        for (P, Wt) in ((GP, wg), (UP, wu)):
                            pe.wait(P.deps_w(), tw, tx)
                            tok = None
                            for kc in range(8):
                                tok = pe.done(pe.e.matmul(P.ap[:, :], Wt.ap[:, kc, fc * 128:(fc + 1) * 128],
                                                          XTg.ap[:, kc, tt * 512:(tt + 1) * 512], start=(kc == 0), stop=(kc == 7)))
                            P.wrote(tok)
                            Wt.read(tok)
                            XTg.read(tok)
                        act.wait(GP.deps_r(), Sb.deps_w())
                        ta = act.done(act.e.activation(out=Sb.ap[:, :], in_=GP.ap[:, :], func=AF.Silu))
                        GP.read(ta)
                        Sb.wrote(ta)
                        dve.wait(ta, UP.deps_r(), H.deps_w())
                        th = dve.done(dve.e.tensor_tensor(out=H.ap[:, fc, :], in0=UP.ap[:, :], in1=Sb.ap[:, :], op=ALU.mult))
                        UP.read(th)
                        Sb.read(th)
                        H.wrote(th, fresh=(fc == 0))
                    for tb in range(4):
                        Y = Ys[yi % 2]
                        yi += 1
                        tile_i = tt * 4 + tb
                        pe.wait(Y.deps_w(), H.deps_r(), tw)
                        tok = None
                        for half in range(2):
                            for fc in range(4):
                                tok = pe.done(pe.e.matmul(Y.ap[:, half * 512:(half + 1) * 512], H.ap[:, fc, tb * 128:(tb + 1) * 128],
                                                          wd.ap[:, fc, half * 512:(half + 1) * 512], start=(fc == 0), stop=(fc == 3)))
                        Y.wrote(tok)
                        H.read(tok)
                        wd.read(tok)
                        a = acc[tile_i]
                        dve.wait(tok, a.deps_w())
                        ty = None
                        for half in range(2):
                            sl = slice(half * 512, (half + 1) * 512)
                            if gate_d is None:
                                if first_acc:
                                    ty = dve.done(dve.e.tensor_copy(out=a.ap[:, sl], in_=Y.ap[:, sl]))
                                else:
                                    ty = dve.done(dve.e.tensor_tensor(out=a.ap[:, sl], in0=Y.ap[:, sl], in1=a.ap[:, sl], op=ALU.add))
                            else:
                                dve.wait(GT.deps_r())
                                gsc = GT.ap[:, tile_i, e:e + 1]
                                if first_acc:
                                    ty = dve.done(dve.e.tensor_scalar(out=a.ap[:, sl], in0=Y.ap[:, sl], scalar1=gsc, scalar2=None,
                                                                      op0=ALU.mult))
                                else:
                                    ty = dve.done(dve.e.scalar_tensor_tensor(out=a.ap[:, sl], in0=Y.ap[:, sl], scalar=gsc,
                                                                             in1=a.ap[:, sl], op0=ALU.mult, op1=ALU.add))
                        Y.read(ty)
                        a.wrote(ty)
                        if gate_d is not None:
                            GT.read(ty)
        for ti in range(ntile):
            k = ei % 2
            ei += 1
            tbg = t0 // 128 + ti
            xt, xn, ot, ob = xts[k], xns[k], ots[k], obs[k]
            a = acc[ti]
            sp.wait(xt.deps_w())
            txl = sp.dma(xt.ap[:, :], xres_d[tbg * 128:(tbg + 1) * 128, :], xsem[k])
            xt.wrote(txl)
            dve.wait(txl, a.deps_r())
            tr = dve.done(dve.e.scalar_tensor_tensor(out=a.ap[:, :], in0=xt.ap[:, :], scalar=ALPHA, in1=a.ap[:, :],
                                                     op0=ALU.mult, op1=ALU.add))
            xt.read(tr)
            a.wrote(tr)
            pool.wait(tln)
            t7 = layernorm_tile(fl, a, stats[k], mvs[k], rss[k], xn, ot, G, Bt)
            sp.wait(t7)
            ts1 = sp.dma(out_d[tbg * 128:(tbg + 1) * 128, :], ot.ap[:, :], ssem[k])
            ot.read(ts1)
            if outb_d is not None:
                act.wait(t7, ob.deps_w())
                tc = act.done(act.e.activation(out=ob.ap[:, :], in_=ot.ap[:, :], func=AF.Copy))
                ot.read(tc)
                ob.wrote(tc)
                sp.wait(tc)
                ts2 = sp.dma(outb_d[tbg * 128:(tbg + 1) * 128, :], ob.ap[:, :], fl.dsem("sb%d" % k))
                ob.read(ts2)
    fl.end()


NSA_FM = [(0, True), (128, True), (256, True), (384, True), (512, True), (640, True), (768, True), (896, True),
          (1024, True), (1152, True),
          (1280, False), (1408, False),
          (1536, True), (1664, True),
          (2048, True), (2176, True)]


def nsa_projT_row(c0):
    return c0 if c0 < 1792 else c0 - 256


def phase_nsa_proj(fl, d):
    pe, act, dve, pool, sp = fl.pe, fl.act, fl.dve, fl.pool, fl.sp
    fl.begin()
    XT = Buf(fl.sb([128, 8, S], BF16, "XT"))
    CT = fl.sb([128, S], F32, "CT")
    ST = fl.sb([128, S], F32, "ST")
    Wn = [Buf(fl.sb([128, 8, 128], BF16, "Wn")) for _ in range(2)]
    Ws = [Buf(fl.sb([128, 8, 128], BF16, "Ws")) for _ in range(2)]
    Wt = Buf(fl.sb([128, 8, 560], BF16, "Wt"))
    tmps = [Buf(fl.sb([128, 512], F32, "tmp")) for _ in range(4)]
    stg = [Buf(fl.sb([128, S], BF16, "stg")) for _ in range(2)]
    vst = [Buf(fl.sb([128, 512], BF16, "vst")) for _ in range(2)]
    gst = [Buf(fl.sb([128, 48], F32, "gst")) for _ in range(2)]
    PJ = [Buf(fl.ps([128, 512], F32, "bank")) for _ in range(6)]
    csem = fl.dsem("const")
    sp.dma(CT[:, :], d["cosT"][:, :], csem)
    sp.dma(ST[:, :], d["sinT"][:, :], csem)
    tc1 = (csem, csem.n)
    tx = load_xT(fl, XT, d["x2b"], None, "xt")
    x_ready = [tx, tc1]
    w_in = d["b_w_in"][0]
    projT = d["projT"]
    ssem = [fl.dsem("st0"), fl.dsem("st1")]
    pj_i = 0
    tmp_i = 0
    for ci, (c0, roped) in enumerate(NSA_FM):
        Wb, Wsb = Wn[ci % 2], Ws[ci % 2]
        sg = stg[ci % 2]
        load_w_cols(fl, Wb, w_in, c0, 128, "wn%d" % (ci % 2))
        if roped:
            load_w_cols(fl, Wsb, w_in, c0, 128, "ws%d" % (ci % 2), swap=True)
        for tt in range(8):
            if roped:
                dests = [(0, sg, sg.ap[0:64, tt * 512:(tt + 1) * 512]), (64, sg, sg.ap[64:128, tt * 512:(tt + 1) * 512])]
                rope_proj_chunk(fl, XT, Wb, Wsb, 128, tt, PJ[0:4], pj_i, CT, ST, tmps, tmp_i, dests, x_ready)
                pj_i += 1
                tmp_i += 1
            else:
                P = PJ[4 + (tt % 2)]
                pe.wait(P.deps_w(), Wb.deps_r(), x_ready)
                tok = None
                for kc in range(8):
                    tok = pe.done(pe.e.matmul(P.ap[:, :], Wb.ap[:, kc, :], XT.ap[:, kc, tt * 512:(tt + 1) * 512],
                                              start=(kc == 0), stop=(kc == 7)))
                Wb.read(tok)
                XT.read(tok)
                P.wrote(tok)
                act.wait(tok, sg.deps_w())
                tv = act.done(act.e.activation(out=sg.ap[:, tt * 512:(tt + 1) * 512], in_=P.ap[:, :], func=AF.Copy))
                P.read(tv)
                sg.wrote(tv, fresh=False)
        r0 = nsa_projT_row(c0)
        sp.wait(sg.deps_r())
        ts = sp.dma(projT[r0:r0 + 128, :], sg.ap[:, :], ssem[ci % 2])
        sg.read(ts)
        sg.w = {}
    wsem = fl.dsem("wt")
    pool.wait(Wt.deps_w())
    wsrc = w_in.rearrange("(kc p) n -> p kc n", p=128)
    pool.dma(Wt.ap[:, :, 0:256], wsrc[:, :, 1792:2048], wsem)
    pool.dma(Wt.ap[:, :, 256:512], wsrc[:, :, 2304:2560], wsem)
    tw = pool.dma(Wt.ap[:, :, 512:560], wsrc[:, :, 2560:2608], wsem)
    Wt.wrote(tw)
    for tb in range(NT):
        P = PJ[(2 * tb) % 6]
        Pg = PJ[(2 * tb + 1) % 6]
        pe.wait(P.deps_w(), Pg.deps_w(), tw, x_ready)
        tok = None
        for kc in range(8):
            tok = pe.done(pe.e.matmul(P.ap[:, :], XT.ap[:, kc, tb * 128:(tb + 1) * 128], Wt.ap[:, kc, 0:512],
                                      start=(kc == 0), stop=(kc == 7)))
        P.wrote(tok)
        tokg = None
        for kc in range(8):
            tokg = pe.done(pe.e.matmul(Pg.ap[:, 0:48], XT.ap[:, kc, tb * 128:(tb + 1) * 128], Wt.ap[:, kc, 512:560],
                                       start=(kc == 0), stop=(kc == 7)))
        Pg.wrote(tokg)
        vs_, gs_ = vst[tb % 2], gst[tb % 2]
        act.wait(tok, vs_.deps_w())
        tv = act.done(act.e.activation(out=vs_.ap[:, :], in_=P.ap[:, :], func=AF.Copy))
        P.read(tv)
        vs_.wrote(tv)
        act.wait(tokg, gs_.deps_w())
        tg_ = act.done(act.e.activation(out=gs_.ap[:, :], in_=Pg.ap[:, 0:48], func=AF.Sigmoid))
        Pg.read(tg_)
        gs_.wrote(tg_)
        sp.wait(tv, tg_)
        t1 = sp.dma(d["vtok"][tb * 128:(tb + 1) * 128, :], vs_.ap[:, :], ssem[tb % 2])
        t2 = sp.dma(d["gl"][tb * 128:(tb + 1) * 128, :], gs_.ap[:, :], fl.dsem("sg%d" % (tb % 2)))
        vs_.read(t1)
        gs_.read(t2)
    fl.end()


class AttnItem:
    def __init__(self):
        self.A = None
        self.B = None
        self.F = None
        self.slot = None


def attn_items(fl, S_ring, PT_ring, O, Kbuf, krows, Qbuf, qsel, q0, Vbuf, W, ktiles, finalize=None, pe_extra_deps=()):
    pe, act, dve = fl.pe, fl.act, fl.dve
    st = {"first": True}
    last_for = {}
    for i, (_, _, qlo, qhi, _) in enumerate(ktiles):
        for qb in range(qlo, qhi):
            last_for[qb] = i
    items = []
    nk = len(ktiles)
    for i, (kc0, vidx, qlo, qhi, masks) in enumerate(ktiles):
        it = AttnItem()

        def A(slot, it=it, kc0=kc0, qlo=qlo, qhi=qhi, masks=masks):
            it.slot = slot
            Sb = S_ring[slot % len(S_ring)]
            p = PT_ring[slot % len(PT_ring)]
            n0, n1 = qlo * 128, qhi * 128
            pe.wait(Sb.deps_w(), Kbuf.deps_r(), Qbuf.deps_r(), pe_extra_deps)
            tok = pe.done(pe.e.matmul(Sb.ap[:, n0:n1], Kbuf.ap[0:krows, kc0:kc0 + 128],
                                      Qbuf.ap[0:krows, qsel, q0 + n0:q0 + n1], start=True, stop=True))
            Sb.wrote(tok)
            Kbuf.read(tok)
            Qbuf.read(tok)
            act.wait(tok, p.deps_w())
            ta = act.done(act.e.activation(out=p.ap[:, n0:n1], in_=Sb.ap[:, n0:n1], func=AF.Exp, scale=0.125))
            Sb.read(ta)
            p.wrote(ta)
            for (qb, mask_ap, mdeps) in masks:
                dve.wait(ta, mdeps)
                tl = dve.done(dve.e.tensor_tensor(out=p.ap[:, qb * 128:(qb + 1) * 128], in0=p.ap[:, qb * 128:(qb + 1) * 128],
                                                  in1=mask_ap, op=ALU.mult))
                p.wrote(tl, fresh=False)

        def B(it=it, i=i, vidx=vidx, qlo=qlo, qhi=qhi):
            p = PT_ring[it.slot % len(PT_ring)]
            for qb in range(qlo, qhi):
                if st["first"]:
                    pe.wait(O.deps_w())
                pe.wait(p.deps_r(), Vbuf.deps_r())
                tok = pe.done(pe.e.matmul(O.ap[:, qb * W:(qb + 1) * W], p.ap[:, qb * 128:(qb + 1) * 128], Vbuf.ap[:, vidx, 0:W],
                                          start=st["first"], stop=(i == nk - 1 and qb == qhi - 1)))
                O.wrote(tok, fresh=st["first"])
                st["first"] = False
                p.read(tok)
                Vbuf.read(tok)

        it.A = A
        it.B = B
        items.append(it)
    items[-1].F = finalize
    return items


def run_items(items, slot0, L=2):
    n = len(items)
    for i in range(n + L):
        if i < n:
            items[i].A(slot0 + i)
        j = i - L
        if j >= 0:
            items[j].B()
            if items[j].F is not None:
                items[j].F()
    return slot0 + n


def phase_nsa_attn(fl, d):
    pe, act, dve, pool, sp = fl.pe, fl.act, fl.dve, fl.pool, fl.sp
    fl.begin()
    projT, vtok, gl = d["projT"], d["vtok"], d["gl"]
    QN = Buf(fl.sb([128, 4, S], BF16, "QN"))
    KE = Buf(fl.sb([128, S], BF16, "KE"))
    KW = Buf(fl.sb([64, S], BF16, "KW"))
    KC = Buf(fl.sb([64, S], BF16, "KC"))
    VC = Buf(fl.sb([64, S], BF16, "VC"))
    VS = Buf(fl.sb([128, NT, 65], BF16, "VS"))
    VW = Buf(fl.sb([128, NT, 65], BF16, "VW"))
    KCC = Buf(fl.sb([64, 256], BF16, "KCC"))
    RC = Buf(fl.sb([128, 2, 129], BF16, "RC"))
    HK = Buf(fl.sb([128, 2, 256], BF16, "HK"))
    HV = Buf(fl.sb([128, 2, 256], BF16, "HV"))
    W1K = fl.sb([64, 32, 256], BF16, "W1K")
    W1V = fl.sb([64, 32, 256], BF16, "W1V")
    W2K = fl.sb([128, 2, 64], BF16, "W2K")
    W2V = fl.sb([128, 2, 64], BF16, "W2V")
    POSK = fl.sb([64, 32], BF16, "POSK")
    POSV = fl.sb([64, 32], BF16, "POSV")
    CB = fl.sb([128, 4], F32, "CB")
    MK = fl.sb([128, 256], BF16, "MK")
    MCM = fl.sb([128, 9, 512], BF16, "MCM")
    FA = fl.sb([128, NT, 64], BF16, "FA")
    FB = fl.sb([128, NT, 64], BF16, "FB")
    IDN = fl.sb([128, 128], BF16, "IDN")
    GS = fl.sb([128, NT, 48], F32, "GS")
    IMP = Buf(fl.sb([128, NT, 64], F32, "IMP"))
    OG = Buf(fl.sb([128, NT, 256], F32, "OG"))
    NBT = [Buf(fl.sb([128, 128], BF16, "NBT")) for _ in range(2)]
    PTs = [Buf(fl.sb([128, 512], BF16, "PT")) for _ in range(3)]
    smalls = [fl.sb([128, 16], F32, "sm") for _ in range(4)]
    otmp = [Buf(fl.sb([128, 4, 64], F32, "otmp")) for _ in range(2)]
    im2 = [fl.sb([128, 64], F32, "im2") for _ in range(2)]
    imm = [fl.sb([128, 64], F32, "imm") for _ in range(2)]
    m8 = [fl.sb([128, 16], F32, "m8") for _ in range(2)]
    S_ring = [Buf(fl.ps([128, 512], F32, "Sb")) for _ in range(3)]
    O_ring = [Buf(fl.ps([128, 512], F32, "Ob")) for _ in range(2)]
    TP = Buf(fl.ps([128, 512], BF16, "TP"))
    CPS = [Buf(fl.ps([128, 512], F32, "CPS")) for _ in range(2)]

    csem = fl.dsem("const")
    cpsem = fl.dsem("constp")
    pool.dma(MK[:, :], d["swa_mask"][:, :], cpsem)
    pool.dma(MCM[:, :, :], d["cmp_mask"].rearrange("n p c -> p n c"), cpsem)
    pool.dma(FA[:, :, :], d["sel_fa"].rearrange("(t p) c -> p t c", p=128), cpsem)
    pool.dma(FB[:, :, :], d["sel_fb"].rearrange("(t p) c -> p t c", p=128), cpsem)
    pool.dma(IDN[:, :], d["ident"][:, :], cpsem)
    pool.dma(KE.ap[64:128, :], d["emat"][:, :], cpsem)
    pool.dma(RC.ap[:, :, 65:129], d["ov_pad"].rearrange("(c p) j -> p c j", p=128), cpsem)
    pool.dma(W1K[:, :, :], d["b_cmp_k_w1"][0].rearrange("(l dd) m -> dd l m", dd=64), cpsem)
    pool.dma(W1V[:, :, :], d["b_cmp_v_w1"][0].rearrange("(l dd) m -> dd l m", dd=64), cpsem)
    pool.dma(W2K[:, :, :], d["b_cmp_k_w2"][0].rearrange("(mc p) dd -> p mc dd", p=128), cpsem)
    pool.dma(W2V[:, :, :], d["b_cmp_v_w2"][0].rearrange("(mc p) dd -> p mc dd", p=128), cpsem)
    pool.dma(POSK[:, :], d["posT_k"][:, :], cpsem)
    pool.dma(POSV[:, :], d["posT_v"][:, :], cpsem)
    tcp = (cpsem, cpsem.n + 0)
    tgs = sp.dma(GS[:, :, :], gl.rearrange("(t p) c -> p t c", p=128), csem)
    dve.wait(tcp)
    t_m = dve.done(dve.e.memset(VS.ap[:, :, 64:65], 1.0))
    t_m = dve.done(dve.e.memset(VW.ap[:, :, 64:65], 1.0))
    t_m = dve.done(dve.e.memset(RC.ap[:, :, 64:65], 1.0))
    t_m = dve.done(dve.e.memset(HK.ap[:, :, :], 0.0))
    t_m = dve.done(dve.e.memset(HV.ap[:, :, :], 0.0))
    t_m = dve.done(dve.e.memset(KCC.ap[:, :], 0.0))
    for nb in NBT:
        t_m = dve.done(dve.e.memset(nb.ap[:, :], 0.0))
    t_init = t_m
    pe.wait(tcp)
    P = CPS[0]
    tok = None
    for which, (W1, POS) in enumerate(((W1K, POSK), (W1V, POSV))):
        for mc in range(2):
            col = which * 2 + mc
            for l in range(32):
                tok = pe.done(pe.e.matmul(P.ap[:, col:col + 1], W1[0:64, l, mc * 128:(mc + 1) * 128], POS[0:64, l:l + 1],
                                          start=(l == 0), stop=(l == 31)))
    P.wrote(tok)
    dve.wait(tok)
    t_cb = dve.done(dve.e.tensor_copy(out=CB[:, :], in_=P.ap[:, 0:4]))
    P.read(t_cb)

    lsem = [fl.dsem("xl0"), fl.dsem("xl1")]
    osem = fl.dsem("ostore_sw")
    s_i = 0
    o_i = 0
    sm_i = 0
    for j in range(4):
        sp.wait(QN.deps_w(), KE.deps_w(), KW.deps_w(), KC.deps_w(), VC.deps_w(), VS.deps_w(), VW.deps_w(), t_init)
        ls = lsem[j % 2]
        for hl in range(4):
            sp.dma(QN.ap[0:64, hl, :], projT[(4 * j + hl) * 64:(4 * j + hl + 1) * 64, :], ls)
        sp.dma(KC.ap[0:64, :], projT[1024 + j * 64:1024 + (j + 1) * 64, :], ls)
        sp.dma(VC.ap[0:64, :], projT[1280 + j * 64:1280 + (j + 1) * 64, :], ls)
        sp.dma(KE.ap[0:64, :], projT[1536 + j * 64:1536 + (j + 1) * 64, :], ls)
        sp.dma(KW.ap[0:64, :], projT[1792 + j * 64:1792 + (j + 1) * 64, :], ls)
        vt3 = vtok.rearrange("(t p) c -> p t c", p=128)
        sp.dma(VS.ap[:, :, 0:64], vt3[:, :, j * 64:(j + 1) * 64], ls)
        tl = sp.dma(VW.ap[:, :, 0:64], vt3[:, :, 256 + j * 64:256 + (j + 1) * 64], ls)
        for b in (QN, KE, KW, KC, VC, VS, VW):
            b.wrote(tl, fresh=False)
            b.r = {}
        ld = [tl, tcp, t_init]

        for which, (src, W1, W2, Hb) in enumerate(((KC, W1K, W2K, HK), (VC, W1V, W2V, HV))):
            for mc in range(2):
                P = CPS[(which * 2 + mc) % 2]
                pe.wait(P.deps_w(), ld)
                tok = None
                for l in range(32):
                    tok = pe.done(pe.e.matmul(P.ap[:, 0:255], W1[0:64, l, mc * 128:(mc + 1) * 128], src.ap[0:64, l:l + 4065:16],
                                              start=(l == 0), stop=(l == 31)))
                P.wrote(tok)
                src.read(tok)
                act.wait(tok, t_cb, Hb.deps_w(), t_init)
                th = act.done(act.e.activation(out=Hb.ap[:, mc, 0:255], in_=P.ap[:, 0:255], func=AF.Gelu_apprx_tanh,
                                               bias=CB[:, which * 2 + mc:which * 2 + mc + 1]))
                P.read(th)
                Hb.wrote(th, fresh=False)
        P = CPS[0]
        pe.wait(P.deps_w(), HK.deps_r())
        tok = None
        for mc in range(2):
            tok = pe.done(pe.e.matmul(P.ap[0:64, 0:255], W2K[:, mc, :], HK.ap[:, mc, 0:255], start=(mc == 0), stop=(mc == 1)))
        P.wrote(tok)
        HK.read(tok)
        act.wait(tok, KCC.deps_w())
        tk = act.done(act.e.activation(out=KCC.ap[0:64, 0:255], in_=P.ap[0:64, 0:255], func=AF.Copy))
        P.read(tk)
        KCC.wrote(tk)
        P = CPS[1]
        pe.wait(P.deps_w(), HV.deps_r())
        tok = None
        for ct in range(2):
            for mc in range(2):
                tok = pe.done(pe.e.matmul(P.ap[:, ct * 64:(ct + 1) * 64], HV.ap[:, mc, ct * 128:(ct + 1) * 128], W2V[:, mc, :],
                                          start=(mc == 0), stop=(mc == 1)))
        P.wrote(tok)
        HV.read(tok)
        act.wait(tok, RC.deps_w())
        tr_ = act.done(act.e.activation(out=RC.ap[:, :, 0:64], in_=P.ap[:, 0:128].rearrange("p (c x) -> p c x", c=2), func=AF.Copy))
        P.read(tr_)
        RC.wrote(tr_)
        HK.w = {}
        HV.w = {}

        mcm_idx = {(0, 0): 0, (0, 1): 1, (0, 2): 2, (0, 3): 3, (0, 4): 4, (1, 4): 5, (1, 5): 6, (1, 6): 7, (1, 7): 8}

        def cmp_final(O, sm, qh, hl, h):
            O3 = O.ap[:, 0:258].rearrange("p (b w) -> p b w", b=2)
            tile0 = qh * 2
            dve.wait(O.deps_r(), tgs, OG.deps_w(), IMP.deps_w())
            f1 = dve.done(dve.e.tensor_scalar(out=sm[:, 0:2], in0=O3[:, :, 64], scalar1=1e-30, scalar2=None, op0=ALU.max))
            dve.wait(f1)
            f2 = dve.done(dve.e.reciprocal(out=sm[:, 0:2], in_=sm[:, 0:2]))
            dve.wait(f2)
            f3 = dve.done(dve.e.tensor_tensor(out=sm[:, 2:4], in0=sm[:, 0:2], in1=GS[:, tile0:tile0 + 2, h * 3 + 0], op=ALU.mult))
            dve.wait(f3)
            f4 = dve.done(dve.e.tensor_tensor(out=OG.ap[:, tile0:tile0 + 2, hl * 64:(hl + 1) * 64], in0=O3[:, :, 0:64],
                                              in1=sm[:, 2:4].unsqueeze(2).to_broadcast([128, 2, 64]), op=ALU.mult))
            OG.wrote(f4, fresh=False)
            f5 = None
            for b2 in range(2):
                if hl == 0:
                    f5 = dve.done(dve.e.tensor_scalar(out=IMP.ap[:, tile0 + b2, :], in0=O3[:, b2, 65:129], scalar1=sm[:, b2:b2 + 1],
                                                      scalar2=None, op0=ALU.mult))
                else:
                    f5 = dve.done(dve.e.scalar_tensor_tensor(out=IMP.ap[:, tile0 + b2, :], in0=O3[:, b2, 65:129],
                                                             scalar=sm[:, b2:b2 + 1], in1=IMP.ap[:, tile0 + b2, :],
                                                             op0=ALU.mult, op1=ALU.add))
            IMP.wrote(f5, fresh=False)
            O.read(f1)
            O.read(f5)

        citems = []
        for hl in range(4):
            h = 4 * j + hl
            for qh in range(16):
                tt = qh // 2
                q0 = qh * 256
                ktl = []
                for ct in range(2):
                    if ct == 1 and tt <= 3:
                        continue
                    masks = []
                    if (ct, tt) in mcm_idx:
                        mi = mcm_idx[(ct, tt)]
                        off = (qh % 2) * 256
                        masks = [(0, MCM[:, mi, off:off + 128], tcp), (1, MCM[:, mi, off + 128:off + 256], tcp)]
                    ktl.append((ct * 128, ct, 0, 2, masks))
                O = O_ring[o_i % 2]
                o_i += 1
                sm = smalls[sm_i % 4]
                sm_i += 1
                citems.extend(attn_items(fl, S_ring, PTs, O, KCC, 64, QN, hl, q0, RC, 129, ktl,
                                         finalize=(lambda O=O, sm=sm, qh=qh, hl=hl, h=h: cmp_final(O, sm, qh, hl, h))))
        s_i = run_items(citems, s_i)

        for tile in range(NT):
            k2 = tile % 2
            i2, im_, mm = im2[k2], imm[k2], m8[k2]
            nb = NBT[k2]
            dve.wait(IMP.deps_r(), tcp)
            g1 = dve.done(dve.e.tensor_tensor(out=im_[:, :], in0=IMP.ap[:, tile, :], in1=FA[:, tile, :], op=ALU.mult))
            dve.wait(g1)
            g2 = dve.done(dve.e.tensor_tensor(out=im_[:, :], in0=im_[:, :], in1=FB[:, tile, :], op=ALU.add))
            dve.wait(g2)
            g3 = dve.done(dve.e.max(out=mm[:, 0:8], in_=im_[:, :]))
            dve.wait(g3)
            g4 = dve.done(dve.e.match_replace(out=i2[:, :], in_to_replace=mm[:, 0:8], in_values=im_[:, :], imm_value=-2.0))
            dve.wait(g4)
            g5 = dve.done(dve.e.max(out=mm[:, 8:16], in_=i2[:, :]))
            dve.wait(g5)
            g6 = dve.done(dve.e.tensor_scalar(out=mm[:, 0:1], in0=mm[:, 15:16], scalar1=0.0, scalar2=None, op0=ALU.max))
            dve.wait(g6, nb.deps_w())
            g7 = dve.done(dve.e.tensor_scalar(out=nb.ap[:, 64:128], in0=im_[:, :], scalar1=mm[:, 0:1], scalar2=1.0,
                                              op0=ALU.is_ge, op1=ALU.subtract))
            nb.wrote(g7)
            IMP.read(g7)
            pe.wait(g7, TP.deps_w(), tcp)
            tt_ = pe.done(pe.e.transpose(TP.ap[:, 0:128], nb.ap[:, :], IDN[:, :]))
            nb.read(tt_)
            TP.wrote(tt_)
            act.wait(tt_, QN.deps_w())
            tq = act.done(act.e.activation(out=QN.ap[64:128, :, tile * 128:(tile + 1) * 128],
                                           in_=TP.ap[64:128, 0:128].unsqueeze(1).to_broadcast([64, 4, 128]), func=AF.Copy))
            TP.read(tq)
            QN.wrote(tq, fresh=False)

        def ws_final(O, sm, ot_, tt, hl, h, br):
            O3 = O.ap[:, 0:260].rearrange("p (b w) -> p b w", b=4)
            dve.wait(O.deps_r(), tgs, ot_.deps_w())
            f1 = dve.done(dve.e.tensor_scalar(out=sm[:, 0:4], in0=O3[:, :, 64], scalar1=1e-30, scalar2=None, op0=ALU.max))
            dve.wait(f1)
            f2 = dve.done(dve.e.reciprocal(out=sm[:, 0:4], in_=sm[:, 0:4]))
            dve.wait(f2)
            f3 = dve.done(dve.e.tensor_tensor(out=sm[:, 4:8], in0=sm[:, 0:4], in1=GS[:, 4 * tt:4 * tt + 4, h * 3 + br], op=ALU.mult))
            dve.wait(f3)
            f4 = dve.done(dve.e.tensor_tensor(out=ot_.ap[:, :, :], in0=O3[:, :, 0:64],
                                              in1=sm[:, 4:8].unsqueeze(2).to_broadcast([128, 4, 64]), op=ALU.mult))
            O.read(f1)
            O.read(f4)
            ot_.wrote(f4)
            pool.wait(f4, OG.deps_r())
            f5 = pool.done(pool.e.tensor_tensor(out=OG.ap[:, 4 * tt:4 * tt + 4, hl * 64:(hl + 1) * 64],
                                                in0=OG.ap[:, 4 * tt:4 * tt + 4, hl * 64:(hl + 1) * 64], in1=ot_.ap[:, :, :], op=ALU.add))
            ot_.read(f5)
            OG.wrote(f5, fresh=False)

        witems = []
        for br in (2, 1):
            for hl in range(4):
                h = 4 * j + hl
                for tt in range(8):
                    ktl = []
                    if br == 2:
                        for kt in range(max(0, 4 * tt - 4), 4 * tt + 4):
                            qlo = max(4 * tt, kt) - 4 * tt
                            qhi = min(4 * tt + 3, kt + 4) - 4 * tt + 1
                            masks = []
                            if 4 * tt <= kt:
                                masks.append((kt - 4 * tt, MK[:, 0:128], tcp))
                            if kt + 4 <= 4 * tt + 3 and kt + 4 >= 4 * tt:
                                masks.append((kt + 4 - 4 * tt, MK[:, 128:256], tcp))
                            ktl.append((kt * 128, kt, qlo, qhi, masks))
                        Kb, krows, Vb = KW, 64, VW
                    else:
                        for kt in range(0, 4 * tt + 4):
                            qlo = max(4 * tt, kt) - 4 * tt
                            masks = []
                            if kt >= 4 * tt:
                                masks.append((kt - 4 * tt, MK[:, 0:128], tcp))
                            ktl.append((kt * 128, kt, qlo, 4, masks))
                        Kb, krows, Vb = KE, 128, VS
                    O = O_ring[o_i % 2]
                    o_i += 1
                    sm = smalls[sm_i % 4]
                    sm_i += 1
                    ot_ = otmp[sm_i % 2]
                    witems.extend(attn_items(fl, S_ring, PTs, O, Kb, krows, QN, hl, tt * 512, Vb, 65, ktl,
                                             finalize=(lambda O=O, sm=sm, ot_=ot_, tt=tt, hl=hl, h=h, br=br: ws_final(O, sm, ot_, tt, hl, h, br))))
        s_i = run_items(witems, s_i)
        pool.wait(OG.deps_r())
        tso = pool.dma(d["attnO"].rearrange("(t p) c -> p t c", p=128)[:, :, j * 256:(j + 1) * 256], OG.ap[:, :, :], osem)
        OG.w = {}
        OG.r = {}
        OG.read(tso)
        IMP.r = {}
    fl.end()


def host_constants():
    inv = (1.0 / (np.float32(10000.0) ** (np.arange(0, 64, 2, dtype=np.float32) / np.float32(64)))).astype(np.float32)
    ang = np.arange(S, dtype=np.float32)[:, None] * inv[None, :]
    cos = np.cos(ang).astype(np.float32).T
    sin = np.sin(ang).astype(np.float32).T
    cosT = np.concatenate([cos, cos, cos, cos], axis=0)
    sinT = np.concatenate([-sin, sin, -sin, sin], axis=0)
    k = np.arange(128)[:, None]
    c = np.arange(256)[None, :]
    swa_mask = np.where(c < 128, c >= k, (c - 128) < k).astype(np.float32)
    out = {"cosT": np.ascontiguousarray(cosT), "sinT": np.ascontiguousarray(sinT), "swa_mask": swa_mask}
    cidx = np.arange(256)
    cs = 16 * cidx
    ce = cs + 32
    ss = 64 * np.arange(64)
    se = ss + 64
    ov = np.clip(np.minimum(ce[:, None], se[None, :]) - np.maximum(cs[:, None], ss[None, :]), 0, None) / 16.0
    ov[255, :] = 0.0
    out["ov_pad"] = ov.astype(np.float32)
    tiles = [(0, 0), (0, 1), (0, 2), (0, 3), (0, 4), (1, 4), (1, 5), (1, 6), (1, 7)]
    cm = np.zeros((9, 128, 512), np.float32)
    for i, (ct, tt) in enumerate(tiles):
        cc = ct * 128 + np.arange(128)[:, None]
        t = tt * 512 + np.arange(512)[None, :]
        cm[i] = ((16 * cc + 31 <= t) & (cc < 255)).astype(np.float32)
    out["cmp_mask"] = cm
    t = np.arange(S)[:, None]
    b = np.arange(64)[None, :]
    cur = t // 64
    forced = (b == 0) | (b == cur) | (b == cur - 1)
    fa = ((b <= cur) & (~forced)).astype(np.float32)
    fb = np.where(b > cur, -1.0, 0.0).astype(np.float32)
    fb = np.where(b == cur - 1, 1.0e4, fb)
    fb = np.where(b == cur, 2.0e4, fb)
    fb = np.where(b == 0, 3.0e4, fb).astype(np.float32)
    out["sel_fa"] = fa
    out["sel_fb"] = fb
    out["ident"] = np.eye(128, dtype=np.float32)
    em = np.zeros((64, S), np.float32)
    em[np.arange(S) // 64, np.arange(S)] = 30000.0
    out["emat"] = em
    return out


INPUT_SHAPES = {
    "a_w_in": [1, 1024, 1536], "a_w_out": [1, 1024, 1024], "a_sinks": [1, 16],
    "b_w_in": [1, 1024, 2608], "b_w_out": [1, 1024, 1024], "b_cmp_pos_k": [1, 32, 64], "b_cmp_pos_v": [1, 32, 64],
    "b_cmp_k_w1": [1, 2048, 256], "b_cmp_k_w2": [1, 256, 64], "b_cmp_v_w1": [1, 2048, 256], "b_cmp_v_w2": [1, 256, 64],
    "ffn_w_gate": [1, 1024, 3584], "ffn_w_up": [1, 1024, 3584], "ffn_w_down": [1, 3584, 1024],
    "moe_router": [1, 1024, 8], "moe_w_gate": [1, 8, 1024, 3584], "moe_w_up": [1, 8, 1024, 3584],
    "moe_w_down": [1, 8, 3584, 1024], "ln_gain": [2, 2, 1024], "ln_bias": [2, 2, 1024],
}


def _needed(k, stop_after):
    if stop_after >= 4:
        return True
    if k.startswith("moe_w"):
        return False
    if stop_after <= 2 and (k.startswith("b_") or k.startswith("moe")):
        return False
    if stop_after <= 1 and k.startswith("ffn"):
        return False
    return True


def build(stop_after=99):
    nc = bass.Bass("TRN2", target_bir_lowering=False)
    d = {}
    d["x"] = nc.dram_tensor("x", [S, D], F32, kind="ExternalInput").ap()
    for k, shp in INPUT_SHAPES.items():
        if not _needed(k, stop_after):
            continue
        d[k] = nc.dram_tensor(k, shp, F32, kind="ExternalInput").ap()
    hc = host_constants()
    for k, v in hc.items():
        d[k] = nc.dram_tensor(k, list(v.shape), F32, kind="ExternalInput").ap()
    d["routerT"] = nc.dram_tensor("routerT", [NE, D], F32, kind="ExternalInput").ap()
    d["posT_k"] = nc.dram_tensor("posT_k", [64, 32], F32, kind="ExternalInput").ap()
    d["posT_v"] = nc.dram_tensor("posT_v", [64, 32], F32, kind="ExternalInput").ap()
    d["projT"] = nc.dram_tensor("projT", [2048, S], BF16, kind="Internal").ap()
    d["vtok"] = nc.dram_tensor("vtok", [S, 512], BF16, kind="Internal").ap()
    d["gl"] = nc.dram_tensor("gl", [S, 48], F32, kind="Internal").ap()
    y = nc.dram_tensor("y", [S, D], F32, kind="ExternalOutput").ap()
    for nm in ("xb0", "attnO", "x1b", "x2b", "x3b"):
        d[nm] = nc.dram_tensor(nm, [S, D], BF16, kind="Internal").ap()
    for nm in ("x1", "x2", "x3"):
        d[nm] = nc.dram_tensor(nm, [S, D], F32, kind="Internal").ap()
    d["gate"] = nc.dram_tensor("gate", [S, NE], F32, kind="Internal").ap()
    fl = Flow(nc)
    d["t_castx"] = phase_cast_x(fl, d["x"], d["xb0"])
    phase_l0_attn(fl, d)
    phase_outproj_ln(fl, d, d["attnO"], d["a_w_out"][0], d["x"], 0, 0, y if stop_after == 1 else d["x1"], d["x1b"])
    if stop_after >= 2:
        phase_ffn(fl, d, d["x1b"], d["x1"], [d["ffn_w_gate"][0]], [d["ffn_w_up"][0]], [d["ffn_w_down"][0]], None, 0, 1,
                  y if stop_after == 2 else d["x2"], d["x2b"])
    if stop_after >= 3:
        phase_nsa_proj(fl, d)
        phase_nsa_attn(fl, d)
        phase_outproj_ln(fl, d, d["attnO"], d["b_w_out"][0], d["x2"], 1, 0, y if stop_after == 3 else d["x3"], d["x3b"],
                         router_d=d["moe_router"][0], gate_d=d["gate"])
    if stop_after >= 4:
        phase_ffn(fl, d, d["x3b"], d["x3"], [d["moe_w_gate"][0, e] for e in range(NE)], [d["moe_w_up"][0, e] for e in range(NE)],
                  [d["moe_w_down"][0, e] for e in range(NE)], d["gate"], 1, 1, y, None)
    fl.barrier()
    fl.gstack.close()
    return nc


_CACHE = {}


def kernel(**inputs):
    stop_after = int(inputs.pop("_stop_after", 99))
    if stop_after not in _CACHE:
        _CACHE[stop_after] = build(stop_after)
    nc = _CACHE[stop_after]
    hc = host_constants()
    shared = {k: np.ascontiguousarray(np.asarray(inputs[k], dtype=np.float32)) for k in INPUT_SHAPES if _needed(k, stop_after)}
    shared.update(hc)
    shared["routerT"] = np.ascontiguousarray(np.asarray(inputs["moe_router"], dtype=np.float32)[0].T)
    shared["posT_k"] = np.ascontiguousarray(np.asarray(inputs["b_cmp_pos_k"], dtype=np.float32)[0].T)
    shared["posT_v"] = np.ascontiguousarray(np.asarray(inputs["b_cmp_pos_v"], dtype=np.float32)[0].T)
    x = np.asarray(inputs["x"], dtype=np.float32)
    in_maps = []
    for c in range(NCORES):
        m = dict(shared)
        m["x"] = np.ascontiguousarray(x[c])
        in_maps.append(m)
    res = run_bass_kernel_spmd(nc, in_maps, core_ids=list(range(NCORES)))
    return np.stack([np.asarray(r["y"], dtype=np.float32) for r in res.results], axis=0)
```

```python
import numpy as np
import ml_dtypes
from contextlib import ExitStack
import concourse.bass as bass
import concourse.mybir as mybir
from concourse.bass_utils import run_bass_kernel_spmd

F32 = mybir.dt.float32
BF16 = mybir.dt.bfloat16
AF = mybir.ActivationFunctionType
ALU = mybir.AluOpType

S = 4096
D = 1024
NT = 32
FF = 3584
NE = 8
ALPHA = float(4.0 ** 0.25)
EPS = 1e-5
NCORES = 8
ROUTED = True


class SemObj:
    def __init__(self, h):
        self.h = h
        self.n = 0


class Buf:
    def __init__(self, ap):
        self.ap = ap
        self.w = {}
        self.r = {}

    @staticmethod
    def _add(d, tok):
        if tok is None:
            return
        s, v = tok
        if d.get(s, 0) < v:
            d[s] = v

    def deps_r(self):
        return list(self.w.items())

    def deps_w(self):
        return list(self.w.items()) + list(self.r.items())

    def wrote(self, tok, fresh=True):
        if fresh:
            self.w = {}
            self.r = {}
        self._add(self.w, tok)

    def read(self, tok):
        self._add(self.r, tok)


class Eng:
    def __init__(self, fl, name, eng):
        self.fl = fl
        self.name = name
        self.e = eng
        self.sem = SemObj(fl.gstack.enter_context(fl.nc.semaphore("s_" + name)))
        self.seen = {}

    def wait(self, *toks):
        for tok in toks:
            if tok is None:
                continue
            if isinstance(tok, list) or (isinstance(tok, tuple) and (len(tok) != 2 or not isinstance(tok[0], SemObj))):
                self.wait(*tok)
                continue
            sem, v = tok
            if sem is None or v <= 0:
                continue
            if self.seen.get(sem, 0) >= v:
                continue
            if sem is self.sem and self.name == "pe":
                continue
            self.e.wait_ge(sem.h, v)
            self.seen[sem] = v

    def done(self, ins):
        self.sem.n += 1
        ins.then_inc(self.sem.h, 1)
        return (self.sem, self.sem.n)

    def dma(self, out, in_, sem, **kw):
        ins = self.e.dma_start(out=out, in_=in_, **kw)
        sem.n += 16
        ins.then_inc(sem.h, 16)
        return (sem, sem.n)


class Flow:
    def __init__(self, nc):
        self.nc = nc
        self.gstack = ExitStack()
        self.pe = Eng(self, "pe", nc.tensor)
        self.act = Eng(self, "act", nc.scalar)
        self.dve = Eng(self, "dve", nc.vector)
        self.pool = Eng(self, "pool", nc.gpsimd)
        self.sp = Eng(self, "sp", nc.sync)
        self.engines = [self.pe, self.act, self.dve, self.pool, self.sp]
        self.dsems = {}
        self.pstack = None
        self.uid = 0

    def dsem(self, name):
        if name not in self.dsems:
            self.dsems[name] = SemObj(self.gstack.enter_context(self.nc.semaphore("d_" + name)))
        return self.dsems[name]

    def begin(self):
        self.pstack = ExitStack()

    def end(self):
        self.barrier()
        self.pstack.close()
        self.pstack = None

    def barrier(self):
        toks = [(e.sem, e.sem.n) for e in self.engines if e.sem.n > 0]
        toks += [(s, s.n) for s in self.dsems.values() if s.n > 0]
        for e in self.engines:
            e.wait([t for t in toks if t[0] is not e.sem])

    def sb(self, shape, dt, name=None):
        self.uid += 1
        return self.pstack.enter_context(self.nc.sbuf_tensor(f"{name or 'sb'}_{self.uid}", list(shape), dt))

    def ps(self, shape, dt=F32, name=None):
        self.uid += 1
        return self.pstack.enter_context(self.nc.psum_tensor(f"{name or 'ps'}_{self.uid}", list(shape), dt))


def load_xT(fl, XT, xb_d, deps, semname, ncols=D, t0=0, nt=S):
    sp = fl.sp
    sem = fl.dsem(semname)
    sp.wait(deps, XT.deps_w())
    tok = None
    step = 1024 if nt >= 1024 else nt
    for kc in range(ncols // 128):
        for q in range(nt // step):
            tok = sp.dma(XT.ap[:, kc, q * step:(q + 1) * step],
                         xb_d[t0 + q * step:t0 + (q + 1) * step, kc * 128:(kc + 1) * 128], sem, transpose=True)
    XT.wrote(tok)
    return tok


def layernorm_tile(fl, r, stats, mv, rs, xn, out_t, G, Bt):
    dve, pool = fl.dve, fl.pool
    dve.wait(r.deps_r())
    t1 = dve.done(dve.e.bn_stats(out=stats[:, 0, :], in_=r.ap[:, 0:512]))
    t2 = dve.done(dve.e.bn_stats(out=stats[:, 1, :], in_=r.ap[:, 512:1024]))
    dve.wait(t1, t2)
    t3 = dve.done(dve.e.bn_aggr(out=mv[:, :], in_=stats[:, :, :].rearrange("p a b -> p (a b)")))
    dve.wait(t3)
    t4a = dve.done(dve.e.tensor_scalar(out=rs[:, :], in0=mv[:, 1:2], scalar1=EPS, scalar2=None, op0=ALU.add))
    fl.act.wait(t4a)
    t4b = fl.act.done(fl.act.e.activation(out=rs[:, :], in_=rs[:, :], func=AF.Sqrt))
    dve.wait(t4b)
    t4 = dve.done(dve.e.reciprocal(out=rs[:, :], in_=rs[:, :]))
    dve.wait(t4, xn.deps_w())
    t5 = dve.done(dve.e.tensor_scalar(out=xn.ap[:, :], in0=r.ap[:, :], scalar1=mv[:, 0:1], scalar2=rs[:, 0:1],
                                      op0=ALU.subtract, op1=ALU.mult))
    r.read(t5)
    xn.wrote(t5)
    pool.wait(t5, out_t.deps_w())
    t6 = pool.done(pool.e.tensor_tensor(out=xn.ap[:, :], in0=xn.ap[:, :], in1=G[:, :], op=ALU.mult))
    pool.wait(t6)
    t7 = pool.done(pool.e.tensor_tensor(out=out_t.ap[:, :], in0=xn.ap[:, :], in1=Bt[:, :], op=ALU.add))
    xn.read(t7)
    xn.wrote(t6, fresh=False)
    out_t.wrote(t7)
    return t7


def load_ln_params(fl, ln_gain_d, ln_bias_d, i, j, ready):
    G = fl.sb([128, D], F32, "lnG")
    Bt = fl.sb([128, D], F32, "lnB")
    sem = fl.dsem("lnp")
    fl.sp.wait(ready)
    fl.sp.dma(G[:, :], ln_gain_d[i, j, :].partition_broadcast(128), sem)
    tok = fl.sp.dma(Bt[:, :], ln_bias_d[i, j, :].partition_broadcast(128), sem)
    return G, Bt, tok


def phase_cast_x(fl, x_d, xb_d):
    sem = fl.dsem("castx")
    tok = None
    for q in range(4):
        tok = fl.pool.dma(xb_d[q * 1024:(q + 1) * 1024, :], x_d[q * 1024:(q + 1) * 1024, :], sem)
    return tok


def rope_proj_chunk(fl, XT, W, Wsw, M, tt, PJ, pj_i, CT, ST, tmps, tmp_i, dests, x_ready):
    pe, dve, pool = fl.pe, fl.dve, fl.pool
    P1 = PJ[(2 * pj_i) % len(PJ)]
    P2 = PJ[(2 * pj_i + 1) % len(PJ)]
    for (P, Wt) in ((P1, W), (P2, Wsw)):
        pe.wait(P.deps_w(), Wt.deps_r(), x_ready)
        tok = None
        for kc in range(8):
            tok = pe.done(pe.e.matmul(P.ap[0:M, :], Wt.ap[:, kc, 0:M], XT.ap[:, kc, tt * 512:(tt + 1) * 512],
                                      start=(kc == 0), stop=(kc == 7)))
        Wt.read(tok)
        XT.read(tok)
        P.wrote(tok)
    T1 = tmps[(2 * tmp_i) % len(tmps)]
    T2 = tmps[(2 * tmp_i + 1) % len(tmps)]
    dve.wait(P1.deps_r(), T1.deps_w())
    t1 = dve.done(dve.e.tensor_tensor(out=T1.ap[0:M, :], in0=P1.ap[0:M, :], in1=CT[0:M, tt * 512:(tt + 1) * 512], op=ALU.mult))
    P1.read(t1)
    T1.wrote(t1)
    dve.wait(P2.deps_r(), T2.deps_w())
    t2 = dve.done(dve.e.tensor_tensor(out=T2.ap[0:M, :], in0=P2.ap[0:M, :], in1=ST[0:M, tt * 512:(tt + 1) * 512], op=ALU.mult))
    P2.read(t2)
    T2.wrote(t2)
    for (row0, dbuf, out_ap) in dests:
        pool.wait(t1, t2, dbuf.deps_w())
        t3 = pool.done(pool.e.tensor_tensor(out=out_ap, in0=T1.ap[row0:row0 + 64, :], in1=T2.ap[row0:row0 + 64, :], op=ALU.add))
        T1.read(t3)
        T2.read(t3)
        dbuf.wrote(t3, fresh=False)


def load_w_cols(fl, Wb, w_d, c0, M, semname, swap=False):
    pool = fl.pool
    sem = fl.dsem(semname)
    pool.wait(Wb.deps_w())
    if not swap:
        src = w_d.rearrange("(kc p) n -> p kc n", p=128)[:, :, c0:c0 + M]
        tok = pool.dma(Wb.ap[:, :, 0:M], src, sem)
    else:
        nh = M // 64
        ncol = w_d.shape[1]
        src5 = w_d[:, 0:(ncol // 64) * 64].rearrange("(kc p) (h two i) -> p kc h two i", p=128, two=2, i=32)
        dst5 = Wb.ap[:, :, 0:M].rearrange("p kc (h two i) -> p kc h two i", two=2, i=32)
        h0 = c0 // 64
        tok = None
        for two in range(2):
            for hh in range(nh):
                tok = pool.dma(dst5[:, :, hh, two, :], src5[:, :, h0 + hh, 1 - two, :], sem)
    Wb.wrote(tok)
    return tok


def phase_l0_attn(fl, d):
    nc = fl.nc
    pe, act, dve, pool, sp = fl.pe, fl.act, fl.dve, fl.pool, fl.sp
    fl.begin()
    XT = Buf(fl.sb([128, 8, S], BF16, "XT"))
    CT = fl.sb([128, S], F32, "CT")
    ST = fl.sb([128, S], F32, "ST")
    MK = fl.sb([128, 256], BF16, "MK")
    es = fl.sb([128, 16], F32, "es")
    KT = Buf(fl.sb([64, S], BF16, "KT"))
    VA = Buf(fl.sb([128, NT, 65], BF16, "VA"))
    QT = Buf(fl.sb([64, 4, S], BF16, "QT"))
    OG = Buf(fl.sb([128, NT, 256], BF16, "OG"))
    Wn = [Buf(fl.sb([128, 8, 128], BF16, "Wn")) for _ in range(2)]
    Ws = [Buf(fl.sb([128, 8, 128], BF16, "Ws")) for _ in range(2)]
    tmps = [Buf(fl.sb([128, 512], F32, "tmp")) for _ in range(4)]
    PTs = [Buf(fl.sb([128, 256], BF16, "PT")) for _ in range(4)]
    lts = [fl.sb([128, 4], F32, "lt") for _ in range(4)]
    banks = [Buf(fl.ps([128, 512], F32, "bank")) for _ in range(8)]
    PJ = banks[0:4]
    STs = []
    for b in banks[4:6]:
        STs.append((Buf(b.ap[:, 0:256]), 0))
        STs.append((Buf(b.ap[:, 256:512]), 0))
    OPs = [banks[6], banks[7], banks[3]]

    csem = fl.dsem("const")
    sp.dma(CT[:, :], d["cosT"][:, :], csem)
    sp.dma(ST[:, :], d["sinT"][:, :], csem)
    sp.dma(es[:, :], d["a_sinks"][0, :].partition_broadcast(128), csem)
    tc1 = (csem, csem.n)
    msem = fl.dsem("constp")
    tmk = pool.dma(MK[:, :], d["swa_mask"][:, :], msem)
    act.wait(tc1)
    tes = act.done(act.e.activation(out=es[:, :], in_=es[:, :], func=AF.Exp))
    tva = dve.done(dve.e.memset(VA.ap[:, :, 64:65], 1.0))
    VA.wrote(tva)
    tx = load_xT(fl, XT, d["xb0"], d["t_castx"], "xt")
    x_ready = [tx, tc1]

    w_in = d["a_w_in"][0]
    pj_i = 0
    tmp_i = 0
    wi = 0
    it = 0
    for j in range(4):
        for c in range(2):
            Wb, Wsb = Wn[wi % 2], Ws[wi % 2]
            wi += 1
            c0 = (4 * j + 2 * c) * 64
            load_w_cols(fl, Wb, w_in, c0, 128, "wn%d" % ((wi - 1) % 2))
            load_w_cols(fl, Wsb, w_in, c0, 128, "ws%d" % ((wi - 1) % 2), swap=True)
            for tt in range(8):
                dests = [(0, QT, QT.ap[0:64, 2 * c, tt * 512:(tt + 1) * 512]),
                         (64, QT, QT.ap[0:64, 2 * c + 1, tt * 512:(tt + 1) * 512])]
                rope_proj_chunk(fl, XT, Wb, Wsb, 128, tt, PJ, pj_i, CT, ST, tmps, tmp_i, dests, x_ready)
                pj_i += 1
                tmp_i += 1
        Wb, Wsb = Wn[wi % 2], Ws[wi % 2]
        wi += 1
        c0 = 1024 + j * 64
        load_w_cols(fl, Wb, w_in, c0, 64, "wn%d" % ((wi - 1) % 2))
        load_w_cols(fl, Wsb, w_in, c0, 64, "ws%d" % ((wi - 1) % 2), swap=True)
        for tt in range(8):
            dests = [(0, KT, KT.ap[0:64, tt * 512:(tt + 1) * 512])]
            rope_proj_chunk(fl, XT, Wb, Wsb, 64, tt, PJ, pj_i, CT, ST, tmps, tmp_i, dests, x_ready)
            pj_i += 1
            tmp_i += 1
        Wb = Wn[wi % 2]
        wi += 1
        c0 = 1280 + j * 64
        load_w_cols(fl, Wb, w_in, c0, 64, "wn%d" % ((wi - 1) % 2))
        for g8 in range(4):
            P = PJ[pj_i % 4]
            pj_i += 1
            pe.wait(P.deps_w(), Wb.deps_r(), x_ready)
            tok = None
            for t8 in range(8):
                tb = g8 * 8 + t8
                for kc in range(8):
                    tok = pe.done(pe.e.matmul(P.ap[:, t8 * 64:(t8 + 1) * 64], XT.ap[:, kc, tb * 128:(tb + 1) * 128],
                                              Wb.ap[:, kc, 0:64], start=(kc == 0), stop=(kc == 7)))
            Wb.read(tok)
            XT.read(tok)
            P.wrote(tok)
            act.wait(tok, VA.deps_w())
            tv = act.done(act.e.activation(out=VA.ap[:, g8 * 8:(g8 + 1) * 8, 0:64],
                                           in_=P.ap[:, :].rearrange("p (a b) -> p a b", a=8), func=AF.Copy))
            P.read(tv)
            VA.wrote(tv, fresh=False)

        for kt in range(NT):
            nq = 256 if kt < NT - 1 else 128
            for hl in range(4):
                sb_, so = STs[it % 4]
                p = PTs[it % 4]
                it += 1
                pe.wait(sb_.deps_w(), KT.deps_r(), QT.deps_r())
                tok = pe.done(pe.e.matmul(sb_.ap[:, so:so + nq], KT.ap[0:64, kt * 128:(kt + 1) * 128],
                                          QT.ap[0:64, hl, kt * 128:kt * 128 + nq], start=True, stop=True))
                sb_.wrote(tok)
                KT.read(tok)
                QT.read(tok)
                act.wait(tok, p.deps_w())
                ta = act.done(act.e.activation(out=p.ap[:, 0:nq], in_=sb_.ap[:, so:so + nq], func=AF.Exp, scale=0.125))
                sb_.read(ta)
                p.wrote(ta)
                dve.wait(ta, tmk)
                td = dve.done(dve.e.tensor_tensor(out=p.ap[:, 0:nq], in0=p.ap[:, 0:nq], in1=MK[:, 0:nq], op=ALU.mult))
                p.wrote(td)
                for qb in ([kt, kt + 1] if kt < NT - 1 else [kt]):
                    o = OPs[qb % 3]
                    first = (qb == kt + 1) or (kt == 0)
                    last = (qb == kt)
                    if first and hl == 0:
                        pe.wait(o.deps_w())
                    pe.wait(td, VA.deps_r())
                    tok = pe.done(pe.e.matmul(o.ap[:, hl * 65:hl * 65 + 65], p.ap[:, (qb - kt) * 128:(qb - kt + 1) * 128],
                                              VA.ap[:, kt, :], start=(first and hl == 0), stop=(last and hl == 3)))
                    p.read(tok)
                    VA.read(tok)
                    o.wrote(tok, fresh=(first and hl == 0))
            o = OPs[kt % 3]
            lt = lts[kt % 4]
            o4 = o.ap[:, 0:260].rearrange("p (h c) -> p h c", h=4)
            dve.wait(o.deps_r(), tes, OG.deps_w())
            f1 = dve.done(dve.e.tensor_tensor(out=lt[:, :], in0=o4[:, :, 64], in1=es[:, 4 * j:4 * j + 4], op=ALU.add))
            dve.wait(f1)
            f2 = dve.done(dve.e.reciprocal(out=lt[:, :], in_=lt[:, :]))
            dve.wait(f2)
            f3 = dve.done(dve.e.tensor_tensor(out=OG.ap[:, kt, :].rearrange("p (h c) -> p h c", h=4), in0=o4[:, :, 0:64],
                                              in1=lt[:, :].unsqueeze(2).to_broadcast([128, 4, 64]), op=ALU.mult))
            o.read(f1)
            o.read(f3)
            OG.wrote(f3, fresh=False)
        osem = fl.dsem("ostore")
        sp.wait(OG.deps_r())
        tso = sp.dma(d["attnO"].rearrange("(t p) c -> p t c", p=128)[:, :, j * 256:(j + 1) * 256], OG.ap[:, :, :], osem)
        OG.read(tso)
        OG.w = {}
    fl.end()


def phase_outproj_ln(fl, d, attn_d, w_out_d, xres_d, ln_i, ln_j, out_d, outb_d, router_d=None, gate_d=None):
    pe, act, dve, pool, sp = fl.pe, fl.act, fl.dve, fl.pool, fl.sp
    fl.begin()
    OT = Buf(fl.sb([128, 8, S], BF16, "OT"))
    Wo = Buf(fl.sb([128, 8, D], BF16, "Wo"))
    G, Bt, tln = load_ln_params(fl, d["ln_gain"], d["ln_bias"], ln_i, ln_j, None)
    xts = [Buf(fl.sb([128, D], F32, "xt")) for _ in range(2)]
    rts = [Buf(fl.sb([128, D], F32, "rt")) for _ in range(2)]
    xns = [Buf(fl.sb([128, D], F32, "xn")) for _ in range(2)]
    ots = [Buf(fl.sb([128, D], F32, "ot")) for _ in range(2)]
    obs = [Buf(fl.sb([128, D], BF16, "ob")) for _ in range(2)]
    stats = [fl.sb([128, 2, 6], F32, "st") for _ in range(2)]
    mvs = [fl.sb([128, 2], F32, "mv") for _ in range(2)]
    rss = [fl.sb([128, 1], F32, "rs") for _ in range(2)]
    Ys = [Buf(fl.ps([128, 1024], F32, "Y")) for _ in range(2)]
    if router_d is not None:
        RB = fl.sb([128, NE, D], F32, "RB")
        rsem = fl.dsem("rb")
        trb = None
        for e in range(NE):
            trb = sp.dma(RB[:, e, :], d["routerT"][e, :].partition_broadcast(128), rsem)
        junk = fl.sb([128, D], F32, "junk")
        jtok = [None]
        lgs = [fl.sb([128, 8], F32, "lg") for _ in range(2)]
        mx8 = [fl.sb([128, 8], F32, "mx8") for _ in range(2)]
        gts = [Buf(fl.sb([128, 8], F32, "gt")) for _ in range(2)]
        g2 = [fl.sb([128, 8], F32, "g2") for _ in range(2)]
        w12 = [fl.sb([128, 2], F32, "w12") for _ in range(2)]
    wsem = fl.dsem("wo")
    pool.wait(Wo.deps_w())
    tw = pool.dma(Wo.ap[:, :, :], w_out_d.rearrange("(kc p) n -> p kc n", p=128), wsem)
    Wo.wrote(tw)
    tot = load_xT(fl, OT, attn_d, None, "xt")
    xsem = [fl.dsem("xl0"), fl.dsem("xl1")]
    ssem = [fl.dsem("st0"), fl.dsem("st1")]
    for tb in range(NT):
        k = tb % 2
        xt, rt, xn, ot, ob, Y = xts[k], rts[k], xns[k], ots[k], obs[k], Ys[k]
        sp.wait(xt.deps_w())
        tx = sp.dma(xt.ap[:, :], xres_d[tb * 128:(tb + 1) * 128, :], xsem[k])
        xt.wrote(tx)
        pe.wait(Y.deps_w(), tw, tot)
        tok = None
        for half in range(2):
            for kc in range(8):
                tok = pe.done(pe.e.matmul(Y.ap[:, half * 512:(half + 1) * 512], OT.ap[:, kc, tb * 128:(tb + 1) * 128],
                                          Wo.ap[:, kc, half * 512:(half + 1) * 512], start=(kc == 0), stop=(kc == 7)))
        Y.wrote(tok)
        dve.wait(tok, tx, rt.deps_w())
        tr = None
        for half in range(2):
            sl = slice(half * 512, (half + 1) * 512)
            tr = dve.done(dve.e.scalar_tensor_tensor(out=rt.ap[:, sl], in0=xt.ap[:, sl], scalar=ALPHA, in1=Y.ap[:, sl],
                                                     op0=ALU.mult, op1=ALU.add))
        Y.read(tr)
        xt.read(tr)
        rt.wrote(tr)
        pool.wait(tln)
        t7 = layernorm_tile(fl, rt, stats[k], mvs[k], rss[k], xn, ot, G, Bt)
        sp.wait(t7)
        ts1 = sp.dma(out_d[tb * 128:(tb + 1) * 128, :], ot.ap[:, :], ssem[k])
        ot.read(ts1)
        act.wait(t7, ob.deps_w())
        tc = act.done(act.e.activation(out=ob.ap[:, :], in_=ot.ap[:, :], func=AF.Copy))
        ot.read(tc)
        ob.wrote(tc)
        sp.wait(tc)
        ts2 = sp.dma(outb_d[tb * 128:(tb + 1) * 128, :], ob.ap[:, :], fl.dsem("sb%d" % k))
        ob.read(ts2)
        if router_d is not None:
            lg, m8, gt, gg, ww = lgs[k], mx8[k], gts[k], g2[k], w12[k]
            dve.wait(t7, trb)
            tl = jtok[0]
            for e in range(NE):
                dve.wait(tl)
                tl = dve.done(dve.e.scalar_tensor_tensor(out=junk[:, :], in0=ot.ap[:, :], scalar=1.0, in1=RB[:, e, :],
                                                         op0=ALU.mult, op1=ALU.mult, accum_out=lg[:, e:e + 1]))
            ot.read(tl)
            jtok[0] = tl
            dve.wait(tl)
            tm = dve.done(dve.e.max(out=m8[:, :], in_=lg[:, :]))
            dve.wait(tm)
            t_a = dve.done(dve.e.tensor_tensor(out=ww[:, 0:1], in0=m8[:, 0:1], in1=m8[:, 1:2], op=ALU.subtract))
            t_b = dve.done(dve.e.tensor_tensor(out=ww[:, 1:2], in0=m8[:, 1:2], in1=m8[:, 0:1], op=ALU.subtract))
            act.wait(t_a, t_b)
            t_s = act.done(act.e.activation(out=ww[:, :], in_=ww[:, :], func=AF.Sigmoid))
            dve.wait(t_s, gt.deps_w())
            t_g1 = dve.done(dve.e.tensor_scalar(out=gt.ap[:, :], in0=lg[:, :], scalar1=m8[:, 0:1], scalar2=ww[:, 0:1],
                                                op0=ALU.is_equal, op1=ALU.mult))
            t_g2 = dve.done(dve.e.tensor_scalar(out=gg[:, :], in0=lg[:, :], scalar1=m8[:, 1:2], scalar2=ww[:, 1:2],
                                                op0=ALU.is_equal, op1=ALU.mult))
            dve.wait(t_g1, t_g2)
            t_g3 = dve.done(dve.e.tensor_tensor(out=gt.ap[:, :], in0=gt.ap[:, :], in1=gg[:, :], op=ALU.add))
            gt.wrote(t_g3)
            sp.wait(t_g3)
            ts3 = sp.dma(gate_d[tb * 128:(tb + 1) * 128, :], gt.ap[:, :], fl.dsem("sg%d" % k))
            gt.read(ts3)
    fl.end()


def phase_ffn(fl, d, xb_d, xres_d, wg_list, wu_list, wd_list, gate_d, ln_i, ln_j, out_d, outb_d, TG=1024):
    pe, act, dve, pool, sp = fl.pe, fl.act, fl.dve, fl.pool, fl.sp
    fl.begin()
    ne = len(wg_list)
    ntile = TG // 128
    XTg = Buf(fl.sb([128, 8, TG], BF16, "XTg"))
    acc = [Buf(fl.sb([128, D], F32, "acc")) for _ in range(ntile)]
    Wg = [Buf(fl.sb([128, 8, 512], BF16, "Wg")) for _ in range(2)]
    Wu = [Buf(fl.sb([128, 8, 512], BF16, "Wu")) for _ in range(2)]
    Wd = [Buf(fl.sb([128, 4, D], BF16, "Wd")) for _ in range(2)]
    Hs = [Buf(fl.sb([128, 4, 512], BF16, "H")) for _ in range(2)]
    Ss = [Buf(fl.sb([128, 512], F32, "Ssil")) for _ in range(2)]
    G, Bt, tln = load_ln_params(fl, d["ln_gain"], d["ln_bias"], ln_i, ln_j, None)
    xts = [Buf(fl.sb([128, D], F32, "xt")) for _ in range(2)]
    xns = [Buf(fl.sb([128, D], F32, "xn")) for _ in range(2)]
    ots = [Buf(fl.sb([128, D], F32, "ot")) for _ in range(2)]
    obs = [Buf(fl.sb([128, D], BF16, "ob")) for _ in range(2)]
    stats = [fl.sb([128, 2, 6], F32, "st") for _ in range(2)]
    mvs = [fl.sb([128, 2], F32, "mv") for _ in range(2)]
    rss = [fl.sb([128, 1], F32, "rs") for _ in range(2)]
    GPs = [Buf(fl.ps([128, 512], F32, "GP")) for _ in range(2)]
    UPs = [Buf(fl.ps([128, 512], F32, "UP")) for _ in range(2)]
    Ys = [Buf(fl.ps([128, 1024], F32, "Y")) for _ in range(2)]
    if gate_d is not None:
        GT = Buf(fl.sb([128, ntile, NE], F32, "GT"))
    wsems = [fl.dsem("ffw0"), fl.dsem("ffw1")]
    xsem = [fl.dsem("xl0"), fl.dsem("xl1")]
    ssem = [fl.dsem("st0"), fl.dsem("st1")]
    gsem = fl.dsem("gl")
    wi = 0
    gi = 0
    yi = 0
    hi = 0
    ei = 0
    for tg in range(S // TG):
        t0 = tg * TG
        tx = load_xT(fl, XTg, xb_d, None, "xt", t0=t0, nt=TG)
        if gate_d is not None:
            sp.wait(GT.deps_w())
            tgl = sp.dma(GT.ap[:, :, :], gate_d[t0:t0 + TG, :].rearrange("(t p) e -> p t e", p=128), gsem)
            GT.wrote(tgl)
        for e in range(ne):
            for fg in range(FF // 512):
                k = wi % 2
                wi += 1
                wg, wu, wd = Wg[k], Wu[k], Wd[k]
                pool.wait(wg.deps_w(), wu.deps_w(), wd.deps_w())
                f0 = fg * 512
                pool.dma(wg.ap[:, :, :], wg_list[e].rearrange("(kc p) n -> p kc n", p=128)[:, :, f0:f0 + 512], wsems[k])
                pool.dma(wu.ap[:, :, :], wu_list[e].rearrange("(kc p) n -> p kc n", p=128)[:, :, f0:f0 + 512], wsems[k])
                tw = pool.dma(wd.ap[:, :, :], wd_list[e][f0:f0 + 512, :].rearrange("(fc p) n -> p fc n", p=128), wsems[k])
                wg.wrote(tw)
                wu.wrote(tw)
                wd.wrote(tw)
                first_acc = (e == 0 and fg == 0)
                for tt in range(TG // 512):
                    H = Hs[hi % 2]
                    hi += 1
                    for fc in range(4):
                        GP, UP, Sb = GPs[gi % 2], UPs[gi % 2], Ss[gi % 2]
                        gi += 1
                        for (P, Wt) in ((GP, wg), (UP, wu)):
                            pe.wait(P.deps_w(), tw, tx)
                            tok = None
                            for kc in range(8):
                                tok = pe.done(pe.e.matmul(P.ap[:, :], Wt.ap[:, kc, fc * 128:(fc + 1) * 128],
                                                          XTg.ap[:, kc, tt * 512:(tt + 1) * 512], start=(kc == 0), stop=(kc == 7)))
                            P.wrote(tok)
                            Wt.read(tok)
                            XTg.read(tok)
                        act.wait(GP.deps_r(), Sb.deps_w())
                        ta = act.done(act.e.activation(out=Sb.ap[:, :], in_=GP.ap[:, :], func=AF.Silu))
                        GP.read(ta)
                        Sb.wrote(ta)
                        dve.wait(ta, UP.deps_r(), H.deps_w())
                        th = dve.done(dve.e.tensor_tensor(out=H.ap[:, fc, :], in0=UP.ap[:, :], in1=Sb.ap[:, :], op=ALU.mult))
                        UP.read(th)
                        Sb.read(th)
                        H.wrote(th, fresh=(fc == 0))
                    for tb in range(4):
                        Y = Ys[yi % 2]
                        yi += 1
                        tile_i = tt * 4 + tb
                        pe.wait(Y.deps_w(), H.deps_r(), tw)
                        tok = None
                        for half in range(2):
                            for fc in range(4):
                                tok = pe.done(pe.e.matmul(Y.ap[:, half * 512:(half + 1) * 512], H.ap[:, fc, tb * 128:(tb + 1) * 128],
                                                          wd.ap[:, fc, half * 512:(half + 1) * 512], start=(fc == 0), stop=(fc == 3)))
                        Y.wrote(tok)
                        H.read(tok)
                        wd.read(tok)
                        a = acc[tile_i]
                        dve.wait(tok, a.deps_w())
                        ty = None
                        for half in range(2):
                            sl = slice(half * 512, (half + 1) * 512)
                            if gate_d is None:
                                if first_acc:
                                    ty = dve.done(dve.e.tensor_copy(out=a.ap[:, sl], in_=Y.ap[:, sl]))
                                else:
                                    ty = dve.done(dve.e.tensor_tensor(out=a.ap[:, sl], in0=Y.ap[:, sl], in1=a.ap[:, sl], op=ALU.add))
                            else:
                                dve.wait(GT.deps_r())
                                gsc = GT.ap[:, tile_i, e:e + 1]
                                if first_acc:
                                    ty = dve.done(dve.e.tensor_scalar(out=a.ap[:, sl], in0=Y.ap[:, sl], scalar1=gsc, scalar2=None,
                                                                      op0=ALU.mult))
                                else:
                                    ty = dve.done(dve.e.scalar_tensor_tensor(out=a.ap[:, sl], in0=Y.ap[:, sl], scalar=gsc,
                                                                             in1=a.ap[:, sl], op0=ALU.mult, op1=ALU.add))
                        Y.read(ty)
                        a.wrote(ty)
                        if gate_d is not None:
                            GT.read(ty)
        for ti in range(ntile):
            k = ei % 2
            ei += 1
            tbg = t0 // 128 + ti
            xt, xn, ot, ob = xts[k], xns[k], ots[k], obs[k]
            a = acc[ti]
            sp.wait(xt.deps_w())
            txl = sp.dma(xt.ap[:, :], xres_d[tbg * 128:(tbg + 1) * 128, :], xsem[k])
            xt.wrote(txl)
            dve.wait(txl, a.deps_r())
            tr = dve.done(dve.e.scalar_tensor_tensor(out=a.ap[:, :], in0=xt.ap[:, :], scalar=ALPHA, in1=a.ap[:, :],
                                                     op0=ALU.mult, op1=ALU.add))
            xt.read(tr)
            a.wrote(tr)
            pool.wait(tln)
            t7 = layernorm_tile(fl, a, stats[k], mvs[k], rss[k], xn, ot, G, Bt)
            sp.wait(t7)
            ts1 = sp.dma(out_d[tbg * 128:(tbg + 1) * 128, :], ot.ap[:, :], ssem[k])
            ot.read(ts1)
            if outb_d is not None:
                act.wait(t7, ob.deps_w())
                tc = act.done(act.e.activation(out=ob.ap[:, :], in_=ot.ap[:, :], func=AF.Copy))
                ot.read(tc)
                ob.wrote(tc)
                sp.wait(tc)
                ts2 = sp.dma(outb_d[tbg * 128:(tbg + 1) * 128, :], ob.ap[:, :], fl.dsem("sb%d" % k))
                ob.read(ts2)
    fl.end()


NSA_FM = [(0, True), (128, True), (256, True), (384, True), (512, True), (640, True), (768, True), (896, True),
          (1024, True), (1152, True),
          (1280, False), (1408, False),
          (1536, True), (1664, True),
          (2048, True), (2176, True)]


def nsa_projT_row(c0):
    return c0 if c0 < 1792 else c0 - 256


def phase_nsa_proj(fl, d):
    pe, act, dve, pool, sp = fl.pe, fl.act, fl.dve, fl.pool, fl.sp
    fl.begin()
    XT = Buf(fl.sb([128, 8, S], BF16, "XT"))
    CT = fl.sb([128, S], F32, "CT")
    ST = fl.sb([128, S], F32, "ST")
    Wn = [Buf(fl.sb([128, 8, 128], BF16, "Wn")) for _ in range(2)]
    Ws = [Buf(fl.sb([128, 8, 128], BF16, "Ws")) for _ in range(2)]
    Wt = Buf(fl.sb([128, 8, 560], BF16, "Wt"))
    tmps = [Buf(fl.sb([128, 512], F32, "tmp")) for _ in range(4)]
    stg = [Buf(fl.sb([128, S], BF16, "stg")) for _ in range(2)]
    vst = [Buf(fl.sb([128, 512], BF16, "vst")) for _ in range(2)]
    gst = [Buf(fl.sb([128, 48], F32, "gst")) for _ in range(2)]
    PJ = [Buf(fl.ps([128, 512], F32, "bank")) for _ in range(6)]
    csem = fl.dsem("const")
    sp.dma(CT[:, :], d["cosT"][:, :], csem)
    sp.dma(ST[:, :], d["sinT"][:, :], csem)
    tc1 = (csem, csem.n)
    tx = load_xT(fl, XT, d["x2b"], None, "xt")
    x_ready = [tx, tc1]
    w_in = d["b_w_in"][0]
    projT = d["projT"]
    ssem = [fl.dsem("st0"), fl.dsem("st1")]
    pj_i = 0
    tmp_i = 0
    for ci, (c0, roped) in enumerate(NSA_FM):
        Wb, Wsb = Wn[ci % 2], Ws[ci % 2]
        sg = stg[ci % 2]
        load_w_cols(fl, Wb, w_in, c0, 128, "wn%d" % (ci % 2))
        if roped:
            load_w_cols(fl, Wsb, w_in, c0, 128, "ws%d" % (ci % 2), swap=True)
        for tt in range(8):
            if roped:
                dests = [(0, sg, sg.ap[0:64, tt * 512:(tt + 1) * 512]), (64, sg, sg.ap[64:128, tt * 512:(tt + 1) * 512])]
                rope_proj_chunk(fl, XT, Wb, Wsb, 128, tt, PJ[0:4], pj_i, CT, ST, tmps, tmp_i, dests, x_ready)
                pj_i += 1
                tmp_i += 1
            else:
                P = PJ[4 + (tt % 2)]
                pe.wait(P.deps_w(), Wb.deps_r(), x_ready)
                tok = None
                for kc in range(8):
                    tok = pe.done(pe.e.matmul(P.ap[:, :], Wb.ap[:, kc, :], XT.ap[:, kc, tt * 512:(tt + 1) * 512],
                                              start=(kc == 0), stop=(kc == 7)))
                Wb.read(tok)
                XT.read(tok)
                P.wrote(tok)
                act.wait(tok, sg.deps_w())
                tv = act.done(act.e.activation(out=sg.ap[:, tt * 512:(tt + 1) * 512], in_=P.ap[:, :], func=AF.Copy))
                P.read(tv)
                sg.wrote(tv, fresh=False)
        r0 = nsa_projT_row(c0)
        sp.wait(sg.deps_r())
        ts = sp.dma(projT[r0:r0 + 128, :], sg.ap[:, :], ssem[ci % 2])
        sg.read(ts)
        sg.w = {}
    wsem = fl.dsem("wt")
    pool.wait(Wt.deps_w())
    wsrc = w_in.rearrange("(kc p) n -> p kc n", p=128)
    pool.dma(Wt.ap[:, :, 0:256], wsrc[:, :, 1792:2048], wsem)
    pool.dma(Wt.ap[:, :, 256:512], wsrc[:, :, 2304:2560], wsem)
    tw = pool.dma(Wt.ap[:, :, 512:560], wsrc[:, :, 2560:2608], wsem)
    Wt.wrote(tw)
    for tb in range(NT):
        P = PJ[(2 * tb) % 6]
        Pg = PJ[(2 * tb + 1) % 6]
        pe.wait(P.deps_w(), Pg.deps_w(), tw, x_ready)
        tok = None
        for kc in range(8):
            tok = pe.done(pe.e.matmul(P.ap[:, :], XT.ap[:, kc, tb * 128:(tb + 1) * 128], Wt.ap[:, kc, 0:512],
                                      start=(kc == 0), stop=(kc == 7)))
        P.wrote(tok)
        tokg = None
        for kc in range(8):
            tokg = pe.done(pe.e.matmul(Pg.ap[:, 0:48], XT.ap[:, kc, tb * 128:(tb + 1) * 128], Wt.ap[:, kc, 512:560],
                                       start=(kc == 0), stop=(kc == 7)))
        Pg.wrote(tokg)
        vs_, gs_ = vst[tb % 2], gst[tb % 2]
        act.wait(tok, vs_.deps_w())
        tv = act.done(act.e.activation(out=vs_.ap[:, :], in_=P.ap[:, :], func=AF.Copy))
        P.read(tv)
        vs_.wrote(tv)
        act.wait(tokg, gs_.deps_w())
        tg_ = act.done(act.e.activation(out=gs_.ap[:, :], in_=Pg.ap[:, 0:48], func=AF.Sigmoid))
        Pg.read(tg_)
        gs_.wrote(tg_)
        sp.wait(tv, tg_)
        t1 = sp.dma(d["vtok"][tb * 128:(tb + 1) * 128, :], vs_.ap[:, :], ssem[tb % 2])
        t2 = sp.dma(d["gl"][tb * 128:(tb + 1) * 128, :], gs_.ap[:, :], fl.dsem("sg%d" % (tb % 2)))
        vs_.read(t1)
        gs_.read(t2)
    fl.end()


class AttnItem:
    def __init__(self):
        self.A = None
        self.B = None
        self.F = None
        self.slot = None


def attn_items(fl, S_ring, PT_ring, O, Kbuf, krows, Qbuf, qsel, q0, Vbuf, W, ktiles, finalize=None, pe_extra_deps=()):
    pe, act, dve = fl.pe, fl.act, fl.dve
    st = {"first": True}
    last_for = {}
    for i, (_, _, qlo, qhi, _) in enumerate(ktiles):
        for qb in range(qlo, qhi):
            last_for[qb] = i
    items = []
    nk = len(ktiles)
    for i, (kc0, vidx, qlo, qhi, masks) in enumerate(ktiles):
        it = AttnItem()

        def A(slot, it=it, kc0=kc0, qlo=qlo, qhi=qhi, masks=masks):
            it.slot = slot
            Sb = S_ring[slot % len(S_ring)]
            p = PT_ring[slot % len(PT_ring)]
            n0, n1 = qlo * 128, qhi * 128
            pe.wait(Sb.deps_w(), Kbuf.deps_r(), Qbuf.deps_r(), pe_extra_deps)
            tok = pe.done(pe.e.matmul(Sb.ap[:, n0:n1], Kbuf.ap[0:krows, kc0:kc0 + 128],
                                      Qbuf.ap[0:krows, qsel, q0 + n0:q0 + n1], start=True, stop=True))
            Sb.wrote(tok)
            Kbuf.read(tok)
            Qbuf.read(tok)
            act.wait(tok, p.deps_w())
            ta = act.done(act.e.activation(out=p.ap[:, n0:n1], in_=Sb.ap[:, n0:n1], func=AF.Exp, scale=0.125))
            Sb.read(ta)
            p.wrote(ta)
            for (qb, mask_ap, mdeps) in masks:
                dve.wait(ta, mdeps)
                tl = dve.done(dve.e.tensor_tensor(out=p.ap[:, qb * 128:(qb + 1) * 128], in0=p.ap[:, qb * 128:(qb + 1) * 128],
                                                  in1=mask_ap, op=ALU.mult))
                p.wrote(tl, fresh=False)

        def B(it=it, i=i, vidx=vidx, qlo=qlo, qhi=qhi):
            p = PT_ring[it.slot % len(PT_ring)]
            for qb in range(qlo, qhi):
                if st["first"]:
                    pe.wait(O.deps_w())
                pe.wait(p.deps_r(), Vbuf.deps_r())
                tok = pe.done(pe.e.matmul(O.ap[:, qb * W:(qb + 1) * W], p.ap[:, qb * 128:(qb + 1) * 128], Vbuf.ap[:, vidx, 0:W],
                                          start=st["first"], stop=(i == nk - 1 and qb == qhi - 1)))
                O.wrote(tok, fresh=st["first"])
                st["first"] = False
                p.read(tok)
                Vbuf.read(tok)

        it.A = A
        it.B = B
        items.append(it)
    items[-1].F = finalize
    return items


def run_items(items, slot0, L=2):
    n = len(items)
    for i in range(n + L):
        if i < n:
            items[i].A(slot0 + i)
        j = i - L
        if j >= 0:
            items[j].B()
            if items[j].F is not None:
                items[j].F()
    return slot0 + n


def phase_nsa_attn(fl, d):
    pe, act, dve, pool, sp = fl.pe, fl.act, fl.dve, fl.pool, fl.sp
    fl.begin()
    projT, vtok, gl = d["projT"], d["vtok"], d["gl"]
    QN = Buf(fl.sb([128, 4, S], BF16, "QN"))
    KE = Buf(fl.sb([128, S], BF16, "KE"))
    KW = Buf(fl.sb([64, S], BF16, "KW"))
    KC = Buf(fl.sb([64, S], BF16, "KC"))
    VC = Buf(fl.sb([64, S], BF16, "VC"))
    VS = Buf(fl.sb([128, NT, 65], BF16, "VS"))
    VW = Buf(fl.sb([128, NT, 65], BF16, "VW"))
    KCC = Buf(fl.sb([64, 256], BF16, "KCC"))
    RC = Buf(fl.sb([128, 2, 129], BF16, "RC"))
    HK = Buf(fl.sb([128, 2, 256], BF16, "HK"))
    HV = Buf(fl.sb([128, 2, 256], BF16, "HV"))
    W1K = fl.sb([64, 32, 256], BF16, "W1K")
    W1V = fl.sb([64, 32, 256], BF16, "W1V")
    W2K = fl.sb([128, 2, 64], BF16, "W2K")
    W2V = fl.sb([128, 2, 64], BF16, "W2V")
    POSK = fl.sb([64, 32], BF16, "POSK")
    POSV = fl.sb([64, 32], BF16, "POSV")
    CB = fl.sb([128, 4], F32, "CB")
    MK = fl.sb([128, 256], BF16, "MK")
    MCM = fl.sb([128, 9, 512], BF16, "MCM")
    FA = fl.sb([128, NT, 64], BF16, "FA")
    FB = fl.sb([128, NT, 64], BF16, "FB")
    IDN = fl.sb([128, 128], BF16, "IDN")
    GS = fl.sb([128, NT, 48], F32, "GS")
    IMP = Buf(fl.sb([128, NT, 64], F32, "IMP"))
    OG = Buf(fl.sb([128, NT, 256], F32, "OG"))
    NBT = [Buf(fl.sb([128, 128], BF16, "NBT")) for _ in range(2)]
    PTs = [Buf(fl.sb([128, 512], BF16, "PT")) for _ in range(3)]
    smalls = [fl.sb([128, 16], F32, "sm") for _ in range(4)]
    otmp = [Buf(fl.sb([128, 4, 64], F32, "otmp")) for _ in range(2)]
    im2 = [fl.sb([128, 64], F32, "im2") for _ in range(2)]
    imm = [fl.sb([128, 64], F32, "imm") for _ in range(2)]
    m8 = [fl.sb([128, 16], F32, "m8") for _ in range(2)]
    S_ring = [Buf(fl.ps([128, 512], F32, "Sb")) for _ in range(3)]
    O_ring = [Buf(fl.ps([128, 512], F32, "Ob")) for _ in range(2)]
    TP = Buf(fl.ps([128, 512], BF16, "TP"))
    CPS = [Buf(fl.ps([128, 512], F32, "CPS")) for _ in range(2)]

    csem = fl.dsem("const")
    cpsem = fl.dsem("constp")
    pool.dma(MK[:, :], d["swa_mask"][:, :], cpsem)
    pool.dma(MCM[:, :, :], d["cmp_mask"].rearrange("n p c -> p n c"), cpsem)
    pool.dma(FA[:, :, :], d["sel_fa"].rearrange("(t p) c -> p t c", p=128), cpsem)
    pool.dma(FB[:, :, :], d["sel_fb"].rearrange("(t p) c -> p t c", p=128), cpsem)
    pool.dma(IDN[:, :], d["ident"][:, :], cpsem)
    pool.dma(KE.ap[64:128, :], d["emat"][:, :], cpsem)
    pool.dma(RC.ap[:, :, 65:129], d["ov_pad"].rearrange("(c p) j -> p c j", p=128), cpsem)
    pool.dma(W1K[:, :, :], d["b_cmp_k_w1"][0].rearrange("(l dd) m -> dd l m", dd=64), cpsem)
    pool.dma(W1V[:, :, :], d["b_cmp_v_w1"][0].rearrange("(l dd) m -> dd l m", dd=64), cpsem)
    pool.dma(W2K[:, :, :], d["b_cmp_k_w2"][0].rearrange("(mc p) dd -> p mc dd", p=128), cpsem)
    pool.dma(W2V[:, :, :], d["b_cmp_v_w2"][0].rearrange("(mc p) dd -> p mc dd", p=128), cpsem)
    pool.dma(POSK[:, :], d["posT_k"][:, :], cpsem)
    pool.dma(POSV[:, :], d["posT_v"][:, :], cpsem)
    tcp = (cpsem, cpsem.n + 0)
    tgs = sp.dma(GS[:, :, :], gl.rearrange("(t p) c -> p t c", p=128), csem)
    dve.wait(tcp)
    t_m = dve.done(dve.e.memset(VS.ap[:, :, 64:65], 1.0))
    t_m = dve.done(dve.e.memset(VW.ap[:, :, 64:65], 1.0))
    t_m = dve.done(dve.e.memset(RC.ap[:, :, 64:65], 1.0))
    t_m = dve.done(dve.e.memset(HK.ap[:, :, :], 0.0))
    t_m = dve.done(dve.e.memset(HV.ap[:, :, :], 0.0))
    t_m = dve.done(dve.e.memset(KCC.ap[:, :], 0.0))
    for nb in NBT:
        t_m = dve.done(dve.e.memset(nb.ap[:, :], 0.0))
    t_init = t_m
    pe.wait(tcp)
    P = CPS[0]
    tok = None
    for which, (W1, POS) in enumerate(((W1K, POSK), (W1V, POSV))):
        for mc in range(2):
            col = which * 2 + mc
            for l in range(32):
                tok = pe.done(pe.e.matmul(P.ap[:, col:col + 1], W1[0:64, l, mc * 128:(mc + 1) * 128], POS[0:64, l:l + 1],
                                          start=(l == 0), stop=(l == 31)))
    P.wrote(tok)
    dve.wait(tok)
    t_cb = dve.done(dve.e.tensor_copy(out=CB[:, :], in_=P.ap[:, 0:4]))
    P.read(t_cb)

    lsem = [fl.dsem("xl0"), fl.dsem("xl1")]
    osem = fl.dsem("ostore_sw")
    s_i = 0
    o_i = 0
    sm_i = 0
    for j in range(4):
        sp.wait(QN.deps_w(), KE.deps_w(), KW.deps_w(), KC.deps_w(), VC.deps_w(), VS.deps_w(), VW.deps_w(), t_init)
        ls = lsem[j % 2]
        for hl in range(4):
            sp.dma(QN.ap[0:64, hl, :], projT[(4 * j + hl) * 64:(4 * j + hl + 1) * 64, :], ls)
        sp.dma(KC.ap[0:64, :], projT[1024 + j * 64:1024 + (j + 1) * 64, :], ls)
        sp.dma(VC.ap[0:64, :], projT[1280 + j * 64:1280 + (j + 1) * 64, :], ls)
        sp.dma(KE.ap[0:64, :], projT[1536 + j * 64:1536 + (j + 1) * 64, :], ls)
        sp.dma(KW.ap[0:64, :], projT[1792 + j * 64:1792 + (j + 1) * 64, :], ls)
        vt3 = vtok.rearrange("(t p) c -> p t c", p=128)
        sp.dma(VS.ap[:, :, 0:64], vt3[:, :, j * 64:(j + 1) * 64], ls)
        tl = sp.dma(VW.ap[:, :, 0:64], vt3[:, :, 256 + j * 64:256 + (j + 1) * 64], ls)
        for b in (QN, KE, KW, KC, VC, VS, VW):
            b.wrote(tl, fresh=False)
            b.r = {}
        ld = [tl, tcp, t_init]

        for which, (src, W1, W2, Hb) in enumerate(((KC, W1K, W2K, HK), (VC, W1V, W2V, HV))):
            for mc in range(2):
                P = CPS[(which * 2 + mc) % 2]
                pe.wait(P.deps_w(), ld)
                tok = None
                for l in range(32):
                    tok = pe.done(pe.e.matmul(P.ap[:, 0:255], W1[0:64, l, mc * 128:(mc + 1) * 128], src.ap[0:64, l:l + 4065:16],
                                              start=(l == 0), stop=(l == 31)))
                P.wrote(tok)
                src.read(tok)
                act.wait(tok, t_cb, Hb.deps_w(), t_init)
                th = act.done(act.e.activation(out=Hb.ap[:, mc, 0:255], in_=P.ap[:, 0:255], func=AF.Gelu_apprx_tanh,
                                               bias=CB[:, which * 2 + mc:which * 2 + mc + 1]))
                P.read(th)
                Hb.wrote(th, fresh=False)
        P = CPS[0]
        pe.wait(P.deps_w(), HK.deps_r())
        tok = None
        for mc in range(2):
            tok = pe.done(pe.e.matmul(P.ap[0:64, 0:255], W2K[:, mc, :], HK.ap[:, mc, 0:255], start=(mc == 0), stop=(mc == 1)))
        P.wrote(tok)
        HK.read(tok)
        act.wait(tok, KCC.deps_w())
        tk = act.done(act.e.activation(out=KCC.ap[0:64, 0:255], in_=P.ap[0:64, 0:255], func=AF.Copy))
        P.read(tk)
        KCC.wrote(tk)
        P = CPS[1]
        pe.wait(P.deps_w(), HV.deps_r())
        tok = None
        for ct in range(2):
            for mc in range(2):
                tok = pe.done(pe.e.matmul(P.ap[:, ct * 64:(ct + 1) * 64], HV.ap[:, mc, ct * 128:(ct + 1) * 128], W2V[:, mc, :],
                                          start=(mc == 0), stop=(mc == 1)))
        P.wrote(tok)
        HV.read(tok)
        act.wait(tok, RC.deps_w())
        tr_ = act.done(act.e.activation(out=RC.ap[:, :, 0:64], in_=P.ap[:, 0:128].rearrange("p (c x) -> p c x", c=2), func=AF.Copy))
        P.read(tr_)
        RC.wrote(tr_)
        HK.w = {}
        HV.w = {}

        mcm_idx = {(0, 0): 0, (0, 1): 1, (0, 2): 2, (0, 3): 3, (0, 4): 4, (1, 4): 5, (1, 5): 6, (1, 6): 7, (1, 7): 8}

        def cmp_final(O, sm, qh, hl, h):
            O3 = O.ap[:, 0:258].rearrange("p (b w) -> p b w", b=2)
            tile0 = qh * 2
            dve.wait(O.deps_r(), tgs, OG.deps_w(), IMP.deps_w())
            f1 = dve.done(dve.e.tensor_scalar(out=sm[:, 0:2], in0=O3[:, :, 64], scalar1=1e-30, scalar2=None, op0=ALU.max))
            dve.wait(f1)
            f2 = dve.done(dve.e.reciprocal(out=sm[:, 0:2], in_=sm[:, 0:2]))
            dve.wait(f2)
            f3 = dve.done(dve.e.tensor_tensor(out=sm[:, 2:4], in0=sm[:, 0:2], in1=GS[:, tile0:tile0 + 2, h * 3 + 0], op=ALU.mult))
            dve.wait(f3)
            f4 = dve.done(dve.e.tensor_tensor(out=OG.ap[:, tile0:tile0 + 2, hl * 64:(hl + 1) * 64], in0=O3[:, :, 0:64],
                                              in1=sm[:, 2:4].unsqueeze(2).to_broadcast([128, 2, 64]), op=ALU.mult))
            OG.wrote(f4, fresh=False)
            f5 = None
            for b2 in range(2):
                if hl == 0:
                    f5 = dve.done(dve.e.tensor_scalar(out=IMP.ap[:, tile0 + b2, :], in0=O3[:, b2, 65:129], scalar1=sm[:, b2:b2 + 1],
                                                      scalar2=None, op0=ALU.mult))
                else:
                    f5 = dve.done(dve.e.scalar_tensor_tensor(out=IMP.ap[:, tile0 + b2, :], in0=O3[:, b2, 65:129],
                                                             scalar=sm[:, b2:b2 + 1], in1=IMP.ap[:, tile0 + b2, :],
                                                             op0=ALU.mult, op1=ALU.add))
            IMP.wrote(f5, fresh=False)
            O.read(f1)
            O.read(f5)

        citems = []
        for hl in range(4):
            h = 4 * j + hl
            for qh in range(16):
                tt = qh // 2
                q0 = qh * 256
                ktl = []
                for ct in range(2):
                    if ct == 1 and tt <= 3:
                        continue
                    masks = []
                    if (ct, tt) in mcm_idx:
                        mi = mcm_idx[(ct, tt)]
                        off = (qh % 2) * 256
                        masks = [(0, MCM[:, mi, off:off + 128], tcp), (1, MCM[:, mi, off + 128:off + 256], tcp)]
                    ktl.append((ct * 128, ct, 0, 2, masks))
                O = O_ring[o_i % 2]
                o_i += 1
                sm = smalls[sm_i % 4]
                sm_i += 1
                citems.extend(attn_items(fl, S_ring, PTs, O, KCC, 64, QN, hl, q0, RC, 129, ktl,
                                         finalize=(lambda O=O, sm=sm, qh=qh, hl=hl, h=h: cmp_final(O, sm, qh, hl, h))))
        s_i = run_items(citems, s_i)

        for tile in range(NT):
            k2 = tile % 2
            i2, im_, mm = im2[k2], imm[k2], m8[k2]
            nb = NBT[k2]
            dve.wait(IMP.deps_r(), tcp)
            g1 = dve.done(dve.e.tensor_tensor(out=im_[:, :], in0=IMP.ap[:, tile, :], in1=FA[:, tile, :], op=ALU.mult))
            dve.wait(g1)
            g2 = dve.done(dve.e.tensor_tensor(out=im_[:, :], in0=im_[:, :], in1=FB[:, tile, :], op=ALU.add))
            dve.wait(g2)
            g3 = dve.done(dve.e.max(out=mm[:, 0:8], in_=im_[:, :]))
            dve.wait(g3)
            g4 = dve.done(dve.e.match_replace(out=i2[:, :], in_to_replace=mm[:, 0:8], in_values=im_[:, :], imm_value=-2.0))
            dve.wait(g4)
            g5 = dve.done(dve.e.max(out=mm[:, 8:16], in_=i2[:, :]))
            dve.wait(g5)
            g6 = dve.done(dve.e.tensor_scalar(out=mm[:, 0:1], in0=mm[:, 15:16], scalar1=0.0, scalar2=None, op0=ALU.max))
            dve.wait(g6, nb.deps_w())
            g7 = dve.done(dve.e.tensor_scalar(out=nb.ap[:, 64:128], in0=im_[:, :], scalar1=mm[:, 0:1], scalar2=1.0,
                                              op0=ALU.is_ge, op1=ALU.subtract))
            nb.wrote(g7)
            IMP.read(g7)
            pe.wait(g7, TP.deps_w(), tcp)
            tt_ = pe.done(pe.e.transpose(TP.ap[:, 0:128], nb.ap[:, :], IDN[:, :]))
            nb.read(tt_)
            TP.wrote(tt_)
            act.wait(tt_, QN.deps_w())
            tq = act.done(act.e.activation(out=QN.ap[64:128, :, tile * 128:(tile + 1) * 128],
                                           in_=TP.ap[64:128, 0:128].unsqueeze(1).to_broadcast([64, 4, 128]), func=AF.Copy))
            TP.read(tq)
            QN.wrote(tq, fresh=False)

        def ws_final(O, sm, ot_, tt, hl, h, br):
            O3 = O.ap[:, 0:260].rearrange("p (b w) -> p b w", b=4)
            dve.wait(O.deps_r(), tgs, ot_.deps_w())
            f1 = dve.done(dve.e.tensor_scalar(out=sm[:, 0:4], in0=O3[:, :, 64], scalar1=1e-30, scalar2=None, op0=ALU.max))
            dve.wait(f1)
            f2 = dve.done(dve.e.reciprocal(out=sm[:, 0:4], in_=sm[:, 0:4]))
            dve.wait(f2)
            f3 = dve.done(dve.e.tensor_tensor(out=sm[:, 4:8], in0=sm[:, 0:4], in1=GS[:, 4 * tt:4 * tt + 4, h * 3 + br], op=ALU.mult))
            dve.wait(f3)
            f4 = dve.done(dve.e.tensor_tensor(out=ot_.ap[:, :, :], in0=O3[:, :, 0:64],
                                              in1=sm[:, 4:8].unsqueeze(2).to_broadcast([128, 4, 64]), op=ALU.mult))
            O.read(f1)
            O.read(f4)
            ot_.wrote(f4)
            pool.wait(f4, OG.deps_r())
            f5 = pool.done(pool.e.tensor_tensor(out=OG.ap[:, 4 * tt:4 * tt + 4, hl * 64:(hl + 1) * 64],
                                                in0=OG.ap[:, 4 * tt:4 * tt + 4, hl * 64:(hl + 1) * 64], in1=ot_.ap[:, :, :], op=ALU.add))
            ot_.read(f5)
            OG.wrote(f5, fresh=False)

        witems = []
        for br in (2, 1):
            for hl in range(4):
                h = 4 * j + hl
                for tt in range(8):
                    ktl = []
                    if br == 2:
                        for kt in range(max(0, 4 * tt - 4), 4 * tt + 4):
                            qlo = max(4 * tt, kt) - 4 * tt
                            qhi = min(4 * tt + 3, kt + 4) - 4 * tt + 1
                            masks = []
                            if 4 * tt <= kt:
                                masks.append((kt - 4 * tt, MK[:, 0:128], tcp))
                            if kt + 4 <= 4 * tt + 3 and kt + 4 >= 4 * tt:
                                masks.append((kt + 4 - 4 * tt, MK[:, 128:256], tcp))
                            ktl.append((kt * 128, kt, qlo, qhi, masks))
                        Kb, krows, Vb = KW, 64, VW
                    else:
                        for kt in range(0, 4 * tt + 4):
                            qlo = max(4 * tt, kt) - 4 * tt
                            masks = []
                            if kt >= 4 * tt:
                                masks.append((kt - 4 * tt, MK[:, 0:128], tcp))
                            ktl.append((kt * 128, kt, qlo, 4, masks))
                        Kb, krows, Vb = KE, 128, VS
                    O = O_ring[o_i % 2]
                    o_i += 1
                    sm = smalls[sm_i % 4]
                    sm_i += 1
                    ot_ = otmp[sm_i % 2]
                    witems.extend(attn_items(fl, S_ring, PTs, O, Kb, krows, QN, hl, tt * 512, Vb, 65, ktl,
                                             finalize=(lambda O=O, sm=sm, ot_=ot_, tt=tt, hl=hl, h=h, br=br: ws_final(O, sm, ot_, tt, hl, h, br))))
        s_i = run_items(witems, s_i)
        pool.wait(OG.deps_r())
        tso = pool.dma(d["attnO"].rearrange("(t p) c -> p t c", p=128)[:, :, j * 256:(j + 1) * 256], OG.ap[:, :, :], osem)
        OG.w = {}
        OG.r = {}
        OG.read(tso)
        IMP.r = {}
    fl.end()


NTILE_R = 24
NSLOT = NTILE_R * 512
I32 = mybir.dt.int32
FG_R = 896


def phase_moe_convert(fl, d):
    sem = fl.dsem("wconv")
    tok = None
    for e in range(NE):
        for fg in range(4):
            b = (e * 4 + fg) * 128
            f0 = fg * FG_R
            for (src, dst) in ((d["moe_w_gate"], d["wgbP"]), (d["moe_w_up"], d["wubP"])):
                tok = fl.pool.dma(dst[b:b + 128, :].rearrange("p (kc n) -> p kc n", kc=8),
                                  src[0, e, :, f0:f0 + FG_R].rearrange("(kc p) n -> p kc n", p=128), sem)
            tok = fl.pool.dma(d["wdbP"][b:b + 128, :].rearrange("p (fc n) -> p fc n", fc=7),
                              d["moe_w_down"][0, e, f0:f0 + FG_R, :].rearrange("(fc p) n -> p fc n", p=128), sem)
    return tok


def phase_moe_routed(fl, d, t_conv):
    nc = fl.nc
    pe, act, dve, pool, sp = fl.pe, fl.act, fl.dve, fl.pool, fl.sp
    gate_d, xb_d, xg_d, ys_d = d["gate"], d["x3b"], d["xg"], d["ys"]
    fl.begin()
    GT = fl.sb([128, NT, NE], F32, "GT")
    M = fl.sb([128, NT, NE], BF16, "M")
    Mf = fl.sb([128, NT, NE], F32, "Mf")
    UT = fl.sb([128, 128], BF16, "UT")
    ON = fl.sb([128, 128], BF16, "ON")
    TT = fl.sb([128, NT, NE], F32, "TT")
    TO = fl.sb([128, NT, NE], F32, "TO")
    SL = fl.sb([128, NT, NE], F32, "SL")
    cnt = fl.sb([128, NE], F32, "cnt")
    cnti = fl.sb([128, NE], I32, "cnti")
    pc = fl.sb([128, NE], F32, "pc")
    ss = fl.sb([128, NE], F32, "ss")
    se = fl.sb([128, NE], F32, "se")
    T512 = fl.sb([128, NTILE_R, NE], F32, "T512")
    cmp3 = fl.sb([128, NTILE_R, NE], F32, "cmp3")
    tef = fl.sb([128, NTILE_R], F32, "tef")
    FGP = fl.sb([128, NTILE_R, 4], F32, "FGP")
    iwf = fl.sb([128, NTILE_R, 4], F32, "iwf")
    idxW = fl.sb([128, NTILE_R, 4], I32, "idxW")
    ssum = fl.sb([128, NT], F32, "ssum")
    sB = fl.sb([128, NT], F32, "sB")
    sA = fl.sb([128, NT], F32, "sA")
    wsum = fl.sb([128, NT], F32, "wsum")
    wB = fl.sb([128, NT], F32, "wB")
    wAB = fl.sb([128, 2, NT], F32, "wAB")
    IAB = fl.sb([128, 2, NT], I32, "IAB")
    xsc = [Buf(fl.sb([128, D], BF16, "xsc")) for _ in range(2)]
    XTt = [Buf(fl.sb([128, 8, 512], BF16, "XTt")) for _ in range(2)]
    Wg = [Buf(fl.sb([128, 8, FG_R], BF16, "Wg")) for _ in range(2)]
    Wu = [Buf(fl.sb([128, 8, FG_R], BF16, "Wu")) for _ in range(2)]
    Wd = [Buf(fl.sb([128, 7, D], BF16, "Wd")) for _ in range(2)]
    Hs = [Buf(fl.sb([128, 7, 512], BF16, "H")) for _ in range(2)]
    Ss = [Buf(fl.sb([128, 512], F32, "Ssil")) for _ in range(2)]
    acc = [[Buf(fl.sb([128, D], F32, "acc")) for _ in range(4)] for _ in range(2)]
    PR = Buf(fl.ps([128, 512], F32, "PR"))
    GPs = [Buf(fl.ps([128, 512], F32, "GP")) for _ in range(2)]
    UPs = [Buf(fl.ps([128, 512], F32, "UP")) for _ in range(2)]
    Ys = [Buf(fl.ps([128, 1024], F32, "Y")) for _ in range(1)]

    ZT = fl.sb([128, 8 * D], BF16, "ZT")
    tz = pool.done(pool.e.memset(ZT[:, :], 0.0))
    act.wait(tz)
    zsem = fl.dsem("zero")
    t_zero = None
    for q in range(NSLOT // 1024):
        t_zero = act.dma(xg_d[q * 1024:(q + 1) * 1024, :].rearrange("(p a) n -> p (a n)", a=8), ZT[:, :], zsem)
    csem = fl.dsem("const")
    cpsem = fl.dsem("constp")
    tg = sp.dma(GT[:, :, :], gate_d.rearrange("(t p) e -> p t e", p=128), csem)
    sp.dma(T512[:, :, :], d["t512"][:, :, :], csem)
    sp.dma(FGP[:, :, :], d["fgp"][:, :, :], csem)
    tc_ = (csem, csem.n)
    pool.dma(UT[:, :], d["utri"][:, :], cpsem)
    tcp = pool.dma(ON[:, :], d["ones128"][:, :], cpsem)
    dve.wait(tc_)
    t = dve.done(dve.e.tensor_scalar(out=Mf[:, :, :], in0=GT[:, :, :], scalar1=0.0, scalar2=None, op0=ALU.is_gt))
    dve.wait(t)
    t = dve.done(dve.e.tensor_copy(out=M[:, :, :], in_=Mf[:, :, :]))
    pe.wait(t, tcp)
    M2 = M[:, :, :].rearrange("p t e -> p (t e)")
    t1 = pe.done(pe.e.matmul(PR.ap[:, 0:256], UT[:, :], M2, start=True, stop=True))
    t2 = pe.done(pe.e.matmul(PR.ap[:, 256:512], ON[:, :], M2, start=True, stop=True))
    dve.wait(t1, t2)
    t = dve.done(dve.e.tensor_copy(out=SL[:, :, :].rearrange("p t e -> p (t e)"), in_=PR.ap[:, 0:256]))
    t = dve.done(dve.e.tensor_copy(out=TT[:, :, :].rearrange("p t e -> p (t e)"), in_=PR.ap[:, 256:512]))
    t = dve.done(dve.e.memset(TO[:, 0, :], 0.0))
    for k in range(1, NT):
        dve.wait(t)
        t = dve.done(dve.e.tensor_tensor(out=TO[:, k, :], in0=TO[:, k - 1, :], in1=TT[:, k - 1, :], op=ALU.add))
    dve.wait(t)
    t = dve.done(dve.e.tensor_tensor(out=cnt[:, :], in0=TO[:, NT - 1, :], in1=TT[:, NT - 1, :], op=ALU.add))
    dve.wait(t)
    t = dve.done(dve.e.tensor_scalar(out=cnt[:, :], in0=cnt[:, :], scalar1=511.0, scalar2=None, op0=ALU.add))
    dve.wait(t)
    t = dve.done(dve.e.tensor_copy(out=cnti[:, :], in_=cnt[:, :]))
    dve.wait(t)
    t = dve.done(dve.e.tensor_scalar(out=cnti[:, :], in0=cnti[:, :], scalar1=9, scalar2=9, op0=ALU.arith_shift_right,
                                     op1=ALU.logical_shift_left))
    dve.wait(t)
    t = dve.done(dve.e.tensor_copy(out=pc[:, :], in_=cnti[:, :]))
    dve.wait(t)
    t = dve.done(dve.e.memset(ss[:, 0:1], 0.0))
    for e in range(1, NE):
        dve.wait(t)
        t = dve.done(dve.e.tensor_tensor(out=ss[:, e:e + 1], in0=ss[:, e - 1:e], in1=pc[:, e - 1:e], op=ALU.add))
    dve.wait(t)
    t = dve.done(dve.e.tensor_tensor(out=se[:, :], in0=ss[:, :], in1=pc[:, :], op=ALU.add))
    dve.wait(t)
    t = dve.done(dve.e.tensor_tensor(out=cmp3[:, :, :], in0=T512[:, :, :], in1=se[:, :].unsqueeze(1).to_broadcast([128, NTILE_R, NE]),
                                     op=ALU.is_ge))
    dve.wait(t)
    t = dve.done(dve.e.tensor_reduce(out=tef[:, :], in_=cmp3[:, :, :], axis=mybir.AxisListType.X, op=ALU.add))
    dve.wait(t)
    t = dve.done(dve.e.tensor_scalar(out=tef[:, :], in0=tef[:, :], scalar1=7.0, scalar2=0.0, op0=ALU.min, op1=ALU.max))
    dve.wait(t)
    t = dve.done(dve.e.tensor_scalar(out=tef[:, :], in0=tef[:, :], scalar1=512.0, scalar2=None, op0=ALU.mult))
    dve.wait(t)
    t = dve.done(dve.e.tensor_tensor(out=iwf[:, :, :], in0=FGP[:, :, :], in1=tef[:, :].unsqueeze(2).to_broadcast([128, NTILE_R, 4]), op=ALU.add))
    dve.wait(t)
    t_te = dve.done(dve.e.tensor_copy(out=idxW[:, :, :], in_=iwf[:, :, :]))
    t = dve.done(dve.e.tensor_tensor(out=SL[:, :, :], in0=SL[:, :, :], in1=TO[:, :, :], op=ALU.add))
    dve.wait(t)
    t = dve.done(dve.e.tensor_tensor(out=SL[:, :, :], in0=SL[:, :, :], in1=ss[:, :].unsqueeze(1).to_broadcast([128, NT, NE]), op=ALU.add))
    dve.wait(t)
    t = dve.done(dve.e.tensor_tensor(out=SL[:, :, :], in0=SL[:, :, :], in1=Mf[:, :, :], op=ALU.mult))
    dve.wait(t)
    t = dve.done(dve.e.tensor_reduce(out=ssum[:, :], in_=SL[:, :, :], axis=mybir.AxisListType.X, op=ALU.add))
    t = dve.done(dve.e.tensor_reduce(out=sB[:, :], in_=SL[:, :, :], axis=mybir.AxisListType.X, op=ALU.max))
    t = dve.done(dve.e.tensor_reduce(out=wsum[:, :], in_=GT[:, :, :], axis=mybir.AxisListType.X, op=ALU.add))
    dve.wait(t)
    t = dve.done(dve.e.tensor_tensor(out=sA[:, :], in0=ssum[:, :], in1=sB[:, :], op=ALU.subtract))
    t = dve.done(dve.e.tensor_tensor(out=TT[:, :, :], in0=SL[:, :, :], in1=sB[:, :].unsqueeze(2).to_broadcast([128, NT, NE]), op=ALU.is_equal))
    dve.wait(t)
    t = dve.done(dve.e.tensor_tensor(out=TT[:, :, :], in0=TT[:, :, :], in1=GT[:, :, :], op=ALU.mult))
    dve.wait(t)
    t = dve.done(dve.e.tensor_reduce(out=wB[:, :], in_=TT[:, :, :], axis=mybir.AxisListType.X, op=ALU.add))
    dve.wait(t)
    t = dve.done(dve.e.tensor_tensor(out=wAB[:, 0, :], in0=wsum[:, :], in1=wB[:, :], op=ALU.subtract))
    t = dve.done(dve.e.tensor_copy(out=wAB[:, 1, :], in_=wB[:, :]))
    t = dve.done(dve.e.tensor_copy(out=IAB[:, 0, :], in_=sA[:, :]))
    t_idx = dve.done(dve.e.tensor_copy(out=IAB[:, 1, :], in_=sB[:, :]))
    sp.wait(t_idx)
    sp.dma(d["r_idx"][:, :, :], IAB[:, :, :], fl.dsem("st0"))
    sp.dma(d["r_w"][:, :, :], wAB[:, :, :], fl.dsem("st1"))

    xsem = [fl.dsem("xl0"), fl.dsem("xl1")]
    scsems = [fl.dsem("scat0"), fl.dsem("scat1")]
    t_sc = None
    for tb in range(NT):
        xs = xsc[tb % 2]
        sp.wait(xs.deps_w())
        tl = sp.dma(xs.ap[:, :], xb_d[tb * 128:(tb + 1) * 128, :], xsem[tb % 2])
        xs.wrote(tl)
        pool.wait(tl, t_idx, t_zero)
        scs = scsems[tb % 2]
        for ab in range(2):
            ins = pool.e.indirect_dma_start(out=xg_d[:, :], out_offset=bass.IndirectOffsetOnAxis(ap=IAB[:, ab, tb:tb + 1], axis=0),
                                            in_=xs.ap[:, :], in_offset=None)
            scs.n += 16
            ins.then_inc(scs.h, 16)
        xs.read((scs, scs.n))
    t_sc = [(scsems[0], scsems[0].n), (scsems[1], scsems[1].n)]

    wsems = [fl.dsem("rw0"), fl.dsem("rw1")]
    xtsem = [fl.dsem("xt0"), fl.dsem("xt1")]
    ysem = [fl.dsem("ys0"), fl.dsem("ys1")]
    wi = 0
    gi = 0
    hi = 0
    t_ys = []
    sp.wait(t_sc)
    for ti in range(NTILE_R):
        xt = XTt[ti % 2]
        sp.wait(xt.deps_w())
        tx = None
        for kc in range(8):
            tx = sp.dma(xt.ap[:, kc, :], xg_d[ti * 512:(ti + 1) * 512, kc * 128:(kc + 1) * 128], xtsem[ti % 2], transpose=True)
        xt.wrote(tx)
        A = acc[ti % 2]
        for fg in range(FF // FG_R):
            k = wi % 2
            wi += 1
            wg, wu, wd = Wg[k], Wu[k], Wd[k]
            f0 = fg * FG_R
            pool.wait(wg.deps_w(), wu.deps_w(), wd.deps_w(), t_te, t_conv)
            tw = None
            for (wt_, srcP) in ((wg, d["wgbP"]), (wu, d["wubP"]), (wd, d["wdbP"])):
                ins = pool.e.indirect_dma_start(out=wt_.ap[:, :, :].rearrange("p a n -> p (a n)"), out_offset=None, in_=srcP[:, :],
                                                in_offset=bass.IndirectOffsetOnAxis(ap=idxW[:, ti, fg:fg + 1], axis=0))
                wsems[k].n += 16
                ins.then_inc(wsems[k].h, 16)
                tw = (wsems[k], wsems[k].n)
            wg.wrote(tw)
            wu.wrote(tw)
            wd.wrote(tw)
            H = Hs[hi % 2]
            hi += 1
            for fc in range(7):
                GP, UP, Sb = GPs[gi % 2], UPs[gi % 2], Ss[gi % 2]
                gi += 1
                for (P, Wt) in ((GP, wg), (UP, wu)):
                    pe.wait(P.deps_w(), tw, tx)
                    tok = None
                    for kc in range(8):
                        tok = pe.done(pe.e.matmul(P.ap[:, :], Wt.ap[:, kc, fc * 128:(fc + 1) * 128], xt.ap[:, kc, :],
                                                  start=(kc == 0), stop=(kc == 7)))
                    P.wrote(tok)
                    Wt.read(tok)
                    xt.read(tok)
                act.wait(GP.deps_r(), Sb.deps_w())
                ta = act.done(act.e.activation(out=Sb.ap[:, :], in_=GP.ap[:, :], func=AF.Silu))
                GP.read(ta)
                Sb.wrote(ta)
                dve.wait(ta, UP.deps_r(), H.deps_w())
                th = dve.done(dve.e.tensor_tensor(out=H.ap[:, fc, :], in0=UP.ap[:, :], in1=Sb.ap[:, :], op=ALU.mult))
                UP.read(th)
                Sb.read(th)
                H.wrote(th, fresh=(fc == 0))
            for tb in range(4):
                Y = Ys[0]
                pe.wait(Y.deps_w(), H.deps_r(), tw)
                tok = None
                for half in range(2):
                    for fc in range(7):
                        tok = pe.done(pe.e.matmul(Y.ap[:, half * 512:(half + 1) * 512], H.ap[:, fc, tb * 128:(tb + 1) * 128],
                                                  wd.ap[:, fc, half * 512:(half + 1) * 512], start=(fc == 0), stop=(fc == 6)))
                Y.wrote(tok)
                H.read(tok)
                wd.read(tok)
                a = A[tb]
                dve.wait(tok, a.deps_w())
                ty = None
                for half in range(2):
                    sl = slice(half * 512, (half + 1) * 512)
                    if fg == 0:
                        ty = dve.done(dve.e.tensor_copy(out=a.ap[:, sl], in_=Y.ap[:, sl]))
                    else:
                        ty = dve.done(dve.e.tensor_tensor(out=a.ap[:, sl], in0=Y.ap[:, sl], in1=a.ap[:, sl], op=ALU.add))
                Y.read(ty)
                a.wrote(ty)
        tys = None
        for tb in range(4):
            a = A[tb]
            act.wait(a.deps_r())
            tys = act.dma(ys_d[ti * 512 + tb * 128:ti * 512 + (tb + 1) * 128, :], a.ap[:, :], ysem[ti % 2])
        for tb in range(4):
            A[tb].read(tys)
        t_ys.append(tys)
    fl.end()

    fl.begin()
    IAB = fl.sb([128, 2, NT], I32, "IAB")
    wAB = fl.sb([128, 2, NT], F32, "wAB")
    G, Bt, tln = load_ln_params(fl, d["ln_gain"], d["ln_bias"], 1, 1, None)
    YA = [Buf(fl.sb([128, D], F32, "YA")) for _ in range(2)]
    YB = [Buf(fl.sb([128, D], F32, "YB")) for _ in range(2)]
    xts = [Buf(fl.sb([128, D], F32, "xt")) for _ in range(2)]
    rts = [Buf(fl.sb([128, D], F32, "rt")) for _ in range(2)]
    xns = [Buf(fl.sb([128, D], F32, "xn")) for _ in range(2)]
    ots = [Buf(fl.sb([128, D], F32, "ot")) for _ in range(2)]
    stats = [fl.sb([128, 2, 6], F32, "st") for _ in range(2)]
    mvs = [fl.sb([128, 2], F32, "mv") for _ in range(2)]
    rss = [fl.sb([128, 1], F32, "rs") for _ in range(2)]
    csem = fl.dsem("const")
    sp.dma(IAB[:, :, :], d["r_idx"][:, :, :], csem)
    tld = sp.dma(wAB[:, :, :], d["r_w"][:, :, :], csem)
    gsem = [fl.dsem("ga0"), fl.dsem("ga1"), fl.dsem("gb0"), fl.dsem("gb1")]
    xsem = [fl.dsem("xl0"), fl.dsem("xl1")]
    ssem = [fl.dsem("st0"), fl.dsem("st1")]
    for tb in range(NT):
        k = tb % 2
        ya, yb, xt, rt, xn, ot = YA[k], YB[k], xts[k], rts[k], xns[k], ots[k]
        pool.wait(tld, ya.deps_w(), yb.deps_w())
        toks = []
        for ab, (yy, sm_) in enumerate(((ya, gsem[k]), (yb, gsem[2 + k]))):
            ins = pool.e.indirect_dma_start(out=yy.ap[:, :], out_offset=None, in_=ys_d[:, :],
                                            in_offset=bass.IndirectOffsetOnAxis(ap=IAB[:, ab, tb:tb + 1], axis=0))
            sm_.n += 16
            ins.then_inc(sm_.h, 16)
            yy.wrote((sm_, sm_.n))
            toks.append((sm_, sm_.n))
        sp.wait(xt.deps_w())
        txl = sp.dma(xt.ap[:, :], d["x3"][tb * 128:(tb + 1) * 128, :], xsem[k])
        xt.wrote(txl)
        dve.wait(toks, txl, rt.deps_w(), tld)
        t1 = dve.done(dve.e.scalar_tensor_tensor(out=rt.ap[:, :], in0=ya.ap[:, :], scalar=wAB[:, 0, tb:tb + 1], in1=xt.ap[:, :],
                                                 op0=ALU.mult, op1=ALU.bypass)) if False else \
            dve.done(dve.e.tensor_scalar(out=rt.ap[:, :], in0=ya.ap[:, :], scalar1=wAB[:, 0, tb:tb + 1], scalar2=None, op0=ALU.mult))
        dve.wait(t1)
        t2 = dve.done(dve.e.scalar_tensor_tensor(out=rt.ap[:, :], in0=yb.ap[:, :], scalar=wAB[:, 1, tb:tb + 1], in1=rt.ap[:, :],
                                                 op0=ALU.mult, op1=ALU.add))
        dve.wait(t2)
        t3 = dve.done(dve.e.scalar_tensor_tensor(out=rt.ap[:, :], in0=xt.ap[:, :], scalar=ALPHA, in1=rt.ap[:, :],
                                                 op0=ALU.mult, op1=ALU.add))
        ya.read(t1)
        yb.read(t2)
        xt.read(t3)
        rt.wrote(t3)
        pool.wait(tln)
        t7 = layernorm_tile(fl, rt, stats[k], mvs[k], rss[k], xn, ot, G, Bt)
        sp.wait(t7)
        ts1 = sp.dma(d["y"][tb * 128:(tb + 1) * 128, :], ot.ap[:, :], ssem[k])
        ot.read(ts1)
    fl.end()


def host_constants():
    inv = (1.0 / (np.float32(10000.0) ** (np.arange(0, 64, 2, dtype=np.float32) / np.float32(64)))).astype(np.float32)
    ang = np.arange(S, dtype=np.float32)[:, None] * inv[None, :]
    cos = np.cos(ang).astype(np.float32).T
    sin = np.sin(ang).astype(np.float32).T
    cosT = np.concatenate([cos, cos, cos, cos], axis=0)
    sinT = np.concatenate([-sin, sin, -sin, sin], axis=0)
    k = np.arange(128)[:, None]
    c = np.arange(256)[None, :]
    swa_mask = np.where(c < 128, c >= k, (c - 128) < k).astype(np.float32)
    out = {"cosT": np.ascontiguousarray(cosT), "sinT": np.ascontiguousarray(sinT), "swa_mask": swa_mask}
    cidx = np.arange(256)
    cs = 16 * cidx
    ce = cs + 32
    ss = 64 * np.arange(64)
    se = ss + 64
    ov = np.clip(np.minimum(ce[:, None], se[None, :]) - np.maximum(cs[:, None], ss[None, :]), 0, None) / 16.0
    ov[255, :] = 0.0
    out["ov_pad"] = ov.astype(np.float32)
    tiles = [(0, 0), (0, 1), (0, 2), (0, 3), (0, 4), (1, 4), (1, 5), (1, 6), (1, 7)]
    cm = np.zeros((9, 128, 512), np.float32)
    for i, (ct, tt) in enumerate(tiles):
        cc = ct * 128 + np.arange(128)[:, None]
        t = tt * 512 + np.arange(512)[None, :]
        cm[i] = ((16 * cc + 31 <= t) & (cc < 255)).astype(np.float32)
    out["cmp_mask"] = cm
    t = np.arange(S)[:, None]
    b = np.arange(64)[None, :]
    cur = t // 64
    forced = (b == 0) | (b == cur) | (b == cur - 1)
    fa = ((b <= cur) & (~forced)).astype(np.float32)
    fb = np.where(b > cur, -1.0, 0.0).astype(np.float32)
    fb = np.where(b == cur - 1, 1.0e4, fb)
    fb = np.where(b == cur, 2.0e4, fb)
    fb = np.where(b == 0, 3.0e4, fb).astype(np.float32)
    out["sel_fa"] = fa
    out["sel_fb"] = fb
    out["ident"] = np.eye(128, dtype=np.float32)
    em = np.zeros((64, S), np.float32)
    em[np.arange(S) // 64, np.arange(S)] = 30000.0
    out["emat"] = em
    pp = np.arange(128)
    out["utri"] = (pp[:, None] < pp[None, :]).astype(np.float32)
    out["ones128"] = np.ones((128, 128), np.float32)
    t512 = np.zeros((128, NTILE_R, NE), np.float32)
    t512[:, :, :] = (512.0 * np.arange(NTILE_R))[None, :, None]
    out["t512"] = t512
    fgp = np.zeros((128, NTILE_R, 4), np.float32)
    fgp[:, :, :] = (128.0 * np.arange(4))[None, None, :] + np.arange(128, dtype=np.float32)[:, None, None]
    out["fgp"] = fgp
    return out


INPUT_SHAPES = {
    "a_w_in": [1, 1024, 1536], "a_w_out": [1, 1024, 1024], "a_sinks": [1, 16],
    "b_w_in": [1, 1024, 2608], "b_w_out": [1, 1024, 1024], "b_cmp_pos_k": [1, 32, 64], "b_cmp_pos_v": [1, 32, 64],
    "b_cmp_k_w1": [1, 2048, 256], "b_cmp_k_w2": [1, 256, 64], "b_cmp_v_w1": [1, 2048, 256], "b_cmp_v_w2": [1, 256, 64],
    "ffn_w_gate": [1, 1024, 3584], "ffn_w_up": [1, 1024, 3584], "ffn_w_down": [1, 3584, 1024],
    "moe_router": [1, 1024, 8], "moe_w_gate": [1, 8, 1024, 3584], "moe_w_up": [1, 8, 1024, 3584],
    "moe_w_down": [1, 8, 3584, 1024], "ln_gain": [2, 2, 1024], "ln_bias": [2, 2, 1024],
}


def _needed(k, stop_after):
    if stop_after >= 4:
        return True
    if k.startswith("moe_w"):
        return False
    if stop_after <= 2 and (k.startswith("b_") or k.startswith("moe")):
        return False
    if stop_after <= 1 and k.startswith("ffn"):
        return False
    return True


def build(stop_after=99):
    nc = bass.Bass("TRN2", target_bir_lowering=False)
    d = {}
    d["x"] = nc.dram_tensor("x", [S, D], F32, kind="ExternalInput").ap()
    for k, shp in INPUT_SHAPES.items():
        if not _needed(k, stop_after):
            continue
        d[k] = nc.dram_tensor(k, shp, F32, kind="ExternalInput").ap()
    hc = host_constants()
    for k, v in hc.items():
        d[k] = nc.dram_tensor(k, list(v.shape), F32, kind="ExternalInput").ap()
    d["routerT"] = nc.dram_tensor("routerT", [NE, D], F32, kind="ExternalInput").ap()
    d["posT_k"] = nc.dram_tensor("posT_k", [64, 32], F32, kind="ExternalInput").ap()
    d["posT_v"] = nc.dram_tensor("posT_v", [64, 32], F32, kind="ExternalInput").ap()
    d["projT"] = nc.dram_tensor("projT", [2048, S], BF16, kind="Internal").ap()
    d["vtok"] = nc.dram_tensor("vtok", [S, 512], BF16, kind="Internal").ap()
    d["gl"] = nc.dram_tensor("gl", [S, 48], F32, kind="Internal").ap()
    y = nc.dram_tensor("y", [S, D], F32, kind="ExternalOutput").ap()
    for nm in ("xb0", "attnO", "x1b", "x2b", "x3b"):
        d[nm] = nc.dram_tensor(nm, [S, D], BF16, kind="Internal").ap()
    for nm in ("x1", "x2", "x3"):
        d[nm] = nc.dram_tensor(nm, [S, D], F32, kind="Internal").ap()
    d["gate"] = nc.dram_tensor("gate", [S, NE], F32, kind="Internal").ap()
    d["y"] = y
    if stop_after >= 4:
        d["wgbP"] = nc.dram_tensor("wgbP", [NE * 4 * 128, 8 * FG_R], BF16, kind="Internal").ap()
        d["wubP"] = nc.dram_tensor("wubP", [NE * 4 * 128, 8 * FG_R], BF16, kind="Internal").ap()
        d["wdbP"] = nc.dram_tensor("wdbP", [NE * 4 * 128, 7 * D], BF16, kind="Internal").ap()
        d["xg"] = nc.dram_tensor("xg", [NSLOT, D], BF16, kind="Internal").ap()
        d["ys"] = nc.dram_tensor("ys", [NSLOT, D], F32, kind="Internal").ap()
        d["r_idx"] = nc.dram_tensor("r_idx", [128, 2, NT], I32, kind="Internal").ap()
        d["r_w"] = nc.dram_tensor("r_w", [128, 2, NT], F32, kind="Internal").ap()
    fl = Flow(nc)
    d["t_castx"] = phase_cast_x(fl, d["x"], d["xb0"])
    phase_l0_attn(fl, d)
    phase_outproj_ln(fl, d, d["attnO"], d["a_w_out"][0], d["x"], 0, 0, y if stop_after == 1 else d["x1"], d["x1b"])
    if stop_after >= 2:
        phase_ffn(fl, d, d["x1b"], d["x1"], [d["ffn_w_gate"][0]], [d["ffn_w_up"][0]], [d["ffn_w_down"][0]], None, 0, 1,
                  y if stop_after == 2 else d["x2"], d["x2b"])
    if stop_after >= 4 and ROUTED:
        d["t_conv"] = phase_moe_convert(fl, d)
    if stop_after >= 3:
        phase_nsa_proj(fl, d)
        phase_nsa_attn(fl, d)
        phase_outproj_ln(fl, d, d["attnO"], d["b_w_out"][0], d["x2"], 1, 0, y if stop_after == 3 else d["x3"], d["x3b"],
                         router_d=d["moe_router"][0], gate_d=d["gate"])
    if stop_after >= 4:
        if ROUTED:
            phase_moe_routed(fl, d, d["t_conv"])
        else:
            phase_ffn(fl, d, d["x3b"], d["x3"], [d["moe_w_gate"][0, e] for e in range(NE)], [d["moe_w_up"][0, e] for e in range(NE)],
                      [d["moe_w_down"][0, e] for e in range(NE)], d["gate"], 1, 1, y, None)
    fl.barrier()
    fl.gstack.close()
    return nc


_CACHE = {}


def kernel(**inputs):
    stop_after = int(inputs.pop("_stop_after", 99))
    if stop_after not in _CACHE:
        _CACHE[stop_after] = build(stop_after)
    nc = _CACHE[stop_after]
    hc = host_constants()
    shared = {k: np.ascontiguousarray(np.asarray(inputs[k], dtype=np.float32)) for k in INPUT_SHAPES if _needed(k, stop_after)}
    shared.update(hc)
    shared["routerT"] = np.ascontiguousarray(np.asarray(inputs["moe_router"], dtype=np.float32)[0].T)
    shared["posT_k"] = np.ascontiguousarray(np.asarray(inputs["b_cmp_pos_k"], dtype=np.float32)[0].T)
    shared["posT_v"] = np.ascontiguousarray(np.asarray(inputs["b_cmp_pos_v"], dtype=np.float32)[0].T)
    x = np.asarray(inputs["x"], dtype=np.float32)
    in_maps = []
    for c in range(NCORES):
        m = dict(shared)
        m["x"] = np.ascontiguousarray(x[c])
        in_maps.append(m)
    res = run_bass_kernel_spmd(nc, in_maps, core_ids=list(range(NCORES)))
    return np.stack([np.asarray(r["y"], dtype=np.float32) for r in res.results], axis=0)
```

```python
import numpy as np
import ml_dtypes
from contextlib import ExitStack
import concourse.bass as bass
import concourse.mybir as mybir
from concourse.bass_utils import run_bass_kernel_spmd

F32 = mybir.dt.float32
BF16 = mybir.dt.bfloat16
AF = mybir.ActivationFunctionType
ALU = mybir.AluOpType

S = 4096
D = 1024
NT = 32
FF = 3584
NE = 8
ALPHA = float(4.0 ** 0.25)
EPS = 1e-5
NCORES = 8
ROUTED = True


class SemObj:
    def __init__(self, h):
        self.h = h
        self.n = 0


class Buf:
    def __init__(self, ap):
        self.ap = ap
        self.w = {}
        self.r = {}

    @staticmethod
    def _add(d, tok):
        if tok is None:
            return
        s, v = tok
        if d.get(s, 0) < v:
            d[s] = v

    def deps_r(self):
        return list(self.w.items())

    def deps_w(self):
        return list(self.w.items()) + list(self.r.items())

    def wrote(self, tok, fresh=True):
        if fresh:
            self.w = {}
            self.r = {}
        self._add(self.w, tok)

    def read(self, tok):
        self._add(self.r, tok)


class Eng:
    def __init__(self, fl, name, eng):
        self.fl = fl
        self.name = name
        self.e = eng
        self.sem = SemObj(fl.gstack.enter_context(fl.nc.semaphore("s_" + name)))
        self.seen = {}

    def wait(self, *toks):
        for tok in toks:
            if tok is None:
                continue
            if isinstance(tok, list) or (isinstance(tok, tuple) and (len(tok) != 2 or not isinstance(tok[0], SemObj))):
                self.wait(*tok)
                continue
            sem, v = tok
            if sem is None or v <= 0:
                continue
            if self.seen.get(sem, 0) >= v:
                continue
            if sem is self.sem and self.name == "pe":
                continue
            self.e.wait_ge(sem.h, v)
            self.seen[sem] = v

    def done(self, ins):
        self.sem.n += 1
        ins.then_inc(self.sem.h, 1)
        return (self.sem, self.sem.n)

    def dma(self, out, in_, sem, **kw):
        ins = self.e.dma_start(out=out, in_=in_, **kw)
        sem.n += 16
        ins.then_inc(sem.h, 16)
        return (sem, sem.n)


class Flow:
    def __init__(self, nc):
        self.nc = nc
        self.gstack = ExitStack()
        self.pe = Eng(self, "pe", nc.tensor)
        self.act = Eng(self, "act", nc.scalar)
        self.dve = Eng(self, "dve", nc.vector)
        self.pool = Eng(self, "pool", nc.gpsimd)
        self.sp = Eng(self, "sp", nc.sync)
        self.engines = [self.pe, self.act, self.dve, self.pool, self.sp]
        self.dsems = {}
        self.pstack = None
        self.uid = 0

    def dsem(self, name):
        if name not in self.dsems:
            self.dsems[name] = SemObj(self.gstack.enter_context(self.nc.semaphore("d_" + name)))
        return self.dsems[name]

    def begin(self):
        self.pstack = ExitStack()

    def end(self):
        self.barrier()
        self.pstack.close()
        self.pstack = None

    def barrier(self):
        toks = [(e.sem, e.sem.n) for e in self.engines if e.sem.n > 0]
        toks += [(s, s.n) for s in self.dsems.values() if s.n > 0]
        for e in self.engines:
            e.wait([t for t in toks if t[0] is not e.sem])

    def sb(self, shape, dt, name=None):
        self.uid += 1
        return self.pstack.enter_context(self.nc.sbuf_tensor(f"{name or 'sb'}_{self.uid}", list(shape), dt))

    def ps(self, shape, dt=F32, name=None):
        self.uid += 1
        return self.pstack.enter_context(self.nc.psum_tensor(f"{name or 'ps'}_{self.uid}", list(shape), dt))


def load_xT(fl, XT, xb_d, deps, semname, ncols=D, t0=0, nt=S):
    sp = fl.sp
    sem = fl.dsem(semname)
    sp.wait(deps, XT.deps_w())
    tok = None
    step = 1024 if nt >= 1024 else nt
    for kc in range(ncols // 128):
        for q in range(nt // step):
            tok = sp.dma(XT.ap[:, kc, q * step:(q + 1) * step],
                         xb_d[t0 + q * step:t0 + (q + 1) * step, kc * 128:(kc + 1) * 128], sem, transpose=True)
    XT.wrote(tok)
    return tok


def layernorm_tile(fl, r, stats, mv, rs, xn, out_t, G, Bt):
    dve, pool = fl.dve, fl.pool
    dve.wait(r.deps_r())
    t1 = dve.done(dve.e.bn_stats(out=stats[:, 0, :], in_=r.ap[:, 0:512]))
    t2 = dve.done(dve.e.bn_stats(out=stats[:, 1, :], in_=r.ap[:, 512:1024]))
    dve.wait(t1, t2)
    t3 = dve.done(dve.e.bn_aggr(out=mv[:, :], in_=stats[:, :, :].rearrange("p a b -> p (a b)")))
    dve.wait(t3)
    t4a = dve.done(dve.e.tensor_scalar(out=rs[:, :], in0=mv[:, 1:2], scalar1=EPS, scalar2=None, op0=ALU.add))
    fl.act.wait(t4a)
    t4b = fl.act.done(fl.act.e.activation(out=rs[:, :], in_=rs[:, :], func=AF.Sqrt))
    dve.wait(t4b)
    t4 = dve.done(dve.e.reciprocal(out=rs[:, :], in_=rs[:, :]))
    dve.wait(t4, xn.deps_w())
    t5 = dve.done(dve.e.tensor_scalar(out=xn.ap[:, :], in0=r.ap[:, :], scalar1=mv[:, 0:1], scalar2=rs[:, 0:1],
                                      op0=ALU.subtract, op1=ALU.mult))
    r.read(t5)
    xn.wrote(t5)
    pool.wait(t5, out_t.deps_w())
    t6 = pool.done(pool.e.tensor_tensor(out=xn.ap[:, :], in0=xn.ap[:, :], in1=G[:, :], op=ALU.mult))
    pool.wait(t6)
    t7 = pool.done(pool.e.tensor_tensor(out=out_t.ap[:, :], in0=xn.ap[:, :], in1=Bt[:, :], op=ALU.add))
    xn.read(t7)
    xn.wrote(t6, fresh=False)
    out_t.wrote(t7)
    return t7


def load_ln_params(fl, ln_gain_d, ln_bias_d, i, j, ready):
    G = fl.sb([128, D], F32, "lnG")
    Bt = fl.sb([128, D], F32, "lnB")
    sem = fl.dsem("lnp")
    fl.sp.wait(ready)
    fl.sp.dma(G[:, :], ln_gain_d[i, j, :].partition_broadcast(128), sem)
    tok = fl.sp.dma(Bt[:, :], ln_bias_d[i, j, :].partition_broadcast(128), sem)
    return G, Bt, tok


def phase_cast_x(fl, x_d, xb_d):
    sem = fl.dsem("castx")
    tok = None
    for q in range(4):
        tok = fl.pool.dma(xb_d[q * 1024:(q + 1) * 1024, :], x_d[q * 1024:(q + 1) * 1024, :], sem)
    return tok


def rope_proj_chunk(fl, XT, W, Wsw, M, tt, PJ, pj_i, CT, ST, tmps, tmp_i, dests, x_ready):
    pe, dve, pool = fl.pe, fl.dve, fl.pool
    P1 = PJ[(2 * pj_i) % len(PJ)]
    P2 = PJ[(2 * pj_i + 1) % len(PJ)]
    for (P, Wt) in ((P1, W), (P2, Wsw)):
        pe.wait(P.deps_w(), Wt.deps_r(), x_ready)
        tok = None
        for kc in range(8):
            tok = pe.done(pe.e.matmul(P.ap[0:M, :], Wt.ap[:, kc, 0:M], XT.ap[:, kc, tt * 512:(tt + 1) * 512],
                                      start=(kc == 0), stop=(kc == 7)))
        Wt.read(tok)
        XT.read(tok)
        P.wrote(tok)
    T1 = tmps[(2 * tmp_i) % len(tmps)]
    T2 = tmps[(2 * tmp_i + 1) % len(tmps)]
    dve.wait(P1.deps_r(), T1.deps_w())
    t1 = dve.done(dve.e.tensor_tensor(out=T1.ap[0:M, :], in0=P1.ap[0:M, :], in1=CT[0:M, tt * 512:(tt + 1) * 512], op=ALU.mult))
    P1.read(t1)
    T1.wrote(t1)
    dve.wait(P2.deps_r(), T2.deps_w())
    t2 = dve.done(dve.e.tensor_tensor(out=T2.ap[0:M, :], in0=P2.ap[0:M, :], in1=ST[0:M, tt * 512:(tt + 1) * 512], op=ALU.mult))
    P2.read(t2)
    T2.wrote(t2)
    for (row0, dbuf, out_ap) in dests:
        pool.wait(t1, t2, dbuf.deps_w())
        t3 = pool.done(pool.e.tensor_tensor(out=out_ap, in0=T1.ap[row0:row0 + 64, :], in1=T2.ap[row0:row0 + 64, :], op=ALU.add))
        T1.read(t3)
        T2.read(t3)
        dbuf.wrote(t3, fresh=False)


def load_w_cols(fl, Wb, w_d, c0, M, semname, swap=False):
    pool = fl.pool
    sem = fl.dsem(semname)
    pool.wait(Wb.deps_w())
    if not swap:
        src = w_d.rearrange("(kc p) n -> p kc n", p=128)[:, :, c0:c0 + M]
        tok = pool.dma(Wb.ap[:, :, 0:M], src, sem)
    else:
        nh = M // 64
        ncol = w_d.shape[1]
        src5 = w_d[:, 0:(ncol // 64) * 64].rearrange("(kc p) (h two i) -> p kc h two i", p=128, two=2, i=32)
        dst5 = Wb.ap[:, :, 0:M].rearrange("p kc (h two i) -> p kc h two i", two=2, i=32)
        h0 = c0 // 64
        tok = None
        for two in range(2):
            for hh in range(nh):
                tok = pool.dma(dst5[:, :, hh, two, :], src5[:, :, h0 + hh, 1 - two, :], sem)
    Wb.wrote(tok)
    return tok


def phase_l0_attn(fl, d):
    nc = fl.nc
    pe, act, dve, pool, sp = fl.pe, fl.act, fl.dve, fl.pool, fl.sp
    fl.begin()
    XT = Buf(fl.sb([128, 8, S], BF16, "XT"))
    CT = fl.sb([128, S], F32, "CT")
    ST = fl.sb([128, S], F32, "ST")
    MK = fl.sb([128, 256], BF16, "MK")
    es = fl.sb([128, 16], F32, "es")
    KT = Buf(fl.sb([64, S], BF16, "KT"))
    VA = Buf(fl.sb([128, NT, 65], BF16, "VA"))
    QT = Buf(fl.sb([64, 4, S], BF16, "QT"))
    OG = Buf(fl.sb([128, NT, 256], BF16, "OG"))
    Wn = [Buf(fl.sb([128, 8, 128], BF16, "Wn")) for _ in range(2)]
    Ws = [Buf(fl.sb([128, 8, 128], BF16, "Ws")) for _ in range(2)]
    tmps = [Buf(fl.sb([128, 512], F32, "tmp")) for _ in range(4)]
    PTs = [Buf(fl.sb([128, 512], BF16, "PT")) for _ in range(3)]
    lts = [fl.sb([128, 4], F32, "lt") for _ in range(4)]
    banks = [Buf(fl.ps([128, 512], F32, "bank")) for _ in range(8)]
    PJ = banks[0:4]
    S_ring = [banks[4], banks[5], banks[3]]
    O_ring = [banks[6], banks[7]]
    o_i = 0

    csem = fl.dsem("const")
    sp.dma(CT[:, :], d["cosT"][:, :], csem)
    sp.dma(ST[:, :], d["sinT"][:, :], csem)
    sp.dma(es[:, :], d["a_sinks"][0, :].partition_broadcast(128), csem)
    tc1 = (csem, csem.n)
    msem = fl.dsem("constp")
    tmk = pool.dma(MK[:, :], d["swa_mask"][:, :], msem)
    act.wait(tc1)
    tes = act.done(act.e.activation(out=es[:, :], in_=es[:, :], func=AF.Exp))
    tva = dve.done(dve.e.memset(VA.ap[:, :, 64:65], 1.0))
    VA.wrote(tva)
    tx = load_xT(fl, XT, d["xb0"], d["t_castx"], "xt")
    x_ready = [tx, tc1]

    w_in = d["a_w_in"][0]
    pj_i = 0
    tmp_i = 0
    wi = 0
    it = 0
    for j in range(4):
        for c in range(2):
            Wb, Wsb = Wn[wi % 2], Ws[wi % 2]
            wi += 1
            c0 = (4 * j + 2 * c) * 64
            load_w_cols(fl, Wb, w_in, c0, 128, "wn%d" % ((wi - 1) % 2))
            load_w_cols(fl, Wsb, w_in, c0, 128, "ws%d" % ((wi - 1) % 2), swap=True)
            for tt in range(8):
                dests = [(0, QT, QT.ap[0:64, 2 * c, tt * 512:(tt + 1) * 512]),
                         (64, QT, QT.ap[0:64, 2 * c + 1, tt * 512:(tt + 1) * 512])]
                rope_proj_chunk(fl, XT, Wb, Wsb, 128, tt, PJ, pj_i, CT, ST, tmps, tmp_i, dests, x_ready)
                pj_i += 1
                tmp_i += 1
        Wb, Wsb = Wn[wi % 2], Ws[wi % 2]
        wi += 1
        c0 = 1024 + j * 64
        load_w_cols(fl, Wb, w_in, c0, 64, "wn%d" % ((wi - 1) % 2))
        load_w_cols(fl, Wsb, w_in, c0, 64, "ws%d" % ((wi - 1) % 2), swap=True)
        for tt in range(8):
            dests = [(0, KT, KT.ap[0:64, tt * 512:(tt + 1) * 512])]
            rope_proj_chunk(fl, XT, Wb, Wsb, 64, tt, PJ, pj_i, CT, ST, tmps, tmp_i, dests, x_ready)
            pj_i += 1
            tmp_i += 1
        Wb = Wn[wi % 2]
        wi += 1
        c0 = 1280 + j * 64
        load_w_cols(fl, Wb, w_in, c0, 64, "wn%d" % ((wi - 1) % 2))
        for g8 in range(4):
            P = PJ[pj_i % 4]
            pj_i += 1
            pe.wait(P.deps_w(), Wb.deps_r(), x_ready)
            tok = None
            for t8 in range(8):
                tb = g8 * 8 + t8
                for kc in range(8):
                    tok = pe.done(pe.e.matmul(P.ap[:, t8 * 64:(t8 + 1) * 64], XT.ap[:, kc, tb * 128:(tb + 1) * 128],
                                              Wb.ap[:, kc, 0:64], start=(kc == 0), stop=(kc == 7)))
            Wb.read(tok)
            XT.read(tok)
            P.wrote(tok)
            act.wait(tok, VA.deps_w())
            tv = act.done(act.e.activation(out=VA.ap[:, g8 * 8:(g8 + 1) * 8, 0:64],
                                           in_=P.ap[:, :].rearrange("p (a b) -> p a b", a=8), func=AF.Copy))
            P.read(tv)
            VA.wrote(tv, fresh=False)

        def l0_final(O, lt, tt, hl, h):
            O3 = O.ap[:, 0:260].rearrange("p (b w) -> p b w", b=4)
            dve.wait(O.deps_r(), tes, OG.deps_w())
            f1 = dve.done(dve.e.tensor_scalar(out=lt[:, :], in0=O3[:, :, 64], scalar1=es[:, h:h + 1], scalar2=None, op0=ALU.add))
            dve.wait(f1)
            f2 = dve.done(dve.e.reciprocal(out=lt[:, :], in_=lt[:, :]))
            dve.wait(f2)
            f3 = dve.done(dve.e.tensor_tensor(out=OG.ap[:, 4 * tt:4 * tt + 4, hl * 64:(hl + 1) * 64], in0=O3[:, :, 0:64],
                                              in1=lt[:, :].unsqueeze(2).to_broadcast([128, 4, 64]), op=ALU.mult))
            O.read(f1)
            O.read(f3)
            OG.wrote(f3, fresh=False)

        aitems = []
        for hl in range(4):
            h = 4 * j + hl
            for tt in range(8):
                ktl = []
                for kt in range(max(0, 4 * tt - 1), 4 * tt + 4):
                    qlo = max(4 * tt, kt) - 4 * tt
                    qhi = min(4 * tt + 3, kt + 1) - 4 * tt + 1
                    masks = []
                    if kt >= 4 * tt:
                        masks.append((kt - 4 * tt, MK[:, 0:128], tmk))
                    if 4 * tt <= kt + 1 <= 4 * tt + 3:
                        masks.append((kt + 1 - 4 * tt, MK[:, 128:256], tmk))
                    ktl.append((kt * 128, kt, qlo, qhi, masks))
                O = O_ring[o_i % 2]
                o_i += 1
                lt = lts[o_i % 4]
                aitems.extend(attn_items(fl, S_ring, PTs, O, KT, 64, QT, hl, tt * 512, VA, 65, ktl,
                                         finalize=(lambda O=O, lt=lt, tt=tt, hl=hl, h=h: l0_final(O, lt, tt, hl, h))))
        it = run_items(aitems, it)
        osem = fl.dsem("ostore")
        sp.wait(OG.deps_r())
        tso = sp.dma(d["attnO"].rearrange("(t p) c -> p t c", p=128)[:, :, j * 256:(j + 1) * 256], OG.ap[:, :, :], osem)
        OG.read(tso)
        OG.w = {}
    fl.end()


def phase_outproj_ln(fl, d, attn_d, w_out_d, xres_d, ln_i, ln_j, out_d, outb_d, router_d=None, gate_d=None):
    pe, act, dve, pool, sp = fl.pe, fl.act, fl.dve, fl.pool, fl.sp
    fl.begin()
    OT = Buf(fl.sb([128, 8, S], BF16, "OT"))
    Wo = Buf(fl.sb([128, 8, D], BF16, "Wo"))
    G, Bt, tln = load_ln_params(fl, d["ln_gain"], d["ln_bias"], ln_i, ln_j, None)
    xts = [Buf(fl.sb([128, D], F32, "xt")) for _ in range(2)]
    rts = [Buf(fl.sb([128, D], F32, "rt")) for _ in range(2)]
    xns = [Buf(fl.sb([128, D], F32, "xn")) for _ in range(2)]
    ots = [Buf(fl.sb([128, D], F32, "ot")) for _ in range(2)]
    obs = [Buf(fl.sb([128, D], BF16, "ob")) for _ in range(2)]
    stats = [fl.sb([128, 2, 6], F32, "st") for _ in range(2)]
    mvs = [fl.sb([128, 2], F32, "mv") for _ in range(2)]
    rss = [fl.sb([128, 1], F32, "rs") for _ in range(2)]
    Ys = [Buf(fl.ps([128, 1024], F32, "Y")) for _ in range(2)]
    if router_d is not None:
        RB = fl.sb([128, NE, D], F32, "RB")
        rsem = fl.dsem("rb")
        trb = None
        for e in range(NE):
            trb = sp.dma(RB[:, e, :], d["routerT"][e, :].partition_broadcast(128), rsem)
        junk = fl.sb([128, D], F32, "junk")
        jtok = [None]
        lgs = [fl.sb([128, 8], F32, "lg") for _ in range(2)]
        mx8 = [fl.sb([128, 8], F32, "mx8") for _ in range(2)]
        gts = [Buf(fl.sb([128, 8], F32, "gt")) for _ in range(2)]
        g2 = [fl.sb([128, 8], F32, "g2") for _ in range(2)]
        w12 = [fl.sb([128, 2], F32, "w12") for _ in range(2)]
    wsem = fl.dsem("wo")
    pool.wait(Wo.deps_w())
    tw = pool.dma(Wo.ap[:, :, :], w_out_d.rearrange("(kc p) n -> p kc n", p=128), wsem)
    Wo.wrote(tw)
    tot = load_xT(fl, OT, attn_d, None, "xt")
    xsem = [fl.dsem("xl0"), fl.dsem("xl1")]
    ssem = [fl.dsem("st0"), fl.dsem("st1")]
    for tb in range(NT):
        k = tb % 2
        xt, rt, xn, ot, ob, Y = xts[k], rts[k], xns[k], ots[k], obs[k], Ys[k]
        sp.wait(xt.deps_w())
        tx = sp.dma(xt.ap[:, :], xres_d[tb * 128:(tb + 1) * 128, :], xsem[k])
        xt.wrote(tx)
        pe.wait(Y.deps_w(), tw, tot)
        tok = None
        for half in range(2):
            for kc in range(8):
                tok = pe.done(pe.e.matmul(Y.ap[:, half * 512:(half + 1) * 512], OT.ap[:, kc, tb * 128:(tb + 1) * 128],
                                          Wo.ap[:, kc, half * 512:(half + 1) * 512], start=(kc == 0), stop=(kc == 7)))
        Y.wrote(tok)
        dve.wait(tok, tx, rt.deps_w())
        tr = None
        for half in range(2):
            sl = slice(half * 512, (half + 1) * 512)
            tr = dve.done(dve.e.scalar_tensor_tensor(out=rt.ap[:, sl], in0=xt.ap[:, sl], scalar=ALPHA, in1=Y.ap[:, sl],
                                                     op0=ALU.mult, op1=ALU.add))
        Y.read(tr)
        xt.read(tr)
        rt.wrote(tr)
        pool.wait(tln)
        t7 = layernorm_tile(fl, rt, stats[k], mvs[k], rss[k], xn, ot, G, Bt)
        sp.wait(t7)
        ts1 = sp.dma(out_d[tb * 128:(tb + 1) * 128, :], ot.ap[:, :], ssem[k])
        ot.read(ts1)
        act.wait(t7, ob.deps_w())
        tc = act.done(act.e.activation(out=ob.ap[:, :], in_=ot.ap[:, :], func=AF.Copy))
        ot.read(tc)
        ob.wrote(tc)
        sp.wait(tc)
        ts2 = sp.dma(outb_d[tb * 128:(tb + 1) * 128, :], ob.ap[:, :], fl.dsem("sb%d" % k))
        ob.read(ts2)
        if router_d is not None:
            lg, m8, gt, gg, ww = lgs[k], mx8[k], gts[k], g2[k], w12[k]
            dve.wait(t7, trb)
            tl = jtok[0]
            for e in range(NE):
                dve.wait(tl)
                tl = dve.done(dve.e.scalar_tensor_tensor(out=junk[:, :], in0=ot.ap[:, :], scalar=1.0, in1=RB[:, e, :],
                                                         op0=ALU.mult, op1=ALU.mult, accum_out=lg[:, e:e + 1]))
            ot.read(tl)
            jtok[0] = tl
            dve.wait(tl)
            tm = dve.done(dve.e.max(out=m8[:, :], in_=lg[:, :]))
            dve.wait(tm)
            t_a = dve.done(dve.e.tensor_tensor(out=ww[:, 0:1], in0=m8[:, 0:1], in1=m8[:, 1:2], op=ALU.subtract))
            t_b = dve.done(dve.e.tensor_tensor(out=ww[:, 1:2], in0=m8[:, 1:2], in1=m8[:, 0:1], op=ALU.subtract))
            act.wait(t_a, t_b)
            t_s = act.done(act.e.activation(out=ww[:, :], in_=ww[:, :], func=AF.Sigmoid))
            dve.wait(t_s, gt.deps_w())
            t_g1 = dve.done(dve.e.tensor_scalar(out=gt.ap[:, :], in0=lg[:, :], scalar1=m8[:, 0:1], scalar2=ww[:, 0:1],
                                                op0=ALU.is_equal, op1=ALU.mult))
            t_g2 = dve.done(dve.e.tensor_scalar(out=gg[:, :], in0=lg[:, :], scalar1=m8[:, 1:2], scalar2=ww[:, 1:2],
                                                op0=ALU.is_equal, op1=ALU.mult))
            dve.wait(t_g1, t_g2)
            t_g3 = dve.done(dve.e.tensor_tensor(out=gt.ap[:, :], in0=gt.ap[:, :], in1=gg[:, :], op=ALU.add))
            gt.wrote(t_g3)
            sp.wait(t_g3)
            ts3 = sp.dma(gate_d[tb * 128:(tb + 1) * 128, :], gt.ap[:, :], fl.dsem("sg%d" % k))
            gt.read(ts3)
    fl.end()


def phase_ffn(fl, d, xb_d, xres_d, wg_list, wu_list, wd_list, gate_d, ln_i, ln_j, out_d, outb_d, TG=1024):
    pe, act, dve, pool, sp = fl.pe, fl.act, fl.dve, fl.pool, fl.sp
    fl.begin()
    ne = len(wg_list)
    ntile = TG // 128
    XTg = Buf(fl.sb([128, 8, TG], BF16, "XTg"))
    acc = [Buf(fl.sb([128, D], F32, "acc")) for _ in range(ntile)]
    Wg = [Buf(fl.sb([128, 8, 512], BF16, "Wg")) for _ in range(2)]
    Wu = [Buf(fl.sb([128, 8, 512], BF16, "Wu")) for _ in range(2)]
    Wd = [Buf(fl.sb([128, 4, D], BF16, "Wd")) for _ in range(2)]
    Hs = [Buf(fl.sb([128, 4, 512], BF16, "H")) for _ in range(2)]
    Ss = [Buf(fl.sb([128, 512], F32, "Ssil")) for _ in range(2)]
    G, Bt, tln = load_ln_params(fl, d["ln_gain"], d["ln_bias"], ln_i, ln_j, None)
    xts = [Buf(fl.sb([128, D], F32, "xt")) for _ in range(2)]
    xns = [Buf(fl.sb([128, D], F32, "xn")) for _ in range(2)]
    ots = [Buf(fl.sb([128, D], F32, "ot")) for _ in range(2)]
    obs = [Buf(fl.sb([128, D], BF16, "ob")) for _ in range(2)]
    stats = [fl.sb([128, 2, 6], F32, "st") for _ in range(2)]
    mvs = [fl.sb([128, 2], F32, "mv") for _ in range(2)]
    rss = [fl.sb([128, 1], F32, "rs") for _ in range(2)]
    GPs = [Buf(fl.ps([128, 512], F32, "GP")) for _ in range(2)]
    UPs = [Buf(fl.ps([128, 512], F32, "UP")) for _ in range(2)]
    Ys = [Buf(fl.ps([128, 1024], F32, "Y")) for _ in range(2)]
    if gate_d is not None:
        GT = Buf(fl.sb([128, ntile, NE], F32, "GT"))
    wsems = [fl.dsem("ffw0"), fl.dsem("ffw1")]
    xsem = [fl.dsem("xl0"), fl.dsem("xl1")]
    ssem = [fl.dsem("st0"), fl.dsem("st1")]
    gsem = fl.dsem("gl")
    wi = 0
    gi = 0
    yi = 0
    hi = 0
    ei = 0
    for tg in range(S // TG):
        t0 = tg * TG
        tx = load_xT(fl, XTg, xb_d, None, "xt", t0=t0, nt=TG)
        if gate_d is not None:
            sp.wait(GT.deps_w())
            tgl = sp.dma(GT.ap[:, :, :], gate_d[t0:t0 + TG, :].rearrange("(t p) e -> p t e", p=128), gsem)
            GT.wrote(tgl)
        for e in range(ne):
            for fg in range(FF // 512):
                k = wi % 2
                wi += 1
                wg, wu, wd = Wg[k], Wu[k], Wd[k]
                pool.wait(wg.deps_w(), wu.deps_w(), wd.deps_w())
                f0 = fg * 512
                pool.dma(wg.ap[:, :, :], wg_list[e].rearrange("(kc p) n -> p kc n", p=128)[:, :, f0:f0 + 512], wsems[k])
                pool.dma(wu.ap[:, :, :], wu_list[e].rearrange("(kc p) n -> p kc n", p=128)[:, :, f0:f0 + 512], wsems[k])
                tw = pool.dma(wd.ap[:, :, :], wd_list[e][f0:f0 + 512, :].rearrange("(fc p) n -> p fc n", p=128), wsems[k])
                wg.wrote(tw)
                wu.wrote(tw)
                wd.wrote(tw)
                first_acc = (e == 0 and fg == 0)
                for tt in range(TG // 512):
                    H = Hs[hi % 2]
                    hi += 1
                    for fc in range(4):
                        GP, UP, Sb = GPs[gi % 2], UPs[gi % 2], Ss[gi % 2]
                        gi += 1
                        for (P, Wt) in ((GP, wg), (UP, wu)):
                            pe.wait(P.deps_w(), tw, tx)
                            tok = None
                            for kc in range(8):
                                tok = pe.done(pe.e.matmul(P.ap[:, :], Wt.ap[:, kc, fc * 128:(fc + 1) * 128],
                                                          XTg.ap[:, kc, tt * 512:(tt + 1) * 512], start=(kc == 0), stop=(kc == 7)))
                            P.wrote(tok)
                            Wt.read(tok)
                            XTg.read(tok)
                        act.wait(GP.deps_r(), Sb.deps_w())
                        ta = act.done(act.e.activation(out=Sb.ap[:, :], in_=GP.ap[:, :], func=AF.Silu))
                        GP.read(ta)
                        Sb.wrote(ta)
                        dve.wait(ta, UP.deps_r(), H.deps_w())
                        th = dve.done(dve.e.tensor_tensor(out=H.ap[:, fc, :], in0=UP.ap[:, :], in1=Sb.ap[:, :], op=ALU.mult))
                        UP.read(th)
                        Sb.read(th)
                        H.wrote(th, fresh=(fc == 0))
                    for tb in range(4):
                        Y = Ys[yi % 2]
                        yi += 1
                        tile_i = tt * 4 + tb
                        pe.wait(Y.deps_w(), H.deps_r(), tw)
                        tok = None
                        for half in range(2):
                            for fc in range(4):
                                tok = pe.done(pe.e.matmul(Y.ap[:, half * 512:(half + 1) * 512], H.ap[:, fc, tb * 128:(tb + 1) * 128],
                                                          wd.ap[:, fc, half * 512:(half + 1) * 512], start=(fc == 0), stop=(fc == 3)))
                        Y.wrote(tok)
                        H.read(tok)
                        wd.read(tok)
                        a = acc[tile_i]
                        dve.wait(tok, a.deps_w())
                        ty = None
                        for half in range(2):
                            sl = slice(half * 512, (half + 1) * 512)
                            if gate_d is None:
                                if first_acc:
                                    ty = dve.done(dve.e.tensor_copy(out=a.ap[:, sl], in_=Y.ap[:, sl]))
                                else:
                                    ty = dve.done(dve.e.tensor_tensor(out=a.ap[:, sl], in0=Y.ap[:, sl], in1=a.ap[:, sl], op=ALU.add))
                            else:
                                dve.wait(GT.deps_r())
                                gsc = GT.ap[:, tile_i, e:e + 1]
                                if first_acc:
                                    ty = dve.done(dve.e.tensor_scalar(out=a.ap[:, sl], in0=Y.ap[:, sl], scalar1=gsc, scalar2=None,
                                                                      op0=ALU.mult))
                                else:
                                    ty = dve.done(dve.e.scalar_tensor_tensor(out=a.ap[:, sl], in0=Y.ap[:, sl], scalar=gsc,
                                                                             in1=a.ap[:, sl], op0=ALU.mult, op1=ALU.add))
                        Y.read(ty)
                        a.wrote(ty)
                        if gate_d is not None:
                            GT.read(ty)
        for ti in range(ntile):
            k = ei % 2
            ei += 1
            tbg = t0 // 128 + ti
            xt, xn, ot, ob = xts[k], xns[k], ots[k], obs[k]
            a = acc[ti]
            sp.wait(xt.deps_w())
            txl = sp.dma(xt.ap[:, :], xres_d[tbg * 128:(tbg + 1) * 128, :], xsem[k])
            xt.wrote(txl)
            dve.wait(txl, a.deps_r())
            tr = dve.done(dve.e.scalar_tensor_tensor(out=a.ap[:, :], in0=xt.ap[:, :], scalar=ALPHA, in1=a.ap[:, :],
                                                     op0=ALU.mult, op1=ALU.add))
            xt.read(tr)
            a.wrote(tr)
            pool.wait(tln)
            t7 = layernorm_tile(fl, a, stats[k], mvs[k], rss[k], xn, ot, G, Bt)
            sp.wait(t7)
            ts1 = sp.dma(out_d[tbg * 128:(tbg + 1) * 128, :], ot.ap[:, :], ssem[k])
            ot.read(ts1)
            if outb_d is not None:
                act.wait(t7, ob.deps_w())
                tc = act.done(act.e.activation(out=ob.ap[:, :], in_=ot.ap[:, :], func=AF.Copy))
                ot.read(tc)
                ob.wrote(tc)
                sp.wait(tc)
                ts2 = sp.dma(outb_d[tbg * 128:(tbg + 1) * 128, :], ob.ap[:, :], fl.dsem("sb%d" % k))
                ob.read(ts2)
    fl.end()


NSA_FM = [(0, True), (128, True), (256, True), (384, True), (512, True), (640, True), (768, True), (896, True),
          (1024, True), (1152, True),
          (1280, False), (1408, False),
          (1536, True), (1664, True),
          (2048, True), (2176, True)]


def nsa_projT_row(c0):
    return c0 if c0 < 1792 else c0 - 256


def phase_nsa_proj(fl, d):
    pe, act, dve, pool, sp = fl.pe, fl.act, fl.dve, fl.pool, fl.sp
    fl.begin()
    XT = Buf(fl.sb([128, 8, S], BF16, "XT"))
    CT = fl.sb([128, S], F32, "CT")
    ST = fl.sb([128, S], F32, "ST")
    Wn = [Buf(fl.sb([128, 8, 128], BF16, "Wn")) for _ in range(2)]
    Ws = [Buf(fl.sb([128, 8, 128], BF16, "Ws")) for _ in range(2)]
    Wt = Buf(fl.sb([128, 8, 560], BF16, "Wt"))
    tmps = [Buf(fl.sb([128, 512], F32, "tmp")) for _ in range(4)]
    stg = [Buf(fl.sb([128, S], BF16, "stg")) for _ in range(2)]
    vst = [Buf(fl.sb([128, 512], BF16, "vst")) for _ in range(2)]
    gst = [Buf(fl.sb([128, 48], F32, "gst")) for _ in range(2)]
    PJ = [Buf(fl.ps([128, 512], F32, "bank")) for _ in range(6)]
    csem = fl.dsem("const")
    sp.dma(CT[:, :], d["cosT"][:, :], csem)
    sp.dma(ST[:, :], d["sinT"][:, :], csem)
    tc1 = (csem, csem.n)
    tx = load_xT(fl, XT, d["x2b"], None, "xt")
    x_ready = [tx, tc1]
    w_in = d["b_w_in"][0]
    projT = d["projT"]
    ssem = [fl.dsem("st0"), fl.dsem("st1")]
    pj_i = 0
    tmp_i = 0
    for ci, (c0, roped) in enumerate(NSA_FM):
        Wb, Wsb = Wn[ci % 2], Ws[ci % 2]
        sg = stg[ci % 2]
        load_w_cols(fl, Wb, w_in, c0, 128, "wn%d" % (ci % 2))
        if roped:
            load_w_cols(fl, Wsb, w_in, c0, 128, "ws%d" % (ci % 2), swap=True)
        for tt in range(8):
            if roped:
                dests = [(0, sg, sg.ap[0:64, tt * 512:(tt + 1) * 512]), (64, sg, sg.ap[64:128, tt * 512:(tt + 1) * 512])]
                rope_proj_chunk(fl, XT, Wb, Wsb, 128, tt, PJ[0:4], pj_i, CT, ST, tmps, tmp_i, dests, x_ready)
                pj_i += 1
                tmp_i += 1
            else:
                P = PJ[4 + (tt % 2)]
                pe.wait(P.deps_w(), Wb.deps_r(), x_ready)
                tok = None
                for kc in range(8):
                    tok = pe.done(pe.e.matmul(P.ap[:, :], Wb.ap[:, kc, :], XT.ap[:, kc, tt * 512:(tt + 1) * 512],
                                              start=(kc == 0), stop=(kc == 7)))
                Wb.read(tok)
                XT.read(tok)
                P.wrote(tok)
                act.wait(tok, sg.deps_w())
                tv = act.done(act.e.activation(out=sg.ap[:, tt * 512:(tt + 1) * 512], in_=P.ap[:, :], func=AF.Copy))
                P.read(tv)
                sg.wrote(tv, fresh=False)
        r0 = nsa_projT_row(c0)
        sp.wait(sg.deps_r())
        ts = sp.dma(projT[r0:r0 + 128, :], sg.ap[:, :], ssem[ci % 2])
        sg.read(ts)
        sg.w = {}
    wsem = fl.dsem("wt")
    pool.wait(Wt.deps_w())
    wsrc = w_in.rearrange("(kc p) n -> p kc n", p=128)
    pool.dma(Wt.ap[:, :, 0:256], wsrc[:, :, 1792:2048], wsem)
    pool.dma(Wt.ap[:, :, 256:512], wsrc[:, :, 2304:2560], wsem)
    tw = pool.dma(Wt.ap[:, :, 512:560], wsrc[:, :, 2560:2608], wsem)
    Wt.wrote(tw)
    for tb in range(NT):
        P = PJ[(2 * tb) % 6]
        Pg = PJ[(2 * tb + 1) % 6]
        pe.wait(P.deps_w(), Pg.deps_w(), tw, x_ready)
        tok = None
        for kc in range(8):
            tok = pe.done(pe.e.matmul(P.ap[:, :], XT.ap[:, kc, tb * 128:(tb + 1) * 128], Wt.ap[:, kc, 0:512],
                                      start=(kc == 0), stop=(kc == 7)))
        P.wrote(tok)
        tokg = None
        for kc in range(8):
            tokg = pe.done(pe.e.matmul(Pg.ap[:, 0:48], XT.ap[:, kc, tb * 128:(tb + 1) * 128], Wt.ap[:, kc, 512:560],
                                       start=(kc == 0), stop=(kc == 7)))
        Pg.wrote(tokg)
        vs_, gs_ = vst[tb % 2], gst[tb % 2]
        act.wait(tok, vs_.deps_w())
        tv = act.done(act.e.activation(out=vs_.ap[:, :], in_=P.ap[:, :], func=AF.Copy))
        P.read(tv)
        vs_.wrote(tv)
        act.wait(tokg, gs_.deps_w())
        tg_ = act.done(act.e.activation(out=gs_.ap[:, :], in_=Pg.ap[:, 0:48], func=AF.Sigmoid))
        Pg.read(tg_)
        gs_.wrote(tg_)
        sp.wait(tv, tg_)
        t1 = sp.dma(d["vtok"][tb * 128:(tb + 1) * 128, :], vs_.ap[:, :], ssem[tb % 2])
        t2 = sp.dma(d["gl"][tb * 128:(tb + 1) * 128, :], gs_.ap[:, :], fl.dsem("sg%d" % (tb % 2)))
        vs_.read(t1)
        gs_.read(t2)
    fl.end()


class AttnItem:
    def __init__(self):
        self.A = None
        self.B = None
        self.F = None
        self.slot = None


def attn_items(fl, S_ring, PT_ring, O, Kbuf, krows, Qbuf, qsel, q0, Vbuf, W, ktiles, finalize=None, pe_extra_deps=()):
    pe, act, dve = fl.pe, fl.act, fl.dve
    st = {"first": True}
    last_for = {}
    for i, (_, _, qlo, qhi, _) in enumerate(ktiles):
        for qb in range(qlo, qhi):
            last_for[qb] = i
    items = []
    nk = len(ktiles)
    for i, (kc0, vidx, qlo, qhi, masks) in enumerate(ktiles):
        it = AttnItem()

        def A(slot, it=it, kc0=kc0, qlo=qlo, qhi=qhi, masks=masks):
            it.slot = slot
            Sb = S_ring[slot % len(S_ring)]
            p = PT_ring[slot % len(PT_ring)]
            n0, n1 = qlo * 128, qhi * 128
            pe.wait(Sb.deps_w(), Kbuf.deps_r(), Qbuf.deps_r(), pe_extra_deps)
            tok = pe.done(pe.e.matmul(Sb.ap[:, n0:n1], Kbuf.ap[0:krows, kc0:kc0 + 128],
                                      Qbuf.ap[0:krows, qsel, q0 + n0:q0 + n1], start=True, stop=True))
            Sb.wrote(tok)
            Kbuf.read(tok)
            Qbuf.read(tok)
            act.wait(tok, p.deps_w())
            ta = act.done(act.e.activation(out=p.ap[:, n0:n1], in_=Sb.ap[:, n0:n1], func=AF.Exp, scale=0.125))
            Sb.read(ta)
            p.wrote(ta)
            for (qb, mask_ap, mdeps) in masks:
                dve.wait(ta, mdeps)
                tl = dve.done(dve.e.tensor_tensor(out=p.ap[:, qb * 128:(qb + 1) * 128], in0=p.ap[:, qb * 128:(qb + 1) * 128],
                                                  in1=mask_ap, op=ALU.mult))
                p.wrote(tl, fresh=False)

        def B(it=it, i=i, vidx=vidx, qlo=qlo, qhi=qhi):
            p = PT_ring[it.slot % len(PT_ring)]
            for qb in range(qlo, qhi):
                if st["first"]:
                    pe.wait(O.deps_w())
                pe.wait(p.deps_r(), Vbuf.deps_r())
                tok = pe.done(pe.e.matmul(O.ap[:, qb * W:(qb + 1) * W], p.ap[:, qb * 128:(qb + 1) * 128], Vbuf.ap[:, vidx, 0:W],
                                          start=st["first"], stop=(i == nk - 1 and qb == qhi - 1)))
                O.wrote(tok, fresh=st["first"])
                st["first"] = False
                p.read(tok)
                Vbuf.read(tok)

        it.A = A
        it.B = B
        items.append(it)
    items[-1].F = finalize
    return items


def run_items(items, slot0, L=2):
    n = len(items)
    for i in range(n + L):
        if i < n:
            items[i].A(slot0 + i)
        j = i - L
        if j >= 0:
            items[j].B()
            if items[j].F is not None:
                items[j].F()
    return slot0 + n


def phase_nsa_attn(fl, d):
    pe, act, dve, pool, sp = fl.pe, fl.act, fl.dve, fl.pool, fl.sp
    fl.begin()
    projT, vtok, gl = d["projT"], d["vtok"], d["gl"]
    QN = Buf(fl.sb([128, 4, S], BF16, "QN"))
    KE = Buf(fl.sb([128, S], BF16, "KE"))
    KW = Buf(fl.sb([64, S], BF16, "KW"))
    KC = Buf(fl.sb([64, S], BF16, "KC"))
    VC = Buf(fl.sb([64, S], BF16, "VC"))
    VS = Buf(fl.sb([128, NT, 65], BF16, "VS"))
    VW = Buf(fl.sb([128, NT, 65], BF16, "VW"))
    KCC = Buf(fl.sb([64, 256], BF16, "KCC"))
    RC = Buf(fl.sb([128, 2, 129], BF16, "RC"))
    HK = Buf(fl.sb([128, 2, 256], BF16, "HK"))
    HV = Buf(fl.sb([128, 2, 256], BF16, "HV"))
    W1K = fl.sb([64, 32, 256], BF16, "W1K")
    W1V = fl.sb([64, 32, 256], BF16, "W1V")
    W2K = fl.sb([128, 2, 64], BF16, "W2K")
    W2V = fl.sb([128, 2, 64], BF16, "W2V")
    POSK = fl.sb([64, 32], BF16, "POSK")
    POSV = fl.sb([64, 32], BF16, "POSV")
    CB = fl.sb([128, 4], F32, "CB")
    MK = fl.sb([128, 256], BF16, "MK")
    MCM = fl.sb([128, 9, 512], BF16, "MCM")
    FA = fl.sb([128, NT, 64], BF16, "FA")
    FB = fl.sb([128, NT, 64], BF16, "FB")
    IDN = fl.sb([128, 128], BF16, "IDN")
    GS = fl.sb([128, NT, 48], F32, "GS")
    IMP = Buf(fl.sb([128, NT, 64], F32, "IMP"))
    OG = Buf(fl.sb([128, NT, 256], F32, "OG"))
    NBT = [Buf(fl.sb([128, 128], BF16, "NBT")) for _ in range(2)]
    PTs = [Buf(fl.sb([128, 512], BF16, "PT")) for _ in range(3)]
    smalls = [fl.sb([128, 16], F32, "sm") for _ in range(4)]
    otmp = [Buf(fl.sb([128, 4, 64], F32, "otmp")) for _ in range(2)]
    im2 = [fl.sb([128, 64], F32, "im2") for _ in range(2)]
    imm = [fl.sb([128, 64], F32, "imm") for _ in range(2)]
    m8 = [fl.sb([128, 16], F32, "m8") for _ in range(2)]
    S_ring = [Buf(fl.ps([128, 512], F32, "Sb")) for _ in range(3)]
    O_ring = [Buf(fl.ps([128, 512], F32, "Ob")) for _ in range(2)]
    TP = Buf(fl.ps([128, 512], BF16, "TP"))
    CPS = [Buf(fl.ps([128, 512], F32, "CPS")) for _ in range(2)]

    csem = fl.dsem("const")
    cpsem = fl.dsem("constp")
    pool.dma(MK[:, :], d["swa_mask"][:, :], cpsem)
    pool.dma(MCM[:, :, :], d["cmp_mask"].rearrange("n p c -> p n c"), cpsem)
    pool.dma(FA[:, :, :], d["sel_fa"].rearrange("(t p) c -> p t c", p=128), cpsem)
    pool.dma(FB[:, :, :], d["sel_fb"].rearrange("(t p) c -> p t c", p=128), cpsem)
    pool.dma(IDN[:, :], d["ident"][:, :], cpsem)
    pool.dma(KE.ap[64:128, :], d["emat"][:, :], cpsem)
    pool.dma(RC.ap[:, :, 65:129], d["ov_pad"].rearrange("(c p) j -> p c j", p=128), cpsem)
    pool.dma(W1K[:, :, :], d["b_cmp_k_w1"][0].rearrange("(l dd) m -> dd l m", dd=64), cpsem)
    pool.dma(W1V[:, :, :], d["b_cmp_v_w1"][0].rearrange("(l dd) m -> dd l m", dd=64), cpsem)
    pool.dma(W2K[:, :, :], d["b_cmp_k_w2"][0].rearrange("(mc p) dd -> p mc dd", p=128), cpsem)
    pool.dma(W2V[:, :, :], d["b_cmp_v_w2"][0].rearrange("(mc p) dd -> p mc dd", p=128), cpsem)
    pool.dma(POSK[:, :], d["posT_k"][:, :], cpsem)
    pool.dma(POSV[:, :], d["posT_v"][:, :], cpsem)
    tcp = (cpsem, cpsem.n + 0)
    tgs = sp.dma(GS[:, :, :], gl.rearrange("(t p) c -> p t c", p=128), csem)
    dve.wait(tcp)
    t_m = dve.done(dve.e.memset(VS.ap[:, :, 64:65], 1.0))
    t_m = dve.done(dve.e.memset(VW.ap[:, :, 64:65], 1.0))
    t_m = dve.done(dve.e.memset(RC.ap[:, :, 64:65], 1.0))
    t_m = dve.done(dve.e.memset(HK.ap[:, :, :], 0.0))
    t_m = dve.done(dve.e.memset(HV.ap[:, :, :], 0.0))
    t_m = dve.done(dve.e.memset(KCC.ap[:, :], 0.0))
    for nb in NBT:
        t_m = dve.done(dve.e.memset(nb.ap[:, :], 0.0))
    t_init = t_m
    pe.wait(tcp)
    P = CPS[0]
    tok = None
    for which, (W1, POS) in enumerate(((W1K, POSK), (W1V, POSV))):
        for mc in range(2):
            col = which * 2 + mc
            for l in range(32):
                tok = pe.done(pe.e.matmul(P.ap[:, col:col + 1], W1[0:64, l, mc * 128:(mc + 1) * 128], POS[0:64, l:l + 1],
                                          start=(l == 0), stop=(l == 31)))
    P.wrote(tok)
    dve.wait(tok)
    t_cb = dve.done(dve.e.tensor_copy(out=CB[:, :], in_=P.ap[:, 0:4]))
    P.read(t_cb)

    lsem = [fl.dsem("xl0"), fl.dsem("xl1")]
    osem = fl.dsem("ostore_sw")
    s_i = 0
    o_i = 0
    sm_i = 0
    for j in range(4):
        sp.wait(QN.deps_w(), KE.deps_w(), KW.deps_w(), KC.deps_w(), VC.deps_w(), VS.deps_w(), VW.deps_w(), t_init)
        ls = lsem[j % 2]
        for hl in range(4):
            sp.dma(QN.ap[0:64, hl, :], projT[(4 * j + hl) * 64:(4 * j + hl + 1) * 64, :], ls)
        sp.dma(KC.ap[0:64, :], projT[1024 + j * 64:1024 + (j + 1) * 64, :], ls)
        sp.dma(VC.ap[0:64, :], projT[1280 + j * 64:1280 + (j + 1) * 64, :], ls)
        sp.dma(KE.ap[0:64, :], projT[1536 + j * 64:1536 + (j + 1) * 64, :], ls)
        sp.dma(KW.ap[0:64, :], projT[1792 + j * 64:1792 + (j + 1) * 64, :], ls)
        vt3 = vtok.rearrange("(t p) c -> p t c", p=128)
        sp.dma(VS.ap[:, :, 0:64], vt3[:, :, j * 64:(j + 1) * 64], ls)
        tl = sp.dma(VW.ap[:, :, 0:64], vt3[:, :, 256 + j * 64:256 + (j + 1) * 64], ls)
        for b in (QN, KE, KW, KC, VC, VS, VW):
            b.wrote(tl, fresh=False)
            b.r = {}
        ld = [tl, tcp, t_init]

        for which, (src, W1, W2, Hb) in enumerate(((KC, W1K, W2K, HK), (VC, W1V, W2V, HV))):
            for mc in range(2):
                P = CPS[(which * 2 + mc) % 2]
                pe.wait(P.deps_w(), ld)
                tok = None
                for l in range(32):
                    tok = pe.done(pe.e.matmul(P.ap[:, 0:255], W1[0:64, l, mc * 128:(mc + 1) * 128], src.ap[0:64, l:l + 4065:16],
                                              start=(l == 0), stop=(l == 31)))
                P.wrote(tok)
                src.read(tok)
                act.wait(tok, t_cb, Hb.deps_w(), t_init)
                th = act.done(act.e.activation(out=Hb.ap[:, mc, 0:255], in_=P.ap[:, 0:255], func=AF.Gelu_apprx_tanh,
                                               bias=CB[:, which * 2 + mc:which * 2 + mc + 1]))
                P.read(th)
                Hb.wrote(th, fresh=False)
        P = CPS[0]
        pe.wait(P.deps_w(), HK.deps_r())
        tok = None
        for mc in range(2):
            tok = pe.done(pe.e.matmul(P.ap[0:64, 0:255], W2K[:, mc, :], HK.ap[:, mc, 0:255], start=(mc == 0), stop=(mc == 1)))
        P.wrote(tok)
        HK.read(tok)
        act.wait(tok, KCC.deps_w())
        tk = act.done(act.e.activation(out=KCC.ap[0:64, 0:255], in_=P.ap[0:64, 0:255], func=AF.Copy))
        P.read(tk)
        KCC.wrote(tk)
        P = CPS[1]
        pe.wait(P.deps_w(), HV.deps_r())
        tok = None
        for ct in range(2):
            for mc in range(2):
                tok = pe.done(pe.e.matmul(P.ap[:, ct * 64:(ct + 1) * 64], HV.ap[:, mc, ct * 128:(ct + 1) * 128], W2V[:, mc, :],
                                          start=(mc == 0), stop=(mc == 1)))
        P.wrote(tok)
        HV.read(tok)
        act.wait(tok, RC.deps_w())
        tr_ = act.done(act.e.activation(out=RC.ap[:, :, 0:64], in_=P.ap[:, 0:128].rearrange("p (c x) -> p c x", c=2), func=AF.Copy))
        P.read(tr_)
        RC.wrote(tr_)
        HK.w = {}
        HV.w = {}

        mcm_idx = {(0, 0): 0, (0, 1): 1, (0, 2): 2, (0, 3): 3, (0, 4): 4, (1, 4): 5, (1, 5): 6, (1, 6): 7, (1, 7): 8}

        def cmp_final(O, sm, qh, hl, h):
            O3 = O.ap[:, 0:258].rearrange("p (b w) -> p b w", b=2)
            tile0 = qh * 2
            dve.wait(O.deps_r(), tgs, OG.deps_w(), IMP.deps_w())
            f1 = dve.done(dve.e.tensor_scalar(out=sm[:, 0:2], in0=O3[:, :, 64], scalar1=1e-30, scalar2=None, op0=ALU.max))
            dve.wait(f1)
            f2 = dve.done(dve.e.reciprocal(out=sm[:, 0:2], in_=sm[:, 0:2]))
            dve.wait(f2)
            f3 = dve.done(dve.e.tensor_tensor(out=sm[:, 2:4], in0=sm[:, 0:2], in1=GS[:, tile0:tile0 + 2, h * 3 + 0], op=ALU.mult))
            dve.wait(f3)
            f4 = dve.done(dve.e.tensor_tensor(out=OG.ap[:, tile0:tile0 + 2, hl * 64:(hl + 1) * 64], in0=O3[:, :, 0:64],
                                              in1=sm[:, 2:4].unsqueeze(2).to_broadcast([128, 2, 64]), op=ALU.mult))
            OG.wrote(f4, fresh=False)
            f5 = None
            for b2 in range(2):
                if hl == 0:
                    f5 = dve.done(dve.e.tensor_scalar(out=IMP.ap[:, tile0 + b2, :], in0=O3[:, b2, 65:129], scalar1=sm[:, b2:b2 + 1],
                                                      scalar2=None, op0=ALU.mult))
                else:
                    f5 = dve.done(dve.e.scalar_tensor_tensor(out=IMP.ap[:, tile0 + b2, :], in0=O3[:, b2, 65:129],
                                                             scalar=sm[:, b2:b2 + 1], in1=IMP.ap[:, tile0 + b2, :],
                                                             op0=ALU.mult, op1=ALU.add))
            IMP.wrote(f5, fresh=False)
            O.read(f1)
            O.read(f5)

        citems = []
        for hl in range(4):
            h = 4 * j + hl
            for qh in range(16):
                tt = qh // 2
                q0 = qh * 256
                ktl = []
                for ct in range(2):
                    if ct == 1 and tt <= 3:
                        continue
                    masks = []
                    if (ct, tt) in mcm_idx:
                        mi = mcm_idx[(ct, tt)]
                        off = (qh % 2) * 256
                        masks = [(0, MCM[:, mi, off:off + 128], tcp), (1, MCM[:, mi, off + 128:off + 256], tcp)]
                    ktl.append((ct * 128, ct, 0, 2, masks))
                O = O_ring[o_i % 2]
                o_i += 1
                sm = smalls[sm_i % 4]
                sm_i += 1
                citems.extend(attn_items(fl, S_ring, PTs, O, KCC, 64, QN, hl, q0, RC, 129, ktl,
                                         finalize=(lambda O=O, sm=sm, qh=qh, hl=hl, h=h: cmp_final(O, sm, qh, hl, h))))
        s_i = run_items(citems, s_i)

        for tile in range(NT):
            k2 = tile % 2
            i2, im_, mm = im2[k2], imm[k2], m8[k2]
            nb = NBT[k2]
            dve.wait(IMP.deps_r(), tcp)
            g1 = dve.done(dve.e.tensor_tensor(out=im_[:, :], in0=IMP.ap[:, tile, :], in1=FA[:, tile, :], op=ALU.mult))
            dve.wait(g1)
            g2 = dve.done(dve.e.tensor_tensor(out=im_[:, :], in0=im_[:, :], in1=FB[:, tile, :], op=ALU.add))
            dve.wait(g2)
            g3 = dve.done(dve.e.max(out=mm[:, 0:8], in_=im_[:, :]))
            dve.wait(g3)
            g4 = dve.done(dve.e.match_replace(out=i2[:, :], in_to_replace=mm[:, 0:8], in_values=im_[:, :], imm_value=-2.0))
            dve.wait(g4)
            g5 = dve.done(dve.e.max(out=mm[:, 8:16], in_=i2[:, :]))
            dve.wait(g5)
            g6 = dve.done(dve.e.tensor_scalar(out=mm[:, 0:1], in0=mm[:, 15:16], scalar1=0.0, scalar2=None, op0=ALU.max))
            dve.wait(g6, nb.deps_w())
            g7 = dve.done(dve.e.tensor_scalar(out=nb.ap[:, 64:128], in0=im_[:, :], scalar1=mm[:, 0:1], scalar2=1.0,
                                              op0=ALU.is_ge, op1=ALU.subtract))
            nb.wrote(g7)
            IMP.read(g7)
            pe.wait(g7, TP.deps_w(), tcp)
            tt_ = pe.done(pe.e.transpose(TP.ap[:, 0:128], nb.ap[:, :], IDN[:, :]))
            nb.read(tt_)
            TP.wrote(tt_)
            act.wait(tt_, QN.deps_w())
            tq = act.done(act.e.activation(out=QN.ap[64:128, :, tile * 128:(tile + 1) * 128],
                                           in_=TP.ap[64:128, 0:128].unsqueeze(1).to_broadcast([64, 4, 128]), func=AF.Copy))
            TP.read(tq)
            QN.wrote(tq, fresh=False)

        def ws_final(O, sm, ot_, tt, hl, h, br):
            O3 = O.ap[:, 0:260].rearrange("p (b w) -> p b w", b=4)
            dve.wait(O.deps_r(), tgs, ot_.deps_w())
            f1 = dve.done(dve.e.tensor_scalar(out=sm[:, 0:4], in0=O3[:, :, 64], scalar1=1e-30, scalar2=None, op0=ALU.max))
            dve.wait(f1)
            f2 = dve.done(dve.e.reciprocal(out=sm[:, 0:4], in_=sm[:, 0:4]))
            dve.wait(f2)
            f3 = dve.done(dve.e.tensor_tensor(out=sm[:, 4:8], in0=sm[:, 0:4], in1=GS[:, 4 * tt:4 * tt + 4, h * 3 + br], op=ALU.mult))
            dve.wait(f3)
            f4 = dve.done(dve.e.tensor_tensor(out=ot_.ap[:, :, :], in0=O3[:, :, 0:64],
                                              in1=sm[:, 4:8].unsqueeze(2).to_broadcast([128, 4, 64]), op=ALU.mult))
            O.read(f1)
            O.read(f4)
            ot_.wrote(f4)
            pool.wait(f4, OG.deps_r())
            f5 = pool.done(pool.e.tensor_tensor(out=OG.ap[:, 4 * tt:4 * tt + 4, hl * 64:(hl + 1) * 64],
                                                in0=OG.ap[:, 4 * tt:4 * tt + 4, hl * 64:(hl + 1) * 64], in1=ot_.ap[:, :, :], op=ALU.add))
            ot_.read(f5)
            OG.wrote(f5, fresh=False)

        witems = []
        for br in (2, 1):
            for hl in range(4):
                h = 4 * j + hl
                for tt in range(8):
                    ktl = []
                    if br == 2:
                        for kt in range(max(0, 4 * tt - 4), 4 * tt + 4):
                            qlo = max(4 * tt, kt) - 4 * tt
                            qhi = min(4 * tt + 3, kt + 4) - 4 * tt + 1
                            masks = []
                            if 4 * tt <= kt:
                                masks.append((kt - 4 * tt, MK[:, 0:128], tcp))
                            if kt + 4 <= 4 * tt + 3 and kt + 4 >= 4 * tt:
                                masks.append((kt + 4 - 4 * tt, MK[:, 128:256], tcp))
                            ktl.append((kt * 128, kt, qlo, qhi, masks))
                        Kb, krows, Vb = KW, 64, VW
                    else:
                        for kt in range(0, 4 * tt + 4):
                            qlo = max(4 * tt, kt) - 4 * tt
                            masks = []
                            if kt >= 4 * tt:
                                masks.append((kt - 4 * tt, MK[:, 0:128], tcp))
                            ktl.append((kt * 128, kt, qlo, 4, masks))
                        Kb, krows, Vb = KE, 128, VS
                    O = O_ring[o_i % 2]
                    o_i += 1
                    sm = smalls[sm_i % 4]
                    sm_i += 1
                    ot_ = otmp[sm_i % 2]
                    witems.extend(attn_items(fl, S_ring, PTs, O, Kb, krows, QN, hl, tt * 512, Vb, 65, ktl,
                                             finalize=(lambda O=O, sm=sm, ot_=ot_, tt=tt, hl=hl, h=h, br=br: ws_final(O, sm, ot_, tt, hl, h, br))))
        s_i = run_items(witems, s_i)
        pool.wait(OG.deps_r())
        tso = pool.dma(d["attnO"].rearrange("(t p) c -> p t c", p=128)[:, :, j * 256:(j + 1) * 256], OG.ap[:, :, :], osem)
        OG.w = {}
        OG.r = {}
        OG.read(tso)
        IMP.r = {}
    fl.end()


NTILE_R = 24
NSLOT = NTILE_R * 512
I32 = mybir.dt.int32
FG_R = 896


def phase_moe_convert(fl, d):
    sem = fl.dsem("wconv")
    tok = None
    for e in range(NE):
        for fg in range(4):
            b = (e * 4 + fg) * 128
            f0 = fg * FG_R
            for (src, dst) in ((d["moe_w_gate"], d["wgbP"]), (d["moe_w_up"], d["wubP"])):
                tok = fl.pool.dma(dst[b:b + 128, :].rearrange("p (kc n) -> p kc n", kc=8),
                                  src[0, e, :, f0:f0 + FG_R].rearrange("(kc p) n -> p kc n", p=128), sem)
            tok = fl.pool.dma(d["wdbP"][b:b + 128, :].rearrange("p (fc n) -> p fc n", fc=7),
                              d["moe_w_down"][0, e, f0:f0 + FG_R, :].rearrange("(fc p) n -> p fc n", p=128), sem)
    return tok


def phase_moe_routed(fl, d, t_conv):
    nc = fl.nc
    pe, act, dve, pool, sp = fl.pe, fl.act, fl.dve, fl.pool, fl.sp
    gate_d, xb_d, xg_d, ys_d = d["gate"], d["x3b"], d["xg"], d["ys"]
    fl.begin()
    GT = fl.sb([128, NT, NE], F32, "GT")
    M = fl.sb([128, NT, NE], BF16, "M")
    Mf = fl.sb([128, NT, NE], F32, "Mf")
    UT = fl.sb([128, 128], BF16, "UT")
    ON = fl.sb([128, 128], BF16, "ON")
    TT = fl.sb([128, NT, NE], F32, "TT")
    TO = fl.sb([128, NT, NE], F32, "TO")
    SL = fl.sb([128, NT, NE], F32, "SL")
    cnt = fl.sb([128, NE], F32, "cnt")
    cnti = fl.sb([128, NE], I32, "cnti")
    pc = fl.sb([128, NE], F32, "pc")
    ss = fl.sb([128, NE], F32, "ss")
    se = fl.sb([128, NE], F32, "se")
    T512 = fl.sb([128, NTILE_R, NE], F32, "T512")
    cmp3 = fl.sb([128, NTILE_R, NE], F32, "cmp3")
    tef = fl.sb([128, NTILE_R], F32, "tef")
    FGP = fl.sb([128, NTILE_R, 4], F32, "FGP")
    iwf = fl.sb([128, NTILE_R, 4], F32, "iwf")
    idxW = fl.sb([128, NTILE_R, 4], I32, "idxW")
    ssum = fl.sb([128, NT], F32, "ssum")
    sB = fl.sb([128, NT], F32, "sB")
    sA = fl.sb([128, NT], F32, "sA")
    wsum = fl.sb([128, NT], F32, "wsum")
    wB = fl.sb([128, NT], F32, "wB")
    wAB = fl.sb([128, 2, NT], F32, "wAB")
    IAB = fl.sb([128, 2, NT], I32, "IAB")
    xsc = [Buf(fl.sb([128, D], BF16, "xsc")) for _ in range(2)]
    XTt = [Buf(fl.sb([128, 8, 512], BF16, "XTt")) for _ in range(2)]
    Wg = [Buf(fl.sb([128, 8, FG_R], BF16, "Wg")) for _ in range(2)]
    Wu = [Buf(fl.sb([128, 8, FG_R], BF16, "Wu")) for _ in range(2)]
    Wd = [Buf(fl.sb([128, 7, D], BF16, "Wd")) for _ in range(2)]
    Hs = [Buf(fl.sb([128, 7, 512], BF16, "H")) for _ in range(2)]
    Ss = [Buf(fl.sb([128, 512], F32, "Ssil")) for _ in range(2)]
    acc = [[Buf(fl.sb([128, D], F32, "acc")) for _ in range(4)] for _ in range(2)]
    GPs = [Buf(fl.ps([128, 512], F32, "GP")) for _ in range(2)]
    UPs = [Buf(fl.ps([128, 512], F32, "UP")) for _ in range(2)]
    YH = [Buf(fl.ps([128, 512], F32, "YH")) for _ in range(4)]
    PR = YH[0]
    yi = 0

    ZT = fl.sb([128, 8 * D], BF16, "ZT")
    tz = pool.done(pool.e.memset(ZT[:, :], 0.0))
    act.wait(tz)
    zsem = fl.dsem("zero")
    t_zero = None
    for q in range(NSLOT // 1024):
        t_zero = act.dma(xg_d[q * 1024:(q + 1) * 1024, :].rearrange("(p a) n -> p (a n)", a=8), ZT[:, :], zsem)
    csem = fl.dsem("const")
    cpsem = fl.dsem("constp")
    tg = sp.dma(GT[:, :, :], gate_d.rearrange("(t p) e -> p t e", p=128), csem)
    sp.dma(T512[:, :, :], d["t512"][:, :, :], csem)
    sp.dma(FGP[:, :, :], d["fgp"][:, :, :], csem)
    tc_ = (csem, csem.n)
    pool.dma(UT[:, :], d["utri"][:, :], cpsem)
    tcp = pool.dma(ON[:, :], d["ones128"][:, :], cpsem)
    dve.wait(tc_)
    t = dve.done(dve.e.tensor_scalar(out=Mf[:, :, :], in0=GT[:, :, :], scalar1=0.0, scalar2=None, op0=ALU.is_gt))
    dve.wait(t)
    t = dve.done(dve.e.tensor_copy(out=M[:, :, :], in_=Mf[:, :, :]))
    pe.wait(t, tcp)
    M2 = M[:, :, :].rearrange("p t e -> p (t e)")
    t1 = pe.done(pe.e.matmul(PR.ap[:, 0:256], UT[:, :], M2, start=True, stop=True))
    t2 = pe.done(pe.e.matmul(PR.ap[:, 256:512], ON[:, :], M2, start=True, stop=True))
    dve.wait(t1, t2)
    t = dve.done(dve.e.tensor_copy(out=SL[:, :, :].rearrange("p t e -> p (t e)"), in_=PR.ap[:, 0:256]))
    t = dve.done(dve.e.tensor_copy(out=TT[:, :, :].rearrange("p t e -> p (t e)"), in_=PR.ap[:, 256:512]))
    t = dve.done(dve.e.memset(TO[:, 0, :], 0.0))
    for k in range(1, NT):
        dve.wait(t)
        t = dve.done(dve.e.tensor_tensor(out=TO[:, k, :], in0=TO[:, k - 1, :], in1=TT[:, k - 1, :], op=ALU.add))
    dve.wait(t)
    t = dve.done(dve.e.tensor_tensor(out=cnt[:, :], in0=TO[:, NT - 1, :], in1=TT[:, NT - 1, :], op=ALU.add))
    dve.wait(t)
    t = dve.done(dve.e.tensor_scalar(out=cnt[:, :], in0=cnt[:, :], scalar1=511.0, scalar2=None, op0=ALU.add))
    dve.wait(t)
    t = dve.done(dve.e.tensor_copy(out=cnti[:, :], in_=cnt[:, :]))
    dve.wait(t)
    t = dve.done(dve.e.tensor_scalar(out=cnti[:, :], in0=cnti[:, :], scalar1=9, scalar2=9, op0=ALU.arith_shift_right,
                                     op1=ALU.logical_shift_left))
    dve.wait(t)
    t = dve.done(dve.e.tensor_copy(out=pc[:, :], in_=cnti[:, :]))
    dve.wait(t)
    t = dve.done(dve.e.memset(ss[:, 0:1], 0.0))
    for e in range(1, NE):
        dve.wait(t)
        t = dve.done(dve.e.tensor_tensor(out=ss[:, e:e + 1], in0=ss[:, e - 1:e], in1=pc[:, e - 1:e], op=ALU.add))
    dve.wait(t)
    t = dve.done(dve.e.tensor_tensor(out=se[:, :], in0=ss[:, :], in1=pc[:, :], op=ALU.add))
    dve.wait(t)
    t = dve.done(dve.e.tensor_tensor(out=cmp3[:, :, :], in0=T512[:, :, :], in1=se[:, :].unsqueeze(1).to_broadcast([128, NTILE_R, NE]),
                                     op=ALU.is_ge))
    dve.wait(t)
    t = dve.done(dve.e.tensor_reduce(out=tef[:, :], in_=cmp3[:, :, :], axis=mybir.AxisListType.X, op=ALU.add))
    dve.wait(t)
    t = dve.done(dve.e.tensor_scalar(out=tef[:, :], in0=tef[:, :], scalar1=7.0, scalar2=0.0, op0=ALU.min, op1=ALU.max))
    dve.wait(t)
    t = dve.done(dve.e.tensor_scalar(out=tef[:, :], in0=tef[:, :], scalar1=512.0, scalar2=None, op0=ALU.mult))
    dve.wait(t)
    t = dve.done(dve.e.tensor_tensor(out=iwf[:, :, :], in0=FGP[:, :, :], in1=tef[:, :].unsqueeze(2).to_broadcast([128, NTILE_R, 4]), op=ALU.add))
    dve.wait(t)
    t_te = dve.done(dve.e.tensor_copy(out=idxW[:, :, :], in_=iwf[:, :, :]))
    t = dve.done(dve.e.tensor_tensor(out=SL[:, :, :], in0=SL[:, :, :], in1=TO[:, :, :], op=ALU.add))
    dve.wait(t)
    t = dve.done(dve.e.tensor_tensor(out=SL[:, :, :], in0=SL[:, :, :], in1=ss[:, :].unsqueeze(1).to_broadcast([128, NT, NE]), op=ALU.add))
    dve.wait(t)
    t = dve.done(dve.e.tensor_tensor(out=SL[:, :, :], in0=SL[:, :, :], in1=Mf[:, :, :], op=ALU.mult))
    dve.wait(t)
    t = dve.done(dve.e.tensor_reduce(out=ssum[:, :], in_=SL[:, :, :], axis=mybir.AxisListType.X, op=ALU.add))
    t = dve.done(dve.e.tensor_reduce(out=sB[:, :], in_=SL[:, :, :], axis=mybir.AxisListType.X, op=ALU.max))
    t = dve.done(dve.e.tensor_reduce(out=wsum[:, :], in_=GT[:, :, :], axis=mybir.AxisListType.X, op=ALU.add))
    dve.wait(t)
    t = dve.done(dve.e.tensor_tensor(out=sA[:, :], in0=ssum[:, :], in1=sB[:, :], op=ALU.subtract))
    t = dve.done(dve.e.tensor_tensor(out=TT[:, :, :], in0=SL[:, :, :], in1=sB[:, :].unsqueeze(2).to_broadcast([128, NT, NE]), op=ALU.is_equal))
    dve.wait(t)
    t = dve.done(dve.e.tensor_tensor(out=TT[:, :, :], in0=TT[:, :, :], in1=GT[:, :, :], op=ALU.mult))
    dve.wait(t)
    t = dve.done(dve.e.tensor_reduce(out=wB[:, :], in_=TT[:, :, :], axis=mybir.AxisListType.X, op=ALU.add))
    dve.wait(t)
    t = dve.done(dve.e.tensor_tensor(out=wAB[:, 0, :], in0=wsum[:, :], in1=wB[:, :], op=ALU.subtract))
    t = dve.done(dve.e.tensor_copy(out=wAB[:, 1, :], in_=wB[:, :]))
    t = dve.done(dve.e.tensor_copy(out=IAB[:, 0, :], in_=sA[:, :]))
    t_idx = dve.done(dve.e.tensor_copy(out=IAB[:, 1, :], in_=sB[:, :]))
    sp.wait(t_idx)
    sp.dma(d["r_idx"][:, :, :], IAB[:, :, :], fl.dsem("st0"))
    sp.dma(d["r_w"][:, :, :], wAB[:, :, :], fl.dsem("st1"))

    xsem = [fl.dsem("xl0"), fl.dsem("xl1")]
    scsems = [fl.dsem("scat0"), fl.dsem("scat1")]
    t_sc = None
    for tb in range(NT):
        xs = xsc[tb % 2]
        sp.wait(xs.deps_w())
        tl = sp.dma(xs.ap[:, :], xb_d[tb * 128:(tb + 1) * 128, :], xsem[tb % 2])
        xs.wrote(tl)
        pool.wait(tl, t_idx, t_zero)
        scs = scsems[tb % 2]
        for ab in range(2):
            ins = pool.e.indirect_dma_start(out=xg_d[:, :], out_offset=bass.IndirectOffsetOnAxis(ap=IAB[:, ab, tb:tb + 1], axis=0),
                                            in_=xs.ap[:, :], in_offset=None)
            scs.n += 16
            ins.then_inc(scs.h, 16)
        xs.read((scs, scs.n))
    t_sc = [(scsems[0], scsems[0].n), (scsems[1], scsems[1].n)]

    wsems = [fl.dsem("rw0"), fl.dsem("rw1")]
    xtsem = [fl.dsem("xt0"), fl.dsem("xt1")]
    ysem = [fl.dsem("ys0"), fl.dsem("ys1")]
    wi = 0
    gi = 0
    hi = 0
    t_ys = []
    sp.wait(t_sc)
    for ti in range(NTILE_R):
        xt = XTt[ti % 2]
        sp.wait(xt.deps_w())
        tx = None
        for kc in range(8):
            tx = sp.dma(xt.ap[:, kc, :], xg_d[ti * 512:(ti + 1) * 512, kc * 128:(kc + 1) * 128], xtsem[ti % 2], transpose=True)
        xt.wrote(tx)
        A = acc[ti % 2]
        for fg in range(FF // FG_R):
            k = wi % 2
            wi += 1
            wg, wu, wd = Wg[k], Wu[k], Wd[k]
            f0 = fg * FG_R
            pool.wait(wg.deps_w(), wu.deps_w(), wd.deps_w(), t_te, t_conv)
            tw = None
            for (wt_, srcP) in ((wg, d["wgbP"]), (wu, d["wubP"]), (wd, d["wdbP"])):
                ins = pool.e.indirect_dma_start(out=wt_.ap[:, :, :].rearrange("p a n -> p (a n)"), out_offset=None, in_=srcP[:, :],
                                                in_offset=bass.IndirectOffsetOnAxis(ap=idxW[:, ti, fg:fg + 1], axis=0))
                wsems[k].n += 16
                ins.then_inc(wsems[k].h, 16)
                tw = (wsems[k], wsems[k].n)
            wg.wrote(tw)
            wu.wrote(tw)
            wd.wrote(tw)
            H = Hs[hi % 2]
            hi += 1
            for fc in range(7):
                GP, UP, Sb = GPs[gi % 2], UPs[gi % 2], Ss[gi % 2]
                gi += 1
                for (P, Wt) in ((GP, wg), (UP, wu)):
                    pe.wait(P.deps_w(), tw, tx)
                    tok = None
                    for kc in range(8):
                        tok = pe.done(pe.e.matmul(P.ap[:, :], Wt.ap[:, kc, fc * 128:(fc + 1) * 128], xt.ap[:, kc, :],
                                                  start=(kc == 0), stop=(kc == 7)))
                    P.wrote(tok)
                    Wt.read(tok)
                    xt.read(tok)
                act.wait(GP.deps_r(), Sb.deps_w())
                ta = act.done(act.e.activation(out=Sb.ap[:, :], in_=GP.ap[:, :], func=AF.Silu))
                GP.read(ta)
                Sb.wrote(ta)
                dve.wait(ta, UP.deps_r(), H.deps_w())
                th = dve.done(dve.e.tensor_tensor(out=H.ap[:, fc, :], in0=UP.ap[:, :], in1=Sb.ap[:, :], op=ALU.mult))
                UP.read(th)
                Sb.read(th)
                H.wrote(th, fresh=(fc == 0))
            for tb in range(4):
                a = A[tb]
                for half in range(2):
                    Y = YH[yi % 4]
                    yi += 1
                    pe.wait(Y.deps_w(), H.deps_r(), tw)
                    tok = None
                    for fc in range(7):
                        tok = pe.done(pe.e.matmul(Y.ap[:, :], H.ap[:, fc, tb * 128:(tb + 1) * 128],
                                                  wd.ap[:, fc, half * 512:(half + 1) * 512], start=(fc == 0), stop=(fc == 6)))
                    Y.wrote(tok)
                    H.read(tok)
                    wd.read(tok)
                    sl = slice(half * 512, (half + 1) * 512)
                    dve.wait(tok, a.deps_w())
                    if fg == 0:
                        ty = dve.done(dve.e.tensor_copy(out=a.ap[:, sl], in_=Y.ap[:, :]))
                    else:
                        ty = dve.done(dve.e.tensor_tensor(out=a.ap[:, sl], in0=Y.ap[:, :], in1=a.ap[:, sl], op=ALU.add))
                    Y.read(ty)
                    a.wrote(ty, fresh=(half == 0))
        tys = None
        for tb in range(4):
            a = A[tb]
            act.wait(a.deps_r())
            tys = act.dma(ys_d[ti * 512 + tb * 128:ti * 512 + (tb + 1) * 128, :], a.ap[:, :], ysem[ti % 2])
        for tb in range(4):
            A[tb].read(tys)
        t_ys.append(tys)
    fl.end()

    fl.begin()
    IAB = fl.sb([128, 2, NT], I32, "IAB")
    wAB = fl.sb([128, 2, NT], F32, "wAB")
    G, Bt, tln = load_ln_params(fl, d["ln_gain"], d["ln_bias"], 1, 1, None)
    YA = [Buf(fl.sb([128, D], F32, "YA")) for _ in range(2)]
    YB = [Buf(fl.sb([128, D], F32, "YB")) for _ in range(2)]
    xts = [Buf(fl.sb([128, D], F32, "xt")) for _ in range(2)]
    rts = [Buf(fl.sb([128, D], F32, "rt")) for _ in range(2)]
    xns = [Buf(fl.sb([128, D], F32, "xn")) for _ in range(2)]
    ots = [Buf(fl.sb([128, D], F32, "ot")) for _ in range(2)]
    stats = [fl.sb([128, 2, 6], F32, "st") for _ in range(2)]
    mvs = [fl.sb([128, 2], F32, "mv") for _ in range(2)]
    rss = [fl.sb([128, 1], F32, "rs") for _ in range(2)]
    csem = fl.dsem("const")
    sp.dma(IAB[:, :, :], d["r_idx"][:, :, :], csem)
    tld = sp.dma(wAB[:, :, :], d["r_w"][:, :, :], csem)
    gsem = [fl.dsem("ga0"), fl.dsem("ga1"), fl.dsem("gb0"), fl.dsem("gb1")]
    xsem = [fl.dsem("xl0"), fl.dsem("xl1")]
    ssem = [fl.dsem("st0"), fl.dsem("st1")]
    for tb in range(NT):
        k = tb % 2
        ya, yb, xt, rt, xn, ot = YA[k], YB[k], xts[k], rts[k], xns[k], ots[k]
        pool.wait(tld, ya.deps_w(), yb.deps_w())
        toks = []
        for ab, (yy, sm_) in enumerate(((ya, gsem[k]), (yb, gsem[2 + k]))):
            ins = pool.e.indirect_dma_start(out=yy.ap[:, :], out_offset=None, in_=ys_d[:, :],
                                            in_offset=bass.IndirectOffsetOnAxis(ap=IAB[:, ab, tb:tb + 1], axis=0))
            sm_.n += 16
            ins.then_inc(sm_.h, 16)
            yy.wrote((sm_, sm_.n))
            toks.append((sm_, sm_.n))
        sp.wait(xt.deps_w())
        txl = sp.dma(xt.ap[:, :], d["x3"][tb * 128:(tb + 1) * 128, :], xsem[k])
        xt.wrote(txl)
        dve.wait(toks, txl, rt.deps_w(), tld)
        t1 = dve.done(dve.e.scalar_tensor_tensor(out=rt.ap[:, :], in0=ya.ap[:, :], scalar=wAB[:, 0, tb:tb + 1], in1=xt.ap[:, :],
                                                 op0=ALU.mult, op1=ALU.bypass)) if False else \
            dve.done(dve.e.tensor_scalar(out=rt.ap[:, :], in0=ya.ap[:, :], scalar1=wAB[:, 0, tb:tb + 1], scalar2=None, op0=ALU.mult))
        dve.wait(t1)
        t2 = dve.done(dve.e.scalar_tensor_tensor(out=rt.ap[:, :], in0=yb.ap[:, :], scalar=wAB[:, 1, tb:tb + 1], in1=rt.ap[:, :],
                                                 op0=ALU.mult, op1=ALU.add))
        dve.wait(t2)
        t3 = dve.done(dve.e.scalar_tensor_tensor(out=rt.ap[:, :], in0=xt.ap[:, :], scalar=ALPHA, in1=rt.ap[:, :],
                                                 op0=ALU.mult, op1=ALU.add))
        ya.read(t1)
        yb.read(t2)
        xt.read(t3)
        rt.wrote(t3)
        pool.wait(tln)
        t7 = layernorm_tile(fl, rt, stats[k], mvs[k], rss[k], xn, ot, G, Bt)
        sp.wait(t7)
        ts1 = sp.dma(d["y"][tb * 128:(tb + 1) * 128, :], ot.ap[:, :], ssem[k])
        ot.read(ts1)
    fl.end()


def host_constants():
    inv = (1.0 / (np.float32(10000.0) ** (np.arange(0, 64, 2, dtype=np.float32) / np.float32(64)))).astype(np.float32)
    ang = np.arange(S, dtype=np.float32)[:, None] * inv[None, :]
    cos = np.cos(ang).astype(np.float32).T
    sin = np.sin(ang).astype(np.float32).T
    cosT = np.concatenate([cos, cos, cos, cos], axis=0)
    sinT = np.concatenate([-sin, sin, -sin, sin], axis=0)
    k = np.arange(128)[:, None]
    c = np.arange(256)[None, :]
    swa_mask = np.where(c < 128, c >= k, (c - 128) < k).astype(np.float32)
    out = {"cosT": np.ascontiguousarray(cosT), "sinT": np.ascontiguousarray(sinT), "swa_mask": swa_mask}
    cidx = np.arange(256)
    cs = 16 * cidx
    ce = cs + 32
    ss = 64 * np.arange(64)
    se = ss + 64
    ov = np.clip(np.minimum(ce[:, None], se[None, :]) - np.maximum(cs[:, None], ss[None, :]), 0, None) / 16.0
    ov[255, :] = 0.0
    out["ov_pad"] = ov.astype(np.float32)
    tiles = [(0, 0), (0, 1), (0, 2), (0, 3), (0, 4), (1, 4), (1, 5), (1, 6), (1, 7)]
    cm = np.zeros((9, 128, 512), np.float32)
    for i, (ct, tt) in enumerate(tiles):
        cc = ct * 128 + np.arange(128)[:, None]
        t = tt * 512 + np.arange(512)[None, :]
        cm[i] = ((16 * cc + 31 <= t) & (cc < 255)).astype(np.float32)
    out["cmp_mask"] = cm
    t = np.arange(S)[:, None]
    b = np.arange(64)[None, :]
    cur = t // 64
    forced = (b == 0) | (b == cur) | (b == cur - 1)
    fa = ((b <= cur) & (~forced)).astype(np.float32)
    fb = np.where(b > cur, -1.0, 0.0).astype(np.float32)
    fb = np.where(b == cur - 1, 1.0e4, fb)
    fb = np.where(b == cur, 2.0e4, fb)
    fb = np.where(b == 0, 3.0e4, fb).astype(np.float32)
    out["sel_fa"] = fa
    out["sel_fb"] = fb
    out["ident"] = np.eye(128, dtype=np.float32)
    em = np.zeros((64, S), np.float32)
    em[np.arange(S) // 64, np.arange(S)] = 30000.0
    out["emat"] = em
    pp = np.arange(128)
    out["utri"] = (pp[:, None] < pp[None, :]).astype(np.float32)
    out["ones128"] = np.ones((128, 128), np.float32)
    t512 = np.zeros((128, NTILE_R, NE), np.float32)
    t512[:, :, :] = (512.0 * np.arange(NTILE_R))[None, :, None]
    out["t512"] = t512
    fgp = np.zeros((128, NTILE_R, 4), np.float32)
    fgp[:, :, :] = (128.0 * np.arange(4))[None, None, :] + np.arange(128, dtype=np.float32)[:, None, None]
    out["fgp"] = fgp
    return out


INPUT_SHAPES = {
    "a_w_in": [1, 1024, 1536], "a_w_out": [1, 1024, 1024], "a_sinks": [1, 16],
    "b_w_in": [1, 1024, 2608], "b_w_out": [1, 1024, 1024], "b_cmp_pos_k": [1, 32, 64], "b_cmp_pos_v": [1, 32, 64],
    "b_cmp_k_w1": [1, 2048, 256], "b_cmp_k_w2": [1, 256, 64], "b_cmp_v_w1": [1, 2048, 256], "b_cmp_v_w2": [1, 256, 64],
    "ffn_w_gate": [1, 1024, 3584], "ffn_w_up": [1, 1024, 3584], "ffn_w_down": [1, 3584, 1024],
    "moe_router": [1, 1024, 8], "moe_w_gate": [1, 8, 1024, 3584], "moe_w_up": [1, 8, 1024, 3584],
    "moe_w_down": [1, 8, 3584, 1024], "ln_gain": [2, 2, 1024], "ln_bias": [2, 2, 1024],
}


def _needed(k, stop_after):
    if stop_after >= 4:
        return True
    if k.startswith("moe_w"):
        return False
    if stop_after <= 2 and (k.startswith("b_") or k.startswith("moe")):
        return False
    if stop_after <= 1 and k.startswith("ffn"):
        return False
    return True


def build(stop_after=99):
    nc = bass.Bass("TRN2", target_bir_lowering=False)
    d = {}
    d["x"] = nc.dram_tensor("x", [S, D], F32, kind="ExternalInput").ap()
    for k, shp in INPUT_SHAPES.items():
        if not _needed(k, stop_after):
            continue
        d[k] = nc.dram_tensor(k, shp, F32, kind="ExternalInput").ap()
    hc = host_constants()
    for k, v in hc.items():
        d[k] = nc.dram_tensor(k, list(v.shape), F32, kind="ExternalInput").ap()
    d["routerT"] = nc.dram_tensor("routerT", [NE, D], F32, kind="ExternalInput").ap()
    d["posT_k"] = nc.dram_tensor("posT_k", [64, 32], F32, kind="ExternalInput").ap()
    d["posT_v"] = nc.dram_tensor("posT_v", [64, 32], F32, kind="ExternalInput").ap()
    d["projT"] = nc.dram_tensor("projT", [2048, S], BF16, kind="Internal").ap()
    d["vtok"] = nc.dram_tensor("vtok", [S, 512], BF16, kind="Internal").ap()
    d["gl"] = nc.dram_tensor("gl", [S, 48], F32, kind="Internal").ap()
    y = nc.dram_tensor("y", [S, D], F32, kind="ExternalOutput").ap()
    for nm in ("xb0", "attnO", "x1b", "x2b", "x3b"):
        d[nm] = nc.dram_tensor(nm, [S, D], BF16, kind="Internal").ap()
    for nm in ("x1", "x2", "x3"):
        d[nm] = nc.dram_tensor(nm, [S, D], F32, kind="Internal").ap()
    d["gate"] = nc.dram_tensor("gate", [S, NE], F32, kind="Internal").ap()
    d["y"] = y
    if stop_after >= 4:
        d["wgbP"] = nc.dram_tensor("wgbP", [NE * 4 * 128, 8 * FG_R], BF16, kind="Internal").ap()
        d["wubP"] = nc.dram_tensor("wubP", [NE * 4 * 128, 8 * FG_R], BF16, kind="Internal").ap()
        d["wdbP"] = nc.dram_tensor("wdbP", [NE * 4 * 128, 7 * D], BF16, kind="Internal").ap()
        d["xg"] = nc.dram_tensor("xg", [NSLOT, D], BF16, kind="Internal").ap()
        d["ys"] = nc.dram_tensor("ys", [NSLOT, D], F32, kind="Internal").ap()
        d["r_idx"] = nc.dram_tensor("r_idx", [128, 2, NT], I32, kind="Internal").ap()
        d["r_w"] = nc.dram_tensor("r_w", [128, 2, NT], F32, kind="Internal").ap()
    fl = Flow(nc)
    d["t_castx"] = phase_cast_x(fl, d["x"], d["xb0"])
    phase_l0_attn(fl, d)
    phase_outproj_ln(fl, d, d["attnO"], d["a_w_out"][0], d["x"], 0, 0, y if stop_after == 1 else d["x1"], d["x1b"])
    if stop_after >= 2:
        phase_ffn(fl, d, d["x1b"], d["x1"], [d["ffn_w_gate"][0]], [d["ffn_w_up"][0]], [d["ffn_w_down"][0]], None, 0, 1,
                  y if stop_after == 2 else d["x2"], d["x2b"])
    if stop_after >= 4 and ROUTED:
        d["t_conv"] = phase_moe_convert(fl, d)
    if stop_after >= 3:
        phase_nsa_proj(fl, d)
        phase_nsa_attn(fl, d)
        phase_outproj_ln(fl, d, d["attnO"], d["b_w_out"][0], d["x2"], 1, 0, y if stop_after == 3 else d["x3"], d["x3b"],
                         router_d=d["moe_router"][0], gate_d=d["gate"])
    if stop_after >= 4:
        if ROUTED:
            phase_moe_routed(fl, d, d["t_conv"])
        else:
            phase_ffn(fl, d, d["x3b"], d["x3"], [d["moe_w_gate"][0, e] for e in range(NE)], [d["moe_w_up"][0, e] for e in range(NE)],
                      [d["moe_w_down"][0, e] for e in range(NE)], d["gate"], 1, 1, y, None)
    fl.barrier()
    fl.gstack.close()
    return nc


_CACHE = {}


def kernel(**inputs):
    stop_after = int(inputs.pop("_stop_after", 99))
    if stop_after not in _CACHE:
        _CACHE[stop_after] = build(stop_after)
    nc = _CACHE[stop_after]
    hc = host_constants()
    shared = {k: np.ascontiguousarray(np.asarray(inputs[k], dtype=np.float32)) for k in INPUT_SHAPES if _needed(k, stop_after)}
    shared.update(hc)
    shared["routerT"] = np.ascontiguousarray(np.asarray(inputs["moe_router"], dtype=np.float32)[0].T)
    shared["posT_k"] = np.ascontiguousarray(np.asarray(inputs["b_cmp_pos_k"], dtype=np.float32)[0].T)
    shared["posT_v"] = np.ascontiguousarray(np.asarray(inputs["b_cmp_pos_v"], dtype=np.float32)[0].T)
    x = np.asarray(inputs["x"], dtype=np.float32)
    in_maps = []
    for c in range(NCORES):
        m = dict(shared)
        m["x"] = np.ascontiguousarray(x[c])
        in_maps.append(m)
    res = run_bass_kernel_spmd(nc, in_maps, core_ids=list(range(NCORES)))
    return np.stack([np.asarray(r["y"], dtype=np.float32) for r in res.results], axis=0)
```

```python
import numpy as np
import ml_dtypes
from contextlib import ExitStack
import concourse.bass as bass
import concourse.mybir as mybir
from concourse.bass_utils import run_bass_kernel_spmd

F32 = mybir.dt.float32
BF16 = mybir.dt.bfloat16
AF = mybir.ActivationFunctionType
ALU = mybir.AluOpType

S = 4096
D = 1024
NT = 32
FF = 3584
NE = 8
ALPHA = float(4.0 ** 0.25)
EPS = 1e-5
NCORES = 8
ROUTED = True


class SemObj:
    def __init__(self, h):
        self.h = h
        self.n = 0


class Buf:
    def __init__(self, ap):
        self.ap = ap
        self.w = {}
        self.r = {}

    @staticmethod
    def _add(d, tok):
        if tok is None:
            return
        s, v = tok
        if d.get(s, 0) < v:
            d[s] = v

    def deps_r(self):
        return list(self.w.items())

    def deps_w(self):
        return list(self.w.items()) + list(self.r.items())

    def wrote(self, tok, fresh=True):
        if fresh:
            self.w = {}
            self.r = {}
        self._add(self.w, tok)

    def read(self, tok):
        self._add(self.r, tok)


class Eng:
    def __init__(self, fl, name, eng):
        self.fl = fl
        self.name = name
        self.e = eng
        self.sem = SemObj(fl.gstack.enter_context(fl.nc.semaphore("s_" + name)))
        self.seen = {}

    def wait(self, *toks):
        for tok in toks:
            if tok is None:
                continue
            if isinstance(tok, list) or (isinstance(tok, tuple) and (len(tok) != 2 or not isinstance(tok[0], SemObj))):
                self.wait(*tok)
                continue
            sem, v = tok
            if sem is None or v <= 0:
                continue
            if self.seen.get(sem, 0) >= v:
                continue
            if sem is self.sem and self.name == "pe":
                continue
            self.e.wait_ge(sem.h, v)
            self.seen[sem] = v

    def done(self, ins):
        self.sem.n += 1
        ins.then_inc(self.sem.h, 1)
        return (self.sem, self.sem.n)

    def dma(self, out, in_, sem, **kw):
        ins = self.e.dma_start(out=out, in_=in_, **kw)
        sem.n += 16
        ins.then_inc(sem.h, 16)
        return (sem, sem.n)


class Flow:
    def __init__(self, nc):
        self.nc = nc
        self.gstack = ExitStack()
        self.pe = Eng(self, "pe", nc.tensor)
        self.act = Eng(self, "act", nc.scalar)
        self.dve = Eng(self, "dve", nc.vector)
        self.pool = Eng(self, "pool", nc.gpsimd)
        self.sp = Eng(self, "sp", nc.sync)
        self.engines = [self.pe, self.act, self.dve, self.pool, self.sp]
        self.dsems = {}
        self.pstack = None
        self.uid = 0

    def dsem(self, name):
        if name not in self.dsems:
            self.dsems[name] = SemObj(self.gstack.enter_context(self.nc.semaphore("d_" + name)))
        return self.dsems[name]

    def begin(self):
        self.pstack = ExitStack()

    def end(self):
        self.barrier()
        self.pstack.close()
        self.pstack = None

    def barrier(self):
        toks = [(e.sem, e.sem.n) for e in self.engines if e.sem.n > 0]
        toks += [(s, s.n) for s in self.dsems.values() if s.n > 0]
        for e in self.engines:
            e.wait([t for t in toks if t[0] is not e.sem])

    def sb(self, shape, dt, name=None):
        self.uid += 1
        return self.pstack.enter_context(self.nc.sbuf_tensor(f"{name or 'sb'}_{self.uid}", list(shape), dt))

    def ps(self, shape, dt=F32, name=None):
        self.uid += 1
        return self.pstack.enter_context(self.nc.psum_tensor(f"{name or 'ps'}_{self.uid}", list(shape), dt))


def load_xT(fl, XT, xb_d, deps, semname, ncols=D, t0=0, nt=S):
    sp = fl.sp
    sem = fl.dsem(semname)
    sp.wait(deps, XT.deps_w())
    tok = None
    step = 1024 if nt >= 1024 else nt
    for kc in range(ncols // 128):
        for q in range(nt // step):
            tok = sp.dma(XT.ap[:, kc, q * step:(q + 1) * step],
                         xb_d[t0 + q * step:t0 + (q + 1) * step, kc * 128:(kc + 1) * 128], sem, transpose=True)
    XT.wrote(tok)
    return tok


def layernorm_tile(fl, r, stats, mv, rs, xn, out_t, G, Bt):
    dve, pool = fl.dve, fl.pool
    dve.wait(r.deps_r())
    t1 = dve.done(dve.e.bn_stats(out=stats[:, 0, :], in_=r.ap[:, 0:512]))
    t2 = dve.done(dve.e.bn_stats(out=stats[:, 1, :], in_=r.ap[:, 512:1024]))
    dve.wait(t1, t2)
    t3 = dve.done(dve.e.bn_aggr(out=mv[:, :], in_=stats[:, :, :].rearrange("p a b -> p (a b)")))
    dve.wait(t3)
    t4a = dve.done(dve.e.tensor_scalar(out=rs[:, :], in0=mv[:, 1:2], scalar1=EPS, scalar2=None, op0=ALU.add))
    fl.act.wait(t4a)
    t4b = fl.act.done(fl.act.e.activation(out=rs[:, :], in_=rs[:, :], func=AF.Sqrt))
    dve.wait(t4b)
    t4 = dve.done(dve.e.reciprocal(out=rs[:, :], in_=rs[:, :]))
    dve.wait(t4, xn.deps_w())
    t5 = dve.done(dve.e.tensor_scalar(out=xn.ap[:, :], in0=r.ap[:, :], scalar1=mv[:, 0:1], scalar2=rs[:, 0:1],
                                      op0=ALU.subtract, op1=ALU.mult))
    r.read(t5)
    xn.wrote(t5)
    pool.wait(t5, out_t.deps_w())
    t6 = pool.done(pool.e.tensor_tensor(out=xn.ap[:, :], in0=xn.ap[:, :], in1=G[:, :], op=ALU.mult))
    pool.wait(t6)
    t7 = pool.done(pool.e.tensor_tensor(out=out_t.ap[:, :], in0=xn.ap[:, :], in1=Bt[:, :], op=ALU.add))
    xn.read(t7)
    xn.wrote(t6, fresh=False)
    out_t.wrote(t7)
    return t7


def load_ln_params(fl, ln_gain_d, ln_bias_d, i, j, ready):
    G = fl.sb([128, D], F32, "lnG")
    Bt = fl.sb([128, D], F32, "lnB")
    sem = fl.dsem("lnp")
    fl.sp.wait(ready)
    fl.sp.dma(G[:, :], ln_gain_d[i, j, :].partition_broadcast(128), sem)
    tok = fl.sp.dma(Bt[:, :], ln_bias_d[i, j, :].partition_broadcast(128), sem)
    return G, Bt, tok


def phase_cast_x(fl, x_d, xb_d):
    sem = fl.dsem("castx")
    tok = None
    for q in range(4):
        tok = fl.pool.dma(xb_d[q * 1024:(q + 1) * 1024, :], x_d[q * 1024:(q + 1) * 1024, :], sem)
    return tok


def rope_proj_chunk(fl, XT, W, Wsw, M, tt, PJ, pj_i, CT, ST, tmps, tmp_i, dests, x_ready):
    pe, dve, pool = fl.pe, fl.dve, fl.pool
    P1 = PJ[(2 * pj_i) % len(PJ)]
    P2 = PJ[(2 * pj_i + 1) % len(PJ)]
    for (P, Wt) in ((P1, W), (P2, Wsw)):
        pe.wait(P.deps_w(), Wt.deps_r(), x_ready)
        tok = None
        for kc in range(8):
            tok = pe.done(pe.e.matmul(P.ap[0:M, :], Wt.ap[:, kc, 0:M], XT.ap[:, kc, tt * 512:(tt + 1) * 512],
                                      start=(kc == 0), stop=(kc == 7)))
        Wt.read(tok)
        XT.read(tok)
        P.wrote(tok)
    T1 = tmps[(2 * tmp_i) % len(tmps)]
    T2 = tmps[(2 * tmp_i + 1) % len(tmps)]
    dve.wait(P1.deps_r(), T1.deps_w())
    t1 = dve.done(dve.e.tensor_tensor(out=T1.ap[0:M, :], in0=P1.ap[0:M, :], in1=CT[0:M, tt * 512:(tt + 1) * 512], op=ALU.mult))
    P1.read(t1)
    T1.wrote(t1)
    dve.wait(P2.deps_r(), T2.deps_w())
    t2 = dve.done(dve.e.tensor_tensor(out=T2.ap[0:M, :], in0=P2.ap[0:M, :], in1=ST[0:M, tt * 512:(tt + 1) * 512], op=ALU.mult))
    P2.read(t2)
    T2.wrote(t2)
    for (row0, dbuf, out_ap) in dests:
        pool.wait(t1, t2, dbuf.deps_w())
        t3 = pool.done(pool.e.tensor_tensor(out=out_ap, in0=T1.ap[row0:row0 + 64, :], in1=T2.ap[row0:row0 + 64, :], op=ALU.add))
        T1.read(t3)
        T2.read(t3)
        dbuf.wrote(t3, fresh=False)


def load_w_cols(fl, Wb, w_d, c0, M, semname, swap=False):
    pool = fl.pool
    sem = fl.dsem(semname)
    pool.wait(Wb.deps_w())
    if not swap:
        src = w_d.rearrange("(kc p) n -> p kc n", p=128)[:, :, c0:c0 + M]
        tok = pool.dma(Wb.ap[:, :, 0:M], src, sem)
    else:
        nh = M // 64
        ncol = w_d.shape[1]
        src5 = w_d[:, 0:(ncol // 64) * 64].rearrange("(kc p) (h two i) -> p kc h two i", p=128, two=2, i=32)
        dst5 = Wb.ap[:, :, 0:M].rearrange("p kc (h two i) -> p kc h two i", two=2, i=32)
        h0 = c0 // 64
        tok = None
        for two in range(2):
            for hh in range(nh):
                tok = pool.dma(dst5[:, :, hh, two, :], src5[:, :, h0 + hh, 1 - two, :], sem)
    Wb.wrote(tok)
    return tok


def phase_l0_attn(fl, d):
    nc = fl.nc
    pe, act, dve, pool, sp = fl.pe, fl.act, fl.dve, fl.pool, fl.sp
    fl.begin()
    XT = Buf(fl.sb([128, 8, S], BF16, "XT"))
    CT = fl.sb([128, S], F32, "CT")
    ST = fl.sb([128, S], F32, "ST")
    MK = fl.sb([128, 256], BF16, "MK")
    es = fl.sb([128, 16], F32, "es")
    KT = Buf(fl.sb([64, S], BF16, "KT"))
    VA = Buf(fl.sb([128, NT, 65], BF16, "VA"))
    QT = Buf(fl.sb([64, 4, S], BF16, "QT"))
    OG = Buf(fl.sb([128, NT, 256], BF16, "OG"))
    Wn = [Buf(fl.sb([128, 8, 128], BF16, "Wn")) for _ in range(2)]
    Ws = [Buf(fl.sb([128, 8, 128], BF16, "Ws")) for _ in range(2)]
    tmps = [Buf(fl.sb([128, 512], F32, "tmp")) for _ in range(4)]
    PTs = [Buf(fl.sb([128, 512], BF16, "PT")) for _ in range(3)]
    lts = [fl.sb([128, 4], F32, "lt") for _ in range(4)]
    banks = [Buf(fl.ps([128, 512], F32, "bank")) for _ in range(8)]
    PJ = banks[0:4]
    S_ring = [banks[4], banks[5], banks[3]]
    O_ring = [banks[6], banks[7]]
    o_i = 0

    csem = fl.dsem("const")
    sp.dma(CT[:, :], d["cosT"][:, :], csem)
    sp.dma(ST[:, :], d["sinT"][:, :], csem)
    sp.dma(es[:, :], d["a_sinks"][0, :].partition_broadcast(128), csem)
    tc1 = (csem, csem.n)
    msem = fl.dsem("constp")
    tmk = pool.dma(MK[:, :], d["swa_mask"][:, :], msem)
    act.wait(tc1)
    tes = act.done(act.e.activation(out=es[:, :], in_=es[:, :], func=AF.Exp))
    tva = dve.done(dve.e.memset(VA.ap[:, :, 64:65], 1.0))
    VA.wrote(tva)
    tx = load_xT(fl, XT, d["xb0"], d["t_castx"], "xt")
    x_ready = [tx, tc1]

    w_in = d["a_w_in"][0]
    pj_i = 0
    tmp_i = 0
    wi = 0
    it = 0
    for j in range(4):
        for c in range(2):
            Wb, Wsb = Wn[wi % 2], Ws[wi % 2]
            wi += 1
            c0 = (4 * j + 2 * c) * 64
            load_w_cols(fl, Wb, w_in, c0, 128, "wn%d" % ((wi - 1) % 2))
            load_w_cols(fl, Wsb, w_in, c0, 128, "ws%d" % ((wi - 1) % 2), swap=True)
            for tt in range(8):
                dests = [(0, QT, QT.ap[0:64, 2 * c, tt * 512:(tt + 1) * 512]),
                         (64, QT, QT.ap[0:64, 2 * c + 1, tt * 512:(tt + 1) * 512])]
                rope_proj_chunk(fl, XT, Wb, Wsb, 128, tt, PJ, pj_i, CT, ST, tmps, tmp_i, dests, x_ready)
                pj_i += 1
                tmp_i += 1
        Wb, Wsb = Wn[wi % 2], Ws[wi % 2]
        wi += 1
        c0 = 1024 + j * 64
        load_w_cols(fl, Wb, w_in, c0, 64, "wn%d" % ((wi - 1) % 2))
        load_w_cols(fl, Wsb, w_in, c0, 64, "ws%d" % ((wi - 1) % 2), swap=True)
        for tt in range(8):
            dests = [(0, KT, KT.ap[0:64, tt * 512:(tt + 1) * 512])]
            rope_proj_chunk(fl, XT, Wb, Wsb, 64, tt, PJ, pj_i, CT, ST, tmps, tmp_i, dests, x_ready)
            pj_i += 1
            tmp_i += 1
        Wb = Wn[wi % 2]
        wi += 1
        c0 = 1280 + j * 64
        load_w_cols(fl, Wb, w_in, c0, 64, "wn%d" % ((wi - 1) % 2))
        for g8 in range(4):
            P = PJ[pj_i % 4]
            pj_i += 1
            pe.wait(P.deps_w(), Wb.deps_r(), x_ready)
            tok = None
            for t8 in range(8):
                tb = g8 * 8 + t8
                for kc in range(8):
                    tok = pe.done(pe.e.matmul(P.ap[:, t8 * 64:(t8 + 1) * 64], XT.ap[:, kc, tb * 128:(tb + 1) * 128],
                                              Wb.ap[:, kc, 0:64], start=(kc == 0), stop=(kc == 7)))
            Wb.read(tok)
            XT.read(tok)
            P.wrote(tok)
            act.wait(tok, VA.deps_w())
            tv = act.done(act.e.activation(out=VA.ap[:, g8 * 8:(g8 + 1) * 8, 0:64],
                                           in_=P.ap[:, :].rearrange("p (a b) -> p a b", a=8), func=AF.Copy))
            P.read(tv)
            VA.wrote(tv, fresh=False)

        def l0_final(O, lt, tt, hl, h):
            O3 = O.ap[:, 0:260].rearrange("p (b w) -> p b w", b=4)
            dve.wait(O.deps_r(), tes, OG.deps_w())
            f1 = dve.done(dve.e.tensor_scalar(out=lt[:, :], in0=O3[:, :, 64], scalar1=es[:, h:h + 1], scalar2=None, op0=ALU.add))
            dve.wait(f1)
            f2 = dve.done(dve.e.reciprocal(out=lt[:, :], in_=lt[:, :]))
            dve.wait(f2)
            f3 = dve.done(dve.e.tensor_tensor(out=OG.ap[:, 4 * tt:4 * tt + 4, hl * 64:(hl + 1) * 64], in0=O3[:, :, 0:64],
                                              in1=lt[:, :].unsqueeze(2).to_broadcast([128, 4, 64]), op=ALU.mult))
            O.read(f1)
            O.read(f3)
            OG.wrote(f3, fresh=False)

        aitems = []
        for hl in range(4):
            h = 4 * j + hl
            for tt in range(8):
                ktl = []
                for kt in range(max(0, 4 * tt - 1), 4 * tt + 4):
                    qlo = max(4 * tt, kt) - 4 * tt
                    qhi = min(4 * tt + 3, kt + 1) - 4 * tt + 1
                    masks = []
                    if kt >= 4 * tt:
                        masks.append((kt - 4 * tt, MK[:, 0:128], tmk))
                    if 4 * tt <= kt + 1 <= 4 * tt + 3:
                        masks.append((kt + 1 - 4 * tt, MK[:, 128:256], tmk))
                    ktl.append((kt * 128, kt, qlo, qhi, masks))
                O = O_ring[o_i % 2]
                o_i += 1
                lt = lts[o_i % 4]
                aitems.extend(attn_items(fl, S_ring, PTs, O, KT, 64, QT, hl, tt * 512, VA, 65, ktl,
                                         finalize=(lambda O=O, lt=lt, tt=tt, hl=hl, h=h: l0_final(O, lt, tt, hl, h))))
        it = run_items(aitems, it)
        osem = fl.dsem("ostore")
        sp.wait(OG.deps_r())
        tso = sp.dma(d["attnO"].rearrange("(t p) c -> p t c", p=128)[:, :, j * 256:(j + 1) * 256], OG.ap[:, :, :], osem)
        OG.read(tso)
        OG.w = {}
    fl.end()


def phase_outproj_ln(fl, d, attn_d, w_out_d, xres_d, ln_i, ln_j, out_d, outb_d, router_d=None, gate_d=None):
    pe, act, dve, pool, sp = fl.pe, fl.act, fl.dve, fl.pool, fl.sp
    fl.begin()
    OT = Buf(fl.sb([128, 8, S], BF16, "OT"))
    Wo = Buf(fl.sb([128, 8, D], BF16, "Wo"))
    G, Bt, tln = load_ln_params(fl, d["ln_gain"], d["ln_bias"], ln_i, ln_j, None)
    xts = [Buf(fl.sb([128, D], F32, "xt")) for _ in range(2)]
    rts = [Buf(fl.sb([128, D], F32, "rt")) for _ in range(2)]
    xns = [Buf(fl.sb([128, D], F32, "xn")) for _ in range(2)]
    ots = [Buf(fl.sb([128, D], F32, "ot")) for _ in range(2)]
    obs = [Buf(fl.sb([128, D], BF16, "ob")) for _ in range(2)]
    stats = [fl.sb([128, 2, 6], F32, "st") for _ in range(2)]
    mvs = [fl.sb([128, 2], F32, "mv") for _ in range(2)]
    rss = [fl.sb([128, 1], F32, "rs") for _ in range(2)]
    Ys = [Buf(fl.ps([128, 1024], F32, "Y")) for _ in range(2)]
    if router_d is not None:
        RB = fl.sb([128, NE, D], F32, "RB")
        rsem = fl.dsem("rb")
        trb = None
        for e in range(NE):
            trb = sp.dma(RB[:, e, :], d["routerT"][e, :].partition_broadcast(128), rsem)
        junk = fl.sb([128, D], F32, "junk")
        jtok = [None]
        lgs = [fl.sb([128, 8], F32, "lg") for _ in range(2)]
        mx8 = [fl.sb([128, 8], F32, "mx8") for _ in range(2)]
        gts = [Buf(fl.sb([128, 8], F32, "gt")) for _ in range(2)]
        g2 = [fl.sb([128, 8], F32, "g2") for _ in range(2)]
        w12 = [fl.sb([128, 2], F32, "w12") for _ in range(2)]
    wsem = fl.dsem("wo")
    pool.wait(Wo.deps_w())
    tw = pool.dma(Wo.ap[:, :, :], w_out_d.rearrange("(kc p) n -> p kc n", p=128), wsem)
    Wo.wrote(tw)
    tot = load_xT(fl, OT, attn_d, None, "xt")
    xsem = [fl.dsem("xl0"), fl.dsem("xl1")]
    ssem = [fl.dsem("st0"), fl.dsem("st1")]
    for tb in range(NT):
        k = tb % 2
        xt, rt, xn, ot, ob, Y = xts[k], rts[k], xns[k], ots[k], obs[k], Ys[k]
        sp.wait(xt.deps_w())
        tx = sp.dma(xt.ap[:, :], xres_d[tb * 128:(tb + 1) * 128, :], xsem[k])
        xt.wrote(tx)
        pe.wait(Y.deps_w(), tw, tot)
        tok = None
        for half in range(2):
            for kc in range(8):
                tok = pe.done(pe.e.matmul(Y.ap[:, half * 512:(half + 1) * 512], OT.ap[:, kc, tb * 128:(tb + 1) * 128],
                                          Wo.ap[:, kc, half * 512:(half + 1) * 512], start=(kc == 0), stop=(kc == 7)))
        Y.wrote(tok)
        dve.wait(tok, tx, rt.deps_w())
        tr = None
        for half in range(2):
            sl = slice(half * 512, (half + 1) * 512)
            tr = dve.done(dve.e.scalar_tensor_tensor(out=rt.ap[:, sl], in0=xt.ap[:, sl], scalar=ALPHA, in1=Y.ap[:, sl],
                                                     op0=ALU.mult, op1=ALU.add))
        Y.read(tr)
        xt.read(tr)
        rt.wrote(tr)
        pool.wait(tln)
        t7 = layernorm_tile(fl, rt, stats[k], mvs[k], rss[k], xn, ot, G, Bt)
        sp.wait(t7)
        ts1 = sp.dma(out_d[tb * 128:(tb + 1) * 128, :], ot.ap[:, :], ssem[k])
        ot.read(ts1)
        act.wait(t7, ob.deps_w())
        tc = act.done(act.e.activation(out=ob.ap[:, :], in_=ot.ap[:, :], func=AF.Copy))
        ot.read(tc)
        ob.wrote(tc)
        sp.wait(tc)
        ts2 = sp.dma(outb_d[tb * 128:(tb + 1) * 128, :], ob.ap[:, :], fl.dsem("sb%d" % k))
        ob.read(ts2)
        if router_d is not None:
            lg, m8, gt, gg, ww = lgs[k], mx8[k], gts[k], g2[k], w12[k]
            dve.wait(t7, trb)
            tl = jtok[0]
            for e in range(NE):
                dve.wait(tl)
                tl = dve.done(dve.e.scalar_tensor_tensor(out=junk[:, :], in0=ot.ap[:, :], scalar=1.0, in1=RB[:, e, :],
                                                         op0=ALU.mult, op1=ALU.mult, accum_out=lg[:, e:e + 1]))
            ot.read(tl)
            jtok[0] = tl
            dve.wait(tl)
            tm = dve.done(dve.e.max(out=m8[:, :], in_=lg[:, :]))
            dve.wait(tm)
            t_a = dve.done(dve.e.tensor_tensor(out=ww[:, 0:1], in0=m8[:, 0:1], in1=m8[:, 1:2], op=ALU.subtract))
            t_b = dve.done(dve.e.tensor_tensor(out=ww[:, 1:2], in0=m8[:, 1:2], in1=m8[:, 0:1], op=ALU.subtract))
            act.wait(t_a, t_b)
            t_s = act.done(act.e.activation(out=ww[:, :], in_=ww[:, :], func=AF.Sigmoid))
            dve.wait(t_s, gt.deps_w())
            t_g1 = dve.done(dve.e.tensor_scalar(out=gt.ap[:, :], in0=lg[:, :], scalar1=m8[:, 0:1], scalar2=ww[:, 0:1],
                                                op0=ALU.is_equal, op1=ALU.mult))
            t_g2 = dve.done(dve.e.tensor_scalar(out=gg[:, :], in0=lg[:, :], scalar1=m8[:, 1:2], scalar2=ww[:, 1:2],
                                                op0=ALU.is_equal, op1=ALU.mult))
            dve.wait(t_g1, t_g2)
            t_g3 = dve.done(dve.e.tensor_tensor(out=gt.ap[:, :], in0=gt.ap[:, :], in1=gg[:, :], op=ALU.add))
            gt.wrote(t_g3)
            sp.wait(t_g3)
            ts3 = sp.dma(gate_d[tb * 128:(tb + 1) * 128, :], gt.ap[:, :], fl.dsem("sg%d" % k))
            gt.read(ts3)
    fl.end()


def phase_ffn(fl, d, xb_d, xres_d, wg_list, wu_list, wd_list, gate_d, ln_i, ln_j, out_d, outb_d, TG=1024):
    pe, act, dve, pool, sp = fl.pe, fl.act, fl.dve, fl.pool, fl.sp
    fl.begin()
    ne = len(wg_list)
    ntile = TG // 128
    XTg = Buf(fl.sb([128, 8, TG], BF16, "XTg"))
    acc = [Buf(fl.sb([128, D], F32, "acc")) for _ in range(ntile)]
    Wg = [Buf(fl.sb([128, 8, 512], BF16, "Wg")) for _ in range(2)]
    Wu = [Buf(fl.sb([128, 8, 512], BF16, "Wu")) for _ in range(2)]
    Wd = [Buf(fl.sb([128, 4, D], BF16, "Wd")) for _ in range(2)]
    Hs = [Buf(fl.sb([128, 4, 512], BF16, "H")) for _ in range(2)]
    Ss = [Buf(fl.sb([128, 512], F32, "Ssil")) for _ in range(2)]
    G, Bt, tln = load_ln_params(fl, d["ln_gain"], d["ln_bias"], ln_i, ln_j, None)
    xts = [Buf(fl.sb([128, D], F32, "xt")) for _ in range(2)]
    xns = [Buf(fl.sb([128, D], F32, "xn")) for _ in range(2)]
    ots = [Buf(fl.sb([128, D], F32, "ot")) for _ in range(2)]
    obs = [Buf(fl.sb([128, D], BF16, "ob")) for _ in range(2)]
    stats = [fl.sb([128, 2, 6], F32, "st") for _ in range(2)]
    mvs = [fl.sb([128, 2], F32, "mv") for _ in range(2)]
    rss = [fl.sb([128, 1], F32, "rs") for _ in range(2)]
    GPs = [Buf(fl.ps([128, 512], F32, "GP")) for _ in range(2)]
    UPs = [Buf(fl.ps([128, 512], F32, "UP")) for _ in range(2)]
    Ys = [Buf(fl.ps([128, 1024], F32, "Y")) for _ in range(2)]
    if gate_d is not None:
        GT = Buf(fl.sb([128, ntile, NE], F32, "GT"))
    wsems = [fl.dsem("ffw0"), fl.dsem("ffw1")]
    xsem = [fl.dsem("xl0"), fl.dsem("xl1")]
    ssem = [fl.dsem("st0"), fl.dsem("st1")]
    gsem = fl.dsem("gl")
    wi = 0
    gi = 0
    yi = 0
    hi = 0
    ei = 0
    for tg in range(S // TG):
        t0 = tg * TG
        tx = load_xT(fl, XTg, xb_d, None, "xt", t0=t0, nt=TG)
        if gate_d is not None:
            sp.wait(GT.deps_w())
            tgl = sp.dma(GT.ap[:, :, :], gate_d[t0:t0 + TG, :].rearrange("(t p) e -> p t e", p=128), gsem)
            GT.wrote(tgl)
        for e in range(ne):
            for fg in range(FF // 512):
                k = wi % 2
                wi += 1
                wg, wu, wd = Wg[k], Wu[k], Wd[k]
                pool.wait(wg.deps_w(), wu.deps_w(), wd.deps_w())
                f0 = fg * 512
                pool.dma(wg.ap[:, :, :], wg_list[e].rearrange("(kc p) n -> p kc n", p=128)[:, :, f0:f0 + 512], wsems[k])
                pool.dma(wu.ap[:, :, :], wu_list[e].rearrange("(kc p) n -> p kc n", p=128)[:, :, f0:f0 + 512], wsems[k])
                tw = pool.dma(wd.ap[:, :, :], wd_list[e][f0:f0 + 512, :].rearrange("(fc p) n -> p fc n", p=128), wsems[k])
                wg.wrote(tw)
                wu.wrote(tw)
                wd.wrote(tw)
                first_acc = (e == 0 and fg == 0)
                for tt in range(TG // 512):
                    H = Hs[hi % 2]
                    hi += 1
                    for fc in range(4):
                        GP, UP, Sb = GPs[gi % 2], UPs[gi % 2], Ss[gi % 2]
                        gi += 1
                        for (P, Wt) in ((GP, wg), (UP, wu)):
                            pe.wait(P.deps_w(), tw, tx)
                            tok = None
                            for kc in range(8):
                                tok = pe.done(pe.e.matmul(P.ap[:, :], Wt.ap[:, kc, fc * 128:(fc + 1) * 128],
                                                          XTg.ap[:, kc, tt * 512:(tt + 1) * 512], start=(kc == 0), stop=(kc == 7)))
                            P.wrote(tok)
                            Wt.read(tok)
                            XTg.read(tok)
                        act.wait(GP.deps_r(), Sb.deps_w())
                        ta = act.done(act.e.activation(out=Sb.ap[:, :], in_=GP.ap[:, :], func=AF.Silu))
                        GP.read(ta)
                        Sb.wrote(ta)
                        dve.wait(ta, UP.deps_r(), H.deps_w())
                        th = dve.done(dve.e.tensor_tensor(out=H.ap[:, fc, :], in0=UP.ap[:, :], in1=Sb.ap[:, :], op=ALU.mult))
                        UP.read(th)
                        Sb.read(th)
                        H.wrote(th, fresh=(fc == 0))
                    for tb in range(4):
                        Y = Ys[yi % 2]
                        yi += 1
                        tile_i = tt * 4 + tb
                        pe.wait(Y.deps_w(), H.deps_r(), tw)
                        tok = None
                        for half in range(2):
                            for fc in range(4):
                                tok = pe.done(pe.e.matmul(Y.ap[:, half * 512:(half + 1) * 512], H.ap[:, fc, tb * 128:(tb + 1) * 128],
                                                          wd.ap[:, fc, half * 512:(half + 1) * 512], start=(fc == 0), stop=(fc == 3)))
                        Y.wrote(tok)
                        H.read(tok)
                        wd.read(tok)
                        a = acc[tile_i]
                        dve.wait(tok, a.deps_w())
                        ty = None
                        for half in range(2):
                            sl = slice(half * 512, (half + 1) * 512)
                            if gate_d is None:
                                if first_acc:
                                    ty = dve.done(dve.e.tensor_copy(out=a.ap[:, sl], in_=Y.ap[:, sl]))
                                else:
                                    ty = dve.done(dve.e.tensor_tensor(out=a.ap[:, sl], in0=Y.ap[:, sl], in1=a.ap[:, sl], op=ALU.add))
                            else:
                                dve.wait(GT.deps_r())
                                gsc = GT.ap[:, tile_i, e:e + 1]
                                if first_acc:
                                    ty = dve.done(dve.e.tensor_scalar(out=a.ap[:, sl], in0=Y.ap[:, sl], scalar1=gsc, scalar2=None,
                                                                      op0=ALU.mult))
                                else:
                                    ty = dve.done(dve.e.scalar_tensor_tensor(out=a.ap[:, sl], in0=Y.ap[:, sl], scalar=gsc,
                                                                             in1=a.ap[:, sl], op0=ALU.mult, op1=ALU.add))
                        Y.read(ty)
                        a.wrote(ty)
                        if gate_d is not None:
                            GT.read(ty)
        for ti in range(ntile):
            k = ei % 2
            ei += 1
            tbg = t0 // 128 + ti
            xt, xn, ot, ob = xts[k], xns[k], ots[k], obs[k]
            a = acc[ti]
            sp.wait(xt.deps_w())
            txl = sp.dma(xt.ap[:, :], xres_d[tbg * 128:(tbg + 1) * 128, :], xsem[k])
            xt.wrote(txl)
            dve.wait(txl, a.deps_r())
            tr = dve.done(dve.e.scalar_tensor_tensor(out=a.ap[:, :], in0=xt.ap[:, :], scalar=ALPHA, in1=a.ap[:, :],
                                                     op0=ALU.mult, op1=ALU.add))
            xt.read(tr)
            a.wrote(tr)
            pool.wait(tln)
            t7 = layernorm_tile(fl, a, stats[k], mvs[k], rss[k], xn, ot, G, Bt)
            sp.wait(t7)
            ts1 = sp.dma(out_d[tbg * 128:(tbg + 1) * 128, :], ot.ap[:, :], ssem[k])
            ot.read(ts1)
            if outb_d is not None:
                act.wait(t7, ob.deps_w())
                tc = act.done(act.e.activation(out=ob.ap[:, :], in_=ot.ap[:, :], func=AF.Copy))
                ot.read(tc)
                ob.wrote(tc)
                sp.wait(tc)
                ts2 = sp.dma(outb_d[tbg * 128:(tbg + 1) * 128, :], ob.ap[:, :], fl.dsem("sb%d" % k))
                ob.read(ts2)
    fl.end()


NSA_FM = [(0, True), (128, True), (256, True), (384, True), (512, True), (640, True), (768, True), (896, True),
          (1024, True), (1152, True),
          (1280, False), (1408, False),
          (1536, True), (1664, True),
          (2048, True), (2176, True)]


def nsa_projT_row(c0):
    return c0 if c0 < 1792 else c0 - 256


def phase_nsa_proj(fl, d):
    pe, act, dve, pool, sp = fl.pe, fl.act, fl.dve, fl.pool, fl.sp
    fl.begin()
    XT = Buf(fl.sb([128, 8, S], BF16, "XT"))
    CT = fl.sb([128, S], F32, "CT")
    ST = fl.sb([128, S], F32, "ST")
    Wn = [Buf(fl.sb([128, 8, 128], BF16, "Wn")) for _ in range(2)]
    Ws = [Buf(fl.sb([128, 8, 128], BF16, "Ws")) for _ in range(2)]
    Wt = Buf(fl.sb([128, 8, 560], BF16, "Wt"))
    tmps = [Buf(fl.sb([128, 512], F32, "tmp")) for _ in range(4)]
    stg = [Buf(fl.sb([128, S], BF16, "stg")) for _ in range(2)]
    vst = [Buf(fl.sb([128, 512], BF16, "vst")) for _ in range(2)]
    gst = [Buf(fl.sb([128, 48], F32, "gst")) for _ in range(2)]
    PJ = [Buf(fl.ps([128, 512], F32, "bank")) for _ in range(6)]
    csem = fl.dsem("const")
    sp.dma(CT[:, :], d["cosT"][:, :], csem)
    sp.dma(ST[:, :], d["sinT"][:, :], csem)
    tc1 = (csem, csem.n)
    tx = load_xT(fl, XT, d["x2b"], None, "xt")
    x_ready = [tx, tc1]
    w_in = d["b_w_in"][0]
    projT = d["projT"]
    ssem = [fl.dsem("st0"), fl.dsem("st1")]
    pj_i = 0
    tmp_i = 0
    for ci, (c0, roped) in enumerate(NSA_FM):
        Wb, Wsb = Wn[ci % 2], Ws[ci % 2]
        sg = stg[ci % 2]
        load_w_cols(fl, Wb, w_in, c0, 128, "wn%d" % (ci % 2))
        if roped:
            load_w_cols(fl, Wsb, w_in, c0, 128, "ws%d" % (ci % 2), swap=True)
        for tt in range(8):
            if roped:
                dests = [(0, sg, sg.ap[0:64, tt * 512:(tt + 1) * 512]), (64, sg, sg.ap[64:128, tt * 512:(tt + 1) * 512])]
                rope_proj_chunk(fl, XT, Wb, Wsb, 128, tt, PJ[0:4], pj_i, CT, ST, tmps, tmp_i, dests, x_ready)
                pj_i += 1
                tmp_i += 1
            else:
                P = PJ[4 + (tt % 2)]
                pe.wait(P.deps_w(), Wb.deps_r(), x_ready)
                tok = None
                for kc in range(8):
                    tok = pe.done(pe.e.matmul(P.ap[:, :], Wb.ap[:, kc, :], XT.ap[:, kc, tt * 512:(tt + 1) * 512],
                                              start=(kc == 0), stop=(kc == 7)))
                Wb.read(tok)
                XT.read(tok)
                P.wrote(tok)
                act.wait(tok, sg.deps_w())
                tv = act.done(act.e.activation(out=sg.ap[:, tt * 512:(tt + 1) * 512], in_=P.ap[:, :], func=AF.Copy))
                P.read(tv)
                sg.wrote(tv, fresh=False)
        r0 = nsa_projT_row(c0)
        sp.wait(sg.deps_r())
        ts = sp.dma(projT[r0:r0 + 128, :], sg.ap[:, :], ssem[ci % 2])
        sg.read(ts)
        sg.w = {}
    wsem = fl.dsem("wt")
    pool.wait(Wt.deps_w())
    wsrc = w_in.rearrange("(kc p) n -> p kc n", p=128)
    pool.dma(Wt.ap[:, :, 0:256], wsrc[:, :, 1792:2048], wsem)
    pool.dma(Wt.ap[:, :, 256:512], wsrc[:, :, 2304:2560], wsem)
    tw = pool.dma(Wt.ap[:, :, 512:560], wsrc[:, :, 2560:2608], wsem)
    Wt.wrote(tw)
    for tb in range(NT):
        P = PJ[(2 * tb) % 6]
        Pg = PJ[(2 * tb + 1) % 6]
        pe.wait(P.deps_w(), Pg.deps_w(), tw, x_ready)
        tok = None
        for kc in range(8):
            tok = pe.done(pe.e.matmul(P.ap[:, :], XT.ap[:, kc, tb * 128:(tb + 1) * 128], Wt.ap[:, kc, 0:512],
                                      start=(kc == 0), stop=(kc == 7)))
        P.wrote(tok)
        tokg = None
        for kc in range(8):
            tokg = pe.done(pe.e.matmul(Pg.ap[:, 0:48], XT.ap[:, kc, tb * 128:(tb + 1) * 128], Wt.ap[:, kc, 512:560],
                                       start=(kc == 0), stop=(kc == 7)))
        Pg.wrote(tokg)
        vs_, gs_ = vst[tb % 2], gst[tb % 2]
        act.wait(tok, vs_.deps_w())
        tv = act.done(act.e.activation(out=vs_.ap[:, :], in_=P.ap[:, :], func=AF.Copy))
        P.read(tv)
        vs_.wrote(tv)
        act.wait(tokg, gs_.deps_w())
        tg_ = act.done(act.e.activation(out=gs_.ap[:, :], in_=Pg.ap[:, 0:48], func=AF.Sigmoid))
        Pg.read(tg_)
        gs_.wrote(tg_)
        sp.wait(tv, tg_)
        t1 = sp.dma(d["vtok"][tb * 128:(tb + 1) * 128, :], vs_.ap[:, :], ssem[tb % 2])
        t2 = sp.dma(d["gl"][tb * 128:(tb + 1) * 128, :], gs_.ap[:, :], fl.dsem("sg%d" % (tb % 2)))
        vs_.read(t1)
        gs_.read(t2)
    fl.end()


class AttnItem:
    def __init__(self):
        self.A = None
        self.B = None
        self.F = None
        self.slot = None


def attn_items(fl, S_ring, PT_ring, O, Kbuf, krows, Qbuf, qsel, q0, Vbuf, W, ktiles, finalize=None, pe_extra_deps=()):
    pe, act, dve = fl.pe, fl.act, fl.dve
    st = {"first": True}
    last_for = {}
    for i, (_, _, qlo, qhi, _) in enumerate(ktiles):
        for qb in range(qlo, qhi):
            last_for[qb] = i
    items = []
    nk = len(ktiles)
    for i, (kc0, vidx, qlo, qhi, masks) in enumerate(ktiles):
        it = AttnItem()

        def A(slot, it=it, kc0=kc0, qlo=qlo, qhi=qhi, masks=masks):
            it.slot = slot
            Sb = S_ring[slot % len(S_ring)]
            p = PT_ring[slot % len(PT_ring)]
            n0, n1 = qlo * 128, qhi * 128
            pe.wait(Sb.deps_w(), Kbuf.deps_r(), Qbuf.deps_r(), pe_extra_deps)
            tok = pe.done(pe.e.matmul(Sb.ap[:, n0:n1], Kbuf.ap[0:krows, kc0:kc0 + 128],
                                      Qbuf.ap[0:krows, qsel, q0 + n0:q0 + n1], start=True, stop=True))
            Sb.wrote(tok)
            Kbuf.read(tok)
            Qbuf.read(tok)
            act.wait(tok, p.deps_w())
            ta = act.done(act.e.activation(out=p.ap[:, n0:n1], in_=Sb.ap[:, n0:n1], func=AF.Exp, scale=0.125))
            Sb.read(ta)
            p.wrote(ta)
            for (qb, mask_ap, mdeps) in masks:
                dve.wait(ta, mdeps)
                tl = dve.done(dve.e.tensor_tensor(out=p.ap[:, qb * 128:(qb + 1) * 128], in0=p.ap[:, qb * 128:(qb + 1) * 128],
                                                  in1=mask_ap, op=ALU.mult))
                p.wrote(tl, fresh=False)

        def B(it=it, i=i, vidx=vidx, qlo=qlo, qhi=qhi):
            p = PT_ring[it.slot % len(PT_ring)]
            for qb in range(qlo, qhi):
                if st["first"]:
                    pe.wait(O.deps_w())
                pe.wait(p.deps_r(), Vbuf.deps_r())
                tok = pe.done(pe.e.matmul(O.ap[:, qb * W:(qb + 1) * W], p.ap[:, qb * 128:(qb + 1) * 128], Vbuf.ap[:, vidx, 0:W],
                                          start=st["first"], stop=(i == nk - 1 and qb == qhi - 1)))
                O.wrote(tok, fresh=st["first"])
                st["first"] = False
                p.read(tok)
                Vbuf.read(tok)

        it.A = A
        it.B = B
        items.append(it)
    items[-1].F = finalize
    return items


def run_items(items, slot0, L=2):
    n = len(items)
    for i in range(n + L):
        if i < n:
            items[i].A(slot0 + i)
        j = i - L
        if j >= 0:
            items[j].B()
            if items[j].F is not None:
                items[j].F()
    return slot0 + n


def phase_nsa_attn(fl, d):
    pe, act, dve, pool, sp = fl.pe, fl.act, fl.dve, fl.pool, fl.sp
    fl.begin()
    projT, vtok, gl = d["projT"], d["vtok"], d["gl"]
    QN = Buf(fl.sb([128, 4, S], BF16, "QN"))
    KE = Buf(fl.sb([128, S], BF16, "KE"))
    KW = Buf(fl.sb([64, S], BF16, "KW"))
    KC = Buf(fl.sb([64, S], BF16, "KC"))
    VC = Buf(fl.sb([64, S], BF16, "VC"))
    VS = Buf(fl.sb([128, NT, 65], BF16, "VS"))
    VW = Buf(fl.sb([128, NT, 65], BF16, "VW"))
    KCC = Buf(fl.sb([64, 256], BF16, "KCC"))
    RC = Buf(fl.sb([128, 2, 129], BF16, "RC"))
    HK = Buf(fl.sb([128, 2, 256], BF16, "HK"))
    HV = Buf(fl.sb([128, 2, 256], BF16, "HV"))
    W1K = fl.sb([64, 32, 256], BF16, "W1K")
    W1V = fl.sb([64, 32, 256], BF16, "W1V")
    W2K = fl.sb([128, 2, 64], BF16, "W2K")
    W2V = fl.sb([128, 2, 64], BF16, "W2V")
    POSK = fl.sb([64, 32], BF16, "POSK")
    POSV = fl.sb([64, 32], BF16, "POSV")
    CB = fl.sb([128, 4], F32, "CB")
    MK = fl.sb([128, 256], BF16, "MK")
    MCM = fl.sb([128, 9, 512], BF16, "MCM")
    FA = fl.sb([128, NT, 64], BF16, "FA")
    FB = fl.sb([128, NT, 64], BF16, "FB")
    IDN = fl.sb([128, 128], BF16, "IDN")
    GS = fl.sb([128, NT, 48], F32, "GS")
    IMP = Buf(fl.sb([128, NT, 64], F32, "IMP"))
    OG = Buf(fl.sb([128, NT, 256], F32, "OG"))
    NBT = [Buf(fl.sb([128, 128], BF16, "NBT")) for _ in range(2)]
    PTs = [Buf(fl.sb([128, 512], BF16, "PT")) for _ in range(3)]
    smalls = [fl.sb([128, 16], F32, "sm") for _ in range(4)]
    otmp = [Buf(fl.sb([128, 4, 64], F32, "otmp")) for _ in range(2)]
    im2 = [fl.sb([128, 64], F32, "im2") for _ in range(2)]
    imm = [fl.sb([128, 64], F32, "imm") for _ in range(2)]
    m8 = [fl.sb([128, 16], F32, "m8") for _ in range(2)]
    S_ring = [Buf(fl.ps([128, 512], F32, "Sb")) for _ in range(3)]
    O_ring = [Buf(fl.ps([128, 512], F32, "Ob")) for _ in range(2)]
    TP = Buf(fl.ps([128, 512], BF16, "TP"))
    CPS = [Buf(fl.ps([128, 512], F32, "CPS")) for _ in range(2)]

    csem = fl.dsem("const")
    cpsem = fl.dsem("constp")
    pool.dma(MK[:, :], d["swa_mask"][:, :], cpsem)
    pool.dma(MCM[:, :, :], d["cmp_mask"].rearrange("n p c -> p n c"), cpsem)
    pool.dma(FA[:, :, :], d["sel_fa"].rearrange("(t p) c -> p t c", p=128), cpsem)
    pool.dma(FB[:, :, :], d["sel_fb"].rearrange("(t p) c -> p t c", p=128), cpsem)
    pool.dma(IDN[:, :], d["ident"][:, :], cpsem)
    pool.dma(KE.ap[64:128, :], d["emat"][:, :], cpsem)
    pool.dma(RC.ap[:, :, 65:129], d["ov_pad"].rearrange("(c p) j -> p c j", p=128), cpsem)
    pool.dma(W1K[:, :, :], d["b_cmp_k_w1"][0].rearrange("(l dd) m -> dd l m", dd=64), cpsem)
    pool.dma(W1V[:, :, :], d["b_cmp_v_w1"][0].rearrange("(l dd) m -> dd l m", dd=64), cpsem)
    pool.dma(W2K[:, :, :], d["b_cmp_k_w2"][0].rearrange("(mc p) dd -> p mc dd", p=128), cpsem)
    pool.dma(W2V[:, :, :], d["b_cmp_v_w2"][0].rearrange("(mc p) dd -> p mc dd", p=128), cpsem)
    pool.dma(POSK[:, :], d["posT_k"][:, :], cpsem)
    pool.dma(POSV[:, :], d["posT_v"][:, :], cpsem)
    tcp = (cpsem, cpsem.n + 0)
    tgs = sp.dma(GS[:, :, :], gl.rearrange("(t p) c -> p t c", p=128), csem)
    dve.wait(tcp)
    t_m = dve.done(dve.e.memset(VS.ap[:, :, 64:65], 1.0))
    t_m = dve.done(dve.e.memset(VW.ap[:, :, 64:65], 1.0))
    t_m = dve.done(dve.e.memset(RC.ap[:, :, 64:65], 1.0))
    t_m = dve.done(dve.e.memset(HK.ap[:, :, :], 0.0))
    t_m = dve.done(dve.e.memset(HV.ap[:, :, :], 0.0))
    t_m = dve.done(dve.e.memset(KCC.ap[:, :], 0.0))
    for nb in NBT:
        t_m = dve.done(dve.e.memset(nb.ap[:, :], 0.0))
    t_init = t_m
    pe.wait(tcp)
    P = CPS[0]
    tok = None
    for which, (W1, POS) in enumerate(((W1K, POSK), (W1V, POSV))):
        for mc in range(2):
            col = which * 2 + mc
            for l in range(32):
                tok = pe.done(pe.e.matmul(P.ap[:, col:col + 1], W1[0:64, l, mc * 128:(mc + 1) * 128], POS[0:64, l:l + 1],
                                          start=(l == 0), stop=(l == 31)))
    P.wrote(tok)
    dve.wait(tok)
    t_cb = dve.done(dve.e.tensor_copy(out=CB[:, :], in_=P.ap[:, 0:4]))
    P.read(t_cb)

    lsem = [fl.dsem("xl0"), fl.dsem("xl1")]
    osem = fl.dsem("ostore_sw")
    s_i = 0
    o_i = 0
    sm_i = 0
    for j in range(4):
        sp.wait(QN.deps_w(), KE.deps_w(), KW.deps_w(), KC.deps_w(), VC.deps_w(), VS.deps_w(), VW.deps_w(), t_init)
        ls = lsem[j % 2]
        for hl in range(4):
            sp.dma(QN.ap[0:64, hl, :], projT[(4 * j + hl) * 64:(4 * j + hl + 1) * 64, :], ls)
        sp.dma(KC.ap[0:64, :], projT[1024 + j * 64:1024 + (j + 1) * 64, :], ls)
        sp.dma(VC.ap[0:64, :], projT[1280 + j * 64:1280 + (j + 1) * 64, :], ls)
        sp.dma(KE.ap[0:64, :], projT[1536 + j * 64:1536 + (j + 1) * 64, :], ls)
        sp.dma(KW.ap[0:64, :], projT[1792 + j * 64:1792 + (j + 1) * 64, :], ls)
        vt3 = vtok.rearrange("(t p) c -> p t c", p=128)
        sp.dma(VS.ap[:, :, 0:64], vt3[:, :, j * 64:(j + 1) * 64], ls)
        tl = sp.dma(VW.ap[:, :, 0:64], vt3[:, :, 256 + j * 64:256 + (j + 1) * 64], ls)
        for b in (QN, KE, KW, KC, VC, VS, VW):
            b.wrote(tl, fresh=False)
            b.r = {}
        ld = [tl, tcp, t_init]

        for which, (src, W1, W2, Hb) in enumerate(((KC, W1K, W2K, HK), (VC, W1V, W2V, HV))):
            for mc in range(2):
                P = CPS[(which * 2 + mc) % 2]
                pe.wait(P.deps_w(), ld)
                tok = None
                for l in range(32):
                    tok = pe.done(pe.e.matmul(P.ap[:, 0:255], W1[0:64, l, mc * 128:(mc + 1) * 128], src.ap[0:64, l:l + 4065:16],
                                              start=(l == 0), stop=(l == 31)))
                P.wrote(tok)
                src.read(tok)
                act.wait(tok, t_cb, Hb.deps_w(), t_init)
                th = act.done(act.e.activation(out=Hb.ap[:, mc, 0:255], in_=P.ap[:, 0:255], func=AF.Gelu_apprx_tanh,
                                               bias=CB[:, which * 2 + mc:which * 2 + mc + 1]))
                P.read(th)
                Hb.wrote(th, fresh=False)
        P = CPS[0]
        pe.wait(P.deps_w(), HK.deps_r())
        tok = None
        for mc in range(2):
            tok = pe.done(pe.e.matmul(P.ap[0:64, 0:255], W2K[:, mc, :], HK.ap[:, mc, 0:255], start=(mc == 0), stop=(mc == 1)))
        P.wrote(tok)
        HK.read(tok)
        act.wait(tok, KCC.deps_w())
        tk = act.done(act.e.activation(out=KCC.ap[0:64, 0:255], in_=P.ap[0:64, 0:255], func=AF.Copy))
        P.read(tk)
        KCC.wrote(tk)
        P = CPS[1]
        pe.wait(P.deps_w(), HV.deps_r())
        tok = None
        for ct in range(2):
            for mc in range(2):
                tok = pe.done(pe.e.matmul(P.ap[:, ct * 64:(ct + 1) * 64], HV.ap[:, mc, ct * 128:(ct + 1) * 128], W2V[:, mc, :],
                                          start=(mc == 0), stop=(mc == 1)))
        P.wrote(tok)
        HV.read(tok)
        act.wait(tok, RC.deps_w())
        tr_ = act.done(act.e.activation(out=RC.ap[:, :, 0:64], in_=P.ap[:, 0:128].rearrange("p (c x) -> p c x", c=2), func=AF.Copy))
        P.read(tr_)
        RC.wrote(tr_)
        HK.w = {}
        HV.w = {}

        mcm_idx = {(0, 0): 0, (0, 1): 1, (0, 2): 2, (0, 3): 3, (0, 4): 4, (1, 4): 5, (1, 5): 6, (1, 6): 7, (1, 7): 8}

        def cmp_final(O, sm, qh, hl, h):
            O3 = O.ap[:, 0:258].rearrange("p (b w) -> p b w", b=2)
            tile0 = qh * 2
            dve.wait(O.deps_r(), tgs, OG.deps_w(), IMP.deps_w())
            f1 = dve.done(dve.e.tensor_scalar(out=sm[:, 0:2], in0=O3[:, :, 64], scalar1=1e-30, scalar2=None, op0=ALU.max))
            dve.wait(f1)
            f2 = dve.done(dve.e.reciprocal(out=sm[:, 0:2], in_=sm[:, 0:2]))
            dve.wait(f2)
            f3 = dve.done(dve.e.tensor_tensor(out=sm[:, 2:4], in0=sm[:, 0:2], in1=GS[:, tile0:tile0 + 2, h * 3 + 0], op=ALU.mult))
            dve.wait(f3)
            f4 = dve.done(dve.e.tensor_tensor(out=OG.ap[:, tile0:tile0 + 2, hl * 64:(hl + 1) * 64], in0=O3[:, :, 0:64],
                                              in1=sm[:, 2:4].unsqueeze(2).to_broadcast([128, 2, 64]), op=ALU.mult))
            OG.wrote(f4, fresh=False)
            f5 = None
            for b2 in range(2):
                if hl == 0:
                    f5 = dve.done(dve.e.tensor_scalar(out=IMP.ap[:, tile0 + b2, :], in0=O3[:, b2, 65:129], scalar1=sm[:, b2:b2 + 1],
                                                      scalar2=None, op0=ALU.mult))
                else:
                    f5 = dve.done(dve.e.scalar_tensor_tensor(out=IMP.ap[:, tile0 + b2, :], in0=O3[:, b2, 65:129],
                                                             scalar=sm[:, b2:b2 + 1], in1=IMP.ap[:, tile0 + b2, :],
                                                             op0=ALU.mult, op1=ALU.add))
            IMP.wrote(f5, fresh=False)
            O.read(f1)
            O.read(f5)

        citems = []
        for hl in range(4):
            h = 4 * j + hl
            for qh in range(16):
                tt = qh // 2
                q0 = qh * 256
                ktl = []
                for ct in range(2):
                    if ct == 1 and tt <= 3:
                        continue
                    masks = []
                    if (ct, tt) in mcm_idx:
                        mi = mcm_idx[(ct, tt)]
                        off = (qh % 2) * 256
                        masks = [(0, MCM[:, mi, off:off + 128], tcp), (1, MCM[:, mi, off + 128:off + 256], tcp)]
                    ktl.append((ct * 128, ct, 0, 2, masks))
                O = O_ring[o_i % 2]
                o_i += 1
                sm = smalls[sm_i % 4]
                sm_i += 1
                citems.extend(attn_items(fl, S_ring, PTs, O, KCC, 64, QN, hl, q0, RC, 129, ktl,
                                         finalize=(lambda O=O, sm=sm, qh=qh, hl=hl, h=h: cmp_final(O, sm, qh, hl, h))))
        s_i = run_items(citems, s_i)

        for tile in range(NT):
            k2 = tile % 2
            i2, im_, mm = im2[k2], imm[k2], m8[k2]
            nb = NBT[k2]
            dve.wait(IMP.deps_r(), tcp)
            g1 = dve.done(dve.e.tensor_tensor(out=im_[:, :], in0=IMP.ap[:, tile, :], in1=FA[:, tile, :], op=ALU.mult))
            dve.wait(g1)
            g2 = dve.done(dve.e.tensor_tensor(out=im_[:, :], in0=im_[:, :], in1=FB[:, tile, :], op=ALU.add))
            dve.wait(g2)
            g3 = dve.done(dve.e.max(out=mm[:, 0:8], in_=im_[:, :]))
            dve.wait(g3)
            g4 = dve.done(dve.e.match_replace(out=i2[:, :], in_to_replace=mm[:, 0:8], in_values=im_[:, :], imm_value=-2.0))
            dve.wait(g4)
            g5 = dve.done(dve.e.max(out=mm[:, 8:16], in_=i2[:, :]))
            dve.wait(g5)
            g6 = dve.done(dve.e.tensor_scalar(out=mm[:, 0:1], in0=mm[:, 15:16], scalar1=0.0, scalar2=None, op0=ALU.max))
            dve.wait(g6, nb.deps_w())
            g7 = dve.done(dve.e.tensor_scalar(out=nb.ap[:, 64:128], in0=im_[:, :], scalar1=mm[:, 0:1], scalar2=1.0,
                                              op0=ALU.is_ge, op1=ALU.subtract))
            nb.wrote(g7)
            IMP.read(g7)
            pe.wait(g7, TP.deps_w(), tcp)
            tt_ = pe.done(pe.e.transpose(TP.ap[:, 0:128], nb.ap[:, :], IDN[:, :]))
            nb.read(tt_)
            TP.wrote(tt_)
            act.wait(tt_, QN.deps_w())
            tq = act.done(act.e.activation(out=QN.ap[64:128, :, tile * 128:(tile + 1) * 128],
                                           in_=TP.ap[64:128, 0:128].unsqueeze(1).to_broadcast([64, 4, 128]), func=AF.Copy))
            TP.read(tq)
            QN.wrote(tq, fresh=False)

        def ws_final(O, sm, ot_, tt, hl, h, br):
            O3 = O.ap[:, 0:260].rearrange("p (b w) -> p b w", b=4)
            dve.wait(O.deps_r(), tgs, ot_.deps_w())
            f1 = dve.done(dve.e.tensor_scalar(out=sm[:, 0:4], in0=O3[:, :, 64], scalar1=1e-30, scalar2=None, op0=ALU.max))
            dve.wait(f1)
            f2 = dve.done(dve.e.reciprocal(out=sm[:, 0:4], in_=sm[:, 0:4]))
            dve.wait(f2)
            f3 = dve.done(dve.e.tensor_tensor(out=sm[:, 4:8], in0=sm[:, 0:4], in1=GS[:, 4 * tt:4 * tt + 4, h * 3 + br], op=ALU.mult))
            dve.wait(f3)
            f4 = dve.done(dve.e.tensor_tensor(out=ot_.ap[:, :, :], in0=O3[:, :, 0:64],
                                              in1=sm[:, 4:8].unsqueeze(2).to_broadcast([128, 4, 64]), op=ALU.mult))
            O.read(f1)
            O.read(f4)
            ot_.wrote(f4)
            pool.wait(f4, OG.deps_r())
            f5 = pool.done(pool.e.tensor_tensor(out=OG.ap[:, 4 * tt:4 * tt + 4, hl * 64:(hl + 1) * 64],
                                                in0=OG.ap[:, 4 * tt:4 * tt + 4, hl * 64:(hl + 1) * 64], in1=ot_.ap[:, :, :], op=ALU.add))
            ot_.read(f5)
            OG.wrote(f5, fresh=False)
            if d["conv_q"]:
                d["conv_q"].pop(0)()

        witems = []
        for br in (2, 1):
            for hl in range(4):
                h = 4 * j + hl
                for tt in range(8):
                    ktl = []
                    if br == 2:
                        for kt in range(max(0, 4 * tt - 4), 4 * tt + 4):
                            qlo = max(4 * tt, kt) - 4 * tt
                            qhi = min(4 * tt + 3, kt + 4) - 4 * tt + 1
                            masks = []
                            if 4 * tt <= kt:
                                masks.append((kt - 4 * tt, MK[:, 0:128], tcp))
                            if kt + 4 <= 4 * tt + 3 and kt + 4 >= 4 * tt:
                                masks.append((kt + 4 - 4 * tt, MK[:, 128:256], tcp))
                            ktl.append((kt * 128, kt, qlo, qhi, masks))
                        Kb, krows, Vb = KW, 64, VW
                    else:
                        for kt in range(0, 4 * tt + 4):
                            qlo = max(4 * tt, kt) - 4 * tt
                            masks = []
                            if kt >= 4 * tt:
                                masks.append((kt - 4 * tt, MK[:, 0:128], tcp))
                            ktl.append((kt * 128, kt, qlo, 4, masks))
                        Kb, krows, Vb = KE, 128, VS
                    O = O_ring[o_i % 2]
                    o_i += 1
                    sm = smalls[sm_i % 4]
                    sm_i += 1
                    ot_ = otmp[sm_i % 2]
                    witems.extend(attn_items(fl, S_ring, PTs, O, Kb, krows, QN, hl, tt * 512, Vb, 65, ktl,
                                             finalize=(lambda O=O, sm=sm, ot_=ot_, tt=tt, hl=hl, h=h, br=br: ws_final(O, sm, ot_, tt, hl, h, br))))
        s_i = run_items(witems, s_i)
        pool.wait(OG.deps_r())
        tso = pool.dma(d["attnO"].rearrange("(t p) c -> p t c", p=128)[:, :, j * 256:(j + 1) * 256], OG.ap[:, :, :], osem)
        OG.w = {}
        OG.r = {}
        OG.read(tso)
        IMP.r = {}
    fl.end()


NTILE_R = 24
NSLOT = NTILE_R * 512
I32 = mybir.dt.int32
FG_R = 896


def phase_moe_convert(fl, d):
    sem = fl.dsem("wconv")
    q = []
    for e in range(NE):
        for fg in range(4):
            b = (e * 4 + fg) * 128
            f0 = fg * FG_R
            for (src, dst) in ((d["moe_w_gate"], d["wgbP"]), (d["moe_w_up"], d["wubP"])):
                q.append(lambda src=src, dst=dst, b=b, f0=f0, e=e: fl.pool.dma(
                    dst[b:b + 128, :].rearrange("p (kc n) -> p kc n", kc=8),
                    src[0, e, :, f0:f0 + FG_R].rearrange("(kc p) n -> p kc n", p=128), sem))
            q.append(lambda b=b, f0=f0, e=e: fl.pool.dma(
                d["wdbP"][b:b + 128, :].rearrange("p (fc n) -> p fc n", fc=7),
                d["moe_w_down"][0, e, f0:f0 + FG_R, :].rearrange("(fc p) n -> p fc n", p=128), sem))
    return q


def phase_moe_routed(fl, d, t_conv):
    nc = fl.nc
    pe, act, dve, pool, sp = fl.pe, fl.act, fl.dve, fl.pool, fl.sp
    gate_d, xb_d, xg_d, ys_d = d["gate"], d["x3b"], d["xg"], d["ys"]
    fl.begin()
    GT = fl.sb([128, NT, NE], F32, "GT")
    M = fl.sb([128, NT, NE], BF16, "M")
    Mf = fl.sb([128, NT, NE], F32, "Mf")
    UT = fl.sb([128, 128], BF16, "UT")
    ON = fl.sb([128, 128], BF16, "ON")
    TT = fl.sb([128, NT, NE], F32, "TT")
    TO = fl.sb([128, NT, NE], F32, "TO")
    SL = fl.sb([128, NT, NE], F32, "SL")
    cnt = fl.sb([128, NE], F32, "cnt")
    cnti = fl.sb([128, NE], I32, "cnti")
    pc = fl.sb([128, NE], F32, "pc")
    ss = fl.sb([128, NE], F32, "ss")
    se = fl.sb([128, NE], F32, "se")
    T512 = fl.sb([128, NTILE_R, NE], F32, "T512")
    cmp3 = fl.sb([128, NTILE_R, NE], F32, "cmp3")
    tef = fl.sb([128, NTILE_R], F32, "tef")
    FGP = fl.sb([128, NTILE_R, 4], F32, "FGP")
    iwf = fl.sb([128, NTILE_R, 4], F32, "iwf")
    idxW = fl.sb([128, NTILE_R, 4], I32, "idxW")
    ssum = fl.sb([128, NT], F32, "ssum")
    sB = fl.sb([128, NT], F32, "sB")
    sA = fl.sb([128, NT], F32, "sA")
    wsum = fl.sb([128, NT], F32, "wsum")
    wB = fl.sb([128, NT], F32, "wB")
    wAB = fl.sb([128, 2, NT], F32, "wAB")
    IAB = fl.sb([128, 2, NT], I32, "IAB")
    xsc = [Buf(fl.sb([128, D], BF16, "xsc")) for _ in range(2)]
    XTt = [Buf(fl.sb([128, 8, 512], BF16, "XTt")) for _ in range(2)]
    Wg = [Buf(fl.sb([128, 8, FG_R], BF16, "Wg")) for _ in range(2)]
    Wu = [Buf(fl.sb([128, 8, FG_R], BF16, "Wu")) for _ in range(2)]
    Wd = [Buf(fl.sb([128, 7, D], BF16, "Wd")) for _ in range(2)]
    Hs = [Buf(fl.sb([128, 7, 512], BF16, "H")) for _ in range(2)]
    Ss = [Buf(fl.sb([128, 512], F32, "Ssil")) for _ in range(2)]
    acc = [[Buf(fl.sb([128, D], F32, "acc")) for _ in range(4)] for _ in range(2)]
    GPs = [Buf(fl.ps([128, 512], F32, "GP")) for _ in range(2)]
    UPs = [Buf(fl.ps([128, 512], F32, "UP")) for _ in range(2)]
    YH = [Buf(fl.ps([128, 512], F32, "YH")) for _ in range(4)]
    PR = YH[0]
    yi = 0

    ZT = fl.sb([128, 8 * D], BF16, "ZT")
    tz = pool.done(pool.e.memset(ZT[:, :], 0.0))
    act.wait(tz)
    zsem = fl.dsem("zero")
    t_zero = None
    for q in range(NSLOT // 1024):
        t_zero = act.dma(xg_d[q * 1024:(q + 1) * 1024, :].rearrange("(p a) n -> p (a n)", a=8), ZT[:, :], zsem)
    csem = fl.dsem("const")
    cpsem = fl.dsem("constp")
    tg = sp.dma(GT[:, :, :], gate_d.rearrange("(t p) e -> p t e", p=128), csem)
    sp.dma(T512[:, :, :], d["t512"][:, :, :], csem)
    sp.dma(FGP[:, :, :], d["fgp"][:, :, :], csem)
    tc_ = (csem, csem.n)
    pool.dma(UT[:, :], d["utri"][:, :], cpsem)
    tcp = pool.dma(ON[:, :], d["ones128"][:, :], cpsem)
    dve.wait(tc_)
    t = dve.done(dve.e.tensor_scalar(out=Mf[:, :, :], in0=GT[:, :, :], scalar1=0.0, scalar2=None, op0=ALU.is_gt))
    dve.wait(t)
    t = dve.done(dve.e.tensor_copy(out=M[:, :, :], in_=Mf[:, :, :]))
    pe.wait(t, tcp)
    M2 = M[:, :, :].rearrange("p t e -> p (t e)")
    t1 = pe.done(pe.e.matmul(PR.ap[:, 0:256], UT[:, :], M2, start=True, stop=True))
    t2 = pe.done(pe.e.matmul(PR.ap[:, 256:512], ON[:, :], M2, start=True, stop=True))
    dve.wait(t1, t2)
    t = dve.done(dve.e.tensor_copy(out=SL[:, :, :].rearrange("p t e -> p (t e)"), in_=PR.ap[:, 0:256]))
    t = dve.done(dve.e.tensor_copy(out=TT[:, :, :].rearrange("p t e -> p (t e)"), in_=PR.ap[:, 256:512]))
    t = dve.done(dve.e.memset(TO[:, 0, :], 0.0))
    for k in range(1, NT):
        dve.wait(t)
        t = dve.done(dve.e.tensor_tensor(out=TO[:, k, :], in0=TO[:, k - 1, :], in1=TT[:, k - 1, :], op=ALU.add))
    dve.wait(t)
    t = dve.done(dve.e.tensor_tensor(out=cnt[:, :], in0=TO[:, NT - 1, :], in1=TT[:, NT - 1, :], op=ALU.add))
    dve.wait(t)
    t = dve.done(dve.e.tensor_scalar(out=cnt[:, :], in0=cnt[:, :], scalar1=511.0, scalar2=None, op0=ALU.add))
    dve.wait(t)
    t = dve.done(dve.e.tensor_copy(out=cnti[:, :], in_=cnt[:, :]))
    dve.wait(t)
    t = dve.done(dve.e.tensor_scalar(out=cnti[:, :], in0=cnti[:, :], scalar1=9, scalar2=9, op0=ALU.arith_shift_right,
                                     op1=ALU.logical_shift_left))
    dve.wait(t)
    t = dve.done(dve.e.tensor_copy(out=pc[:, :], in_=cnti[:, :]))
    dve.wait(t)
    t = dve.done(dve.e.memset(ss[:, 0:1], 0.0))
    for e in range(1, NE):
        dve.wait(t)
        t = dve.done(dve.e.tensor_tensor(out=ss[:, e:e + 1], in0=ss[:, e - 1:e], in1=pc[:, e - 1:e], op=ALU.add))
    dve.wait(t)
    t = dve.done(dve.e.tensor_tensor(out=se[:, :], in0=ss[:, :], in1=pc[:, :], op=ALU.add))
    dve.wait(t)
    t = dve.done(dve.e.tensor_tensor(out=cmp3[:, :, :], in0=T512[:, :, :], in1=se[:, :].unsqueeze(1).to_broadcast([128, NTILE_R, NE]),
                                     op=ALU.is_ge))
    dve.wait(t)
    t = dve.done(dve.e.tensor_reduce(out=tef[:, :], in_=cmp3[:, :, :], axis=mybir.AxisListType.X, op=ALU.add))
    dve.wait(t)
    t = dve.done(dve.e.tensor_scalar(out=tef[:, :], in0=tef[:, :], scalar1=7.0, scalar2=0.0, op0=ALU.min, op1=ALU.max))
    dve.wait(t)
    t = dve.done(dve.e.tensor_scalar(out=tef[:, :], in0=tef[:, :], scalar1=512.0, scalar2=None, op0=ALU.mult))
    dve.wait(t)
    t = dve.done(dve.e.tensor_tensor(out=iwf[:, :, :], in0=FGP[:, :, :], in1=tef[:, :].unsqueeze(2).to_broadcast([128, NTILE_R, 4]), op=ALU.add))
    dve.wait(t)
    t_te = dve.done(dve.e.tensor_copy(out=idxW[:, :, :], in_=iwf[:, :, :]))
    t = dve.done(dve.e.tensor_tensor(out=SL[:, :, :], in0=SL[:, :, :], in1=TO[:, :, :], op=ALU.add))
    dve.wait(t)
    t = dve.done(dve.e.tensor_tensor(out=SL[:, :, :], in0=SL[:, :, :], in1=ss[:, :].unsqueeze(1).to_broadcast([128, NT, NE]), op=ALU.add))
    dve.wait(t)
    t = dve.done(dve.e.tensor_tensor(out=SL[:, :, :], in0=SL[:, :, :], in1=Mf[:, :, :], op=ALU.mult))
    dve.wait(t)
    t = dve.done(dve.e.tensor_reduce(out=ssum[:, :], in_=SL[:, :, :], axis=mybir.AxisListType.X, op=ALU.add))
    t = dve.done(dve.e.tensor_reduce(out=sB[:, :], in_=SL[:, :, :], axis=mybir.AxisListType.X, op=ALU.max))
    t = dve.done(dve.e.tensor_reduce(out=wsum[:, :], in_=GT[:, :, :], axis=mybir.AxisListType.X, op=ALU.add))
    dve.wait(t)
    t = dve.done(dve.e.tensor_tensor(out=sA[:, :], in0=ssum[:, :], in1=sB[:, :], op=ALU.subtract))
    t = dve.done(dve.e.tensor_tensor(out=TT[:, :, :], in0=SL[:, :, :], in1=sB[:, :].unsqueeze(2).to_broadcast([128, NT, NE]), op=ALU.is_equal))
    dve.wait(t)
    t = dve.done(dve.e.tensor_tensor(out=TT[:, :, :], in0=TT[:, :, :], in1=GT[:, :, :], op=ALU.mult))
    dve.wait(t)
    t = dve.done(dve.e.tensor_reduce(out=wB[:, :], in_=TT[:, :, :], axis=mybir.AxisListType.X, op=ALU.add))
    dve.wait(t)
    t = dve.done(dve.e.tensor_tensor(out=wAB[:, 0, :], in0=wsum[:, :], in1=wB[:, :], op=ALU.subtract))
    t = dve.done(dve.e.tensor_copy(out=wAB[:, 1, :], in_=wB[:, :]))
    t = dve.done(dve.e.tensor_copy(out=IAB[:, 0, :], in_=sA[:, :]))
    t_idx = dve.done(dve.e.tensor_copy(out=IAB[:, 1, :], in_=sB[:, :]))
    sp.wait(t_idx)
    sp.dma(d["r_idx"][:, :, :], IAB[:, :, :], fl.dsem("st0"))
    sp.dma(d["r_w"][:, :, :], wAB[:, :, :], fl.dsem("st1"))

    xsem = [fl.dsem("xl0"), fl.dsem("xl1")]
    scsems = [fl.dsem("scat0"), fl.dsem("scat1")]
    t_sc = None
    for tb in range(NT):
        xs = xsc[tb % 2]
        sp.wait(xs.deps_w())
        tl = sp.dma(xs.ap[:, :], xb_d[tb * 128:(tb + 1) * 128, :], xsem[tb % 2])
        xs.wrote(tl)
        pool.wait(tl, t_idx, t_zero)
        scs = scsems[tb % 2]
        for ab in range(2):
            ins = pool.e.indirect_dma_start(out=xg_d[:, :], out_offset=bass.IndirectOffsetOnAxis(ap=IAB[:, ab, tb:tb + 1], axis=0),
                                            in_=xs.ap[:, :], in_offset=None)
            scs.n += 16
            ins.then_inc(scs.h, 16)
        xs.read((scs, scs.n))
    t_sc = [(scsems[0], scsems[0].n), (scsems[1], scsems[1].n)]

    wsems = [fl.dsem("rw0"), fl.dsem("rw1")]
    xtsem = [fl.dsem("xt0"), fl.dsem("xt1")]
    ysem = [fl.dsem("ys0"), fl.dsem("ys1")]
    wi = 0
    gi = 0
    hi = 0
    t_ys = []
    sp.wait(t_sc)
    for ti in range(NTILE_R):
        xt = XTt[ti % 2]
        sp.wait(xt.deps_w())
        tx = None
        for kc in range(8):
            tx = sp.dma(xt.ap[:, kc, :], xg_d[ti * 512:(ti + 1) * 512, kc * 128:(kc + 1) * 128], xtsem[ti % 2], transpose=True)
        xt.wrote(tx)
        A = acc[ti % 2]
        for fg in range(FF // FG_R):
            k = wi % 2
            wi += 1
            wg, wu, wd = Wg[k], Wu[k], Wd[k]
            f0 = fg * FG_R
            pool.wait(wg.deps_w(), wu.deps_w(), wd.deps_w(), t_te, t_conv)
            tw = None
            for (wt_, srcP) in ((wg, d["wgbP"]), (wu, d["wubP"]), (wd, d["wdbP"])):
                ins = pool.e.indirect_dma_start(out=wt_.ap[:, :, :].rearrange("p a n -> p (a n)"), out_offset=None, in_=srcP[:, :],
                                                in_offset=bass.IndirectOffsetOnAxis(ap=idxW[:, ti, fg:fg + 1], axis=0))
                wsems[k].n += 16
                ins.then_inc(wsems[k].h, 16)
                tw = (wsems[k], wsems[k].n)
            wg.wrote(tw)
            wu.wrote(tw)
            wd.wrote(tw)
            H = Hs[hi % 2]
            hi += 1
            for fc in range(7):
                GP, UP, Sb = GPs[gi % 2], UPs[gi % 2], Ss[gi % 2]
                gi += 1
                for (P, Wt) in ((GP, wg), (UP, wu)):
                    pe.wait(P.deps_w(), tw, tx)
                    tok = None
                    for kc in range(8):
                        tok = pe.done(pe.e.matmul(P.ap[:, :], Wt.ap[:, kc, fc * 128:(fc + 1) * 128], xt.ap[:, kc, :],
                                                  start=(kc == 0), stop=(kc == 7)))
                    P.wrote(tok)
                    Wt.read(tok)
                    xt.read(tok)
                act.wait(GP.deps_r(), Sb.deps_w())
                ta = act.done(act.e.activation(out=Sb.ap[:, :], in_=GP.ap[:, :], func=AF.Silu))
                GP.read(ta)
                Sb.wrote(ta)
                dve.wait(ta, UP.deps_r(), H.deps_w())
                th = dve.done(dve.e.tensor_tensor(out=H.ap[:, fc, :], in0=UP.ap[:, :], in1=Sb.ap[:, :], op=ALU.mult))
                UP.read(th)
                Sb.read(th)
                H.wrote(th, fresh=(fc == 0))
            for tb in range(4):
                a = A[tb]
                for half in range(2):
                    Y = YH[yi % 4]
                    yi += 1
                    pe.wait(Y.deps_w(), H.deps_r(), tw)
                    tok = None
                    for fc in range(7):
                        tok = pe.done(pe.e.matmul(Y.ap[:, :], H.ap[:, fc, tb * 128:(tb + 1) * 128],
                                                  wd.ap[:, fc, half * 512:(half + 1) * 512], start=(fc == 0), stop=(fc == 6)))
                    Y.wrote(tok)
                    H.read(tok)
                    wd.read(tok)
                    sl = slice(half * 512, (half + 1) * 512)
                    dve.wait(tok, a.deps_w())
                    if fg == 0:
                        ty = dve.done(dve.e.tensor_copy(out=a.ap[:, sl], in_=Y.ap[:, :]))
                    else:
                        ty = dve.done(dve.e.tensor_tensor(out=a.ap[:, sl], in0=Y.ap[:, :], in1=a.ap[:, sl], op=ALU.add))
                    Y.read(ty)
                    a.wrote(ty, fresh=(half == 0))
        tys = None
        for tb in range(4):
            a = A[tb]
            act.wait(a.deps_r())
            tys = act.dma(ys_d[ti * 512 + tb * 128:ti * 512 + (tb + 1) * 128, :], a.ap[:, :], ysem[ti % 2])
        for tb in range(4):
            A[tb].read(tys)
        t_ys.append(tys)
    fl.end()

    fl.begin()
    IAB = fl.sb([128, 2, NT], I32, "IAB")
    wAB = fl.sb([128, 2, NT], F32, "wAB")
    G, Bt, tln = load_ln_params(fl, d["ln_gain"], d["ln_bias"], 1, 1, None)
    YA = [Buf(fl.sb([128, D], F32, "YA")) for _ in range(2)]
    YB = [Buf(fl.sb([128, D], F32, "YB")) for _ in range(2)]
    xts = [Buf(fl.sb([128, D], F32, "xt")) for _ in range(2)]
    rts = [Buf(fl.sb([128, D], F32, "rt")) for _ in range(2)]
    xns = [Buf(fl.sb([128, D], F32, "xn")) for _ in range(2)]
    ots = [Buf(fl.sb([128, D], F32, "ot")) for _ in range(2)]
    stats = [fl.sb([128, 2, 6], F32, "st") for _ in range(2)]
    mvs = [fl.sb([128, 2], F32, "mv") for _ in range(2)]
    rss = [fl.sb([128, 1], F32, "rs") for _ in range(2)]
    csem = fl.dsem("const")
    sp.dma(IAB[:, :, :], d["r_idx"][:, :, :], csem)
    tld = sp.dma(wAB[:, :, :], d["r_w"][:, :, :], csem)
    gsem = [fl.dsem("ga0"), fl.dsem("ga1"), fl.dsem("gb0"), fl.dsem("gb1")]
    xsem = [fl.dsem("xl0"), fl.dsem("xl1")]
    ssem = [fl.dsem("st0"), fl.dsem("st1")]
    for tb in range(NT):
        k = tb % 2
        ya, yb, xt, rt, xn, ot = YA[k], YB[k], xts[k], rts[k], xns[k], ots[k]
        pool.wait(tld, ya.deps_w(), yb.deps_w())
        toks = []
        for ab, (yy, sm_) in enumerate(((ya, gsem[k]), (yb, gsem[2 + k]))):
            ins = pool.e.indirect_dma_start(out=yy.ap[:, :], out_offset=None, in_=ys_d[:, :],
                                            in_offset=bass.IndirectOffsetOnAxis(ap=IAB[:, ab, tb:tb + 1], axis=0))
            sm_.n += 16
            ins.then_inc(sm_.h, 16)
            yy.wrote((sm_, sm_.n))
            toks.append((sm_, sm_.n))
        sp.wait(xt.deps_w())
        txl = sp.dma(xt.ap[:, :], d["x3"][tb * 128:(tb + 1) * 128, :], xsem[k])
        xt.wrote(txl)
        dve.wait(toks, txl, rt.deps_w(), tld)
        t1 = dve.done(dve.e.scalar_tensor_tensor(out=rt.ap[:, :], in0=ya.ap[:, :], scalar=wAB[:, 0, tb:tb + 1], in1=xt.ap[:, :],
                                                 op0=ALU.mult, op1=ALU.bypass)) if False else \
            dve.done(dve.e.tensor_scalar(out=rt.ap[:, :], in0=ya.ap[:, :], scalar1=wAB[:, 0, tb:tb + 1], scalar2=None, op0=ALU.mult))
        dve.wait(t1)
        t2 = dve.done(dve.e.scalar_tensor_tensor(out=rt.ap[:, :], in0=yb.ap[:, :], scalar=wAB[:, 1, tb:tb + 1], in1=rt.ap[:, :],
                                                 op0=ALU.mult, op1=ALU.add))
        dve.wait(t2)
        t3 = dve.done(dve.e.scalar_tensor_tensor(out=rt.ap[:, :], in0=xt.ap[:, :], scalar=ALPHA, in1=rt.ap[:, :],
                                                 op0=ALU.mult, op1=ALU.add))
        ya.read(t1)
        yb.read(t2)
        xt.read(t3)
        rt.wrote(t3)
        pool.wait(tln)
        t7 = layernorm_tile(fl, rt, stats[k], mvs[k], rss[k], xn, ot, G, Bt)
        sp.wait(t7)
        ts1 = sp.dma(d["y"][tb * 128:(tb + 1) * 128, :], ot.ap[:, :], ssem[k])
        ot.read(ts1)
    fl.end()


def host_constants():
    inv = (1.0 / (np.float32(10000.0) ** (np.arange(0, 64, 2, dtype=np.float32) / np.float32(64)))).astype(np.float32)
    ang = np.arange(S, dtype=np.float32)[:, None] * inv[None, :]
    cos = np.cos(ang).astype(np.float32).T
    sin = np.sin(ang).astype(np.float32).T
    cosT = np.concatenate([cos, cos, cos, cos], axis=0)
    sinT = np.concatenate([-sin, sin, -sin, sin], axis=0)
    k = np.arange(128)[:, None]
    c = np.arange(256)[None, :]
    swa_mask = np.where(c < 128, c >= k, (c - 128) < k).astype(np.float32)
    out = {"cosT": np.ascontiguousarray(cosT), "sinT": np.ascontiguousarray(sinT), "swa_mask": swa_mask}
    cidx = np.arange(256)
    cs = 16 * cidx
    ce = cs + 32
    ss = 64 * np.arange(64)
    se = ss + 64
    ov = np.clip(np.minimum(ce[:, None], se[None, :]) - np.maximum(cs[:, None], ss[None, :]), 0, None) / 16.0
    ov[255, :] = 0.0
    out["ov_pad"] = ov.astype(np.float32)
    tiles = [(0, 0), (0, 1), (0, 2), (0, 3), (0, 4), (1, 4), (1, 5), (1, 6), (1, 7)]
    cm = np.zeros((9, 128, 512), np.float32)
    for i, (ct, tt) in enumerate(tiles):
        cc = ct * 128 + np.arange(128)[:, None]
        t = tt * 512 + np.arange(512)[None, :]
        cm[i] = ((16 * cc + 31 <= t) & (cc < 255)).astype(np.float32)
    out["cmp_mask"] = cm
    t = np.arange(S)[:, None]
    b = np.arange(64)[None, :]
    cur = t // 64
    forced = (b == 0) | (b == cur) | (b == cur - 1)
    fa = ((b <= cur) & (~forced)).astype(np.float32)
    fb = np.where(b > cur, -1.0, 0.0).astype(np.float32)
    fb = np.where(b == cur - 1, 1.0e4, fb)
    fb = np.where(b == cur, 2.0e4, fb)
    fb = np.where(b == 0, 3.0e4, fb).astype(np.float32)
    out["sel_fa"] = fa
    out["sel_fb"] = fb
    out["ident"] = np.eye(128, dtype=np.float32)
    em = np.zeros((64, S), np.float32)
    em[np.arange(S) // 64, np.arange(S)] = 30000.0
    out["emat"] = em
    pp = np.arange(128)
    out["utri"] = (pp[:, None] < pp[None, :]).astype(np.float32)
    out["ones128"] = np.ones((128, 128), np.float32)
    t512 = np.zeros((128, NTILE_R, NE), np.float32)
    t512[:, :, :] = (512.0 * np.arange(NTILE_R))[None, :, None]
    out["t512"] = t512
    fgp = np.zeros((128, NTILE_R, 4), np.float32)
    fgp[:, :, :] = (128.0 * np.arange(4))[None, None, :] + np.arange(128, dtype=np.float32)[:, None, None]
    out["fgp"] = fgp
    return out


INPUT_SHAPES = {
    "a_w_in": [1, 1024, 1536], "a_w_out": [1, 1024, 1024], "a_sinks": [1, 16],
    "b_w_in": [1, 1024, 2608], "b_w_out": [1, 1024, 1024], "b_cmp_pos_k": [1, 32, 64], "b_cmp_pos_v": [1, 32, 64],
    "b_cmp_k_w1": [1, 2048, 256], "b_cmp_k_w2": [1, 256, 64], "b_cmp_v_w1": [1, 2048, 256], "b_cmp_v_w2": [1, 256, 64],
    "ffn_w_gate": [1, 1024, 3584], "ffn_w_up": [1, 1024, 3584], "ffn_w_down": [1, 3584, 1024],
    "moe_router": [1, 1024, 8], "moe_w_gate": [1, 8, 1024, 3584], "moe_w_up": [1, 8, 1024, 3584],
    "moe_w_down": [1, 8, 3584, 1024], "ln_gain": [2, 2, 1024], "ln_bias": [2, 2, 1024],
}


def _needed(k, stop_after):
    if stop_after >= 4:
        return True
    if k.startswith("moe_w"):
        return False
    if stop_after <= 2 and (k.startswith("b_") or k.startswith("moe")):
        return False
    if stop_after <= 1 and k.startswith("ffn"):
        return False
    return True


def build(stop_after=99):
    nc = bass.Bass("TRN2", target_bir_lowering=False)
    d = {}
    d["x"] = nc.dram_tensor("x", [S, D], F32, kind="ExternalInput").ap()
    for k, shp in INPUT_SHAPES.items():
        if not _needed(k, stop_after):
            continue
        d[k] = nc.dram_tensor(k, shp, F32, kind="ExternalInput").ap()
    hc = host_constants()
    for k, v in hc.items():
        d[k] = nc.dram_tensor(k, list(v.shape), F32, kind="ExternalInput").ap()
    d["routerT"] = nc.dram_tensor("routerT", [NE, D], F32, kind="ExternalInput").ap()
    d["posT_k"] = nc.dram_tensor("posT_k", [64, 32], F32, kind="ExternalInput").ap()
    d["posT_v"] = nc.dram_tensor("posT_v", [64, 32], F32, kind="ExternalInput").ap()
    d["projT"] = nc.dram_tensor("projT", [2048, S], BF16, kind="Internal").ap()
    d["vtok"] = nc.dram_tensor("vtok", [S, 512], BF16, kind="Internal").ap()
    d["gl"] = nc.dram_tensor("gl", [S, 48], F32, kind="Internal").ap()
    y = nc.dram_tensor("y", [S, D], F32, kind="ExternalOutput").ap()
    for nm in ("xb0", "attnO", "x1b", "x2b", "x3b"):
        d[nm] = nc.dram_tensor(nm, [S, D], BF16, kind="Internal").ap()
    for nm in ("x1", "x2", "x3"):
        d[nm] = nc.dram_tensor(nm, [S, D], F32, kind="Internal").ap()
    d["gate"] = nc.dram_tensor("gate", [S, NE], F32, kind="Internal").ap()
    d["y"] = y
    if stop_after >= 4:
        d["wgbP"] = nc.dram_tensor("wgbP", [NE * 4 * 128, 8 * FG_R], BF16, kind="Internal").ap()
        d["wubP"] = nc.dram_tensor("wubP", [NE * 4 * 128, 8 * FG_R], BF16, kind="Internal").ap()
        d["wdbP"] = nc.dram_tensor("wdbP", [NE * 4 * 128, 7 * D], BF16, kind="Internal").ap()
        d["xg"] = nc.dram_tensor("xg", [NSLOT, D], BF16, kind="Internal").ap()
        d["ys"] = nc.dram_tensor("ys", [NSLOT, D], F32, kind="Internal").ap()
        d["r_idx"] = nc.dram_tensor("r_idx", [128, 2, NT], I32, kind="Internal").ap()
        d["r_w"] = nc.dram_tensor("r_w", [128, 2, NT], F32, kind="Internal").ap()
    fl = Flow(nc)
    d["t_castx"] = phase_cast_x(fl, d["x"], d["xb0"])
    phase_l0_attn(fl, d)
    phase_outproj_ln(fl, d, d["attnO"], d["a_w_out"][0], d["x"], 0, 0, y if stop_after == 1 else d["x1"], d["x1b"])
    if stop_after >= 2:
        phase_ffn(fl, d, d["x1b"], d["x1"], [d["ffn_w_gate"][0]], [d["ffn_w_up"][0]], [d["ffn_w_down"][0]], None, 0, 1,
                  y if stop_after == 2 else d["x2"], d["x2b"])
    d["conv_q"] = phase_moe_convert(fl, d) if (stop_after >= 4 and ROUTED) else []
    if stop_after >= 3:
        phase_nsa_proj(fl, d)
        phase_nsa_attn(fl, d)
        phase_outproj_ln(fl, d, d["attnO"], d["b_w_out"][0], d["x2"], 1, 0, y if stop_after == 3 else d["x3"], d["x3b"],
                         router_d=d["moe_router"][0], gate_d=d["gate"])
    if stop_after >= 4:
        if ROUTED:
            for f in d["conv_q"]:
                f()
            d["conv_q"] = []
            wc = fl.dsem("wconv")
            phase_moe_routed(fl, d, (wc, wc.n))
        else:
            phase_ffn(fl, d, d["x3b"], d["x3"], [d["moe_w_gate"][0, e] for e in range(NE)], [d["moe_w_up"][0, e] for e in range(NE)],
                      [d["moe_w_down"][0, e] for e in range(NE)], d["gate"], 1, 1, y, None)
    fl.barrier()
    fl.gstack.close()
    return nc


_CACHE = {}


def kernel(**inputs):
    stop_after = int(inputs.pop("_stop_after", 99))
    if stop_after not in _CACHE:
        _CACHE[stop_after] = build(stop_after)
    nc = _CACHE[stop_after]
    hc = host_constants()
    shared = {k: np.ascontiguousarray(np.asarray(inputs[k], dtype=np.float32)) for k in INPUT_SHAPES if _needed(k, stop_after)}
    shared.update(hc)
    shared["routerT"] = np.ascontiguousarray(np.asarray(inputs["moe_router"], dtype=np.float32)[0].T)
    shared["posT_k"] = np.ascontiguousarray(np.asarray(inputs["b_cmp_pos_k"], dtype=np.float32)[0].T)
    shared["posT_v"] = np.ascontiguousarray(np.asarray(inputs["b_cmp_pos_v"], dtype=np.float32)[0].T)
    x = np.asarray(inputs["x"], dtype=np.float32)
    in_maps = []
    for c in range(NCORES):
        m = dict(shared)
        m["x"] = np.ascontiguousarray(x[c])
        in_maps.append(m)
    res = run_bass_kernel_spmd(nc, in_maps, core_ids=list(range(NCORES)))
    return np.stack([np.asarray(r["y"], dtype=np.float32) for r in res.results], axis=0)
```
